# Optimizing a Trainium2 kernel written in Bass

```python
import math
import jax
import jax.numpy as jnp
from jax import lax

D_MODEL = 1024
BATCH = 16
SEQ = 2048
DEPTH = 4

N_EVEN = (DEPTH + 1) // 2
N_ODD = DEPTH // 2
N_VRES = max(N_EVEN - 1, 0)

MIX_HALF = D_MODEL // 2

RW_WIDTH = MIX_HALF
RW_HEAD_DIM = 64
RW_HEADS = RW_WIDTH // RW_HEAD_DIM
RW_DECAY_RANK = 64
RW_ICLR_RANK = 64
RW_GATE_RANK = 128
RW_VRES_RANK = 32
RW_SHIFT_COLS = 3 * RW_WIDTH + RW_DECAY_RANK + RW_ICLR_RANK + RW_GATE_RANK
RW_SPLITS = (RW_WIDTH, 2 * RW_WIDTH, 3 * RW_WIDTH, 3 * RW_WIDTH + RW_DECAY_RANK,
             3 * RW_WIDTH + RW_DECAY_RANK + RW_ICLR_RANK)
RW_LN_EPS = 64e-5

POOL_WIDTH = MIX_HALF
POOL_WINDOWS = (2, 4, 8, 16)
POOL_GROUPS = len(POOL_WINDOWS)
POOL_GROUP_DIM = POOL_WIDTH // POOL_GROUPS

EVEN_IN = RW_SHIFT_COLS + POOL_WIDTH
EVEN_MIX = RW_WIDTH + POOL_WIDTH

DA_WIDTH = MIX_HALF
DA_HEAD_DIM = 64
DA_VALUE_DIM = 2 * DA_HEAD_DIM
DA_HEADS = DA_WIDTH // DA_VALUE_DIM
DA_QK_COLS = DA_HEADS * 2 * DA_HEAD_DIM
ROPE_THETA = 10000.0
Q_BLOCK = 128
SUBLN_EPS = 1e-5

S5_WIDTH = MIX_HALF
S5_GROUP_DIM = 16
S5_GROUPS = S5_WIDTH // S5_GROUP_DIM
S5_STATE = 64

ODD_IN = 2 * DA_QK_COLS + DA_WIDTH + S5_WIDTH
ODD_MIX = DA_WIDTH + S5_WIDTH

MOE_GROUPS = 4
MOE_EXPERTS_PER_GROUP = 8
MOE_EXPERTS = MOE_GROUPS * MOE_EXPERTS_PER_GROUP
MOE_TOP_K = 2
MOE_HIDDEN = D_MODEL // 2
MOE_BLOCK = 128

RMS_EPS = 1e-6

kernel_name = 'hybrid_rwkv7_pool_diffattn_s5_hmoe'


def rms_norm(x, g, eps=RMS_EPS):
    xf = x.astype(jnp.float32)
    y = xf * lax.rsqrt(jnp.mean(xf * xf, axis=-1, keepdims=True) + eps)
    return (y * g.astype(jnp.float32)).astype(x.dtype)


def token_shift(u):
    return jnp.pad(u, ((0, 0), (1, 0), (0, 0)))[:, :-1]


def rwkv7_recurrence(r, w, k, v, a, b):
    bsz, _, n_heads, n = r.shape
    seq = tuple(jnp.moveaxis(t.astype(jnp.float32), 1, 0) for t in (r, w, k, v, a, b))

    def step(state, inp):
        r_t, w_t, k_t, v_t, a_t, b_t = inp
        sa = jnp.einsum('bhij,bhj->bhi', state, a_t)
        state = (state * w_t[:, :, None, :] + sa[..., None] * b_t[:, :, None, :]
                 + v_t[..., None] * k_t[:, :, None, :])
        return state, jnp.einsum('bhij,bhj->bhi', state, r_t)

    s0 = jnp.zeros((bsz, n_heads, n, n), jnp.float32)
    _, y = lax.scan(step, s0, seq)
    return jnp.moveaxis(y, 0, 1)


def multiscale_pool(u, pool_w, pool_scale):
    bsz, t_len, _ = u.shape
    uf = u.astype(jnp.float32).reshape(bsz, t_len, POOL_GROUPS, POOL_GROUP_DIM)
    csum = jnp.cumsum(uf, axis=1)
    n_seen = jnp.arange(1, t_len + 1, dtype=jnp.float32)
    diffs = []
    for gi, win in enumerate(POOL_WINDOWS):
        c = csum[:, :, gi]
        lagged = jnp.pad(c, ((0, 0), (win, 0), (0, 0)))[:, :t_len]
        mean = (c - lagged) / jnp.minimum(n_seen, float(win))[None, :, None]
        diffs.append(mean - uf[:, :, gi])
    d = jnp.stack(diffs, axis=2).astype(u.dtype)
    y = jnp.einsum('btgc,gcd->btgd', d, pool_w)
    return y.reshape(bsz, t_len, POOL_WIDTH) * pool_scale


def even_mixer(h, v_first, w_in, mu, w0, w2, a0, a2, g2, k_k, k_a, r_k, ln_g, ln_b, vres,
               pool_w, pool_scale, w_out):
    bsz, t_len, _ = h.shape
    u = h @ w_in
    u_rw, u_pool = u[..., :RW_SHIFT_COLS], u[..., RW_SHIFT_COLS:]
    m = u_rw + (token_shift(u_rw) - u_rw) * mu
    r, k, v, dw, da, dg = jnp.split(m, RW_SPLITS, axis=-1)
    w = -jax.nn.softplus(-(w0 + jnp.tanh(dw) @ w2)) - 0.5
    decay = jnp.exp(-jnp.exp(w.astype(jnp.float32)))
    a = jax.nn.sigmoid(a0 + da @ a2)
    g = jax.nn.sigmoid(dg) @ g2
    if v_first is None:
        v_first = v
    else:
        v0, v1, v2 = vres
        v = v + (v_first - v) * jax.nn.sigmoid(v0 + (v @ v1) @ v2)

    def heads(t):
        return t.reshape(bsz, t_len, RW_HEADS, RW_HEAD_DIM)

    kk = heads(k * k_k).astype(jnp.float32)
    kk = kk / jnp.maximum(jnp.sqrt(jnp.sum(kk * kk, axis=-1, keepdims=True)), 1e-12)
    k = k * (1.0 + (a - 1.0) * k_a)
    rh, kh, vh, ah = heads(r), heads(k), heads(v), heads(a)
    y = rwkv7_recurrence(rh, heads(decay), kh, vh, -kk, kk * ah.astype(jnp.float32))
    mean = jnp.mean(y, axis=-1, keepdims=True)
    var = jnp.mean(jnp.square(y - mean), axis=-1, keepdims=True)
    y = ((y - mean) * lax.rsqrt(var + RW_LN_EPS)).reshape(bsz, t_len, RW_WIDTH)
    y = (y * ln_g + ln_b).astype(h.dtype)
    bonus = jnp.sum(rh * kh * r_k, axis=-1, keepdims=True) * vh
    o_rw = (y + bonus.reshape(bsz, t_len, RW_WIDTH)) * g
    o_pool = multiscale_pool(u_pool, pool_w, pool_scale)
    return jnp.concatenate([o_rw, o_pool], axis=-1) @ w_out, v_first


def rope_tables(t_len, dim):
    inv_freq = ROPE_THETA ** (-jnp.arange(0, dim, 2, dtype=jnp.float32) / dim)
    ang = jnp.arange(t_len, dtype=jnp.float32)[:, None] * inv_freq[None, :]
    return jnp.cos(ang), jnp.sin(ang)


def apply_rope(x, cos, sin):
    xf = x.astype(jnp.float32)
    x1, x2 = jnp.split(xf, 2, axis=-1)
    c = cos[:, None, None, :]
    s = sin[:, None, None, :]
    return jnp.concatenate([x1 * c - x2 * s, x2 * c + x1 * s], axis=-1).astype(x.dtype)


def diff_attention(q, k, v, lam):
    t_len = q.shape[1]
    scale = DA_HEAD_DIM ** -0.5
    pos = jnp.arange(t_len)
    outs = []
    for s in range(0, t_len, Q_BLOCK):
        e = s + Q_BLOCK
        sc = jnp.einsum('bqhmd,bkhmd->bhmqk', q[:, s:e], k[:, :e]).astype(jnp.float32) * scale
        mask = pos[s:e, None] >= pos[None, :e]
        p = jax.nn.softmax(jnp.where(mask, sc, -jnp.inf), axis=-1)
        attn = p[:, :, 0] - lam * p[:, :, 1]
        outs.append(jnp.einsum('bhqk,bkhd->bqhd', attn.astype(v.dtype), v[:, :e]))
    return jnp.concatenate(outs, axis=1)


def s5_mixer(u, a_re, a_im, log_step, b_re, b_im, c_re, c_im, d_skip, w_glu):
    bsz, t_len, _ = u.shape
    f32 = jnp.float32
    uf = u.astype(f32).reshape(bsz, t_len, S5_GROUPS, S5_GROUP_DIM)
    lam = lax.complex(jnp.minimum(a_re.astype(f32), -1e-4), a_im.astype(f32))
    step = jnp.exp(log_step.astype(f32))
    lam_bar = jnp.exp(lam * step)
    b_bar = ((lam_bar - 1.0) / lam)[..., None] * lax.complex(b_re.astype(f32), b_im.astype(f32))
    c_mat = lax.complex(c_re.astype(f32), c_im.astype(f32))
    bu = jnp.einsum('gpc,btgc->btgp', b_bar, uf.astype(jnp.complex64))
    a_seq = jnp.broadcast_to(lam_bar, bu.shape)

    def combine(left, right):
        a_l, b_l = left
        a_r, b_r = right
        return a_r * a_l, a_r * b_l + b_r

    _, states = lax.associative_scan(combine, (a_seq, bu), axis=1)
    y = jnp.einsum('gcp,btgp->btgc', c_mat, states).real + d_skip.astype(f32) * uf
    z = jax.nn.gelu(y.reshape(bsz, t_len, S5_WIDTH)).astype(u.dtype)
    return z * jax.nn.sigmoid(z @ w_glu)


def odd_mixer(h, layer_idx, w_in, q_norm, k_norm, lam_q1, lam_k1, lam_q2, lam_k2, subln,
              a_re, a_im, log_step, b_re, b_im, c_re, c_im, d_skip, w_glu, w_out):
    bsz, t_len, _ = h.shape
    f32 = jnp.float32
    u = h @ w_in
    q, k, v, u_s5 = jnp.split(u, (DA_QK_COLS, 2 * DA_QK_COLS, 2 * DA_QK_COLS + DA_WIDTH), axis=-1)
    q = q.reshape(bsz, t_len, DA_HEADS, 2, DA_HEAD_DIM)
    k = k.reshape(bsz, t_len, DA_HEADS, 2, DA_HEAD_DIM)
    v = v.reshape(bsz, t_len, DA_HEADS, DA_VALUE_DIM)
    cos, sin = rope_tables(t_len, DA_HEAD_DIM)
    q = apply_rope(rms_norm(q, q_norm), cos, sin)
    k = apply_rope(rms_norm(k, k_norm), cos, sin)
    lam_init = 0.8 - 0.6 * math.exp(-0.3 * layer_idx)
    lam = (jnp.exp(jnp.sum(lam_q1.astype(f32) * lam_k1.astype(f32)))
           - jnp.exp(jnp.sum(lam_q2.astype(f32) * lam_k2.astype(f32))) + lam_init)
    o = diff_attention(q, k, v, lam)
    o = rms_norm(o, subln, SUBLN_EPS) * (1.0 - lam_init)
    o_attn = o.reshape(bsz, t_len, DA_WIDTH)
    o_ssm = s5_mixer(u_s5, a_re, a_im, log_step, b_re, b_im, c_re, c_im, d_skip, w_glu)
    return jnp.concatenate([o_attn, o_ssm], axis=-1) @ w_out


def routed_experts(hf, expert_idx, gates, w_gate, w_up, w_down):
    n_tok, d = hf.shape
    n_slot = n_tok * MOE_TOP_K
    flat_e = expert_idx.reshape(-1)
    flat_tok = jnp.arange(n_slot, dtype=jnp.int32) // MOE_TOP_K
    flat_g = gates.reshape(-1)
    order = jnp.argsort(flat_e)
    sorted_e = flat_e[order]
    counts = jnp.bincount(flat_e, length=MOE_EXPERTS)
    padded = (counts + MOE_BLOCK - 1) // MOE_BLOCK * MOE_BLOCK
    seg_start = jnp.cumsum(counts) - counts
    cum_padded = jnp.cumsum(padded)
    pad_start = cum_padded - padded
    dest = pad_start[sorted_e] + jnp.arange(n_slot, dtype=jnp.int32) - seg_start[sorted_e]
    n_blocks = -(-n_slot // MOE_BLOCK) + MOE_EXPERTS
    n_pad = n_blocks * MOE_BLOCK
    buf_tok = jnp.full((n_pad,), n_tok, jnp.int32).at[dest].set(flat_tok[order])
    buf_gate = jnp.zeros((n_pad,), jnp.float32).at[dest].set(flat_g[order])
    block_start = jnp.arange(n_blocks, dtype=cum_padded.dtype) * MOE_BLOCK
    block_expert = jnp.minimum(jnp.searchsorted(cum_padded, block_start, side='right'),
                               MOE_EXPERTS - 1).astype(jnp.int32)
    h_pad = jnp.concatenate([hf, jnp.zeros((1, d), hf.dtype)], axis=0)

    def run_block(args):
        tok, e, gt = args
        xb = h_pad[tok]
        hidden = jax.nn.silu(xb @ w_gate[e]) * (xb @ w_up[e])
        return (hidden @ w_down[e]) * gt[:, None].astype(xb.dtype)

    ys = lax.map(run_block, (buf_tok.reshape(n_blocks, MOE_BLOCK), block_expert,
                             buf_gate.reshape(n_blocks, MOE_BLOCK)))
    out = jnp.zeros((n_tok + 1, d), hf.dtype).at[buf_tok].add(ys.reshape(n_pad, d))
    return out[:n_tok]


def hier_moe(h, w_group, b_group, w_expert, b_expert, w_gate, w_up, w_down):
    bsz, t_len, d = h.shape
    n_tok = bsz * t_len
    hf = h.reshape(n_tok, d)
    rows = jnp.arange(n_tok)
    g_logits = (hf @ w_group + b_group).astype(jnp.float32)
    g_prob = jax.nn.softmax(g_logits, axis=-1)
    g_top = jnp.argmax(g_logits, axis=-1).astype(jnp.int32)
    g_w = g_prob[rows, g_top]
    e_logits = (hf @ w_expert + b_expert).astype(jnp.float32)
    e_logits = e_logits.reshape(n_tok, MOE_GROUPS, MOE_EXPERTS_PER_GROUP)[rows, g_top]
    e_prob = jax.nn.softmax(e_logits, axis=-1)
    top_p, top_j = lax.top_k(e_prob, MOE_TOP_K)
    gates = g_w[:, None] * top_p / jnp.sum(top_p, axis=-1, keepdims=True)
    expert_idx = g_top[:, None] * MOE_EXPERTS_PER_GROUP + top_j.astype(jnp.int32)
    y = routed_experts(hf, expert_idx, gates, w_gate, w_up, w_down)
    return y.reshape(bsz, t_len, d)


def setup_inputs(seed: int = 0) -> dict:
    key = jax.random.key(seed)
    keys = jax.random.split(key, 64)
    counter = [0]

    def nk():
        counter[0] += 1
        return keys[counter[0] - 1]

    f32 = jnp.float32

    def nrm(shape, scale):
        return scale * jax.random.normal(nk(), shape, f32)

    def gain(shape, base=1.0, noise=0.02):
        return base + noise * jax.random.normal(nk(), shape, f32)

    def unif(shape, lo, hi):
        return jax.random.uniform(nk(), shape, f32, lo, hi)

    d = D_MODEL
    res = (2.0 * DEPTH) ** -0.5
    return {
        'x': nrm((BATCH, SEQ, d), 1.0),
        'norm_mix_g': gain((DEPTH, d)),
        'norm_ffn_g': gain((DEPTH, d)),
        'even_w_in': nrm((N_EVEN, d, EVEN_IN), d ** -0.5),
        'rw_mu': unif((N_EVEN, RW_SHIFT_COLS), 0.0, 1.0),
        'rw_w0': unif((N_EVEN, RW_WIDTH), -6.0, -1.0),
        'rw_w2': nrm((N_EVEN, RW_DECAY_RANK, RW_WIDTH), RW_DECAY_RANK ** -0.5),
        'rw_a0': nrm((N_EVEN, RW_WIDTH), 0.1),
        'rw_a2': nrm((N_EVEN, RW_ICLR_RANK, RW_WIDTH), RW_ICLR_RANK ** -0.5),
        'rw_g2': nrm((N_EVEN, RW_GATE_RANK, RW_WIDTH), RW_GATE_RANK ** -0.5),
        'rw_k_k': gain((N_EVEN, RW_WIDTH), 0.85, 0.05),
        'rw_k_a': gain((N_EVEN, RW_WIDTH), 1.0, 0.05),
        'rw_r_k': nrm((N_EVEN, RW_HEADS, RW_HEAD_DIM), 0.1),
        'rw_ln_g': gain((N_EVEN, RW_WIDTH)),
        'rw_ln_b': nrm((N_EVEN, RW_WIDTH), 0.02),
        'rw_v0': nrm((N_VRES, RW_WIDTH), 0.1),
        'rw_v1': nrm((N_VRES, RW_WIDTH, RW_VRES_RANK), RW_WIDTH ** -0.5),
        'rw_v2': nrm((N_VRES, RW_VRES_RANK, RW_WIDTH), RW_VRES_RANK ** -0.5),
        'pool_w': nrm((N_EVEN, POOL_GROUPS, POOL_GROUP_DIM, POOL_GROUP_DIM), POOL_GROUP_DIM ** -0.5),
        'pool_scale': gain((N_EVEN, POOL_WIDTH)),
        'even_w_out': nrm((N_EVEN, EVEN_MIX, d), EVEN_MIX ** -0.5 * res),
        'odd_w_in': nrm((N_ODD, d, ODD_IN), d ** -0.5),
        'da_q_norm': gain((N_ODD, DA_HEAD_DIM)),
        'da_k_norm': gain((N_ODD, DA_HEAD_DIM)),
        'da_lam_q1': nrm((N_ODD, DA_HEAD_DIM), 0.1),
        'da_lam_k1': nrm((N_ODD, DA_HEAD_DIM), 0.1),
        'da_lam_q2': nrm((N_ODD, DA_HEAD_DIM), 0.1),
        'da_lam_k2': nrm((N_ODD, DA_HEAD_DIM), 0.1),
        'da_subln': gain((N_ODD, DA_VALUE_DIM)),
        's5_a_re': gain((N_ODD, S5_GROUPS, S5_STATE), -0.5, 0.01),
        's5_a_im': math.pi * jnp.arange(S5_STATE, dtype=f32) + nrm((N_ODD, S5_GROUPS, S5_STATE), 0.01),
        's5_log_step': unif((N_ODD, S5_GROUPS, S5_STATE), math.log(1e-3), math.log(1e-1)),
        's5_b_re': nrm((N_ODD, S5_GROUPS, S5_STATE, S5_GROUP_DIM), (2.0 * S5_GROUP_DIM) ** -0.5),
        's5_b_im': nrm((N_ODD, S5_GROUPS, S5_STATE, S5_GROUP_DIM), (2.0 * S5_GROUP_DIM) ** -0.5),
        's5_c_re': nrm((N_ODD, S5_GROUPS, S5_GROUP_DIM, S5_STATE), (2.0 * S5_STATE) ** -0.5),
        's5_c_im': nrm((N_ODD, S5_GROUPS, S5_GROUP_DIM, S5_STATE), (2.0 * S5_STATE) ** -0.5),
        's5_d': nrm((N_ODD, S5_GROUPS, S5_GROUP_DIM), 1.0),
        's5_w_glu': nrm((N_ODD, S5_WIDTH, S5_WIDTH), S5_WIDTH ** -0.5),
        'odd_w_out': nrm((N_ODD, ODD_MIX, d), ODD_MIX ** -0.5 * res),
        'moe_w_group': nrm((DEPTH, d, MOE_GROUPS), d ** -0.5),
        'moe_b_group': nrm((DEPTH, MOE_GROUPS), 0.01),
        'moe_w_expert': nrm((DEPTH, d, MOE_EXPERTS), d ** -0.5),
        'moe_b_expert': nrm((DEPTH, MOE_EXPERTS), 0.01),
        'moe_w_gate': nrm((DEPTH, MOE_EXPERTS, d, MOE_HIDDEN), d ** -0.5),
        'moe_w_up': nrm((DEPTH, MOE_EXPERTS, d, MOE_HIDDEN), d ** -0.5),
        'moe_w_down': nrm((DEPTH, MOE_EXPERTS, MOE_HIDDEN, d), MOE_HIDDEN ** -0.5 * res),
    }


def reference(x, norm_mix_g, norm_ffn_g,
              even_w_in, rw_mu, rw_w0, rw_w2, rw_a0, rw_a2, rw_g2, rw_k_k, rw_k_a, rw_r_k,
              rw_ln_g, rw_ln_b, rw_v0, rw_v1, rw_v2, pool_w, pool_scale, even_w_out,
              odd_w_in, da_q_norm, da_k_norm, da_lam_q1, da_lam_k1, da_lam_q2, da_lam_k2, da_subln,
              s5_a_re, s5_a_im, s5_log_step, s5_b_re, s5_b_im, s5_c_re, s5_c_im, s5_d, s5_w_glu,
              odd_w_out,
              moe_w_group, moe_b_group, moe_w_expert, moe_b_expert, moe_w_gate, moe_w_up, moe_w_down):
    v_first = None
    for layer in range(DEPTH):
        i = layer // 2
        h = rms_norm(x, norm_mix_g[layer])
        if layer % 2 == 0:
            vres = None if v_first is None else (rw_v0[i - 1], rw_v1[i - 1], rw_v2[i - 1])
            mix, v_first = even_mixer(h, v_first, even_w_in[i], rw_mu[i], rw_w0[i], rw_w2[i],
                                      rw_a0[i], rw_a2[i], rw_g2[i], rw_k_k[i], rw_k_a[i], rw_r_k[i],
                                      rw_ln_g[i], rw_ln_b[i], vres, pool_w[i], pool_scale[i],
                                      even_w_out[i])
        else:
            mix = odd_mixer(h, layer, odd_w_in[i], da_q_norm[i], da_k_norm[i], da_lam_q1[i],
                            da_lam_k1[i], da_lam_q2[i], da_lam_k2[i], da_subln[i], s5_a_re[i],
                            s5_a_im[i], s5_log_step[i], s5_b_re[i], s5_b_im[i], s5_c_re[i],
                            s5_c_im[i], s5_d[i], s5_w_glu[i], odd_w_out[i])
        x = x + mix
        h = rms_norm(x, norm_ffn_g[layer])
        x = x + hier_moe(h, moe_w_group[layer], moe_b_group[layer], moe_w_expert[layer],
                         moe_b_expert[layer], moe_w_gate[layer], moe_w_up[layer], moe_w_down[layer])
    return x
```

```python
import numpy as np
from contextlib import ExitStack
import concourse.bass as bass
import concourse.mybir as mybir
from concourse.bass_utils import run_bass_kernel_spmd

F32 = mybir.dt.float32
BF16 = mybir.dt.bfloat16
I32 = mybir.dt.int32
U32 = mybir.dt.uint32
AF = mybir.ActivationFunctionType
ALU = mybir.AluOpType
AX = mybir.AxisListType

SAME_ENGINE_SYNC = True
EPOCH = 20000
DBG = {}


class Buf:
    __slots__ = ("w", "r", "name")

    def __init__(self, name=""):
        self.w = None
        self.r = {}
        self.name = name


class V:
    __slots__ = ("ap", "buf")

    def __init__(self, ap, buf):
        self.ap = ap
        self.buf = buf

    def __getitem__(self, k):
        return V(self.ap[k], self.buf)

    def re(self, s, **kw):
        return V(self.ap.rearrange(s, **kw), self.buf)

    def bc(self, shape):
        return V(self.ap.to_broadcast(list(shape)), self.buf)

    def pbc(self, n):
        return V(self.ap.partition_broadcast(n), self.buf)

    def bitcast(self, dt):
        return V(self.ap.bitcast(dt), self.buf)

    def sub(self, buf):
        return V(self.ap, buf)

    @property
    def shape(self):
        return self.ap.shape


ENGS = ("pe", "act", "dve", "pool", "sp")


class MK:
    def __init__(self, nc, stack, n_dma_slots=8):
        self.nc = nc
        self.stack = stack
        self.ops = {e: [] for e in ENGS}
        self.root = stack
        self.esem = {}
        self.cnt = {e: 0 for e in ENGS}
        self.seen = {e: {} for e in ENGS}
        self.dma_slots = {}
        self.dma_n = {}
        for q in ("sp", "pool", "act"):
            self.dma_slots[q] = [stack.enter_context(nc.semaphore("dma_%s_%d" % (q, i)))
                                 for i in range(n_dma_slots)]
            self.dma_n[q] = 0
        self.semobj = {}
        self.n_ops = 0
        self.uid = 0

    def sb(self, shape, dtype, name=None):
        self.uid += 1
        name = name or "t%d" % self.uid
        t = self.stack.enter_context(self.nc.sbuf_tensor(name + "_%d" % self.uid, list(shape), dtype))
        return V(t[:], Buf(name))

    def ps(self, shape, dtype=F32, name=None):
        self.uid += 1
        name = name or "p%d" % self.uid
        t = self.stack.enter_context(self.nc.psum_tensor(name + "_%d" % self.uid, list(shape), dtype))
        return V(t[:], Buf(name))

    def dram(self, name, shape, dtype, kind="Internal"):
        t = self.nc.dram_tensor(name, list(shape), dtype, kind=kind)
        return V(t.ap(), Buf(name))

    def _tok_need(self, e, tok, waits):
        if tok is None:
            return
        semkey, val, src, is_dma = tok
        if src == e and not is_dma:
            if e == "pe" or not SAME_ENGINE_SYNC:
                return
        if self.seen[e].get(semkey, 0) >= val:
            return
        if waits.get(semkey, 0) < val:
            waits[semkey] = val

    def op(self, e, fn, reads=(), writes=(), dma=False):
        waits = {}
        pend = getattr(self, "pending", {}).pop(e, None)
        if pend:
            waits.update(pend)
        rb = [v.buf for v in reads if v is not None]
        wb = [v.buf for v in writes if v is not None]
        for b in rb:
            self._tok_need(e, b.w, waits)
        for b in wb:
            self._tok_need(e, b.w, waits)
            for t in b.r.values():
                self._tok_need(e, t, waits)
        if dma:
            slots = self.dma_slots[e]
            n = self.dma_n[e]
            self.dma_n[e] = n + 1
            sem = slots[n % len(slots)]
            val = 16 * (n // len(slots) + 1)
            semkey = ("d", e, n % len(slots))
            self.semobj[semkey] = sem
            if val > 16:
                if self.seen[e].get(semkey, 0) < val - 16 and waits.get(semkey, 0) < val - 16:
                    waits[semkey] = val - 16
            tok = (semkey, val, e, True)
            inc = (sem, 16)
        else:
            self.cnt[e] += 1
            ep = (self.cnt[e] - 1) // EPOCH
            semkey = ("c", e, ep)
            if semkey not in self.esem:
                self.esem[semkey] = self.root.enter_context(self.nc.semaphore("sem_%s_%d" % (e, ep)))
            self.semobj[semkey] = self.esem[semkey]
            tok = (semkey, (self.cnt[e] - 1) % EPOCH + 1, e, False)
            inc = (self.esem[semkey], 1)
        for k, v in waits.items():
            self.seen[e][k] = v
        for b in wb:
            b.w = tok
            b.r = {}
        for b in rb:
            old = b.r.get(tok[0])
            if old is None or old[1] < tok[1]:
                b.r[tok[0]] = tok
        self.ops[e].append(([(self.semobj[k], v) for k, v in waits.items()], fn, inc))
        self.n_ops += 1
        return tok

    def emit(self):
        nc = self.nc
        fin = []
        for q in ("sp", "pool", "act"):
            n = self.dma_n[q]
            slots = self.dma_slots[q]
            for i in range(min(n, len(slots))):
                uses = (n - i + len(slots) - 1) // len(slots)
                fin.append((slots[i], 16 * uses))
        for e in ("pe", "act", "dve", "pool"):
            if self.cnt[e]:
                ep = (self.cnt[e] - 1) // EPOCH
                fin.append((self.esem[("c", e, ep)], (self.cnt[e] - 1) % EPOCH + 1))
        with nc.Block() as block:
            def run(eng, lst, final=None):
                for waits, fn, inc in lst:
                    for s, v in waits:
                        eng.wait_ge(s, v)
                    fn(eng).then_inc(inc[0], inc[1])
                if final:
                    for s, v in final:
                        eng.wait_ge(s, v)

            @block.tensor
            def _(eng):
                run(eng, self.ops["pe"])

            @block.scalar
            def _(eng):
                run(eng, self.ops["act"])

            @block.vector
            def _(eng):
                run(eng, self.ops["dve"])

            @block.gpsimd
            def _(eng):
                run(eng, self.ops["pool"])

            @block.sync
            def _(eng):
                run(eng, self.ops["sp"], fin)

    def dma(self, out, in_, q="sp", **kw):
        return self.op(q, lambda eng: eng.dma_start(out=out.ap, in_=in_.ap, **kw),
                       reads=[in_], writes=[out], dma=True)

    def mm(self, out, lhsT, rhs, start=True, stop=True, **kw):
        return self.op("pe", lambda eng: eng.matmul(out.ap, lhsT.ap, rhs.ap, start=start, stop=stop, **kw),
                       reads=[lhsT, rhs], writes=[out])

    def transpose(self, out, in_, ident):
        return self.op("pe", lambda eng: eng.transpose(out.ap, in_.ap, ident.ap),
                       reads=[in_, ident], writes=[out])

    def act(self, out, in_, func, bias=None, scale=1.0, accum_out=None, e="act"):
        reads = [in_]
        kw = {}
        if isinstance(bias, V):
            reads.append(bias)
            kw["bias"] = bias.ap
        elif bias is not None:
            kw["bias"] = bias
        if isinstance(scale, V):
            reads.append(scale)
            kw["scale"] = scale.ap
        else:
            kw["scale"] = scale
        writes = [out]
        if accum_out is not None:
            writes.append(accum_out)
            kw["accum_out"] = accum_out.ap
        return self.op(e, lambda eng: eng.activation(out=out.ap, in_=in_.ap, func=func, **kw),
                       reads=reads, writes=writes)

    def tt(self, out, in0, in1, op, e="dve"):
        return self.op(e, lambda eng: eng.tensor_tensor(out=out.ap, in0=in0.ap, in1=in1.ap, op=op),
                       reads=[in0, in1], writes=[out])

    def ts(self, out, in0, s1, s2=None, op0=ALU.mult, op1=None, accum_out=None, e="dve"):
        reads = [in0]
        a1 = s1
        if isinstance(s1, V):
            reads.append(s1)
            a1 = s1.ap
        a2 = s2
        if isinstance(s2, V):
            reads.append(s2)
            a2 = s2.ap
        kw = {}
        if op1 is not None:
            kw["op1"] = op1
        writes = [out]
        if accum_out is not None:
            writes.append(accum_out)
            kw["accum_out"] = accum_out.ap
        return self.op(e, lambda eng: eng.tensor_scalar(out=out.ap, in0=in0.ap, scalar1=a1, scalar2=a2,
                                                        op0=op0, **kw),
                       reads=reads, writes=writes)

    def stt(self, out, in0, scalar, in1, op0, op1, e="dve"):
        reads = [in0, in1]
        a = scalar
        if isinstance(scalar, V):
            reads.append(scalar)
            a = scalar.ap
        return self.op(e, lambda eng: eng.scalar_tensor_tensor(out=out.ap, in0=in0.ap, scalar=a, in1=in1.ap,
                                                               op0=op0, op1=op1),
                       reads=reads, writes=[out])

    def copy(self, out, in_, e="dve"):
        if e == "act":
            return self.op(e, lambda eng: eng.copy(out=out.ap, in_=in_.ap), reads=[in_], writes=[out])
        return self.op(e, lambda eng: eng.tensor_copy(out=out.ap, in_=in_.ap), reads=[in_], writes=[out])

    def memset(self, out, val, e="pool"):
        return self.op(e, lambda eng: eng.memset(out.ap, val), writes=[out])

    def reduce(self, out, in_, op=ALU.add, axis=AX.X, e="dve"):
        return self.op(e, lambda eng: eng.tensor_reduce(out=out.ap, in_=in_.ap, axis=axis, op=op),
                       reads=[in_], writes=[out])

    def recip(self, out, in_):
        return self.op("dve", lambda eng: eng.reciprocal(out=out.ap, in_=in_.ap), reads=[in_], writes=[out])

    def scan(self, out, d0, d1, init, op0=ALU.mult, op1=ALU.add):
        reads = [d0, d1]
        a = init
        if isinstance(init, V):
            reads.append(init)
            a = init.ap
        return self.op("dve", lambda eng: eng.tensor_tensor_scan(out=out.ap, data0=d0.ap, data1=d1.ap,
                                                                 initial=a, op0=op0, op1=op1),
                       reads=reads, writes=[out])

    def barrier(self):
        fin = {}
        for q in ("sp", "pool", "act"):
            n = self.dma_n[q]
            slots = self.dma_slots[q]
            for i in range(min(n, len(slots))):
                uses = (n - i + len(slots) - 1) // len(slots)
                fin[("d", q, i)] = 16 * uses
        for e in ("pe", "act", "dve", "pool"):
            if self.cnt[e]:
                ep = (self.cnt[e] - 1) // EPOCH
                fin[("c", e, ep)] = (self.cnt[e] - 1) % EPOCH + 1
        for e in ENGS:
            waits = {}
            for k, v in fin.items():
                if k[0] == "c" and k[1] == e:
                    continue
                if self.seen[e].get(k, 0) < v:
                    waits[k] = v
                    self.seen[e][k] = v
            if waits:
                if e == "sp":
                    continue_fn = None
                self.pending = getattr(self, "pending", {})
                self.pending.setdefault(e, {}).update(waits)

    def scope(self):
        return _Scope(self)


class _Scope:
    def __init__(self, mk):
        self.mk = mk

    def __enter__(self):
        self.old = self.mk.stack
        self.st = ExitStack()
        self.mk.stack = self.st
        return self

    def __exit__(self, *a):
        self.mk.barrier()
        self.mk.stack = self.old
        self.st.close()
        return False


D = 1024
T = 2048
NSEQ = 2
NTOK = NSEQ * T
NT = NTOK // 128
CAP = 512
NE = 32
NSLOT = NE * CAP
NROW_TL = NSLOT + 128
RMS_EPS = 1e-6


class DR:
    def __init__(self, mk, name, shape, dtype, kind="Internal", rows_per=128):
        self.t = mk.nc.dram_tensor(name, list(shape), dtype, kind=kind)
        self.ap = self.t.ap()
        self.rows_per = rows_per
        n = (shape[0] + rows_per - 1) // rows_per
        self.bufs = [Buf("%s_%d" % (name, i)) for i in range(n)]
        self.whole = Buf(name)

    def rows(self, r0, n):
        assert r0 % self.rows_per == 0 and n <= self.rows_per
        return V(self.ap[r0:r0 + n], self.bufs[r0 // self.rows_per])

    def all(self):
        return [V(self.ap, b) for b in self.bufs]


class Builder:
    def __init__(self, nc, stack):
        self.nc = nc
        self.mk = MK(nc, stack)
        mk = self.mk
        self.inp = {}
        self.c_ident = self.ext_in("c_ident", [128, 128], F32)
        self.c_tri = self.ext_in("c_tri", [128, 128], F32)
        self.ext_in("c_blk", [128, 128], F32)
        self.ext_in("c_masks", [3, 128, 128], F32)
        self.ident32 = mk.sb([128, 128], F32, "ident32")
        self.identb = mk.sb([128, 128], BF16, "identb")
        self.trib = mk.sb([128, 128], BF16, "trib")
        self.onesb = mk.sb([128, 128], BF16, "onesb")
        mk.dma(self.ident32, self.c_ident)
        mk.dma(self.identb, self.c_ident, q="pool")
        mk.dma(self.trib, self.c_tri, q="pool")
        mk.memset(self.onesb, 1.0)
        self.eps_t = mk.sb([128, 1], F32, "eps_t")
        mk.memset(self.eps_t, RMS_EPS)
        self.bank = [mk.ps([128, 512], F32, "bank%d" % i) for i in range(8)]

    def scratch(self, name, shape, dtype, rows_per=128):
        if not hasattr(self, "_scr"):
            self._scr = {}
        if name not in self._scr:
            self._scr[name] = DR(self.mk, name, shape, dtype, rows_per=rows_per)
        return self._scr[name]

    def ext_in(self, name, shape, dtype=F32):
        v = self.mk.dram(name, shape, dtype, kind="ExternalInput")
        self.inp[name] = v
        return v

    def rms_tile(self, xt, gbc, h32, eps=RMS_EPS, sq=None, small=None):
        mk = self.mk
        ss, rstd = small
        mk.act(sq, xt, AF.Square, accum_out=ss)
        mk.act(rstd, ss, AF.Sqrt, scale=1.0 / D, bias=self.eps_t)
        mk.recip(rstd, rstd)
        mk.stt(h32, xt, rstd, gbc, ALU.mult, ALU.mult)

    def moe(self, xres, layer, P):
        mk = self.mk
        with mk.scope():
            Hrows = self.scratch("hrows", [NTOK + 128, D], BF16)
            Ybuf = self.scratch("ybuf", [NROW_TL, D], F32)
            TokL = self.scratch("tokl", [NROW_TL, 8], I32, rows_per=NROW_TL)
            gbc = mk.sb([128, D], F32, "gbc")
            mk.dma(gbc, P["norm_ffn_g"][layer:layer + 1, :].pbc(128))
            wr = mk.sb([128, 8, 36], F32, "wr")
            mk.dma(wr, P["w_router"][layer].re("(k p) n -> p k n", p=128))
            brt = mk.sb([128, 36], F32, "brt")
            mk.dma(brt, P["b_router"][layer:layer + 1, :].pbc(128))
            maskall = mk.sb([128, NT, 32], BF16, "maskall")
            E1all = mk.sb([128, NT, 32], F32, "E1all")
            E2all = mk.sb([128, NT, 32], F32, "E2all")
            gate1 = mk.sb([128, NT], F32, "gate1")
            gate2 = mk.sb([128, NT], F32, "gate2")
            slot1 = mk.sb([128, NT], I32, "slot1")
            slot2 = mk.sb([128, NT], I32, "slot2")
            base = mk.sb([128, 32], F32, "base")
            lim = mk.sb([128, 32], F32, "lim")
            trash = mk.sb([128, 1], F32, "trash")
            zero_t = mk.sb([128, D], F32, "zero_t")
            sent = mk.sb([128, (NROW_TL // 128) * 8], I32, "sent")
            mk.op("pool", lambda eng: eng.iota(base.ap, [[CAP, 32]], base=-1, channel_multiplier=0,
                                                allow_small_or_imprecise_dtypes=True), writes=[base])
            mk.ts(lim, base, float(CAP) + 0.5, None, op0=ALU.add)
            mk.op("pool", lambda eng: eng.iota(trash.ap, [[0, 1]], base=NSLOT, channel_multiplier=1,
                                                allow_small_or_imprecise_dtypes=True), writes=[trash])
            mk.memset(zero_t, 0.0)
            mk.op("pool", lambda eng: eng.iota(sent.ap, [[0, (NROW_TL // 128) * 8]], base=NTOK,
                                                channel_multiplier=0), writes=[sent])
            mk.dma(V(TokL.ap.rearrange("(p r) c -> p (r c)", p=128), TokL.bufs[0]), sent)
            mk.dma(Hrows.rows(NTOK, 128), zero_t.bitcast(BF16)[:, 0:D])
            mk.dma(Ybuf.rows(NSLOT, 128), zero_t)

            xts = [mk.sb([128, D], F32, "xt%d" % i) for i in range(2)]
            sqs = [mk.sb([128, D], F32, "sq%d" % i) for i in range(2)]
            h32s = [mk.sb([128, D], F32, "h32%d" % i) for i in range(2)]
            hbs = [mk.sb([128, D], BF16, "hb%d" % i) for i in range(2)]
            hT32s = [mk.sb([128, 8, 128], F32, "hT32%d" % i) for i in range(2)]
            smalls = [(mk.sb([128, 1], F32), mk.sb([128, 1], F32)) for i in range(2)]
            rt = [dict(lg=mk.sb([128, 36], F32), gmax=mk.sb([128, 1], F32), ngmax=mk.sb([128, 1], F32),
                       ohg=mk.sb([128, 4], F32), eg=mk.sb([128, 4], F32), gsum=mk.sb([128, 1], F32),
                       gw=mk.sb([128, 1], F32), tmp3=mk.sb([128, 4, 8], F32), sel=mk.sb([128, 8], F32),
                       mx8=mk.sb([128, 8], F32), oh1=mk.sb([128, 8], F32), oh2=mk.sb([128, 8], F32),
                       dd=mk.sb([128, 1], F32), r1=mk.sb([128, 1], F32)) for i in range(2)]
            for i in range(NT):
                b = i % 2
                xt, sq, h32, hb, hT32, R = xts[b], sqs[b], h32s[b], hbs[b], hT32s[b], rt[b]
                mk.dma(xt, xres.rows(i * 128, 128))
                self.rms_tile(xt, gbc, h32, sq=sq, small=smalls[b])
                mk.copy(hb, h32, e="pool")
                mk.dma(Hrows.rows(i * 128, 128), hb)
                for half in range(2):
                    pb = self.bank[half]
                    for kk in range(4):
                        k = half * 4 + kk
                        mk.transpose(pb[:, kk * 128:(kk + 1) * 128], h32[:, k * 128:(k + 1) * 128], self.ident32)
                    mk.copy(hT32[:, half * 4:(half + 1) * 4, :].re("p k t -> p (k t)"), pb, e="act")
                pl = self.bank[2 + b]
                for k in range(8):
                    mk.mm(pl[:, 0:36], hT32[:, k, :], wr[:, k, :], start=(k == 0), stop=(k == 7))
                lg = R["lg"]
                mk.tt(lg, pl[:, 0:36], brt, ALU.add)
                mk.reduce(R["gmax"], lg[:, 0:4], op=ALU.max)
                mk.ts(R["ohg"], lg[:, 0:4], R["gmax"], None, op0=ALU.is_equal)
                mk.ts(R["ngmax"], R["gmax"], -1.0, None, op0=ALU.mult)
                mk.act(R["eg"], lg[:, 0:4], AF.Exp, bias=R["ngmax"], accum_out=R["gsum"])
                mk.recip(R["gw"], R["gsum"])
                mk.tt(R["tmp3"], lg[:, 4:36].re("p (g e) -> p g e", g=4),
                      V(R["ohg"].ap.unsqueeze(2).to_broadcast([128, 4, 8]), R["ohg"].buf), ALU.mult)
                mk.reduce(R["sel"], R["tmp3"].re("p g e -> p e g"), op=ALU.add)
                mk.op("dve", lambda eng, R=R: eng.max(out=R["mx8"].ap, in_=R["sel"].ap),
                      reads=[R["sel"]], writes=[R["mx8"]])
                mk.ts(R["oh1"], R["sel"], R["mx8"][:, 0:1], None, op0=ALU.is_equal)
                mk.ts(R["oh2"], R["sel"], R["mx8"][:, 1:2], None, op0=ALU.is_equal)
                mk.tt(R["dd"], R["mx8"][:, 1:2], R["mx8"][:, 0:1], ALU.subtract)
                mk.act(R["dd"], R["dd"], AF.Exp)
                mk.ts(R["dd"], R["dd"], 1.0, None, op0=ALU.add)
                mk.recip(R["r1"], R["dd"])
                mk.tt(gate1[:, i:i + 1], R["gw"], R["r1"], ALU.mult)
                mk.tt(gate2[:, i:i + 1], R["gw"], gate1[:, i:i + 1], ALU.subtract)
                ohg_b = V(R["ohg"].ap.unsqueeze(2).to_broadcast([128, 4, 8]), R["ohg"].buf)
                for oh, Eall in ((R["oh1"], E1all), (R["oh2"], E2all)):
                    oh_b = V(oh.ap.unsqueeze(1).to_broadcast([128, 4, 8]), oh.buf)
                    mk.tt(Eall[:, i, :].re("p (g e) -> p g e", g=4), ohg_b, oh_b, ALU.mult)
                mk.tt(maskall[:, i, :], E1all[:, i, :], E2all[:, i, :], ALU.add)

            posf = [mk.sb([128, 32], F32) for i in range(2)]
            okm = [mk.sb([128, 32], F32) for i in range(2)]
            tmpe = [mk.sb([128, 32], F32) for i in range(2)]
            sl = [mk.sb([128, 2], F32) for i in range(2)]
            tokid = [mk.sb([128, 8], I32) for i in range(2)]
            for i in range(NT):
                b = i % 2
                pc = self.bank[4 + b]
                mk.mm(pc[:, 0:32], self.trib, maskall[:, i, :])
                mk.mm(pc[:, 32:64], self.onesb, maskall[:, i, :])
                mk.tt(posf[b], pc[:, 0:32], base, ALU.add)
                mk.tt(base, base, pc[:, 32:64], ALU.add)
                mk.tt(okm[b], posf[b], lim, ALU.is_lt)
                mk.ts(posf[b], posf[b], trash, None, op0=ALU.subtract)
                mk.tt(posf[b], posf[b], okm[b], ALU.mult)
                mk.ts(posf[b], posf[b], trash, None, op0=ALU.add)
                for j, (Eall, slot) in enumerate(((E1all, slot1), (E2all, slot2))):
                    mk.tt(tmpe[b], posf[b], Eall[:, i, :], ALU.mult)
                    mk.reduce(sl[b][:, j:j + 1], tmpe[b], op=ALU.add)
                    mk.copy(slot[:, i:i + 1], sl[b][:, j:j + 1])
                mk.op("pool", lambda eng, t=tokid[b], i=i: eng.iota(t.ap, [[0, 8]], base=i * 128,
                                                                    channel_multiplier=1), writes=[tokid[b]])
                for slot in (slot1, slot2):
                    mk.op("pool", lambda eng, slot=slot, i=i, t=tokid[b]: eng.indirect_dma_start(
                        out=TokL.ap, out_offset=bass.IndirectOffsetOnAxis(ap=slot.ap[:, i:i + 1], axis=0),
                        in_=t.ap, in_offset=None),
                        reads=[slot, tokid[b]], writes=[V(TokL.ap, TokL.bufs[0])], dma=True)

            NCT = CAP // 128
            wg = [mk.sb([128, 8, 512], BF16, "wg%d" % i) for i in range(2)]
            wu = [mk.sb([128, 8, 512], BF16, "wu%d" % i) for i in range(2)]
            wd = [mk.sb([128, 4, D], BF16, "wd%d" % i) for i in range(2)]
            idx = [mk.sb([128, 8], I32) for i in range(4)]
            xg = [mk.sb([128, D], BF16) for i in range(4)]
            xgT = [mk.sb([128, 8, CAP], BF16) for i in range(2)]
            hidT = [mk.sb([128, 4, CAP], BF16) for i in range(2)]
            sil = [mk.sb([128, CAP], F32) for i in range(2)]
            yrow = [mk.sb([128, D], F32) for i in range(2)]
            nslot = 0
            ny = 0
            for e in range(NE):
                b = e % 2
                mk.dma(wg[b], P["moe_w_gate"][layer, e].re("(k p) n -> p k n", p=128), q="pool")
                mk.dma(wu[b], P["moe_w_up"][layer, e].re("(k p) n -> p k n", p=128), q="pool")
                mk.dma(wd[b], P["moe_w_down"][layer, e].re("(k p) n -> p k n", p=128), q="pool")
                for j in range(NCT):
                    s = nslot % 4
                    nslot += 1
                    r0 = e * CAP + j * 128
                    mk.dma(idx[s], V(TokL.ap[r0:r0 + 128, :], TokL.bufs[0]))
                    mk.op("pool", lambda eng, s=s: eng.indirect_dma_start(
                        out=xg[s].ap, out_offset=None, in_=Hrows.ap,
                        in_offset=bass.IndirectOffsetOnAxis(ap=idx[s].ap[:, 0:1], axis=0)),
                        reads=[idx[s]] + Hrows.all(), writes=[xg[s]], dma=True)
                    pb = self.bank[j % 2]
                    pbb = pb.bitcast(BF16)
                    for k in range(8):
                        mk.transpose(pbb[:, k * 128:(k + 1) * 128], xg[s][:, k * 128:(k + 1) * 128], self.identb)
                    mk.copy(xgT[b][:, :, j * 128:(j + 1) * 128], pbb.re("p (k t) -> p k t", k=8),
                            e=("act" if j % 2 else "dve"))
                for c in range(4):
                    pg = self.bank[2 + (c % 2)]
                    pu = self.bank[4 + (c % 2)]
                    for k in range(8):
                        mk.mm(pg, wg[b][:, k, c * 128:(c + 1) * 128], xgT[b][:, k, :], start=(k == 0), stop=(k == 7))
                    for k in range(8):
                        mk.mm(pu, wu[b][:, k, c * 128:(c + 1) * 128], xgT[b][:, k, :], start=(k == 0), stop=(k == 7))
                    mk.act(sil[c % 2], pg, AF.Silu)
                    mk.tt(hidT[b][:, c, :], sil[c % 2], pu, ALU.mult)
                for j in range(NCT):
                    yb = ny % 2
                    ny += 1
                    for half in range(2):
                        pd = self.bank[6 + half]
                        for c in range(4):
                            mk.mm(pd, hidT[b][:, c, j * 128:(j + 1) * 128], wd[b][:, c, half * 512:(half + 1) * 512],
                                  start=(c == 0), stop=(c == 3))
                        mk.copy(yrow[yb][:, half * 512:(half + 1) * 512], pd, e=("act" if half else "dve"))
                    mk.dma(Ybuf.rows(e * CAP + j * 128, 128), yrow[yb])

            y1 = [mk.sb([128, D], F32) for i in range(2)]
            y2 = [mk.sb([128, D], F32) for i in range(2)]
            for i in range(NT):
                b = i % 2
                xt = xts[b]
                mk.dma(xt, xres.rows(i * 128, 128))
                for slot, yy in ((slot1, y1[b]), (slot2, y2[b])):
                    mk.op("pool", lambda eng, slot=slot, yy=yy, i=i: eng.indirect_dma_start(
                        out=yy.ap, out_offset=None, in_=Ybuf.ap,
                        in_offset=bass.IndirectOffsetOnAxis(ap=slot.ap[:, i:i + 1], axis=0)),
                        reads=[slot] + Ybuf.all(), writes=[yy], dma=True)
                mk.stt(xt, y1[b], gate1[:, i:i + 1], xt, ALU.mult, ALU.add)
                mk.stt(xt, y2[b], gate2[:, i:i + 1], xt, ALU.mult, ALU.add)
                mk.dma(xres.rows(i * 128, 128), xt)


def host_consts():
    ident = np.eye(128, dtype=np.float32)
    tri = np.triu(np.ones((128, 128), np.float32))
    return {"c_ident": ident, "c_tri": tri}


MOE_KEYS = ("norm_ffn_g", "w_router", "b_router", "moe_w_gate", "moe_w_up", "moe_w_down")


def host_moe_params(inputs):
    out = {}
    out["norm_ffn_g"] = np.ascontiguousarray(inputs["norm_ffn_g"], dtype=np.float32)
    out["w_router"] = np.ascontiguousarray(
        np.concatenate([inputs["moe_w_group"], inputs["moe_w_expert"]], axis=-1), dtype=np.float32)
    out["b_router"] = np.ascontiguousarray(
        np.concatenate([inputs["moe_b_group"], inputs["moe_b_expert"]], axis=-1), dtype=np.float32)
    for k in ("moe_w_gate", "moe_w_up", "moe_w_down"):
        out[k] = np.ascontiguousarray(inputs[k], dtype=np.float32)
    return out


TWO_PI = 6.283185307179586
CW1 = 6.28125
CW2 = TWO_PI - CW1
PI_SAFE = 3.1415925


def _rr_sin(self, dst, X, tmpf, tmpi, phase=0.0, e="dve"):
    mk = self.mk
    mk.ts(tmpf, X, 1.0 / TWO_PI, 0.5 + phase / TWO_PI, op0=ALU.mult, op1=ALU.add, e=e)
    mk.copy(tmpi, tmpf, e=e)
    mk.copy(tmpf, tmpi, e=e)
    mk.stt(dst, tmpf, -CW1, X, ALU.mult, ALU.add)
    mk.stt(dst, tmpf, -CW2, dst, ALU.mult, ALU.add)
    if phase:
        mk.ts(dst, dst, phase, None, op0=ALU.add, e=e)
    mk.ts(tmpf, dst, -PI_SAFE, TWO_PI, op0=ALU.is_lt, op1=ALU.mult, e=e)
    mk.tt(dst, dst, tmpf, ALU.add, e=e)
    mk.ts(tmpf, dst, PI_SAFE, TWO_PI, op0=ALU.is_gt, op1=ALU.mult, e=e)
    mk.tt(dst, dst, tmpf, ALU.subtract, e=e)
    mk.ts(dst, dst, PI_SAFE, -PI_SAFE, op0=ALU.min, op1=ALU.max, e=e)
    mk.act(dst, dst, AF.Sin)


Builder.rr_sin = _rr_sin


def _proj_in(self, xres, g_row, W, ncols, fm_blocks, uT, tm_range, u_tm):
    mk = self.mk
    with mk.scope():
        gbc = mk.sb([128, D], F32, "gbc")
        mk.dma(gbc, g_row.pbc(128))
        Wb = mk.sb([128, 8, ncols], BF16, "Wb")
        for k in range(8):
            mk.dma(Wb[:, k, :], W[k * 128:(k + 1) * 128, :], q="pool")
        xts = [mk.sb([128, D], F32) for i in range(2)]
        sqs = [mk.sb([128, D], F32) for i in range(2)]
        hbs = [mk.sb([128, D], BF16) for i in range(2)]
        smalls = [(mk.sb([128, 1], F32), mk.sb([128, 1], F32)) for i in range(2)]
        hT = [mk.sb([128, 8, 512], BF16) for i in range(2)]
        ev = [mk.sb([128, 512], F32) for i in range(4)]
        nev = 0
        for gidx in range(NTOK // 512):
            hb_ = hT[gidx % 2]
            for tl in range(4):
                i = gidx * 4 + tl
                b = i % 2
                mk.dma(xts[b], xres.rows(i * 128, 128))
                self.rms_tile(xts[b], gbc, hbs[b], sq=sqs[b], small=smalls[b])
                pbb = self.bank[b].bitcast(BF16)
                for k in range(8):
                    mk.transpose(pbb[:, k * 128:(k + 1) * 128], hbs[b][:, k * 128:(k + 1) * 128], self.identb)
                mk.copy(hb_[:, :, tl * 128:(tl + 1) * 128], pbb.re("p (k t) -> p k t", k=8),
                        e=("act" if tl % 2 else "pool_never") if False else ("act" if tl % 2 else "dve"))
            for bi, cb in enumerate(fm_blocks):
                pb = self.bank[2 + (bi % 3)]
                for k in range(8):
                    mk.mm(pb, Wb[:, k, cb * 128:(cb + 1) * 128], hb_[:, k, :], start=(k == 0), stop=(k == 7))
                t = ev[nev % 4]
                mk.copy(t, pb, e=("act" if nev % 2 else "dve"))
                nev += 1
                mk.dma(V(uT.ap[bi * 128:(bi + 1) * 128, gidx * 512:(gidx + 1) * 512], uT.bufs[bi]), t)
            if tm_range is not None:
                c0, c1 = tm_range
                for tl in range(4):
                    i = gidx * 4 + tl
                    for cc in range(c0, c1, 512):
                        pb = self.bank[5 + (nev % 3)]
                        for k in range(8):
                            mk.mm(pb, hb_[:, k, tl * 128:(tl + 1) * 128], Wb[:, k, cc:cc + 512],
                                  start=(k == 0), stop=(k == 7))
                        t = ev[nev % 4]
                        mk.copy(t, pb, e=("act" if nev % 2 else "dve"))
                        nev += 1
                        mk.dma(V(u_tm.ap[i * 128:(i + 1) * 128, cc - c0:cc - c0 + 512], u_tm.bufs[i]), t)


Builder.proj_in = _proj_in


def _proj_out(self, xres_in, xres_out, oTd, Wout):
    mk = self.mk
    with mk.scope():
        Wb = mk.sb([128, 8, D], BF16, "Wob")
        for k in range(8):
            mk.dma(Wb[:, k, :], Wout[k * 128:(k + 1) * 128, :], q="pool")
        xts = [mk.sb([128, D], F32) for i in range(2)]
        ot = [mk.sb([128, 8, 512], BF16) for i in range(2)]
        for gi in range(NTOK // 512):
            o_ = ot[gi % 2]
            for k in range(8):
                mk.dma(o_[:, k, :], V(oTd.ap[k * 128:(k + 1) * 128, gi * 512:(gi + 1) * 512], oTd.bufs[k]))
            for tl in range(4):
                i = gi * 4 + tl
                b = i % 2
                mk.dma(xts[b], xres_in.rows(i * 128, 128))
                for half in range(2):
                    pb = self.bank[(i % 2) * 2 + half]
                    for k in range(8):
                        mk.mm(pb, o_[:, k, tl * 128:(tl + 1) * 128], Wb[:, k, half * 512:(half + 1) * 512],
                              start=(k == 0), stop=(k == 7))
                    mk.tt(xts[b][:, half * 512:(half + 1) * 512], xts[b][:, half * 512:(half + 1) * 512], pb, ALU.add)
                mk.dma(xres_out.rows(i * 128, 128), xts[b])


Builder.proj_out = _proj_out


def _attn(self, u_tm, oT, P, layer):
    import math
    mk = self.mk
    oi = layer // 2
    lam_init = 0.8 - 0.6 * math.exp(-0.3 * layer)
    with mk.scope():
        gqk = mk.sb([128, D], F32, "gqk")
        mk.dma(gqk, P["da_qk_gain"][oi:oi + 1, :].pbc(128))
        subg = mk.sb([128, 128], F32, "subg")
        mk.dma(subg, P["da_subln"][oi:oi + 1, :].pbc(128))
        mk.ts(subg, subg, 1.0 - lam_init, None, op0=ALU.mult)
        lamv = mk.sb([128, 4, 64], F32, "lamv")
        mk.dma(lamv.re("p a d -> p (a d)"), P["da_lam"][oi:oi + 1].re("o a d -> o (a d)").pbc(128))
        lt = mk.sb([128, 2, 64], F32)
        ls = mk.sb([128, 2], F32)
        mk.tt(lt[:, 0, :], lamv[:, 0, :], lamv[:, 1, :], ALU.mult)
        mk.tt(lt[:, 1, :], lamv[:, 2, :], lamv[:, 3, :], ALU.mult)
        mk.reduce(ls, lt, op=ALU.add)
        mk.act(ls, ls, AF.Exp)
        nlam = mk.sb([128, 1], F32, "nlam")
        mk.tt(nlam, ls[:, 1:2], ls[:, 0:1], ALU.subtract)
        mk.ts(nlam, nlam, -lam_init, None, op0=ALU.add)
        eps5 = mk.sb([128, 1], F32)
        mk.memset(eps5, 1e-5)
        nshift = mk.sb([128, 1], F32)
        mk.memset(nshift, -4.0)
        zb = mk.sb([128, 512], BF16, "zb")
        mk.memset(zb, 0.0)
        jf = mk.sb([128, 32], F32)
        mk.op("pool", lambda eng: eng.iota(jf.ap, [[1, 32]], base=0, channel_multiplier=0,
                                            allow_small_or_imprecise_dtypes=True), writes=[jf])
        mk.act(jf, jf, AF.Exp, scale=-math.log(10000.0) / 32.0)
        posf = mk.sb([128, 16], F32)
        mk.op("pool", lambda eng: eng.iota(posf.ap, [[128, 16]], base=0, channel_multiplier=1,
                                            allow_small_or_imprecise_dtypes=True), writes=[posf])
        ang = mk.sb([128, 16, 32], F32)
        mk.tt(ang, V(jf.ap.unsqueeze(1).to_broadcast([128, 16, 32]), jf.buf),
              V(posf.ap.unsqueeze(2).to_broadcast([128, 16, 32]), posf.buf), ALU.mult)
        sint = mk.sb([128, 16, 32], F32, "sint")
        cost = mk.sb([128, 16, 32], F32, "cost")
        tf = mk.sb([128, 16, 32], F32)
        ti = mk.sb([128, 16, 32], I32)
        self.rr_sin(sint, ang, tf, ti)
        self.rr_sin(cost, ang, tf, ti, phase=math.pi / 2)

        QT = mk.sb([128, 4, T], BF16, "QT")
        KT = mk.sb([128, 4, T], BF16, "KT")
        Vt = mk.sb([128, 16, 512], BF16, "Vt")
        qk = [mk.sb([128, 16, 2, 32], F32) for i in range(2)]
        sq = mk.sb([128, 16, 64], F32)
        ss = mk.sb([128, 16], F32)
        ta = mk.sb([128, 16, 32], F32)
        tb = mk.sb([128, 16, 32], F32)
        qr = [mk.sb([128, 16, 2, 32], BF16) for i in range(2)]
        pts = [mk.sb([128, 512], BF16) for i in range(3)]
        rl = mk.sb([128, 8], F32)
        ob32 = [mk.sb([128, 128], F32) for i in range(2)]
        obb = [mk.sb([128, 128], BF16) for i in range(2)]
        junk = mk.sb([128, 128], F32)
        ss1 = [mk.sb([128, 1], F32) for i in range(2)]
        npt = 0
        nfin = 0
        ostage = [mk.sb([128, T], BF16, "ostage%d" % i) for i in range(2)]
        for s in range(NSEQ):
            mk.dma(Vt, V(u_tm.ap[s * T:(s + 1) * T, 1024:1536].rearrange("(i p) c -> p i c", p=128),
                         u_tm.whole), q="pool", )
            for i in range(16):
                b = i % 2
                row0 = s * T + i * 128
                q_ = qk[b]
                qf = q_.re("p g m d -> p (g m d)")
                mk.dma(qf, V(u_tm.ap[row0:row0 + 128, 0:1024], u_tm.bufs[row0 // 128]))
                mk.act(sq.re("p g d -> p (g d)"), qf, AF.Square)
                mk.reduce(ss, sq, op=ALU.add)
                mk.act(ss, ss, AF.Sqrt, scale=1.0 / 64.0, bias=self.eps_t)
                mk.recip(ss, ss)
                q3 = q_.re("p g m d -> p g (m d)")
                mk.tt(q3, q3, V(ss.ap.unsqueeze(2).to_broadcast([128, 16, 64]), ss.buf), ALU.mult)
                mk.tt(qf, qf, gqk, ALU.mult)
                cb_ = V(cost.ap[:, i, :].unsqueeze(1).to_broadcast([128, 16, 32]), cost.buf)
                sb_ = V(sint.ap[:, i, :].unsqueeze(1).to_broadcast([128, 16, 32]), sint.buf)
                x1 = q_[:, :, 0, :]
                x2 = q_[:, :, 1, :]
                mk.tt(ta, x1, cb_, ALU.mult)
                mk.tt(tb, x2, sb_, ALU.mult, e="pool")
                mk.tt(qr[b][:, :, 0, :], ta, tb, ALU.subtract)
                mk.tt(ta, x2, cb_, ALU.mult)
                mk.tt(tb, x1, sb_, ALU.mult, e="pool")
                mk.tt(qr[b][:, :, 1, :], ta, tb, ALU.add)
                qrf = qr[b].re("p g m d -> p (g m d)")
                pbb = self.bank[6 + b].bitcast(BF16)
                for k in range(8):
                    mk.transpose(pbb[:, k * 128:(k + 1) * 128], qrf[:, k * 128:(k + 1) * 128], self.identb)
                mk.copy(QT[:, :, i * 128:(i + 1) * 128], pbb[:, 0:512].re("p (h t) -> p h t", h=4), e="act")
                mk.copy(KT[:, :, i * 128:(i + 1) * 128], pbb[:, 512:1024].re("p (h t) -> p h t", h=4), e="act")
            for h in range(4):
                for qc in range(4):
                    O = [self.bank[2], self.bank[3]]
                    Lb = self.bank[4]
                    mk.mm(O[0], zb[:, 0:128], zb)
                    mk.mm(O[1], zb[:, 0:128], zb)
                    mk.mm(Lb[:, 0:8], zb[:, 0:128], zb[:, 0:8])
                    for m in range(2):
                        for kt in range(4 * qc + 4):
                            q0 = max(kt * 128, qc * 512)
                            nq = (qc + 1) * 512 - q0
                            S = self.bank[npt % 2]
                            Pt = pts[npt % 3]
                            npt += 1
                            mk.mm(S[:, 0:nq], KT[m * 64:(m + 1) * 64, h, kt * 128:(kt + 1) * 128],
                                  QT[m * 64:(m + 1) * 64, h, q0:q0 + nq])
                            mk.act(Pt[:, 0:nq], S[:, 0:nq], AF.Exp, scale=0.125, bias=nshift)
                            if kt >= 4 * qc:
                                mk.tt(Pt[:, 0:128], Pt[:, 0:128], self.trib, ALU.mult, e="pool")
                            for qb in range(max(kt, 4 * qc), 4 * qc + 4):
                                ql = qb - 4 * qc
                                c0 = qb * 128 - q0
                                mk.mm(O[m][:, ql * 128:(ql + 1) * 128], Pt[:, c0:c0 + 128],
                                      Vt[:, kt, h * 128:(h + 1) * 128], start=False, stop=(kt == qb),
                                      skip_group_check=True)
                                mk.mm(Lb[:, m * 4 + ql:m * 4 + ql + 1], Pt[:, c0:c0 + 128], self.onesb[:, 0:1],
                                      start=False, stop=(kt == qb), skip_group_check=True)
                    mk.recip(rl, Lb[:, 0:8])
                    mk.ts(rl[:, 4:8], rl[:, 4:8], nlam, None, op0=ALU.mult)
                    for ql in range(4):
                        fb = nfin % 2
                        nfin += 1
                        o = ob32[fb]
                        mk.ts(o, O[0][:, ql * 128:(ql + 1) * 128], rl[:, ql:ql + 1], None, op0=ALU.mult)
                        mk.stt(o, O[1][:, ql * 128:(ql + 1) * 128], rl[:, 4 + ql:5 + ql], o, ALU.mult, ALU.add)
                        mk.act(junk, o, AF.Square, accum_out=ss1[fb])
                        mk.act(ss1[fb], ss1[fb], AF.Sqrt, scale=1.0 / 128.0, bias=eps5)
                        mk.recip(ss1[fb], ss1[fb])
                        mk.stt(obb[fb], o, ss1[fb], subg, ALU.mult, ALU.mult)
                        ptr = self.bank[5].bitcast(BF16)
                        mk.transpose(ptr[:, fb * 128:(fb + 1) * 128], obb[fb], self.identb)
                        t0 = (4 * qc + ql) * 128
                        mk.copy(ostage[h % 2][:, t0:t0 + 128], ptr[:, fb * 128:(fb + 1) * 128], e="act")
                mk.dma(V(oT.ap[h * 128:(h + 1) * 128, s * T:(s + 1) * T], oT.bufs[h]), ostage[h % 2])


Builder.attn = _attn


def _s5_params(self, a_re, a_im, lstep, shape, want_coef):
    import math
    mk = self.mk
    n = lambda: mk.sb(shape, F32)
    are, step, lr, th, rho = n(), n(), n(), n(), n()
    mk.ts(are, a_re, -1e-4, None, op0=ALU.min)
    mk.act(step, lstep, AF.Exp)
    mk.tt(lr, are, step, ALU.mult)
    mk.tt(th, a_im, step, ALU.mult)
    mk.act(rho, lr, AF.Exp)
    out = dict(rho=rho, th=th)
    if want_coef:
        sn, cs, tf, x, y, den, cre, cim = n(), n(), n(), n(), n(), n(), n(), n()
        ti = mk.sb(shape, I32)
        self.rr_sin(sn, th, tf, ti)
        self.rr_sin(cs, th, tf, ti, phase=math.pi / 2)
        mk.tt(x, rho, cs, ALU.mult)
        mk.ts(x, x, -1.0, None, op0=ALU.add)
        mk.tt(y, rho, sn, ALU.mult)
        mk.tt(den, are, are, ALU.mult)
        mk.tt(tf, a_im, a_im, ALU.mult)
        mk.tt(den, den, tf, ALU.add)
        mk.recip(den, den)
        mk.tt(cre, x, are, ALU.mult)
        mk.tt(tf, y, a_im, ALU.mult)
        mk.tt(cre, cre, tf, ALU.add)
        mk.tt(cre, cre, den, ALU.mult)
        mk.tt(cim, y, are, ALU.mult)
        mk.tt(tf, x, a_im, ALU.mult)
        mk.tt(cim, cim, tf, ALU.subtract)
        mk.tt(cim, cim, den, ALU.mult)
        out.update(cre=cre, cim=cim)
    return out


Builder.s5_params = _s5_params


def _s5(self, uT, oT, P, layer):
    import math
    mk = self.mk
    oi = layer // 2
    with mk.scope():
        bbr = mk.sb([128, 4, 128], BF16, "bbr")
        bbi = mk.sb([128, 4, 128], BF16, "bbi")
        rho = mk.sb([128, 16], F32, "rho16")
        theta = mk.sb([128, 16], F32, "th16")
        bfr = mk.sb([128, 16, 128], BF16, "bfr")
        bfi = mk.sb([128, 16, 128], BF16, "bfi")
        Cfr = mk.sb([128, 16, 128], BF16, "Cfr")
        Cfi = mk.sb([128, 16, 128], BF16, "Cfi")
        with mk.scope():
            rep = mk.sb([128, 3, 512], F32, "rep")
            mk.dma(rep, P["s5_rep"][oi].re("a p j s -> p a (j s)"))
            pr_ = self.s5_params(rep[:, 0, :], rep[:, 1, :], rep[:, 2, :], [128, 512], True)
            Bre = mk.sb([128, 512], F32)
            Bim = mk.sb([128, 512], F32)
            mk.dma(Bre, P["s5_bbd_re"][oi].re("p j s -> p (j s)"))
            mk.dma(Bim, P["s5_bbd_im"][oi].re("p j s -> p (j s)"))
            t1p = mk.sb([128, 512], F32)
            t2p = mk.sb([128, 512], F32)
            mk.tt(t1p, Bre, pr_["cre"], ALU.mult)
            mk.tt(t2p, Bim, pr_["cim"], ALU.mult)
            mk.tt(bbr.re("p j s -> p (j s)"), t1p, t2p, ALU.subtract)
            mk.tt(t1p, Bre, pr_["cim"], ALU.mult)
            mk.tt(t2p, Bim, pr_["cre"], ALU.mult)
            mk.tt(bbi.re("p j s -> p (j s)"), t1p, t2p, ALU.add)
            mk.memset(bfr, 0.0)
            mk.memset(bfi, 0.0)
            for q in range(4):
                for (src, dst) in ((bbr, bfr), (bbi, bfi)):
                    mk.copy(dst[32 * q:32 * q + 32].re("p (j q) s -> p j q s", q=4)[:, :, q, :],
                            src[32 * q:32 * q + 32, :, :], e="pool")
            st = mk.sb([128, 3, 16], F32, "st")
            mk.dma(st, P["s5_st"][oi].re("a p b -> p a b"))
            ps_ = self.s5_params(st[:, 0, :], st[:, 1, :], st[:, 2, :], [128, 16], False)
            mk.copy(rho, ps_["rho"])
            mk.copy(theta, ps_["th"])
        Cre = mk.sb([128, 16, 32], BF16, "Cre")
        nCim = mk.sb([128, 16, 32], BF16, "nCim")
        cim32 = mk.sb([128, 16, 32], F32)
        mk.dma(Cre, P["s5_cbd_re"][oi], q="pool")
        mk.dma(cim32, P["s5_cbd_im"][oi])
        mk.ts(nCim, cim32, -1.0, None, op0=ALU.mult)
        mk.memset(Cfr, 0.0)
        mk.memset(Cfi, 0.0)
        for q in range(4):
            for (src, dst) in ((Cre, Cfr), (nCim, Cfi)):
                mk.copy(dst.re("p (j q) c -> p j q c", q=4)[:, :, q, 32 * q:32 * q + 32],
                        src.re("p (j q) c -> p j q c", q=4)[:, :, q, :], e="pool")
        dcol = mk.sb([128, 4], F32, "dcol")
        mk.dma(dcol, P["s5_dcol"][oi])
        tio = mk.sb([128, T], F32, "tio")
        mk.op("pool", lambda eng: eng.iota(tio.ap, [[1, T]], base=0, channel_multiplier=0,
                                            allow_small_or_imprecise_dtypes=True), writes=[tio])
        ubb = mk.sb([128, NTOK], BF16, "ubb")
        zT = [mk.sb([128, NTOK], BF16, "zT%d" % i) for i in range(4)]
        yT = mk.sb([128, NTOK], F32, "yT")
        sint = mk.sb([128, T], F32, "sint")
        cost = mk.sb([128, T], F32, "cost")
        gre = mk.sb([128, T], F32, "gre")
        gim = mk.sb([128, T], F32, "gim")
        wre = mk.sb([128, T], F32, "wre")
        wim = mk.sb([128, T], F32, "wim")
        xre = mk.sb([128, T], BF16, "xre")
        xim = mk.sb([128, T], BF16, "xim")
        t1 = mk.sb([128, 512], F32)
        t2 = mk.sb([128, 512], F32)
        nb = 0
        for cb in range(4):
            mk.dma(ubb, V(uT.ap[cb * 128:(cb + 1) * 128, :], uT.bufs[cb]), q="pool")
            for q in range(4):
                sbi = 4 * cb + q
                mk.ts(gre, tio, theta[:, sbi:sbi + 1], None, op0=ALU.mult)
                self.rr_sin(sint, gre, gim, wre.bitcast(I32))
                self.rr_sin(cost, gre, gim, wre.bitcast(I32), phase=math.pi / 2)
                rho_b = V(rho.ap[:, sbi:sbi + 1].to_broadcast([128, T]), rho.buf)
                for s in range(NSEQ):
                    for ch in range(4):
                        tok0 = s * T + ch * 512
                        sl = slice(ch * 512, (ch + 1) * 512)
                        pr = self.bank[nb % 2]
                        pi = self.bank[2 + nb % 2]
                        nb += 1
                        mk.mm(pr, bfr[:, sbi, :], ubb[:, tok0:tok0 + 512])
                        mk.mm(pi, bfi[:, sbi, :], ubb[:, tok0:tok0 + 512])
                        mk.tt(t1, pr, cost[:, sl], ALU.mult)
                        mk.tt(t2, pi, sint[:, sl], ALU.mult)
                        mk.tt(gre[:, sl], t1, t2, ALU.add, e="pool")
                        mk.tt(t1, pi, cost[:, sl], ALU.mult)
                        mk.tt(t2, pr, sint[:, sl], ALU.mult)
                        mk.tt(gim[:, sl], t1, t2, ALU.subtract, e="pool")
                    mk.scan(wre, rho_b, gre, 0.0)
                    mk.scan(wim, rho_b, gim, 0.0)
                    mk.tt(gre, cost, wre, ALU.mult, e="pool")
                    mk.tt(gim, sint, wim, ALU.mult, e="pool")
                    mk.tt(xre, gre, gim, ALU.subtract, e="pool")
                    mk.tt(gre, sint, wre, ALU.mult)
                    mk.tt(gim, cost, wim, ALU.mult)
                    mk.tt(xim, gre, gim, ALU.add)
                    for ch in range(4):
                        tok0 = s * T + ch * 512
                        sl = slice(ch * 512, (ch + 1) * 512)
                        py = self.bank[4 + ch % 2]
                        mk.mm(py, Cfr[:, sbi, :], xre[:, sl], start=True, stop=False)
                        mk.mm(py, Cfi[:, sbi, :], xim[:, sl], start=False, stop=True)
                        if q == 0:
                            mk.copy(yT[:, tok0:tok0 + 512], py, e="act")
                        else:
                            mk.tt(yT[:, tok0:tok0 + 512], yT[:, tok0:tok0 + 512], py, ALU.add)
            for s in range(NSEQ):
                ys = yT[:, s * T:(s + 1) * T]
                mk.dma(gre, V(uT.ap[cb * 128:(cb + 1) * 128, s * T:(s + 1) * T], uT.bufs[cb]))
                mk.stt(ys, gre, dcol[:, cb:cb + 1], ys, ALU.mult, ALU.add)
                mk.tt(gim, ys, ys, ALU.mult, e="pool")
                mk.ts(gim, gim, 0.044715, 1.0, op0=ALU.mult, op1=ALU.add)
                mk.tt(gim, gim, ys, ALU.mult, e="pool")
                mk.act(gim, gim, AF.Sigmoid, scale=2.0 * math.sqrt(2.0 / math.pi))
                mk.tt(zT[cb][:, s * T:(s + 1) * T], ys, gim, ALU.mult)
        wgl = mk.sb([128, 4, 512], BF16, "wgl")
        for k in range(4):
            mk.dma(wgl[:, k, :], P["s5_w_glu"][oi, k * 128:(k + 1) * 128, :], q="pool")
        sg = [mk.sb([128, 512], F32) for i in range(4)]
        obuf = [mk.sb([128, 512], BF16) for i in range(4)]
        for ch in range(NTOK // 512):
            sl = slice(ch * 512, (ch + 1) * 512)
            for cbo in range(4):
                pg = self.bank[cbo]
                for k in range(4):
                    mk.mm(pg, wgl[:, k, cbo * 128:(cbo + 1) * 128], zT[k][:, sl], start=(k == 0), stop=(k == 3))
                mk.act(sg[cbo], pg, AF.Sigmoid)
            for cbo in range(4):
                ob_ = obuf[(ch * 4 + cbo) % 4]
                mk.tt(ob_, zT[cbo][:, sl], sg[cbo], ALU.mult, e=("pool" if cbo % 2 else "dve"))
                mk.dma(V(oT.ap[(4 + cbo) * 128:(5 + cbo) * 128, sl], oT.bufs[4 + cbo]), ob_)


Builder.s5 = _s5


def _odd_layer(self, xres_in, xres_out, layer, P):
    mk = self.mk
    oi = layer // 2
    u_tm = self.scratch("u_tm", [NTOK, 1536], F32)
    uT = self.scratch("uTo", [512, NTOK], F32)
    self.proj_in(xres_in, P["norm_mix_g"][layer:layer + 1, :], P["odd_w_in"][oi], 2048,
                 [12, 13, 14, 15], uT, (0, 1536), u_tm)
    oT = self.scratch("oTd", [D, NTOK], BF16)
    self.attn(u_tm, oT, P, layer)
    self.s5(uT, oT, P, layer)
    self.proj_out(xres_in, xres_out, oT, P["odd_w_out"][oi])


Builder.odd_layer = _odd_layer


def host_odd_params(inputs):
    f = lambda a: np.ascontiguousarray(a, dtype=np.float32)
    out = {}
    out["norm_mix_g"] = f(inputs["norm_mix_g"])
    out["odd_w_in"] = f(inputs["odd_w_in"])
    out["odd_w_out"] = f(inputs["odd_w_out"])
    n_odd = inputs["odd_w_in"].shape[0]
    out["da_qk_gain"] = f(np.concatenate([np.tile(inputs["da_q_norm"], (1, 8)),
                                          np.tile(inputs["da_k_norm"], (1, 8))], axis=1))
    out["da_lam"] = f(np.stack([inputs["da_lam_q1"], inputs["da_lam_k1"],
                                inputs["da_lam_q2"], inputs["da_lam_k2"]], axis=1))
    out["da_subln"] = f(inputs["da_subln"])
    three = np.stack([inputs["s5_a_re"], inputs["s5_a_im"], inputs["s5_log_step"]], axis=1)
    t16 = three.reshape(n_odd, 3, 16, 128)
    rep = t16.reshape(n_odd, 3, 4, 4, 128)
    rep = np.transpose(rep, (0, 1, 3, 2, 4))
    rep = np.repeat(rep[:, :, :, None, :, :], 32, axis=3)
    out["s5_rep"] = f(rep.reshape(n_odd, 3, 128, 4, 128))
    out["s5_st"] = f(np.transpose(t16, (0, 1, 3, 2)))
    for nm, key in (("s5_bbd_re", "s5_b_re"), ("s5_bbd_im", "s5_b_im")):
        Bm = inputs[key]
        bd = np.zeros((n_odd, 4, 2, 16, 4, 2, 64), np.float32)
        for j in range(4):
            for q in range(4):
                for gl in range(2):
                    g = 2 * (4 * j + q) + gl
                    bd[:, q, gl, :, j, gl, :] = np.transpose(Bm[:, g], (0, 2, 1))
        out[nm] = f(bd.reshape(n_odd, 128, 4, 128))
    for nm, key in (("s5_cbd_re", "s5_c_re"), ("s5_cbd_im", "s5_c_im")):
        Cm = inputs[key]
        bd = np.zeros((n_odd, 2, 64, 16, 2, 16), np.float32)
        for sbi in range(16):
            for gl in range(2):
                bd[:, gl, :, sbi, gl, :] = np.transpose(Cm[:, 2 * sbi + gl], (0, 2, 1))
        out[nm] = f(bd.reshape(n_odd, 128, 16, 32))
    out["s5_dcol"] = f(np.transpose(inputs["s5_d"].reshape(n_odd, 4, 128), (0, 2, 1)))
    out["s5_w_glu"] = f(inputs["s5_w_glu"])
    return out


LCH = 64
NCH = T // LCH
DECAY_C = 0.6065306597126334


def _load_shift_mix(self, dst, uT, blk, s, mu_col, U, dtmp):
    mk = self.mk
    mk.dma(U[:, 1:T + 1], V(uT.ap[blk * 128:(blk + 1) * 128, s * T:(s + 1) * T], uT.bufs[blk]))
    mk.tt(dtmp, U[:, 0:T], U[:, 1:T + 1], ALU.subtract, e="pool")
    mk.stt(dst, dtmp, mu_col, U[:, 1:T + 1], ALU.mult, ALU.add)


Builder.load_shift_mix = _load_shift_mix


def _rwkv(self, uT, oTd, P, layer, vfirst):
    mk = self.mk
    ei = layer // 2
    has_vres = layer > 0
    with mk.scope():
        mu = mk.sb([128, 14], F32, "mu")
        mk.dma(mu, P["rw_mu_col"][ei])
        cols = mk.sb([128, 7, 4], F32, "cols")
        mk.dma(cols, P["rw_cols"][ei])
        w0c, a0c, kkc, kac, rkc, lngc, lnbc = [cols[:, i, :] for i in range(7)]
        w2a2 = mk.sb([128, 512], BF16, "w2a2")
        mk.dma(w2a2, P["rw_w2a2"][ei], q="pool")
        g2b = mk.sb([128, 512], BF16, "g2b")
        mk.dma(g2b, P["rw_g2"][ei], q="pool")
        blk = mk.sb([128, 128], BF16, "blk")
        mk.dma(blk, self.inp["c_blk"], q="pool")
        masks = mk.sb([128, 3, 128], BF16, "masks")
        mk.dma(masks, self.inp["c_masks"].re("a p c -> p a c"), q="pool")

        def mb(i):
            return V(masks.ap[:, i, :].unsqueeze(1).to_broadcast([128, 2, 128]), masks.buf)
        mLs, mUs, mUi = mb(0), mb(1), mb(2)
        rmask = mk.sb([128, T], BF16, "rmask")
        tB = mk.sb([128, T], F32, "tB")
        r32 = mk.sb([128, T], F32, "r32")
        mk.op("pool", lambda eng: eng.iota(tB.ap.rearrange("p (c l) -> p c l", l=LCH), [[0, NCH], [1, LCH]],
                                            base=0, channel_multiplier=0, allow_small_or_imprecise_dtypes=True),
              writes=[tB])
        mk.ts(rmask, tB, 1.0, None, op0=ALU.min)
        lneps = mk.sb([128, 1], F32)
        mk.memset(lneps, 64e-5)
        zb = mk.sb([128, 128], BF16, "zb")
        mk.memset(zb, 0.0)
        U = mk.sb([128, T + 1], F32, "U")
        mk.memset(U[:, 0:1], 0.0)
        dtmp = tB
        m_ = r32
        lr12 = mk.sb([128, T], BF16, "lr12")
        sdg = mk.sb([128, T], BF16, "sdg")
        tbf = mk.sb([128, T], BF16, "tbf")
        if has_vres:
            v0c = mk.sb([128, 4], F32, "v0c")
            mk.dma(v0c, P["rw_v0col"][ei - 1])
            v1b = mk.sb([128, 4, 32], BF16, "v1b")
            mk.dma(v1b, P["rw_v1"][ei - 1].re("(k p) n -> p k n", p=128), q="pool")
            v2b = mk.sb([32, 512], BF16, "v2b")
            mk.dma(v2b, P["rw_v2"][ei - 1], q="pool")
            t32b = mk.sb([32, T], BF16, "t32b")
        k32 = mk.sb([128, T], F32, "k32")
        a16 = mk.sb([128, T], BF16, "a16")
        lw = mk.sb([128, T], F32, "lw")
        cl = mk.sb([128, T], F32, "cl")
        v32 = cl
        tA = mk.sb([128, T], F32, "tA")
        gT = mk.sb([128, T], BF16, "gT")
        bon = mk.sb([128, T], BF16, "bon")
        aT_ = mk.sb([128, T], BF16, "aT_")
        bT_ = mk.sb([128, T], BF16, "bT_")
        kT_ = mk.sb([128, T], BF16, "kT_")
        vT_ = tbf
        a_bd = mk.sb([128, 2, T], BF16, "a_bd")
        b_bd = mk.sb([128, 2, T], BF16, "b_bd")
        r_bd = mk.sb([128, 2, T], BF16, "r_bd")
        for t_ in (a_bd, b_bd, r_bd):
            mk.memset(t_, 0.0)
        TMav = mk.sb([128, 16, 2, 128], BF16, "TMav")
        TMbk = mk.sb([128, 16, 2, 2, 128], BF16, "TMbk")
        mk.memset(TMbk, 0.0)
        DL = mk.sb([128, NCH], F32, "DL")
        H32 = mk.sb([128, 128], F32, "H32")
        Hb = mk.sb([128, 128], BF16, "Hb")
        ht1 = mk.sb([128, 128], F32, "ht1")
        oacc = mk.sb([128, T], BF16, "oacc")

        def grp():
            d = dict(X=[mk.sb([128, 2, 128], BF16) for _ in range(2)], XT=[mk.sb([128, 2, 128], BF16) for _ in range(2)],
                     AakT=mk.sb([128, 2, 128], BF16), ArbT=mk.sb([128, 2, 128], BF16), ArkT=mk.sb([128, 2, 128], BF16),
                     Z=mk.sb([128, 2, 2, 64], BF16), T1T=mk.sb([128, 2, 128], BF16), G1=mk.sb([128, 2, 128], F32),
                     QT=mk.sb([128, 2, 128], BF16), yn=mk.sb([128, 128], BF16), ot=mk.sb([128, 128], F32),
                     st6=mk.sb([128, 2, 6], F32), mv=mk.sb([128, 2, 2], F32), rs=mk.sb([128, 2], F32))
            mk.memset(d["T1T"], 0.0)
            mk.memset(d["G1"], 0.0)
            mk.memset(d["QT"], 0.0)
            return d
        G = [grp(), grp()]
        B_ = self.bank

        def reg(b, c0, c1):
            return V(B_[b].ap[:, c0:c1], B_[b].buf)
        pN, pNT = reg(0, 0, 256), reg(0, 256, 512)
        pAk, pRb = reg(1, 0, 256), reg(1, 256, 512)
        pRk, pZ2, pTr = reg(2, 0, 256), reg(2, 256, 384), reg(2, 384, 512)
        pZa = [reg(3, 0, 256), reg(3, 256, 512)]
        pX = [reg(4, 0, 256), reg(4, 256, 512)]
        pXT = [reg(5, 0, 256), reg(5, 256, 512)]
        pT, pG = reg(6, 0, 256), reg(6, 256, 512)
        pQ, pY, pHp = reg(3, 0, 256), reg(7, 0, 128), reg(2, 256, 384)
        pbig = [B_[5], B_[6], B_[7]]

        def v3(t):
            return t.re("p (h c) -> p h c", h=2)

        for s in range(NSEQ):
            tsl = slice(s * T, (s + 1) * T)
            self.load_shift_mix(m_, uT, 12, s, mu[:, 12:13], U, dtmp)
            mk.act(lr12[0:64, :], m_[0:64, :], AF.Tanh)
            mk.copy(lr12[64:128, :], m_[64:128, :], e="pool")
            self.load_shift_mix(m_, uT, 13, s, mu[:, 13:14], U, dtmp)
            mk.act(sdg, m_, AF.Sigmoid)
            if has_vres:
                pv = [B_[i] for i in range(4)]
                for hb in range(4):
                    self.load_shift_mix(m_, uT, 8 + hb, s, mu[:, 8 + hb:9 + hb], U, dtmp)
                    mk.copy(tbf, m_, e="act")
                    for ch in range(4):
                        mk.mm(pv[ch][0:32, :], v1b[:, hb, :], tbf[:, ch * 512:(ch + 1) * 512],
                              start=(hb == 0), stop=(hb == 3))
                for ch in range(4):
                    mk.copy(t32b[:, ch * 512:(ch + 1) * 512], pv[ch][0:32, :], e="act")
            for hb in range(4):
                hsl = slice(hb * 128, (hb + 1) * 128)
                self.load_shift_mix(r32, uT, hb, s, mu[:, hb:hb + 1], U, dtmp)
                self.load_shift_mix(k32, uT, 4 + hb, s, mu[:, 4 + hb:5 + hb], U, dtmp)
                self.load_shift_mix(v32, uT, 8 + hb, s, mu[:, 8 + hb:9 + hb], U, dtmp)
                for ch in range(4):
                    csl = slice(ch * 512, (ch + 1) * 512)
                    pb = pbig[ch % 3]
                    mk.mm(pb, w2a2[0:64, hsl], lr12[0:64, csl])
                    mk.act(lw[:, csl], pb, AF.Sigmoid, bias=w0c[:, hb:hb + 1])
                    pb = pbig[(ch + 1) % 3]
                    mk.mm(pb, w2a2[64:128, hsl], lr12[64:128, csl])
                    mk.act(a16[:, csl], pb, AF.Sigmoid, bias=a0c[:, hb:hb + 1])
                    pb = pbig[(ch + 2) % 3]
                    mk.mm(pb, g2b[:, hsl], sdg[:, csl])
                    mk.copy(gT[:, csl], pb, e="act")
                    if has_vres:
                        pb = pbig[ch % 3]
                        mk.mm(pb, v2b[:, hsl], t32b[:, csl])
                        mk.act(tA[:, csl], pb, AF.Sigmoid, bias=v0c[:, hb:hb + 1])
                mk.ts(lw, lw, -DECAY_C, None, op0=ALU.mult)
                if has_vres:
                    mk.dma(tB, V(vfirst.ap[hb * 128:(hb + 1) * 128, tsl], vfirst.bufs[hb]))
                    mk.tt(tB, tB, v32, ALU.subtract, e="pool")
                    mk.tt(tB, tB, tA, ALU.mult, e="pool")
                    mk.tt(v32, v32, tB, ALU.add, e="pool")
                else:
                    mk.dma(V(vfirst.ap[hb * 128:(hb + 1) * 128, tsl], vfirst.bufs[hb]), v32, q=DBG.get("st_q", "pool"))
                mk.ts(tA, k32, kkc[:, hb:hb + 1], None, op0=ALU.mult)
                mk.tt(tbf, tA, tA, ALU.mult, e="pool")
                for ch in range(4):
                    csl = slice(ch * 512, (ch + 1) * 512)
                    pb = pbig[ch % 3]
                    mk.mm(pb, blk, tbf[:, csl])
                    mk.act(tB[:, csl], pb, AF.Sqrt)
                mk.ts(tB, tB, 1e-12, None, op0=ALU.max)
                mk.recip(tB, tB)
                mk.tt(tA, tA, tB, ALU.mult)
                mk.ts(tB, a16, -1.0, kac[:, hb:hb + 1], op0=ALU.add, op1=ALU.mult)
                mk.ts(tB, tB, 1.0, None, op0=ALU.add)
                mk.tt(k32, k32, tB, ALU.mult)
                mk.tt(tB, r32, k32, ALU.mult, e="pool")
                mk.ts(tbf, tB, rkc[:, hb:hb + 1], None, op0=ALU.mult)
                for ch in range(4):
                    csl = slice(ch * 512, (ch + 1) * 512)
                    pb = pbig[ch % 3]
                    mk.mm(pb, blk, tbf[:, csl])
                    mk.tt(bon[:, csl], pb, v32[:, csl], ALU.mult)
                mk.copy(vT_, v32, e="act")
                mk.scan(cl, rmask, lw, 0.0)
                mk.act(tB, cl, AF.Exp)
                mk.copy(DL, tB.re("p (c l) -> p c l", l=LCH)[:, :, LCH - 1], e="pool")
                mk.tt(r_bd[0:64, 0, :], r32[0:64, :], tB[0:64, :], ALU.mult)
                mk.tt(r_bd[64:128, 1, :], r32[64:128, :], tB[64:128, :], ALU.mult, e="pool")
                mk.tt(cl, cl, lw, ALU.subtract, e="pool")
                mk.act(tB, cl, AF.Exp)
                mk.tt(tB, tB, tA, ALU.mult)
                mk.ts(aT_, tB, -1.0, None, op0=ALU.mult)
                mk.copy(a_bd[0:64, 0, :], aT_[0:64, :], e="pool")
                mk.copy(a_bd[64:128, 1, :], aT_[64:128, :], e="act")
                mk.tt(cl, cl, lw, ALU.add, e="pool")
                mk.act(tB, cl, AF.Exp, scale=-1.0)
                mk.tt(kT_, k32, tB, ALU.mult)
                mk.tt(tA, tA, a16, ALU.mult, e="pool")
                mk.tt(bT_, tA, tB, ALU.mult)
                mk.copy(b_bd[0:64, 0, :], bT_[0:64, :], e="pool")
                mk.copy(b_bd[64:128, 1, :], bT_[64:128, :], e="act")
                for p in range(16):
                    ptm = B_[5 + p % 2].bitcast(BF16)
                    psl = slice(p * 128, (p + 1) * 128)
                    for qi, src in enumerate((aT_, vT_, bT_, kT_)):
                        mk.transpose(ptm[:, qi * 128:(qi + 1) * 128], src[:, psl], self.identb)
                    mk.copy(TMav[:, p, :, :], ptm[:, 0:256].re("p (q c) -> p q c", q=2), e="act")
                    for c in range(2):
                        mk.copy(TMbk[64 * c:64 * c + 64, p, c, :, :],
                                ptm[64 * c:64 * c + 64, 256:512].re("p (q c) -> p q c", q=2), e=("dve" if c else "act"))
                mk.memset(H32, 0.0)
                mk.memset(Hb, 0.0)
                for p in range(DBG.get("ngrp", 16)):
                    g = G[p % 2]
                    t0 = p * 128
                    gsl = slice(t0, t0 + 128)
                    mk.mm(pN, aT_[:, gsl], b_bd[:, :, gsl])
                    mk.mm(pNT, bT_[:, gsl], a_bd[:, :, gsl])
                    mk.mm(pAk, kT_[:, gsl], a_bd[:, :, gsl])
                    mk.mm(pRb, bT_[:, gsl], r_bd[:, :, gsl])
                    mk.mm(pRk, kT_[:, gsl], r_bd[:, :, gsl])
                    mk.tt(g["X"][0], v3(pN), mLs, ALU.mult)
                    mk.tt(g["XT"][0], v3(pNT), mUs, ALU.mult)
                    mk.tt(g["AakT"], v3(pAk), mUs, ALU.mult)
                    mk.tt(g["ArbT"], v3(pRb), mUi, ALU.mult)
                    mk.tt(g["ArkT"], v3(pRk), mUi, ALU.mult)
                    if DBG.get('gstage', 9) < 2:
                        continue
                    for hd in range(2):
                        mk.mm(pZ2[:, 64 * hd:64 * hd + 64], g["AakT"][:, hd, :], TMav[:, p, 1, 64 * hd:64 * hd + 64])
                    Z = g["Z"]
                    mk.copy(Z[:, 0, :, :], TMav[:, p, 0, :].re("p (h j) -> p h j", h=2), e="pool")
                    mk.copy(Z[:, 1, :, :], pZ2.re("p (h i) -> p h i", h=2), e="act")
                    if DBG.get('gstage', 9) < 3:
                        continue
                    for lev in range(6):
                        X, XT = g["X"][lev % 2], g["XT"][lev % 2]
                        pz = pZa[lev % 2]
                        for hd in range(2):
                            mk.mm(pz[:, hd * 128:(hd + 1) * 128], XT[:, hd, :], Z[:, :, hd, :])
                        if lev < 5:
                            Xn, XTn = g["X"][(lev + 1) % 2], g["XT"][(lev + 1) % 2]
                            for hd in range(2):
                                mk.mm(pX[lev % 2][:, hd * 128:(hd + 1) * 128], XT[:, hd, :], X[:, hd, :])
                                mk.mm(pXT[lev % 2][:, hd * 128:(hd + 1) * 128], X[:, hd, :], XT[:, hd, :])
                            mk.copy(Xn.re("p h c -> p (h c)"), pX[lev % 2], e="act")
                            mk.copy(XTn.re("p h c -> p (h c)"), pXT[lev % 2], e="act")
                        mk.tt(Z, Z, pz.re("p (h w c) -> p w h c", h=2, w=2), ALU.add)
                    if DBG.get('gstage', 9) < 4:
                        continue
                    Wb_ = Z[:, 0, :, :].re("p h j -> p (h j)")
                    Ub_ = Z[:, 1, :, :].re("p h j -> p (h j)")
                    mk.mm(pT, Wb_, TMbk[:, p, :, 0, :])
                    for c in range(2):
                        mk.mm(pG[:, 128 * c:128 * c + 128], TMbk[:, p, c, 0, :], Ub_, start=True, stop=False)
                        mk.mm(pG[:, 128 * c:128 * c + 128], TMbk[:, p, c, 1, :], TMav[:, p, 1, :],
                              start=False, stop=True)
                    mk.mm(pQ, Wb_, g["ArbT"].re("p h t -> p (h t)"))
                    for hd in range(2):
                        hs = slice(64 * hd, 64 * hd + 64)
                        mk.copy(g["T1T"][hs, :, hs], pT[hs, :].re("p (c h j) -> p c h j", c=2, h=2)[:, :, hd, :], e="act")
                        mk.copy(g["G1"][hs, :, hs], pG[hs, :].re("p (c h i) -> p c h i", c=2, h=2)[:, :, hd, :], e="act")
                        for c in range(2):
                            mk.tt(g["QT"][hs, c, 64 * c:64 * c + 64], pQ[hs, hd * 128 + 64 * c:hd * 128 + 64 * c + 64],
                                  r_bd[hs, hd, t0 + 64 * c:t0 + 64 * c + 64], ALU.add)
                    if DBG.get('gstage', 9) < 5:
                        continue
                    mk.mm(pY, zb, zb)
                    for hd in range(2):
                        hs = slice(64 * hd, 64 * hd + 64)
                        mk.mm(pY[:, hs], g["ArbT"][:, hd, :], Z[:, 1, hd, :], start=False, stop=False,
                              skip_group_check=True)
                        mk.mm(pY[:, hs], g["ArkT"][:, hd, :], TMav[:, p, 1, hs], start=False, stop=False,
                              skip_group_check=True)
                    for c in range(2):
                        ci = 2 * p + c
                        cs = slice(64 * c, 64 * c + 64)
                        mk.mm(pY, g["QT"][:, c, :], Hb, start=False, stop=True, skip_group_check=True)
                        mk.mm(pHp, g["T1T"][:, c, :], Hb)
                        mk.tt(ht1, pHp, H32, ALU.add)
                        mk.tt(ht1, ht1, g["G1"][:, c, :], ALU.add)
                        mk.ts(H32, ht1, DL[:, ci:ci + 1], None, op0=ALU.mult)
                        mk.copy(Hb, H32, e="act")
                    if DBG.get('gstage', 9) < 6:
                        continue
                    for hd in range(2):
                        ysl = pY[:, 64 * hd:64 * hd + 64]
                        mk.op("dve", lambda eng, o=g["st6"][:, hd, :], i_=ysl: eng.bn_stats(out=o.ap, in_=i_.ap),
                              reads=[ysl], writes=[g["st6"]])
                        mk.op("dve", lambda eng, o=g["mv"][:, hd, :], i_=g["st6"][:, hd, :]: eng.bn_aggr(out=o.ap, in_=i_.ap),
                              reads=[g["st6"]], writes=[g["mv"]])
                    mk.act(g["rs"], g["mv"][:, :, 1], AF.Sqrt, bias=lneps)
                    mk.recip(g["rs"], g["rs"])
                    for hd in range(2):
                        if DBG.get("rw_tap") == "y":
                            mk.copy(g["yn"][:, 64 * hd:64 * hd + 64], pY[:, 64 * hd:64 * hd + 64])
                            continue
                        mk.ts(g["yn"][:, 64 * hd:64 * hd + 64], pY[:, 64 * hd:64 * hd + 64], g["mv"][:, hd, 0:1],
                              g["rs"][:, hd:hd + 1], op0=ALU.subtract, op1=ALU.mult)
                    ptr = pTr.bitcast(BF16)
                    mk.transpose(ptr[:, 0:128], g["yn"], self.identb)
                    tap = DBG.get("rw_tap")
                    if tap in ("y", "yn"):
                        mk.copy(oacc[:, gsl], ptr[:, 0:128])
                    elif tap == "bon":
                        mk.copy(oacc[:, gsl], bon[:, gsl])
                    elif tap == "g":
                        mk.copy(oacc[:, gsl], gT[:, gsl])
                    else:
                        mk.ts(g["ot"], ptr[:, 0:128], lngc[:, hb:hb + 1], lnbc[:, hb:hb + 1], op0=ALU.mult, op1=ALU.add)
                        mk.tt(g["ot"], g["ot"], bon[:, gsl], ALU.add, e="pool")
                        mk.tt(oacc[:, gsl], g["ot"], gT[:, gsl], ALU.mult, e="pool")
                mk.dma(V(oTd.ap[hb * 128:(hb + 1) * 128, tsl], oTd.bufs[hb]), oacc, q=DBG.get("st_q", "pool"))


Builder.rwkv = _rwkv


def _pool(self, uT, oT, P, layer):
    mk = self.mk
    ei = layer // 2
    with mk.scope():
        pwb = mk.sb([128, 4, 128], BF16, "pwb")
        mk.dma(pwb, P["pool_w"][ei].re("g c d -> c g d"), q="pool")
        psc = mk.sb([128, 4], F32, "psc")
        mk.dma(psc, P["pool_scale_col"][ei])
        A = [mk.sb([128, 16 + T], F32, "pA%d" % i) for i in range(2)]
        U0 = mk.sb([128, 16 + T], F32, "pU")
        for t_ in A + [U0]:
            mk.memset(t_[:, 0:16], 0.0)
        rcw = mk.sb([128, T], F32, "rcw")
        dT = mk.sb([128, T], BF16, "dT")
        tmp = mk.sb([128, T], F32, "ptmp")
        pob = [mk.sb([128, 512], BF16) for i in range(2)]
        for gi in range(4):
            win = 2 ** (gi + 1)
            mk.op("pool", lambda eng: eng.iota(rcw.ap, [[1, T]], base=1, channel_multiplier=0,
                                                allow_small_or_imprecise_dtypes=True), writes=[rcw])
            mk.ts(rcw, rcw, float(win), None, op0=ALU.min)
            mk.recip(rcw, rcw)
            for s in range(NSEQ if DBG.get("pool_stage", 9) >= 2 else 0):
                mk.dma(U0[:, 16:], V(uT.ap[(14 + gi) * 128:(15 + gi) * 128, s * T:(s + 1) * T], uT.bufs[14 + gi]))
                src = U0
                for lev in range(gi + 1):
                    sh = 2 ** lev
                    dst = A[lev % 2]
                    mk.tt(dst[:, 16:], src[:, 16:], src[:, 16 - sh:16 - sh + T], ALU.add, e=("pool" if lev % 2 else "dve"))
                    src = dst
                mk.tt(tmp, src[:, 16:], rcw, ALU.mult)
                mk.tt(dT, tmp, U0[:, 16:], ALU.subtract, e="pool")
                for ch in range(4 if DBG.get("pool_stage", 9) >= 3 else 0):
                    pb = self.bank[ch % 2]
                    mk.mm(pb, pwb[:, gi, :], dT[:, ch * 512:(ch + 1) * 512])
                    ob_ = pob[ch % 2]
                    if DBG.get("pool_stage", 9) >= 4:
                        mk.ts(ob_, pb, psc[:, gi:gi + 1], None, op0=ALU.mult)
                    if DBG.get("pool_stage", 9) >= 5:
                        mk.dma(V(oT.ap[(4 + gi) * 128:(5 + gi) * 128, s * T + ch * 512:s * T + (ch + 1) * 512],
                                 oT.bufs[4 + gi]), ob_, q=DBG.get("pool_q", "pool"))


Builder.pool = _pool


def _even_layer(self, xres_in, xres_out, layer, P, vfirst):
    mk = self.mk
    ei = layer // 2
    uT = self.scratch("uTe", [2304, NTOK], F32)
    self.proj_in(xres_in, P["norm_mix_g"][layer:layer + 1, :], P["even_w_in"][ei], 2304,
                 list(range(18)), uT, None, None)
    oT = self.scratch("oTd", [D, NTOK], BF16)
    if not DBG.get("no_rwkv"):
        self.rwkv(uT, oT, P, layer, vfirst)
    if not DBG.get("no_pool"):
        self.pool(uT, oT, P, layer)
    self.proj_out(xres_in, xres_out, oT, P["even_w_out"][ei])


Builder.even_layer = _even_layer


def host_even_params(inputs):
    f = lambda a: np.ascontiguousarray(a, dtype=np.float32)
    out = {}
    out["norm_mix_g"] = f(inputs["norm_mix_g"])
    out["even_w_in"] = f(inputs["even_w_in"])
    out["even_w_out"] = f(inputs["even_w_out"])
    n_even = inputs["even_w_in"].shape[0]
    out["rw_mu_col"] = f(np.transpose(inputs["rw_mu"].reshape(n_even, 14, 128), (0, 2, 1)))
    colp = [inputs["rw_w0"], inputs["rw_a0"], inputs["rw_k_k"], inputs["rw_k_a"],
            inputs["rw_r_k"].reshape(n_even, 512), inputs["rw_ln_g"], inputs["rw_ln_b"]]
    out["rw_cols"] = f(np.stack([np.transpose(c.reshape(n_even, 4, 128), (0, 2, 1)) for c in colp], axis=2))
    out["rw_w2a2"] = f(np.concatenate([inputs["rw_w2"], inputs["rw_a2"]], axis=1))
    out["rw_g2"] = f(inputs["rw_g2"])
    nv = inputs["rw_v0"].shape[0]
    out["rw_v0col"] = f(np.transpose(inputs["rw_v0"].reshape(nv, 4, 128), (0, 2, 1)))
    out["rw_v1"] = f(inputs["rw_v1"])
    out["rw_v2"] = f(inputs["rw_v2"])
    out["pool_w"] = f(inputs["pool_w"])
    out["pool_scale_col"] = f(np.transpose(inputs["pool_scale"].reshape(n_even, 4, 128), (0, 2, 1)))
    return out


def host_consts2():
    c = host_consts()
    blk = np.kron(np.eye(2, dtype=np.float32), np.ones((64, 64), np.float32))
    lo = np.tril(np.ones((64, 64), np.float32), -1)
    e2 = np.eye(2, dtype=np.float32)
    mLs = np.kron(e2, lo)
    mUs = np.kron(e2, lo.T)
    mUi = np.kron(e2, np.triu(np.ones((64, 64), np.float32)))
    c["c_blk"] = blk
    c["c_masks"] = np.stack([mLs, mUs, mUi]).astype(np.float32)
    return c


def mk_check(mk):
    val = {}
    pos = {e: 0 for e in ENGS}
    total = sum(len(mk.ops[e]) for e in ENGS)
    done = 0
    while done < total:
        prog = False
        for e in ENGS:
            lst = mk.ops[e]
            while pos[e] < len(lst):
                waits, fn, inc = lst[pos[e]]
                if all(val.get(id(s), 0) >= v for s, v in waits):
                    val[id(inc[0])] = val.get(id(inc[0]), 0) + inc[1]
                    pos[e] += 1
                    done += 1
                    prog = True
                else:
                    break
        if not prog:
            names = {id(v): k for k, v in mk.semobj.items()}
            for e in ENGS:
                if pos[e] < len(mk.ops[e]):
                    waits, fn, inc = mk.ops[e][pos[e]]
                    print("STUCK", e, pos[e], [(names.get(id(s)), v, val.get(id(s), 0)) for s, v in waits])
            return False
    return True


_PROG = {}


def host_params(inputs):
    hp = {}
    hp.update(host_even_params(inputs))
    hp.update(host_odd_params(inputs))
    hp.update(host_moe_params(inputs))
    hp.update(host_consts2())
    return hp


def build_program(shapes, n_layers=4):
    nc = bass.Bass("TRN2", target_bir_lowering=False)
    with ExitStack() as st:
        B = Builder(nc, st)
        mk = B.mk
        x_in = DR(mk, "x_in", [NTOK, D], F32, kind="ExternalInput")
        y = DR(mk, "y", [NTOK, D], F32, kind="ExternalOutput")
        P = {}
        for k, shp in shapes.items():
            if k in B.inp:
                continue
            P[k] = B.ext_in(k, list(shp))
        vf = B.scratch("vfirst", [512, NTOK], F32)
        for layer in range(n_layers):
            xin = x_in if layer == 0 else y
            if layer % 2 == 0:
                B.even_layer(xin, y, layer, P, vf)
            else:
                B.odd_layer(xin, y, layer, P)
            B.moe(y, layer, P)
        mk.emit()
    return nc


def kernel(**inputs):
    inputs = {k: np.asarray(v) for k, v in inputs.items()}
    hp = host_params(inputs)
    key = "full"
    if key not in _PROG:
        _PROG[key] = build_program({k: v.shape for k, v in hp.items()})
    nc = _PROG[key]
    x = np.ascontiguousarray(inputs["x"], dtype=np.float32)
    nb = x.shape[0]
    n_cores = 8
    per = nb // n_cores
    in_maps = []
    for c in range(n_cores):
        m = dict(hp)
        m["x_in"] = np.ascontiguousarray(x[c * per:(c + 1) * per].reshape(NTOK, D))
        in_maps.append(m)
    res = run_bass_kernel_spmd(nc, in_maps, core_ids=list(range(n_cores)))
    out = np.stack([np.asarray(r["y"], dtype=np.float32).reshape(per, T, D) for r in res.results], axis=0)
    return out.reshape(nb, T, D)
```

```python
import numpy as np
from contextlib import ExitStack
import concourse.bass as bass
import concourse.mybir as mybir
from concourse.bass_utils import run_bass_kernel_spmd

F32 = mybir.dt.float32
BF16 = mybir.dt.bfloat16
I32 = mybir.dt.int32
U32 = mybir.dt.uint32
AF = mybir.ActivationFunctionType
ALU = mybir.AluOpType
AX = mybir.AxisListType

SAME_ENGINE_SYNC = True
EPOCH = 20000
DBG = {}


class Buf:
    __slots__ = ("w", "r", "name")

    def __init__(self, name=""):
        self.w = None
        self.r = {}
        self.name = name


class V:
    __slots__ = ("ap", "buf")

    def __init__(self, ap, buf):
        self.ap = ap
        self.buf = buf

    def __getitem__(self, k):
        return V(self.ap[k], self.buf)

    def re(self, s, **kw):
        return V(self.ap.rearrange(s, **kw), self.buf)

    def bc(self, shape):
        return V(self.ap.to_broadcast(list(shape)), self.buf)

    def pbc(self, n):
        return V(self.ap.partition_broadcast(n), self.buf)

    def bitcast(self, dt):
        return V(self.ap.bitcast(dt), self.buf)

    def sub(self, buf):
        return V(self.ap, buf)

    @property
    def shape(self):
        return self.ap.shape


ENGS = ("pe", "act", "dve", "pool", "sp")


class MK:
    def __init__(self, nc, stack, n_dma_slots=8):
        self.nc = nc
        self.stack = stack
        self.ops = {e: [] for e in ENGS}
        self.root = stack
        self.esem = {}
        self.cnt = {e: 0 for e in ENGS}
        self.seen = {e: {} for e in ENGS}
        self.dma_slots = {}
        self.dma_n = {}
        for q in ("sp", "pool", "act"):
            self.dma_slots[q] = [stack.enter_context(nc.semaphore("dma_%s_%d" % (q, i)))
                                 for i in range(n_dma_slots)]
            self.dma_n[q] = 0
        self.semobj = {}
        self.n_ops = 0
        self.uid = 0

    def sb(self, shape, dtype, name=None):
        self.uid += 1
        name = name or "t%d" % self.uid
        t = self.stack.enter_context(self.nc.sbuf_tensor(name + "_%d" % self.uid, list(shape), dtype))
        return V(t[:], Buf(name))

    def ps(self, shape, dtype=F32, name=None):
        self.uid += 1
        name = name or "p%d" % self.uid
        t = self.stack.enter_context(self.nc.psum_tensor(name + "_%d" % self.uid, list(shape), dtype))
        return V(t[:], Buf(name))

    def dram(self, name, shape, dtype, kind="Internal"):
        t = self.nc.dram_tensor(name, list(shape), dtype, kind=kind)
        return V(t.ap(), Buf(name))

    def _tok_need(self, e, tok, waits):
        if tok is None:
            return
        semkey, val, src, is_dma = tok
        if src == e and not is_dma:
            if e == "pe" or not SAME_ENGINE_SYNC:
                return
        if self.seen[e].get(semkey, 0) >= val:
            return
        if waits.get(semkey, 0) < val:
            waits[semkey] = val

    def op(self, e, fn, reads=(), writes=(), dma=False):
        waits = {}
        pend = getattr(self, "pending", {}).pop(e, None)
        if pend:
            waits.update(pend)
        rb = [v.buf for v in reads if v is not None]
        wb = [v.buf for v in writes if v is not None]
        for b in rb:
            self._tok_need(e, b.w, waits)
        for b in wb:
            self._tok_need(e, b.w, waits)
            for t in b.r.values():
                self._tok_need(e, t, waits)
        if dma:
            slots = self.dma_slots[e]
            n = self.dma_n[e]
            self.dma_n[e] = n + 1
            sem = slots[n % len(slots)]
            val = 16 * (n // len(slots) + 1)
            semkey = ("d", e, n % len(slots))
            self.semobj[semkey] = sem
            if val > 16:
                if self.seen[e].get(semkey, 0) < val - 16 and waits.get(semkey, 0) < val - 16:
                    waits[semkey] = val - 16
            tok = (semkey, val, e, True)
            inc = (sem, 16)
        else:
            self.cnt[e] += 1
            ep = (self.cnt[e] - 1) // EPOCH
            semkey = ("c", e, ep)
            if semkey not in self.esem:
                self.esem[semkey] = self.root.enter_context(self.nc.semaphore("sem_%s_%d" % (e, ep)))
            self.semobj[semkey] = self.esem[semkey]
            tok = (semkey, (self.cnt[e] - 1) % EPOCH + 1, e, False)
            inc = (self.esem[semkey], 1)
        for k, v in waits.items():
            self.seen[e][k] = v
        for b in wb:
            b.w = tok
            b.r = {}
        for b in rb:
            old = b.r.get(tok[0])
            if old is None or old[1] < tok[1]:
                b.r[tok[0]] = tok
        self.ops[e].append(([(self.semobj[k], v) for k, v in waits.items()], fn, inc))
        self.n_ops += 1
        return tok

    def emit(self):
        nc = self.nc
        fin = []
        for q in ("sp", "pool", "act"):
            n = self.dma_n[q]
            slots = self.dma_slots[q]
            for i in range(min(n, len(slots))):
                uses = (n - i + len(slots) - 1) // len(slots)
                fin.append((slots[i], 16 * uses))
        for e in ("pe", "act", "dve", "pool"):
            if self.cnt[e]:
                ep = (self.cnt[e] - 1) // EPOCH
                fin.append((self.esem[("c", e, ep)], (self.cnt[e] - 1) % EPOCH + 1))
        with nc.Block() as block:
            def run(eng, lst, final=None):
                for waits, fn, inc in lst:
                    for s, v in waits:
                        eng.wait_ge(s, v)
                    fn(eng).then_inc(inc[0], inc[1])
                if final:
                    for s, v in final:
                        eng.wait_ge(s, v)

            @block.tensor
            def _(eng):
                run(eng, self.ops["pe"])

            @block.scalar
            def _(eng):
                run(eng, self.ops["act"])

            @block.vector
            def _(eng):
                run(eng, self.ops["dve"])

            @block.gpsimd
            def _(eng):
                run(eng, self.ops["pool"])

            @block.sync
            def _(eng):
                run(eng, self.ops["sp"], fin)

    def dma(self, out, in_, q="sp", **kw):
        return self.op(q, lambda eng: eng.dma_start(out=out.ap, in_=in_.ap, **kw),
                       reads=[in_], writes=[out], dma=True)

    def mm(self, out, lhsT, rhs, start=True, stop=True, **kw):
        return self.op("pe", lambda eng: eng.matmul(out.ap, lhsT.ap, rhs.ap, start=start, stop=stop, **kw),
                       reads=[lhsT, rhs], writes=[out])

    def transpose(self, out, in_, ident):
        return self.op("pe", lambda eng: eng.transpose(out.ap, in_.ap, ident.ap),
                       reads=[in_, ident], writes=[out])

    def act(self, out, in_, func, bias=None, scale=1.0, accum_out=None, e="act"):
        reads = [in_]
        kw = {}
        if isinstance(bias, V):
            reads.append(bias)
            kw["bias"] = bias.ap
        elif bias is not None:
            kw["bias"] = bias
        if isinstance(scale, V):
            reads.append(scale)
            kw["scale"] = scale.ap
        else:
            kw["scale"] = scale
        writes = [out]
        if accum_out is not None:
            writes.append(accum_out)
            kw["accum_out"] = accum_out.ap
        return self.op(e, lambda eng: eng.activation(out=out.ap, in_=in_.ap, func=func, **kw),
                       reads=reads, writes=writes)

    def tt(self, out, in0, in1, op, e="dve"):
        return self.op(e, lambda eng: eng.tensor_tensor(out=out.ap, in0=in0.ap, in1=in1.ap, op=op),
                       reads=[in0, in1], writes=[out])

    def ts(self, out, in0, s1, s2=None, op0=ALU.mult, op1=None, accum_out=None, e="dve"):
        reads = [in0]
        a1 = s1
        if isinstance(s1, V):
            reads.append(s1)
            a1 = s1.ap
        a2 = s2
        if isinstance(s2, V):
            reads.append(s2)
            a2 = s2.ap
        kw = {}
        if op1 is not None:
            kw["op1"] = op1
        writes = [out]
        if accum_out is not None:
            writes.append(accum_out)
            kw["accum_out"] = accum_out.ap
        return self.op(e, lambda eng: eng.tensor_scalar(out=out.ap, in0=in0.ap, scalar1=a1, scalar2=a2,
                                                        op0=op0, **kw),
                       reads=reads, writes=writes)

    def stt(self, out, in0, scalar, in1, op0, op1, e="dve"):
        reads = [in0, in1]
        a = scalar
        if isinstance(scalar, V):
            reads.append(scalar)
            a = scalar.ap
        return self.op(e, lambda eng: eng.scalar_tensor_tensor(out=out.ap, in0=in0.ap, scalar=a, in1=in1.ap,
                                                               op0=op0, op1=op1),
                       reads=reads, writes=[out])

    def copy(self, out, in_, e="dve"):
        if e == "act":
            return self.op(e, lambda eng: eng.copy(out=out.ap, in_=in_.ap), reads=[in_], writes=[out])
        return self.op(e, lambda eng: eng.tensor_copy(out=out.ap, in_=in_.ap), reads=[in_], writes=[out])

    def memset(self, out, val, e="pool"):
        return self.op(e, lambda eng: eng.memset(out.ap, val), writes=[out])

    def reduce(self, out, in_, op=ALU.add, axis=AX.X, e="dve"):
        return self.op(e, lambda eng: eng.tensor_reduce(out=out.ap, in_=in_.ap, axis=axis, op=op),
                       reads=[in_], writes=[out])

    def recip(self, out, in_):
        return self.op("dve", lambda eng: eng.reciprocal(out=out.ap, in_=in_.ap), reads=[in_], writes=[out])

    def scan(self, out, d0, d1, init, op0=ALU.mult, op1=ALU.add):
        reads = [d0, d1]
        a = init
        if isinstance(init, V):
            reads.append(init)
            a = init.ap
        return self.op("dve", lambda eng: eng.tensor_tensor_scan(out=out.ap, data0=d0.ap, data1=d1.ap,
                                                                 initial=a, op0=op0, op1=op1),
                       reads=reads, writes=[out])

    def barrier(self):
        fin = {}
        for q in ("sp", "pool", "act"):
            n = self.dma_n[q]
            slots = self.dma_slots[q]
            for i in range(min(n, len(slots))):
                uses = (n - i + len(slots) - 1) // len(slots)
                fin[("d", q, i)] = 16 * uses
        for e in ("pe", "act", "dve", "pool"):
            if self.cnt[e]:
                ep = (self.cnt[e] - 1) // EPOCH
                fin[("c", e, ep)] = (self.cnt[e] - 1) % EPOCH + 1
        for e in ENGS:
            waits = {}
            for k, v in fin.items():
                if k[0] == "c" and k[1] == e:
                    continue
                if self.seen[e].get(k, 0) < v:
                    waits[k] = v
                    self.seen[e][k] = v
            if waits:
                if e == "sp":
                    continue_fn = None
                self.pending = getattr(self, "pending", {})
                self.pending.setdefault(e, {}).update(waits)

    def scope(self):
        return _Scope(self)


class _Scope:
    def __init__(self, mk):
        self.mk = mk

    def __enter__(self):
        self.old = self.mk.stack
        self.st = ExitStack()
        self.mk.stack = self.st
        return self

    def __exit__(self, *a):
        self.mk.barrier()
        self.mk.stack = self.old
        self.st.close()
        return False


D = 1024
T = 2048
NSEQ = 2
NTOK = NSEQ * T
NT = NTOK // 128
CAP = 512
NE = 32
NSLOT = NE * CAP
NROW_TL = NSLOT + 128
RMS_EPS = 1e-6


class DR:
    def __init__(self, mk, name, shape, dtype, kind="Internal", rows_per=128):
        self.t = mk.nc.dram_tensor(name, list(shape), dtype, kind=kind)
        self.ap = self.t.ap()
        self.rows_per = rows_per
        n = (shape[0] + rows_per - 1) // rows_per
        self.bufs = [Buf("%s_%d" % (name, i)) for i in range(n)]
        self.whole = Buf(name)

    def rows(self, r0, n):
        assert r0 % self.rows_per == 0 and n <= self.rows_per
        return V(self.ap[r0:r0 + n], self.bufs[r0 // self.rows_per])

    def all(self):
        return [V(self.ap, b) for b in self.bufs]


class Builder:
    def __init__(self, nc, stack):
        self.nc = nc
        self.mk = MK(nc, stack)
        mk = self.mk
        self.inp = {}
        self.c_ident = self.ext_in("c_ident", [128, 128], F32)
        self.c_tri = self.ext_in("c_tri", [128, 128], F32)
        self.ext_in("c_blk", [128, 128], F32)
        self.ext_in("c_masks", [3, 128, 128], F32)
        self.ident32 = mk.sb([128, 128], F32, "ident32")
        self.identb = mk.sb([128, 128], BF16, "identb")
        self.trib = mk.sb([128, 128], BF16, "trib")
        self.onesb = mk.sb([128, 128], BF16, "onesb")
        mk.dma(self.ident32, self.c_ident)
        mk.dma(self.identb, self.c_ident, q="pool")
        mk.dma(self.trib, self.c_tri, q="pool")
        mk.memset(self.onesb, 1.0)
        self.eps_t = mk.sb([128, 1], F32, "eps_t")
        mk.memset(self.eps_t, RMS_EPS)
        self.psum_all = mk.ps([128, 4096], F32, "psum_all")
        self.bank = [V(self.psum_all.ap[:, i * 512:(i + 1) * 512], Buf("bank%d" % i)) for i in range(8)]

    def scratch(self, name, shape, dtype, rows_per=128):
        if not hasattr(self, "_scr"):
            self._scr = {}
        if name not in self._scr:
            self._scr[name] = DR(self.mk, name, shape, dtype, rows_per=rows_per)
        return self._scr[name]

    def ext_in(self, name, shape, dtype=F32):
        v = self.mk.dram(name, shape, dtype, kind="ExternalInput")
        self.inp[name] = v
        return v

    def rms_tile(self, xt, gbc, h32, eps=RMS_EPS, sq=None, small=None):
        mk = self.mk
        ss, rstd = small
        mk.act(sq, xt, AF.Square, accum_out=ss)
        mk.act(rstd, ss, AF.Sqrt, scale=1.0 / D, bias=self.eps_t)
        mk.recip(rstd, rstd)
        mk.stt(h32, xt, rstd, gbc, ALU.mult, ALU.mult)

    def moe(self, xres, layer, P):
        mk = self.mk
        with mk.scope():
            Hrows = self.scratch("hrows", [NTOK + 128, D], BF16)
            Ybuf = self.scratch("ybuf", [NROW_TL, D], F32)
            TokL = self.scratch("tokl", [NROW_TL, 8], I32, rows_per=NROW_TL)
            gbc = mk.sb([128, D], F32, "gbc")
            mk.dma(gbc, P["norm_ffn_g"][layer:layer + 1, :].pbc(128))
            wr = mk.sb([128, 8, 36], F32, "wr")
            mk.dma(wr, P["w_router"][layer].re("(k p) n -> p k n", p=128))
            brt = mk.sb([128, 36], F32, "brt")
            mk.dma(brt, P["b_router"][layer:layer + 1, :].pbc(128))
            maskall = mk.sb([128, NT, 32], BF16, "maskall")
            E1all = mk.sb([128, NT, 32], F32, "E1all")
            E2all = mk.sb([128, NT, 32], F32, "E2all")
            gate1 = mk.sb([128, NT], F32, "gate1")
            gate2 = mk.sb([128, NT], F32, "gate2")
            slot1 = mk.sb([128, NT], I32, "slot1")
            slot2 = mk.sb([128, NT], I32, "slot2")
            base = mk.sb([128, 32], F32, "base")
            lim = mk.sb([128, 32], F32, "lim")
            trash = mk.sb([128, 1], F32, "trash")
            zero_t = mk.sb([128, D], F32, "zero_t")
            sent = mk.sb([128, (NROW_TL // 128) * 8], I32, "sent")
            mk.op("pool", lambda eng: eng.iota(base.ap, [[CAP, 32]], base=-1, channel_multiplier=0,
                                                allow_small_or_imprecise_dtypes=True), writes=[base])
            mk.ts(lim, base, float(CAP) + 0.5, None, op0=ALU.add)
            mk.op("pool", lambda eng: eng.iota(trash.ap, [[0, 1]], base=NSLOT, channel_multiplier=1,
                                                allow_small_or_imprecise_dtypes=True), writes=[trash])
            mk.memset(zero_t, 0.0)
            mk.op("pool", lambda eng: eng.iota(sent.ap, [[0, (NROW_TL // 128) * 8]], base=NTOK,
                                                channel_multiplier=0), writes=[sent])
            mk.dma(V(TokL.ap.rearrange("(p r) c -> p (r c)", p=128), TokL.bufs[0]), sent)
            mk.dma(Hrows.rows(NTOK, 128), zero_t.bitcast(BF16)[:, 0:D])
            mk.dma(Ybuf.rows(NSLOT, 128), zero_t)

            xts = [mk.sb([128, D], F32, "xt%d" % i) for i in range(2)]
            sqs = [mk.sb([128, D], F32, "sq%d" % i) for i in range(2)]
            h32s = [mk.sb([128, D], F32, "h32%d" % i) for i in range(2)]
            hbs = [mk.sb([128, D], BF16, "hb%d" % i) for i in range(2)]
            hT32s = [mk.sb([128, 8, 128], F32, "hT32%d" % i) for i in range(2)]
            smalls = [(mk.sb([128, 1], F32), mk.sb([128, 1], F32)) for i in range(2)]
            rt = [dict(lg=mk.sb([128, 36], F32), gmax=mk.sb([128, 1], F32), ngmax=mk.sb([128, 1], F32),
                       ohg=mk.sb([128, 4], F32), eg=mk.sb([128, 4], F32), gsum=mk.sb([128, 1], F32),
                       gw=mk.sb([128, 1], F32), tmp3=mk.sb([128, 4, 8], F32), sel=mk.sb([128, 8], F32),
                       mx8=mk.sb([128, 8], F32), oh1=mk.sb([128, 8], F32), oh2=mk.sb([128, 8], F32),
                       dd=mk.sb([128, 1], F32), r1=mk.sb([128, 1], F32)) for i in range(2)]
            for i in range(NT):
                b = i % 2
                xt, sq, h32, hb, hT32, R = xts[b], sqs[b], h32s[b], hbs[b], hT32s[b], rt[b]
                mk.dma(xt, xres.rows(i * 128, 128))
                self.rms_tile(xt, gbc, h32, sq=sq, small=smalls[b])
                mk.copy(hb, h32, e="pool")
                mk.dma(Hrows.rows(i * 128, 128), hb)
                for half in range(2):
                    pb = self.bank[half]
                    for kk in range(4):
                        k = half * 4 + kk
                        mk.transpose(pb[:, kk * 128:(kk + 1) * 128], h32[:, k * 128:(k + 1) * 128], self.ident32)
                    mk.copy(hT32[:, half * 4:(half + 1) * 4, :].re("p k t -> p (k t)"), pb, e="act")
                pl = self.bank[2 + b]
                for k in range(8):
                    mk.mm(pl[:, 0:36], hT32[:, k, :], wr[:, k, :], start=(k == 0), stop=(k == 7))
                lg = R["lg"]
                mk.tt(lg, pl[:, 0:36], brt, ALU.add)
                mk.reduce(R["gmax"], lg[:, 0:4], op=ALU.max)
                mk.ts(R["ohg"], lg[:, 0:4], R["gmax"], None, op0=ALU.is_equal)
                mk.ts(R["ngmax"], R["gmax"], -1.0, None, op0=ALU.mult)
                mk.act(R["eg"], lg[:, 0:4], AF.Exp, bias=R["ngmax"], accum_out=R["gsum"])
                mk.recip(R["gw"], R["gsum"])
                mk.tt(R["tmp3"], lg[:, 4:36].re("p (g e) -> p g e", g=4),
                      V(R["ohg"].ap.unsqueeze(2).to_broadcast([128, 4, 8]), R["ohg"].buf), ALU.mult)
                mk.reduce(R["sel"], R["tmp3"].re("p g e -> p e g"), op=ALU.add)
                mk.op("dve", lambda eng, R=R: eng.max(out=R["mx8"].ap, in_=R["sel"].ap),
                      reads=[R["sel"]], writes=[R["mx8"]])
                mk.ts(R["oh1"], R["sel"], R["mx8"][:, 0:1], None, op0=ALU.is_equal)
                mk.ts(R["oh2"], R["sel"], R["mx8"][:, 1:2], None, op0=ALU.is_equal)
                mk.tt(R["dd"], R["mx8"][:, 1:2], R["mx8"][:, 0:1], ALU.subtract)
                mk.act(R["dd"], R["dd"], AF.Exp)
                mk.ts(R["dd"], R["dd"], 1.0, None, op0=ALU.add)
                mk.recip(R["r1"], R["dd"])
                mk.tt(gate1[:, i:i + 1], R["gw"], R["r1"], ALU.mult)
                mk.tt(gate2[:, i:i + 1], R["gw"], gate1[:, i:i + 1], ALU.subtract)
                ohg_b = V(R["ohg"].ap.unsqueeze(2).to_broadcast([128, 4, 8]), R["ohg"].buf)
                for oh, Eall in ((R["oh1"], E1all), (R["oh2"], E2all)):
                    oh_b = V(oh.ap.unsqueeze(1).to_broadcast([128, 4, 8]), oh.buf)
                    mk.tt(Eall[:, i, :].re("p (g e) -> p g e", g=4), ohg_b, oh_b, ALU.mult)
                mk.tt(maskall[:, i, :], E1all[:, i, :], E2all[:, i, :], ALU.add)

            posf = [mk.sb([128, 32], F32) for i in range(2)]
            okm = [mk.sb([128, 32], F32) for i in range(2)]
            tmpe = [mk.sb([128, 32], F32) for i in range(2)]
            sl = [mk.sb([128, 2], F32) for i in range(2)]
            tokid = [mk.sb([128, 8], I32) for i in range(2)]
            for i in range(NT):
                b = i % 2
                pc = self.bank[4 + b]
                mk.mm(pc[:, 0:32], self.trib, maskall[:, i, :])
                mk.mm(pc[:, 32:64], self.onesb, maskall[:, i, :])
                mk.tt(posf[b], pc[:, 0:32], base, ALU.add)
                mk.tt(base, base, pc[:, 32:64], ALU.add)
                mk.tt(okm[b], posf[b], lim, ALU.is_lt)
                mk.ts(posf[b], posf[b], trash, None, op0=ALU.subtract)
                mk.tt(posf[b], posf[b], okm[b], ALU.mult)
                mk.ts(posf[b], posf[b], trash, None, op0=ALU.add)
                for j, (Eall, slot) in enumerate(((E1all, slot1), (E2all, slot2))):
                    mk.tt(tmpe[b], posf[b], Eall[:, i, :], ALU.mult)
                    mk.reduce(sl[b][:, j:j + 1], tmpe[b], op=ALU.add)
                    mk.copy(slot[:, i:i + 1], sl[b][:, j:j + 1])
                mk.op("pool", lambda eng, t=tokid[b], i=i: eng.iota(t.ap, [[0, 8]], base=i * 128,
                                                                    channel_multiplier=1), writes=[tokid[b]])
                for slot in (slot1, slot2):
                    mk.op("pool", lambda eng, slot=slot, i=i, t=tokid[b]: eng.indirect_dma_start(
                        out=TokL.ap, out_offset=bass.IndirectOffsetOnAxis(ap=slot.ap[:, i:i + 1], axis=0),
                        in_=t.ap, in_offset=None),
                        reads=[slot, tokid[b]], writes=[V(TokL.ap, TokL.bufs[0])], dma=True)

            NCT = CAP // 128
            wg = [mk.sb([128, 8, 512], BF16, "wg%d" % i) for i in range(2)]
            wu = [mk.sb([128, 8, 512], BF16, "wu%d" % i) for i in range(2)]
            wd = [mk.sb([128, 4, D], BF16, "wd%d" % i) for i in range(2)]
            idx = [mk.sb([128, 8], I32) for i in range(4)]
            xg = [mk.sb([128, D], BF16) for i in range(4)]
            xgT = [mk.sb([128, 8, CAP], BF16) for i in range(2)]
            hidT = [mk.sb([128, 4, CAP], BF16) for i in range(2)]
            sil = [mk.sb([128, CAP], F32) for i in range(2)]
            yrow = [mk.sb([128, D], F32) for i in range(2)]
            nslot = 0
            ny = 0
            for e in range(NE):
                b = e % 2
                mk.dma(wg[b], P["moe_w_gate"][layer, e].re("(k p) n -> p k n", p=128), q="pool")
                mk.dma(wu[b], P["moe_w_up"][layer, e].re("(k p) n -> p k n", p=128), q="pool")
                mk.dma(wd[b], P["moe_w_down"][layer, e].re("(k p) n -> p k n", p=128), q="pool")
                for j in range(NCT):
                    s = nslot % 4
                    nslot += 1
                    r0 = e * CAP + j * 128
                    mk.dma(idx[s], V(TokL.ap[r0:r0 + 128, :], TokL.bufs[0]))
                    mk.op("pool", lambda eng, s=s: eng.indirect_dma_start(
                        out=xg[s].ap, out_offset=None, in_=Hrows.ap,
                        in_offset=bass.IndirectOffsetOnAxis(ap=idx[s].ap[:, 0:1], axis=0)),
                        reads=[idx[s]] + Hrows.all(), writes=[xg[s]], dma=True)
                    pb = self.bank[j % 2]
                    pbb = pb.bitcast(BF16)
                    for k in range(8):
                        mk.transpose(pbb[:, k * 128:(k + 1) * 128], xg[s][:, k * 128:(k + 1) * 128], self.identb)
                    mk.copy(xgT[b][:, :, j * 128:(j + 1) * 128], pbb.re("p (k t) -> p k t", k=8),
                            e=("act" if j % 2 else "dve"))
                for c in range(4):
                    pg = self.bank[2 + (c % 2)]
                    pu = self.bank[4 + (c % 2)]
                    for k in range(8):
                        mk.mm(pg, wg[b][:, k, c * 128:(c + 1) * 128], xgT[b][:, k, :], start=(k == 0), stop=(k == 7))
                    for k in range(8):
                        mk.mm(pu, wu[b][:, k, c * 128:(c + 1) * 128], xgT[b][:, k, :], start=(k == 0), stop=(k == 7))
                    mk.act(sil[c % 2], pg, AF.Silu)
                    mk.tt(hidT[b][:, c, :], sil[c % 2], pu, ALU.mult)
                for j in range(NCT):
                    yb = ny % 2
                    ny += 1
                    for half in range(2):
                        pd = self.bank[6 + half]
                        for c in range(4):
                            mk.mm(pd, hidT[b][:, c, j * 128:(j + 1) * 128], wd[b][:, c, half * 512:(half + 1) * 512],
                                  start=(c == 0), stop=(c == 3))
                        mk.copy(yrow[yb][:, half * 512:(half + 1) * 512], pd, e=("act" if half else "dve"))
                    mk.dma(Ybuf.rows(e * CAP + j * 128, 128), yrow[yb])

            y1 = [mk.sb([128, D], F32) for i in range(2)]
            y2 = [mk.sb([128, D], F32) for i in range(2)]
            for i in range(NT):
                b = i % 2
                xt = xts[b]
                mk.dma(xt, xres.rows(i * 128, 128))
                for slot, yy in ((slot1, y1[b]), (slot2, y2[b])):
                    mk.op("pool", lambda eng, slot=slot, yy=yy, i=i: eng.indirect_dma_start(
                        out=yy.ap, out_offset=None, in_=Ybuf.ap,
                        in_offset=bass.IndirectOffsetOnAxis(ap=slot.ap[:, i:i + 1], axis=0)),
                        reads=[slot] + Ybuf.all(), writes=[yy], dma=True)
                mk.stt(xt, y1[b], gate1[:, i:i + 1], xt, ALU.mult, ALU.add)
                mk.stt(xt, y2[b], gate2[:, i:i + 1], xt, ALU.mult, ALU.add)
                mk.dma(xres.rows(i * 128, 128), xt)


def host_consts():
    ident = np.eye(128, dtype=np.float32)
    tri = np.triu(np.ones((128, 128), np.float32))
    return {"c_ident": ident, "c_tri": tri}


MOE_KEYS = ("norm_ffn_g", "w_router", "b_router", "moe_w_gate", "moe_w_up", "moe_w_down")


def host_moe_params(inputs):
    out = {}
    out["norm_ffn_g"] = np.ascontiguousarray(inputs["norm_ffn_g"], dtype=np.float32)
    out["w_router"] = np.ascontiguousarray(
        np.concatenate([inputs["moe_w_group"], inputs["moe_w_expert"]], axis=-1), dtype=np.float32)
    out["b_router"] = np.ascontiguousarray(
        np.concatenate([inputs["moe_b_group"], inputs["moe_b_expert"]], axis=-1), dtype=np.float32)
    for k in ("moe_w_gate", "moe_w_up", "moe_w_down"):
        out[k] = np.ascontiguousarray(inputs[k], dtype=np.float32)
    return out


TWO_PI = 6.283185307179586
CW1 = 6.28125
CW2 = TWO_PI - CW1
PI_SAFE = 3.1415925


def _rr_sin(self, dst, X, tmpf, tmpi, phase=0.0, e="dve"):
    mk = self.mk
    mk.ts(tmpf, X, 1.0 / TWO_PI, 0.5 + phase / TWO_PI, op0=ALU.mult, op1=ALU.add, e=e)
    mk.copy(tmpi, tmpf, e=e)
    mk.copy(tmpf, tmpi, e=e)
    mk.stt(dst, tmpf, -CW1, X, ALU.mult, ALU.add)
    mk.stt(dst, tmpf, -CW2, dst, ALU.mult, ALU.add)
    if phase:
        mk.ts(dst, dst, phase, None, op0=ALU.add, e=e)
    mk.ts(tmpf, dst, -PI_SAFE, TWO_PI, op0=ALU.is_lt, op1=ALU.mult, e=e)
    mk.tt(dst, dst, tmpf, ALU.add, e=e)
    mk.ts(tmpf, dst, PI_SAFE, TWO_PI, op0=ALU.is_gt, op1=ALU.mult, e=e)
    mk.tt(dst, dst, tmpf, ALU.subtract, e=e)
    mk.ts(dst, dst, PI_SAFE, -PI_SAFE, op0=ALU.min, op1=ALU.max, e=e)
    mk.act(dst, dst, AF.Sin)


Builder.rr_sin = _rr_sin


def _proj_in(self, xres, g_row, W, ncols, fm_blocks, uT, tm_range, u_tm):
    mk = self.mk
    with mk.scope():
        gbc = mk.sb([128, D], F32, "gbc")
        mk.dma(gbc, g_row.pbc(128))
        Wb = mk.sb([128, 8, ncols], BF16, "Wb")
        for k in range(8):
            mk.dma(Wb[:, k, :], W[k * 128:(k + 1) * 128, :], q="pool")
        xts = [mk.sb([128, D], F32) for i in range(2)]
        sqs = [mk.sb([128, D], F32) for i in range(2)]
        hbs = [mk.sb([128, D], BF16) for i in range(2)]
        smalls = [(mk.sb([128, 1], F32), mk.sb([128, 1], F32)) for i in range(2)]
        hT = [mk.sb([128, 8, 512], BF16) for i in range(2)]
        ev = [mk.sb([128, 512], F32) for i in range(4)]
        nev = 0
        for gidx in range(NTOK // 512):
            hb_ = hT[gidx % 2]
            for tl in range(4):
                i = gidx * 4 + tl
                b = i % 2
                mk.dma(xts[b], xres.rows(i * 128, 128))
                self.rms_tile(xts[b], gbc, hbs[b], sq=sqs[b], small=smalls[b])
                pbb = self.bank[b].bitcast(BF16)
                for k in range(8):
                    mk.transpose(pbb[:, k * 128:(k + 1) * 128], hbs[b][:, k * 128:(k + 1) * 128], self.identb)
                mk.copy(hb_[:, :, tl * 128:(tl + 1) * 128], pbb.re("p (k t) -> p k t", k=8),
                        e=("act" if tl % 2 else "pool_never") if False else ("act" if tl % 2 else "dve"))
            for bi, cb in enumerate(fm_blocks):
                pb = self.bank[2 + (bi % 3)]
                for k in range(8):
                    mk.mm(pb, Wb[:, k, cb * 128:(cb + 1) * 128], hb_[:, k, :], start=(k == 0), stop=(k == 7))
                t = ev[nev % 4]
                mk.copy(t, pb, e=("act" if nev % 2 else "dve"))
                nev += 1
                mk.dma(V(uT.ap[bi * 128:(bi + 1) * 128, gidx * 512:(gidx + 1) * 512], uT.bufs[bi]), t)
            if tm_range is not None:
                c0, c1 = tm_range
                for tl in range(4):
                    i = gidx * 4 + tl
                    for cc in range(c0, c1, 512):
                        pb = self.bank[5 + (nev % 3)]
                        for k in range(8):
                            mk.mm(pb, hb_[:, k, tl * 128:(tl + 1) * 128], Wb[:, k, cc:cc + 512],
                                  start=(k == 0), stop=(k == 7))
                        t = ev[nev % 4]
                        mk.copy(t, pb, e=("act" if nev % 2 else "dve"))
                        nev += 1
                        mk.dma(V(u_tm.ap[i * 128:(i + 1) * 128, cc - c0:cc - c0 + 512], u_tm.bufs[i]), t)


Builder.proj_in = _proj_in


def _proj_out(self, xres_in, xres_out, oTd, Wout):
    mk = self.mk
    with mk.scope():
        Wb = mk.sb([128, 8, D], BF16, "Wob")
        for k in range(8):
            mk.dma(Wb[:, k, :], Wout[k * 128:(k + 1) * 128, :], q="pool")
        xts = [mk.sb([128, D], F32) for i in range(2)]
        ot = [mk.sb([128, 8, 512], BF16) for i in range(2)]
        for gi in range(NTOK // 512):
            o_ = ot[gi % 2]
            for k in range(8):
                mk.dma(o_[:, k, :], V(oTd.ap[k * 128:(k + 1) * 128, gi * 512:(gi + 1) * 512], oTd.bufs[k]))
            for tl in range(4):
                i = gi * 4 + tl
                b = i % 2
                mk.dma(xts[b], xres_in.rows(i * 128, 128))
                for half in range(2):
                    pb = self.bank[(i % 2) * 2 + half]
                    for k in range(8):
                        mk.mm(pb, o_[:, k, tl * 128:(tl + 1) * 128], Wb[:, k, half * 512:(half + 1) * 512],
                              start=(k == 0), stop=(k == 7))
                    mk.tt(xts[b][:, half * 512:(half + 1) * 512], xts[b][:, half * 512:(half + 1) * 512], pb, ALU.add)
                mk.dma(xres_out.rows(i * 128, 128), xts[b])


Builder.proj_out = _proj_out


def _attn(self, u_tm, oT, P, layer):
    import math
    mk = self.mk
    oi = layer // 2
    lam_init = 0.8 - 0.6 * math.exp(-0.3 * layer)
    with mk.scope():
        gqk = mk.sb([128, D], F32, "gqk")
        mk.dma(gqk, P["da_qk_gain"][oi:oi + 1, :].pbc(128))
        subg = mk.sb([128, 128], F32, "subg")
        mk.dma(subg, P["da_subln"][oi:oi + 1, :].pbc(128))
        mk.ts(subg, subg, 1.0 - lam_init, None, op0=ALU.mult)
        lamv = mk.sb([128, 4, 64], F32, "lamv")
        mk.dma(lamv.re("p a d -> p (a d)"), P["da_lam"][oi:oi + 1].re("o a d -> o (a d)").pbc(128))
        lt = mk.sb([128, 2, 64], F32)
        ls = mk.sb([128, 2], F32)
        mk.tt(lt[:, 0, :], lamv[:, 0, :], lamv[:, 1, :], ALU.mult)
        mk.tt(lt[:, 1, :], lamv[:, 2, :], lamv[:, 3, :], ALU.mult)
        mk.reduce(ls, lt, op=ALU.add)
        mk.act(ls, ls, AF.Exp)
        nlam = mk.sb([128, 1], F32, "nlam")
        mk.tt(nlam, ls[:, 1:2], ls[:, 0:1], ALU.subtract)
        mk.ts(nlam, nlam, -lam_init, None, op0=ALU.add)
        eps5 = mk.sb([128, 1], F32)
        mk.memset(eps5, 1e-5)
        nshift = mk.sb([128, 1], F32)
        mk.memset(nshift, -4.0)
        zb = mk.sb([128, 512], BF16, "zb")
        mk.memset(zb, 0.0)
        jf = mk.sb([128, 32], F32)
        mk.op("pool", lambda eng: eng.iota(jf.ap, [[1, 32]], base=0, channel_multiplier=0,
                                            allow_small_or_imprecise_dtypes=True), writes=[jf])
        mk.act(jf, jf, AF.Exp, scale=-math.log(10000.0) / 32.0)
        posf = mk.sb([128, 16], F32)
        mk.op("pool", lambda eng: eng.iota(posf.ap, [[128, 16]], base=0, channel_multiplier=1,
                                            allow_small_or_imprecise_dtypes=True), writes=[posf])
        ang = mk.sb([128, 16, 32], F32)
        mk.tt(ang, V(jf.ap.unsqueeze(1).to_broadcast([128, 16, 32]), jf.buf),
              V(posf.ap.unsqueeze(2).to_broadcast([128, 16, 32]), posf.buf), ALU.mult)
        sint = mk.sb([128, 16, 32], F32, "sint")
        cost = mk.sb([128, 16, 32], F32, "cost")
        tf = mk.sb([128, 16, 32], F32)
        ti = mk.sb([128, 16, 32], I32)
        self.rr_sin(sint, ang, tf, ti)
        self.rr_sin(cost, ang, tf, ti, phase=math.pi / 2)

        QT = mk.sb([128, 4, T], BF16, "QT")
        KT = mk.sb([128, 4, T], BF16, "KT")
        Vt = mk.sb([128, 16, 512], BF16, "Vt")
        qk = [mk.sb([128, 16, 2, 32], F32) for i in range(2)]
        sq = mk.sb([128, 16, 64], F32)
        ss = mk.sb([128, 16], F32)
        ta = mk.sb([128, 16, 32], F32)
        tb = mk.sb([128, 16, 32], F32)
        qr = [mk.sb([128, 16, 2, 32], BF16) for i in range(2)]
        pts = [mk.sb([128, 512], BF16) for i in range(3)]
        rls = [mk.sb([128, 8], F32) for i in range(2)]
        ob32 = [mk.sb([128, 128], F32) for i in range(2)]
        obb = [mk.sb([128, 128], BF16) for i in range(2)]
        junk = mk.sb([128, 128], F32)
        ss1 = [mk.sb([128, 1], F32) for i in range(2)]
        npt = 0
        nfin = 0
        ostage = [mk.sb([128, T], BF16, "ostage%d" % i) for i in range(2)]
        for s in range(NSEQ):
            mk.dma(Vt, V(u_tm.ap[s * T:(s + 1) * T, 1024:1536].rearrange("(i p) c -> p i c", p=128),
                         u_tm.whole), q="pool", )
            for i in range(16):
                b = i % 2
                row0 = s * T + i * 128
                q_ = qk[b]
                qf = q_.re("p g m d -> p (g m d)")
                mk.dma(qf, V(u_tm.ap[row0:row0 + 128, 0:1024], u_tm.bufs[row0 // 128]))
                mk.act(sq.re("p g d -> p (g d)"), qf, AF.Square)
                mk.reduce(ss, sq, op=ALU.add)
                mk.act(ss, ss, AF.Sqrt, scale=1.0 / 64.0, bias=self.eps_t)
                mk.recip(ss, ss)
                q3 = q_.re("p g m d -> p g (m d)")
                mk.tt(q3, q3, V(ss.ap.unsqueeze(2).to_broadcast([128, 16, 64]), ss.buf), ALU.mult)
                mk.tt(qf, qf, gqk, ALU.mult)
                cb_ = V(cost.ap[:, i, :].unsqueeze(1).to_broadcast([128, 16, 32]), cost.buf)
                sb_ = V(sint.ap[:, i, :].unsqueeze(1).to_broadcast([128, 16, 32]), sint.buf)
                x1 = q_[:, :, 0, :]
                x2 = q_[:, :, 1, :]
                mk.tt(ta, x1, cb_, ALU.mult)
                mk.tt(tb, x2, sb_, ALU.mult, e="pool")
                mk.tt(qr[b][:, :, 0, :], ta, tb, ALU.subtract)
                mk.tt(ta, x2, cb_, ALU.mult)
                mk.tt(tb, x1, sb_, ALU.mult, e="pool")
                mk.tt(qr[b][:, :, 1, :], ta, tb, ALU.add)
                qrf = qr[b].re("p g m d -> p (g m d)")
                pbb = self.bank[6 + b].bitcast(BF16)
                for k in range(8):
                    mk.transpose(pbb[:, k * 128:(k + 1) * 128], qrf[:, k * 128:(k + 1) * 128], self.identb)
                mk.copy(QT[:, :, i * 128:(i + 1) * 128], pbb[:, 0:512].re("p (h t) -> p h t", h=4), e="act")
                mk.copy(KT[:, :, i * 128:(i + 1) * 128], pbb[:, 512:1024].re("p (h t) -> p h t", h=4), e="act")
            units = [(h, qc) for h in range(4) for qc in range(4)]

            def osets(u):
                par = u % 2
                O = [self.bank[2], self.bank[3]] if par == 0 else [self.bank[6], self.bank[7]]
                Lb = V(self.bank[4].ap[:, 8 * par:8 * par + 8], self.bank[4].buf)
                return O, Lb

            def main(u):
                nonlocal npt
                h, qc = units[u]
                O, Lb = osets(u)
                mk.mm(O[0], zb[:, 0:128], zb)
                mk.mm(O[1], zb[:, 0:128], zb)
                mk.mm(Lb, zb[:, 0:128], zb[:, 0:8])
                steps = [(m, kt) for m in range(2) for kt in range(4 * qc + 4)]
                info = []

                def issue_S(i):
                    nonlocal npt
                    m, kt = steps[i]
                    q0 = max(kt * 128, qc * 512)
                    nq = (qc + 1) * 512 - q0
                    S = self.bank[npt % 2]
                    Pt = pts[npt % 3]
                    npt += 1
                    mk.mm(S[:, 0:nq], KT[m * 64:(m + 1) * 64, h, kt * 128:(kt + 1) * 128],
                          QT[m * 64:(m + 1) * 64, h, q0:q0 + nq])
                    info.append((S, Pt, q0, nq))

                issue_S(0)
                for i, (m, kt) in enumerate(steps):
                    if i + 1 < len(steps):
                        issue_S(i + 1)
                    S, Pt, q0, nq = info[i]
                    mk.act(Pt[:, 0:nq], S[:, 0:nq], AF.Exp, scale=0.125, bias=nshift)
                    if kt >= 4 * qc:
                        mk.tt(Pt[:, 0:128], Pt[:, 0:128], self.trib, ALU.mult, e="pool")
                    for qb in range(max(kt, 4 * qc), 4 * qc + 4):
                        ql = qb - 4 * qc
                        c0 = qb * 128 - q0
                        mk.mm(O[m][:, ql * 128:(ql + 1) * 128], Pt[:, c0:c0 + 128],
                              Vt[:, kt, h * 128:(h + 1) * 128], start=False, stop=(kt == qb),
                              skip_group_check=True)
                        mk.mm(Lb[:, m * 4 + ql:m * 4 + ql + 1], Pt[:, c0:c0 + 128], self.onesb[:, 0:1],
                              start=False, stop=(kt == qb), skip_group_check=True)

            def fin(u):
                nonlocal nfin
                h, qc = units[u]
                O, Lb = osets(u)
                rl = rls[u % 2]
                mk.recip(rl, Lb)
                mk.ts(rl[:, 4:8], rl[:, 4:8], nlam, None, op0=ALU.mult)
                for ql in range(4):
                    fb = nfin % 2
                    nfin += 1
                    o = ob32[fb]
                    mk.ts(o, O[0][:, ql * 128:(ql + 1) * 128], rl[:, ql:ql + 1], None, op0=ALU.mult)
                    mk.stt(o, O[1][:, ql * 128:(ql + 1) * 128], rl[:, 4 + ql:5 + ql], o, ALU.mult, ALU.add)
                    mk.act(junk, o, AF.Square, accum_out=ss1[fb])
                    mk.act(ss1[fb], ss1[fb], AF.Sqrt, scale=1.0 / 128.0, bias=eps5)
                    mk.recip(ss1[fb], ss1[fb])
                    mk.stt(obb[fb], o, ss1[fb], subg, ALU.mult, ALU.mult)
                    ptr = self.bank[5].bitcast(BF16)
                    mk.transpose(ptr[:, fb * 128:(fb + 1) * 128], obb[fb], self.identb)
                    t0 = (4 * qc + ql) * 128
                    mk.copy(ostage[h % 2][:, t0:t0 + 128], ptr[:, fb * 128:(fb + 1) * 128], e="act")
                if qc == 3:
                    mk.dma(V(oT.ap[h * 128:(h + 1) * 128, s * T:(s + 1) * T], oT.bufs[h]), ostage[h % 2])

            main(0)
            for u in range(len(units)):
                if u + 1 < len(units):
                    main(u + 1)
                fin(u)


Builder.attn = _attn


def _s5_params(self, a_re, a_im, lstep, shape, want_coef):
    import math
    mk = self.mk
    n = lambda: mk.sb(shape, F32)
    are, step, lr, th, rho = n(), n(), n(), n(), n()
    mk.ts(are, a_re, -1e-4, None, op0=ALU.min)
    mk.act(step, lstep, AF.Exp)
    mk.tt(lr, are, step, ALU.mult)
    mk.tt(th, a_im, step, ALU.mult)
    mk.act(rho, lr, AF.Exp)
    out = dict(rho=rho, th=th)
    if want_coef:
        sn, cs, tf, x, y, den, cre, cim = n(), n(), n(), n(), n(), n(), n(), n()
        ti = mk.sb(shape, I32)
        self.rr_sin(sn, th, tf, ti)
        self.rr_sin(cs, th, tf, ti, phase=math.pi / 2)
        mk.tt(x, rho, cs, ALU.mult)
        mk.ts(x, x, -1.0, None, op0=ALU.add)
        mk.tt(y, rho, sn, ALU.mult)
        mk.tt(den, are, are, ALU.mult)
        mk.tt(tf, a_im, a_im, ALU.mult)
        mk.tt(den, den, tf, ALU.add)
        mk.recip(den, den)
        mk.tt(cre, x, are, ALU.mult)
        mk.tt(tf, y, a_im, ALU.mult)
        mk.tt(cre, cre, tf, ALU.add)
        mk.tt(cre, cre, den, ALU.mult)
        mk.tt(cim, y, are, ALU.mult)
        mk.tt(tf, x, a_im, ALU.mult)
        mk.tt(cim, cim, tf, ALU.subtract)
        mk.tt(cim, cim, den, ALU.mult)
        out.update(cre=cre, cim=cim)
    return out


Builder.s5_params = _s5_params


def _s5(self, uT, oT, P, layer):
    import math
    mk = self.mk
    oi = layer // 2
    with mk.scope():
        bbr = mk.sb([128, 4, 128], BF16, "bbr")
        bbi = mk.sb([128, 4, 128], BF16, "bbi")
        rho = mk.sb([128, 16], F32, "rho16")
        theta = mk.sb([128, 16], F32, "th16")
        bfr = mk.sb([128, 16, 128], BF16, "bfr")
        bfi = mk.sb([128, 16, 128], BF16, "bfi")
        Cfr = mk.sb([128, 16, 128], BF16, "Cfr")
        Cfi = mk.sb([128, 16, 128], BF16, "Cfi")
        with mk.scope():
            rep = mk.sb([128, 3, 512], F32, "rep")
            mk.dma(rep, P["s5_rep"][oi].re("a p j s -> p a (j s)"))
            pr_ = self.s5_params(rep[:, 0, :], rep[:, 1, :], rep[:, 2, :], [128, 512], True)
            Bre = mk.sb([128, 512], F32)
            Bim = mk.sb([128, 512], F32)
            mk.dma(Bre, P["s5_bbd_re"][oi].re("p j s -> p (j s)"))
            mk.dma(Bim, P["s5_bbd_im"][oi].re("p j s -> p (j s)"))
            t1p = mk.sb([128, 512], F32)
            t2p = mk.sb([128, 512], F32)
            mk.tt(t1p, Bre, pr_["cre"], ALU.mult)
            mk.tt(t2p, Bim, pr_["cim"], ALU.mult)
            mk.tt(bbr.re("p j s -> p (j s)"), t1p, t2p, ALU.subtract)
            mk.tt(t1p, Bre, pr_["cim"], ALU.mult)
            mk.tt(t2p, Bim, pr_["cre"], ALU.mult)
            mk.tt(bbi.re("p j s -> p (j s)"), t1p, t2p, ALU.add)
            mk.memset(bfr, 0.0)
            mk.memset(bfi, 0.0)
            for q in range(4):
                for (src, dst) in ((bbr, bfr), (bbi, bfi)):
                    mk.copy(dst[32 * q:32 * q + 32].re("p (j q) s -> p j q s", q=4)[:, :, q, :],
                            src[32 * q:32 * q + 32, :, :], e="pool")
            st = mk.sb([128, 3, 16], F32, "st")
            mk.dma(st, P["s5_st"][oi].re("a p b -> p a b"))
            ps_ = self.s5_params(st[:, 0, :], st[:, 1, :], st[:, 2, :], [128, 16], False)
            mk.copy(rho, ps_["rho"])
            mk.copy(theta, ps_["th"])
        Cre = mk.sb([128, 16, 32], BF16, "Cre")
        nCim = mk.sb([128, 16, 32], BF16, "nCim")
        cim32 = mk.sb([128, 16, 32], F32)
        mk.dma(Cre, P["s5_cbd_re"][oi], q="pool")
        mk.dma(cim32, P["s5_cbd_im"][oi])
        mk.ts(nCim, cim32, -1.0, None, op0=ALU.mult)
        mk.memset(Cfr, 0.0)
        mk.memset(Cfi, 0.0)
        for q in range(4):
            for (src, dst) in ((Cre, Cfr), (nCim, Cfi)):
                mk.copy(dst.re("p (j q) c -> p j q c", q=4)[:, :, q, 32 * q:32 * q + 32],
                        src.re("p (j q) c -> p j q c", q=4)[:, :, q, :], e="pool")
        dcol = mk.sb([128, 4], F32, "dcol")
        mk.dma(dcol, P["s5_dcol"][oi])
        cbase = mk.sb([128, 16, 64], F32, "cbase")
        sbase = mk.sb([128, 16, 64], F32, "sbase")
        cstep = mk.sb([128, 16, 32], F32, "cstep")
        sstep = mk.sb([128, 16, 32], F32, "sstep")
        with mk.scope():
            rio = mk.sb([128, 64], F32)
            mk.op("pool", lambda eng: eng.iota(rio.ap, [[1, 64]], base=0, channel_multiplier=0,
                                                allow_small_or_imprecise_dtypes=True), writes=[rio])
            kio = mk.sb([128, 32], F32)
            mk.op("pool", lambda eng: eng.iota(kio.ap, [[64, 32]], base=0, channel_multiplier=0,
                                                allow_small_or_imprecise_dtypes=True), writes=[kio])
            angb = mk.sb([128, 16, 64], F32)
            angs = mk.sb([128, 16, 32], F32)
            mk.tt(angb, V(theta.ap.unsqueeze(2).to_broadcast([128, 16, 64]), theta.buf),
                  V(rio.ap.unsqueeze(1).to_broadcast([128, 16, 64]), rio.buf), ALU.mult)
            mk.tt(angs, V(theta.ap.unsqueeze(2).to_broadcast([128, 16, 32]), theta.buf),
                  V(kio.ap.unsqueeze(1).to_broadcast([128, 16, 32]), kio.buf), ALU.mult)
            tfb = mk.sb([128, 16, 64], F32)
            tib = mk.sb([128, 16, 64], I32)
            self.rr_sin(sbase, angb, tfb, tib)
            self.rr_sin(cbase, angb, tfb, tib, phase=math.pi / 2)
            self.rr_sin(sstep, angs, tfb[:, :, 0:32], tib[:, :, 0:32])
            self.rr_sin(cstep, angs, tfb[:, :, 0:32], tib[:, :, 0:32], phase=math.pi / 2)
        ubb = mk.sb([128, NTOK], BF16, "ubb")
        zTd = self.scratch("zTd", [512, NTOK], BF16)
        yT = mk.sb([128, NTOK], F32, "yT")
        sint2 = [mk.sb([128, T], F32, "sint%d" % i) for i in range(2)]
        cost2 = [mk.sb([128, T], F32, "cost%d" % i) for i in range(2)]
        gre2 = [mk.sb([128, T], F32, "gre%d" % i) for i in range(2)]
        gim2 = [mk.sb([128, T], F32, "gim%d" % i) for i in range(2)]
        wre2 = [mk.sb([128, T], F32, "wre%d" % i) for i in range(2)]
        wim2 = [mk.sb([128, T], F32, "wim%d" % i) for i in range(2)]
        xre2 = [mk.sb([128, T], BF16, "xre%d" % i) for i in range(2)]
        xim2 = [mk.sb([128, T], BF16, "xim%d" % i) for i in range(2)]
        tt1 = mk.sb([128, T], F32, "tt1")
        tt2 = mk.sb([128, T], F32, "tt2")
        bur_f = mk.sb([128, T], F32, "bur_f")
        bui_f = mk.sb([128, T], F32, "bui_f")
        ubb2 = [ubb, ubb]
        state = dict(nb=0)

        def tables(sbi):
            sint, cost = sint2[sbi % 2], cost2[sbi % 2]
            gre, gim, wre, wim = gre2[0], gim2[0], wre2[0], wim2[0]
            cs_b = V(cstep.ap[:, sbi, :].unsqueeze(2).to_broadcast([128, 32, 64]), cstep.buf)
            ss_b = V(sstep.ap[:, sbi, :].unsqueeze(2).to_broadcast([128, 32, 64]), sstep.buf)
            cb_b = V(cbase.ap[:, sbi, :].unsqueeze(1).to_broadcast([128, 32, 64]), cbase.buf)
            sb_b = V(sbase.ap[:, sbi, :].unsqueeze(1).to_broadcast([128, 32, 64]), sbase.buf)
            v3_ = lambda t_: t_.re("p (k r) -> p k r", r=64)
            mk.tt(v3_(tt1), cs_b, cb_b, ALU.mult)
            mk.tt(v3_(tt2), ss_b, sb_b, ALU.mult, e="pool")
            mk.tt(cost, tt1, tt2, ALU.subtract)
            mk.tt(v3_(tt1), ss_b, cb_b, ALU.mult, e="pool")
            mk.tt(v3_(tt2), cs_b, sb_b, ALU.mult)
            mk.tt(sint, tt1, tt2, ALU.add, e="pool")

        def multi(b0, nb_):
            return V(self.psum_all.ap[:, b0 * 512:(b0 + nb_) * 512], self.bank[b0].buf)

        def stageA(it):
            sbi, s = it // NSEQ, it % NSEQ
            cb = sbi // 4
            sint, cost = sint2[sbi % 2], cost2[sbi % 2]
            gre, gim, wre, wim = gre2[it % 2], gim2[it % 2], wre2[it % 2], wim2[it % 2]
            ub_ = ubb2[cb % 2]
            rho_b = V(rho.ap[:, sbi:sbi + 1].to_broadcast([128, T]), rho.buf)
            for ch in range(4):
                tok0 = s * T + ch * 512
                mk.mm(self.bank[ch], bfr[:, sbi, :], ub_[:, tok0:tok0 + 512])
            for ch in range(4):
                tok0 = s * T + ch * 512
                mk.mm(self.bank[4 + ch], bfi[:, sbi, :], ub_[:, tok0:tok0 + 512])
            rd_r = [self.bank[i] for i in range(1, 4)]
            rd_i = [self.bank[i] for i in range(5, 8)]
            src_r, src_i = multi(0, 4), multi(4, 4)
            mk.op("act", lambda eng: eng.copy(out=bur_f.ap, in_=src_r.ap), reads=[src_r] + rd_r, writes=[bur_f])
            mk.op("act", lambda eng: eng.copy(out=bui_f.ap, in_=src_i.ap), reads=[src_i] + rd_i, writes=[bui_f])
            mk.tt(tt1, bur_f, cost, ALU.mult)
            mk.tt(gre, bui_f, sint, ALU.mult, e="pool")
            mk.tt(gre, gre, tt1, ALU.add)
            mk.tt(tt2, bui_f, cost, ALU.mult)
            mk.tt(gim, bur_f, sint, ALU.mult, e="pool")
            mk.tt(gim, tt2, gim, ALU.subtract)
            mk.scan(wre, rho_b, gre, 0.0)
            mk.scan(wim, rho_b, gim, 0.0)

        def stageB(it):
            sbi, s = it // NSEQ, it % NSEQ
            cb, q = sbi // 4, sbi % 4
            sint, cost = sint2[sbi % 2], cost2[sbi % 2]
            gre, gim, wre, wim = gre2[it % 2], gim2[it % 2], wre2[it % 2], wim2[it % 2]
            xre, xim = xre2[it % 2], xim2[it % 2]
            mk.tt(gre, cost, wre, ALU.mult, e="pool")
            mk.tt(gim, sint, wim, ALU.mult)
            mk.tt(xre, gre, gim, ALU.subtract)
            mk.tt(wre, sint, wre, ALU.mult)
            mk.tt(wim, cost, wim, ALU.mult, e="pool")
            mk.tt(xim, wre, wim, ALU.add)
            for ch in range(4):
                sl = slice(ch * 512, (ch + 1) * 512)
                py = self.bank[ch]
                mk.mm(py, Cfr[:, sbi, :], xre[:, sl], start=True, stop=False)
                mk.mm(py, Cfi[:, sbi, :], xim[:, sl], start=False, stop=True)
            src_y = multi(0, 4)
            rd_y = [self.bank[i] for i in range(1, 4)]
            ysl = yT[:, s * T:(s + 1) * T]
            if q == 0:
                mk.op("act", lambda eng: eng.copy(out=ysl.ap, in_=src_y.ap), reads=[src_y] + rd_y, writes=[ysl])
            else:
                mk.op("act", lambda eng: eng.copy(out=bur_f.ap, in_=src_y.ap), reads=[src_y] + rd_y, writes=[bur_f])
                mk.tt(ysl, ysl, bur_f, ALU.add, e="pool")

        def finish(cb):
            for s in range(NSEQ):
                ys = yT[:, s * T:(s + 1) * T]
                mk.dma(tt1, V(uT.ap[cb * 128:(cb + 1) * 128, s * T:(s + 1) * T], uT.bufs[cb]))
                mk.stt(ys, tt1, dcol[:, cb:cb + 1], ys, ALU.mult, ALU.add)
                mk.tt(tt2, ys, ys, ALU.mult, e="pool")
                mk.ts(tt2, tt2, 0.044715, 1.0, op0=ALU.mult, op1=ALU.add)
                mk.tt(tt2, tt2, ys, ALU.mult, e="pool")
                mk.act(tt2, tt2, AF.Sigmoid, scale=2.0 * math.sqrt(2.0 / math.pi))
                mk.tt(xre2[s % 2], ys, tt2, ALU.mult)
                mk.dma(V(zTd.ap[cb * 128:(cb + 1) * 128, s * T:(s + 1) * T], zTd.bufs[cb]), xre2[s % 2], q="pool")

        NIT = 16 * NSEQ
        for cb in range(4):
            if cb == 0:
                mk.dma(ubb2[0], V(uT.ap[0:128, :], uT.bufs[0]), q="pool")
                tables(0)
                stageA(0)
            for q in range(4):
                sbi = 4 * cb + q
                for s in range(NSEQ):
                    it = sbi * NSEQ + s
                    nxt = it + 1
                    if nxt < NIT and (nxt // NSEQ) // 4 == cb:
                        if nxt % NSEQ == 0:
                            tables(nxt // NSEQ)
                        stageA(nxt)
                    stageB(it)
            finish(cb)
            nxt = (4 * cb + 4) * NSEQ
            if nxt < NIT:
                mk.dma(ubb2[(cb + 1) % 2], V(uT.ap[(cb + 1) * 128:(cb + 2) * 128, :], uT.bufs[cb + 1]), q="pool")
                tables(nxt // NSEQ)
                stageA(nxt)
    with mk.scope():
        wgl = mk.sb([128, 4, 512], BF16, "wgl")
        for k in range(4):
            mk.dma(wgl[:, k, :], P["s5_w_glu"][oi, k * 128:(k + 1) * 128, :], q="pool")
        sg = [mk.sb([128, 512], F32) for i in range(4)]
        obuf = [mk.sb([128, 512], BF16) for i in range(4)]
        zc = [mk.sb([128, 4, 512], BF16, "zc%d" % i) for i in range(2)]
        for ch in range(NTOK // 512):
            sl = slice(ch * 512, (ch + 1) * 512)
            zch = zc[ch % 2]
            for k in range(4):
                mk.dma(zch[:, k, :], V(zTd.ap[k * 128:(k + 1) * 128, sl], zTd.bufs[k]))
            for cbo in range(4):
                pg = self.bank[cbo]
                for k in range(4):
                    mk.mm(pg, wgl[:, k, cbo * 128:(cbo + 1) * 128], zch[:, k, :], start=(k == 0), stop=(k == 3))
                mk.act(sg[cbo], pg, AF.Sigmoid)
            for cbo in range(4):
                ob_ = obuf[(ch * 4 + cbo) % 4]
                mk.tt(ob_, zch[:, cbo, :], sg[cbo], ALU.mult, e=("pool" if cbo % 2 else "dve"))
                mk.dma(V(oT.ap[(4 + cbo) * 128:(5 + cbo) * 128, sl], oT.bufs[4 + cbo]), ob_)


Builder.s5 = _s5


def _odd_layer(self, xres_in, xres_out, layer, P):
    mk = self.mk
    oi = layer // 2
    u_tm = self.scratch("u_tm", [NTOK, 1536], F32)
    uT = self.scratch("uTo", [512, NTOK], F32)
    self.proj_in(xres_in, P["norm_mix_g"][layer:layer + 1, :], P["odd_w_in"][oi], 2048,
                 [12, 13, 14, 15], uT, (0, 1536), u_tm)
    oT = self.scratch("oTd", [D, NTOK], BF16)
    if not DBG.get("no_attn"):
        self.attn(u_tm, oT, P, layer)
    if not DBG.get("no_s5"):
        self.s5(uT, oT, P, layer)
    self.proj_out(xres_in, xres_out, oT, P["odd_w_out"][oi])


Builder.odd_layer = _odd_layer


def host_odd_params(inputs):
    f = lambda a: np.ascontiguousarray(a, dtype=np.float32)
    out = {}
    out["norm_mix_g"] = f(inputs["norm_mix_g"])
    out["odd_w_in"] = f(inputs["odd_w_in"])
    out["odd_w_out"] = f(inputs["odd_w_out"])
    n_odd = inputs["odd_w_in"].shape[0]
    out["da_qk_gain"] = f(np.concatenate([np.tile(inputs["da_q_norm"], (1, 8)),
                                          np.tile(inputs["da_k_norm"], (1, 8))], axis=1))
    out["da_lam"] = f(np.stack([inputs["da_lam_q1"], inputs["da_lam_k1"],
                                inputs["da_lam_q2"], inputs["da_lam_k2"]], axis=1))
    out["da_subln"] = f(inputs["da_subln"])
    three = np.stack([inputs["s5_a_re"], inputs["s5_a_im"], inputs["s5_log_step"]], axis=1)
    t16 = three.reshape(n_odd, 3, 16, 128)
    rep = t16.reshape(n_odd, 3, 4, 4, 128)
    rep = np.transpose(rep, (0, 1, 3, 2, 4))
    rep = np.repeat(rep[:, :, :, None, :, :], 32, axis=3)
    out["s5_rep"] = f(rep.reshape(n_odd, 3, 128, 4, 128))
    out["s5_st"] = f(np.transpose(t16, (0, 1, 3, 2)))
    for nm, key in (("s5_bbd_re", "s5_b_re"), ("s5_bbd_im", "s5_b_im")):
        Bm = inputs[key]
        bd = np.zeros((n_odd, 4, 2, 16, 4, 2, 64), np.float32)
        for j in range(4):
            for q in range(4):
                for gl in range(2):
                    g = 2 * (4 * j + q) + gl
                    bd[:, q, gl, :, j, gl, :] = np.transpose(Bm[:, g], (0, 2, 1))
        out[nm] = f(bd.reshape(n_odd, 128, 4, 128))
    for nm, key in (("s5_cbd_re", "s5_c_re"), ("s5_cbd_im", "s5_c_im")):
        Cm = inputs[key]
        bd = np.zeros((n_odd, 2, 64, 16, 2, 16), np.float32)
        for sbi in range(16):
            for gl in range(2):
                bd[:, gl, :, sbi, gl, :] = np.transpose(Cm[:, 2 * sbi + gl], (0, 2, 1))
        out[nm] = f(bd.reshape(n_odd, 128, 16, 32))
    out["s5_dcol"] = f(np.transpose(inputs["s5_d"].reshape(n_odd, 4, 128), (0, 2, 1)))
    out["s5_w_glu"] = f(inputs["s5_w_glu"])
    return out


LCH = 64
NCH = T // LCH
DECAY_C = 0.6065306597126334


def _load_shift_mix(self, dst, uT, blk, s, mu_col, U, dtmp):
    mk = self.mk
    mk.dma(U[:, 1:T + 1], V(uT.ap[blk * 128:(blk + 1) * 128, s * T:(s + 1) * T], uT.bufs[blk]))
    mk.tt(dtmp, U[:, 0:T], U[:, 1:T + 1], ALU.subtract, e="pool")
    mk.stt(dst, dtmp, mu_col, U[:, 1:T + 1], ALU.mult, ALU.add)


Builder.load_shift_mix = _load_shift_mix


def _rwkv(self, uT, oTd, P, layer, vfirst):
    mk = self.mk
    ei = layer // 2
    has_vres = layer > 0
    with mk.scope():
        mu = mk.sb([128, 14], F32, "mu")
        mk.dma(mu, P["rw_mu_col"][ei])
        cols = mk.sb([128, 7, 4], F32, "cols")
        mk.dma(cols, P["rw_cols"][ei])
        w0c, a0c, kkc, kac, rkc, lngc, lnbc = [cols[:, i, :] for i in range(7)]
        w2a2 = mk.sb([128, 512], BF16, "w2a2")
        mk.dma(w2a2, P["rw_w2a2"][ei], q="pool")
        g2b = mk.sb([128, 512], BF16, "g2b")
        mk.dma(g2b, P["rw_g2"][ei], q="pool")
        blk = mk.sb([128, 128], BF16, "blk")
        mk.dma(blk, self.inp["c_blk"], q="pool")
        masks = mk.sb([128, 3, 128], BF16, "masks")
        mk.dma(masks, self.inp["c_masks"].re("a p c -> p a c"), q="pool")

        def mb(i):
            return V(masks.ap[:, i, :].unsqueeze(1).to_broadcast([128, 2, 128]), masks.buf)
        mLs, mUs, mUi = mb(0), mb(1), mb(2)
        rmask = mk.sb([128, T], BF16, "rmask")
        tB = mk.sb([128, T], F32, "tB")
        r32 = mk.sb([128, T], F32, "r32")
        mk.op("pool", lambda eng: eng.iota(tB.ap.rearrange("p (c l) -> p c l", l=LCH), [[0, NCH], [1, LCH]],
                                            base=0, channel_multiplier=0, allow_small_or_imprecise_dtypes=True),
              writes=[tB])
        mk.ts(rmask, tB, 1.0, None, op0=ALU.min)
        lneps = mk.sb([128, 1], F32)
        mk.memset(lneps, 64e-5)
        zb = mk.sb([128, 128], BF16, "zb")
        mk.memset(zb, 0.0)
        U = mk.sb([128, T + 1], F32, "U")
        mk.memset(U[:, 0:1], 0.0)
        dtmp = tB
        m_ = r32
        lr12 = mk.sb([128, T], BF16, "lr12")
        sdg = mk.sb([128, T], BF16, "sdg")
        tbf = mk.sb([128, T], BF16, "tbf")
        if has_vres:
            v0c = mk.sb([128, 4], F32, "v0c")
            mk.dma(v0c, P["rw_v0col"][ei - 1])
            v1b = mk.sb([128, 4, 32], BF16, "v1b")
            mk.dma(v1b, P["rw_v1"][ei - 1].re("(k p) n -> p k n", p=128), q="pool")
            v2b = mk.sb([32, 512], BF16, "v2b")
            mk.dma(v2b, P["rw_v2"][ei - 1], q="pool")
            t32b = mk.sb([32, T], BF16, "t32b")
        k32 = mk.sb([128, T], F32, "k32")
        a16 = mk.sb([128, T], BF16, "a16")
        lw = mk.sb([128, T], F32, "lw")
        cl = mk.sb([128, T], F32, "cl")
        v32 = cl
        tA = mk.sb([128, T], F32, "tA")
        gT = mk.sb([128, T], BF16, "gT")
        bon = mk.sb([128, T], BF16, "bon")
        aT_ = mk.sb([128, T], BF16, "aT_")
        bT_ = mk.sb([128, T], BF16, "bT_")
        kT_ = mk.sb([128, T], BF16, "kT_")
        vT_ = tbf
        a_bd = mk.sb([128, 2, T], BF16, "a_bd")
        b_bd = mk.sb([128, 2, T], BF16, "b_bd")
        r_bd = mk.sb([128, 2, T], BF16, "r_bd")
        for t_ in (a_bd, b_bd, r_bd):
            mk.memset(t_, 0.0)
        TMav = mk.sb([128, 16, 2, 128], BF16, "TMav")
        TMbk = mk.sb([128, 16, 2, 2, 128], BF16, "TMbk")
        mk.memset(TMbk, 0.0)
        DL = mk.sb([128, NCH], F32, "DL")
        H32 = mk.sb([128, 128], F32, "H32")
        Hb = mk.sb([128, 128], BF16, "Hb")
        ht1 = mk.sb([128, 128], F32, "ht1")
        oacc = mk.sb([128, T], BF16, "oacc")

        def grp():
            d = dict(X=[mk.sb([128, 2, 128], BF16) for _ in range(2)], XT=[mk.sb([128, 2, 128], BF16) for _ in range(2)],
                     AakT=mk.sb([128, 2, 128], BF16), ArbT=mk.sb([128, 2, 128], BF16), ArkT=mk.sb([128, 2, 128], BF16),
                     Z=mk.sb([128, 2, 2, 64], BF16), T1T=mk.sb([128, 2, 128], BF16), G1=mk.sb([128, 2, 128], F32),
                     QT=mk.sb([128, 2, 128], BF16), yn=mk.sb([128, 128], BF16), ot=mk.sb([128, 128], F32),
                     st6=mk.sb([128, 2, 6], F32), mv=mk.sb([128, 2, 2], F32), rs=mk.sb([128, 2], F32))
            mk.memset(d["T1T"], 0.0)
            mk.memset(d["G1"], 0.0)
            mk.memset(d["QT"], 0.0)
            return d
        G = [grp(), grp()]
        B_ = self.bank

        def reg(b, c0, c1):
            return V(B_[b].ap[:, c0:c1], B_[b].buf)
        pN, pNT = reg(0, 0, 256), reg(0, 256, 512)
        pAk, pRb = reg(1, 0, 256), reg(1, 256, 512)
        pRk, pZ2 = reg(2, 0, 256), reg(2, 256, 384)
        pZa = [reg(3, 0, 256), reg(3, 256, 512)]
        pX, pXT = reg(4, 0, 256), reg(4, 256, 512)
        pHp, pTr = reg(5, 0, 128), reg(5, 128, 256)
        pT, pG = reg(6, 0, 256), reg(6, 256, 512)
        pQ, pY = reg(3, 0, 256), reg(7, 0, 128)
        pbig = [B_[5], B_[6], B_[7]]

        def v3(t):
            return t.re("p (h c) -> p h c", h=2)

        for s in range(NSEQ):
            tsl = slice(s * T, (s + 1) * T)
            self.load_shift_mix(m_, uT, 12, s, mu[:, 12:13], U, dtmp)
            mk.act(lr12[0:64, :], m_[0:64, :], AF.Tanh)
            mk.copy(lr12[64:128, :], m_[64:128, :], e="pool")
            self.load_shift_mix(m_, uT, 13, s, mu[:, 13:14], U, dtmp)
            mk.act(sdg, m_, AF.Sigmoid)
            if has_vres:
                pv = [B_[i] for i in range(4)]
                for hb in range(4):
                    self.load_shift_mix(m_, uT, 8 + hb, s, mu[:, 8 + hb:9 + hb], U, dtmp)
                    mk.copy(tbf, m_, e="act")
                    for ch in range(4):
                        mk.mm(pv[ch][0:32, :], v1b[:, hb, :], tbf[:, ch * 512:(ch + 1) * 512],
                              start=(hb == 0), stop=(hb == 3))
                for ch in range(4):
                    mk.copy(t32b[:, ch * 512:(ch + 1) * 512], pv[ch][0:32, :], e="act")
            for hb in range(4):
                hsl = slice(hb * 128, (hb + 1) * 128)
                self.load_shift_mix(r32, uT, hb, s, mu[:, hb:hb + 1], U, dtmp)
                self.load_shift_mix(k32, uT, 4 + hb, s, mu[:, 4 + hb:5 + hb], U, dtmp)
                self.load_shift_mix(v32, uT, 8 + hb, s, mu[:, 8 + hb:9 + hb], U, dtmp)
                for ch in range(4):
                    csl = slice(ch * 512, (ch + 1) * 512)
                    pb = pbig[ch % 3]
                    mk.mm(pb, w2a2[0:64, hsl], lr12[0:64, csl])
                    mk.act(lw[:, csl], pb, AF.Sigmoid, bias=w0c[:, hb:hb + 1])
                    pb = pbig[(ch + 1) % 3]
                    mk.mm(pb, w2a2[64:128, hsl], lr12[64:128, csl])
                    mk.act(a16[:, csl], pb, AF.Sigmoid, bias=a0c[:, hb:hb + 1])
                    pb = pbig[(ch + 2) % 3]
                    mk.mm(pb, g2b[:, hsl], sdg[:, csl])
                    mk.copy(gT[:, csl], pb, e="act")
                    if has_vres:
                        pb = pbig[ch % 3]
                        mk.mm(pb, v2b[:, hsl], t32b[:, csl])
                        mk.act(tA[:, csl], pb, AF.Sigmoid, bias=v0c[:, hb:hb + 1])
                mk.ts(lw, lw, -DECAY_C, None, op0=ALU.mult)
                if has_vres:
                    mk.dma(tB, V(vfirst.ap[hb * 128:(hb + 1) * 128, tsl], vfirst.bufs[hb]))
                    mk.tt(tB, tB, v32, ALU.subtract, e="pool")
                    mk.tt(tB, tB, tA, ALU.mult, e="pool")
                    mk.tt(v32, v32, tB, ALU.add, e="pool")
                else:
                    mk.dma(V(vfirst.ap[hb * 128:(hb + 1) * 128, tsl], vfirst.bufs[hb]), v32, q=DBG.get("st_q", "pool"))
                mk.ts(tA, k32, kkc[:, hb:hb + 1], None, op0=ALU.mult)
                mk.tt(tbf, tA, tA, ALU.mult, e="pool")
                for ch in range(4):
                    csl = slice(ch * 512, (ch + 1) * 512)
                    pb = pbig[ch % 3]
                    mk.mm(pb, blk, tbf[:, csl])
                    mk.act(tB[:, csl], pb, AF.Sqrt)
                mk.ts(tB, tB, 1e-12, None, op0=ALU.max)
                mk.recip(tB, tB)
                mk.tt(tA, tA, tB, ALU.mult)
                mk.ts(tB, a16, -1.0, kac[:, hb:hb + 1], op0=ALU.add, op1=ALU.mult)
                mk.ts(tB, tB, 1.0, None, op0=ALU.add)
                mk.tt(k32, k32, tB, ALU.mult)
                mk.tt(tB, r32, k32, ALU.mult, e="pool")
                mk.ts(tbf, tB, rkc[:, hb:hb + 1], None, op0=ALU.mult)
                for ch in range(4):
                    csl = slice(ch * 512, (ch + 1) * 512)
                    pb = pbig[ch % 3]
                    mk.mm(pb, blk, tbf[:, csl])
                    mk.tt(bon[:, csl], pb, v32[:, csl], ALU.mult)
                mk.copy(vT_, v32, e="act")
                mk.scan(cl, rmask, lw, 0.0)
                mk.act(tB, cl, AF.Exp)
                mk.copy(DL, tB.re("p (c l) -> p c l", l=LCH)[:, :, LCH - 1], e="pool")
                mk.tt(r_bd[0:64, 0, :], r32[0:64, :], tB[0:64, :], ALU.mult)
                mk.tt(r_bd[64:128, 1, :], r32[64:128, :], tB[64:128, :], ALU.mult, e="pool")
                mk.tt(cl, cl, lw, ALU.subtract, e="pool")
                mk.act(tB, cl, AF.Exp)
                mk.tt(tB, tB, tA, ALU.mult)
                mk.ts(aT_, tB, -1.0, None, op0=ALU.mult)
                mk.copy(a_bd[0:64, 0, :], aT_[0:64, :], e="pool")
                mk.copy(a_bd[64:128, 1, :], aT_[64:128, :], e="act")
                mk.tt(cl, cl, lw, ALU.add, e="pool")
                mk.act(tB, cl, AF.Exp, scale=-1.0)
                mk.tt(kT_, k32, tB, ALU.mult)
                mk.tt(tA, tA, a16, ALU.mult, e="pool")
                mk.tt(bT_, tA, tB, ALU.mult)
                mk.copy(b_bd[0:64, 0, :], bT_[0:64, :], e="pool")
                mk.copy(b_bd[64:128, 1, :], bT_[64:128, :], e="act")
                for p in range(16):
                    ptm = B_[5 + p % 2].bitcast(BF16)
                    psl = slice(p * 128, (p + 1) * 128)
                    for qi, src in enumerate((aT_, vT_, bT_, kT_)):
                        mk.transpose(ptm[:, qi * 128:(qi + 1) * 128], src[:, psl], self.identb)
                    mk.copy(TMav[:, p, :, :], ptm[:, 0:256].re("p (q c) -> p q c", q=2), e="act")
                    for c in range(2):
                        mk.copy(TMbk[64 * c:64 * c + 64, p, c, :, :],
                                ptm[64 * c:64 * c + 64, 256:512].re("p (q c) -> p q c", q=2), e=("dve" if c else "act"))
                mk.memset(H32, 0.0)
                mk.memset(Hb, 0.0)
                def local(p):
                    g = G[p % 2]
                    t0 = p * 128
                    gsl = slice(t0, t0 + 128)
                    mk.mm(pN, aT_[:, gsl], b_bd[:, :, gsl])
                    mk.mm(pNT, bT_[:, gsl], a_bd[:, :, gsl])
                    mk.mm(pAk, kT_[:, gsl], a_bd[:, :, gsl])
                    mk.mm(pRb, bT_[:, gsl], r_bd[:, :, gsl])
                    mk.mm(pRk, kT_[:, gsl], r_bd[:, :, gsl])
                    yield
                    mk.tt(g["X"][0], v3(pN), mLs, ALU.mult)
                    mk.tt(g["XT"][0], v3(pNT), mUs, ALU.mult)
                    mk.tt(g["AakT"], v3(pAk), mUs, ALU.mult)
                    mk.tt(g["ArbT"], v3(pRb), mUi, ALU.mult)
                    mk.tt(g["ArkT"], v3(pRk), mUi, ALU.mult)
                    yield
                    for hd in range(2):
                        mk.mm(pZ2[:, 64 * hd:64 * hd + 64], g["AakT"][:, hd, :], TMav[:, p, 1, 64 * hd:64 * hd + 64])
                    Z = g["Z"]
                    mk.copy(Z[:, 0, :, :], TMav[:, p, 0, :].re("p (h j) -> p h j", h=2), e="pool")
                    yield
                    mk.copy(Z[:, 1, :, :], pZ2.re("p (h i) -> p h i", h=2), e="act")
                    yield
                    for lev in range(6):
                        X, XT = g["X"][lev % 2], g["XT"][lev % 2]
                        pz = pZa[lev % 2]
                        for hd in range(2):
                            mk.mm(pz[:, hd * 128:(hd + 1) * 128], XT[:, hd, :], Z[:, :, hd, :])
                        if lev < 5:
                            Xn, XTn = g["X"][(lev + 1) % 2], g["XT"][(lev + 1) % 2]
                            for hd in range(2):
                                mk.mm(pX[:, hd * 128:(hd + 1) * 128], XT[:, hd, :], X[:, hd, :])
                                mk.mm(pXT[:, hd * 128:(hd + 1) * 128], X[:, hd, :], XT[:, hd, :])
                        yield
                        if lev < 5:
                            mk.copy(Xn.re("p h c -> p (h c)"), pX, e="act")
                            mk.copy(XTn.re("p h c -> p (h c)"), pXT, e="act")
                        mk.tt(Z, Z, pz.re("p (h w c) -> p w h c", h=2, w=2), ALU.add)
                        yield
                    Wb_ = Z[:, 0, :, :].re("p h j -> p (h j)")
                    Ub_ = Z[:, 1, :, :].re("p h j -> p (h j)")
                    mk.mm(pT, Wb_, TMbk[:, p, :, 0, :])
                    for c in range(2):
                        mk.mm(pG[:, 128 * c:128 * c + 128], TMbk[:, p, c, 0, :], Ub_, start=True, stop=False)
                        mk.mm(pG[:, 128 * c:128 * c + 128], TMbk[:, p, c, 1, :], TMav[:, p, 1, :],
                              start=False, stop=True)
                    mk.mm(pQ, Wb_, g["ArbT"].re("p h t -> p (h t)"))
                    yield
                    for hd in range(2):
                        hs = slice(64 * hd, 64 * hd + 64)
                        mk.copy(g["T1T"][hs, :, hs], pT[hs, :].re("p (c h j) -> p c h j", c=2, h=2)[:, :, hd, :], e="act")
                        mk.copy(g["G1"][hs, :, hs], pG[hs, :].re("p (c h i) -> p c h i", c=2, h=2)[:, :, hd, :], e="act")
                        for c in range(2):
                            mk.tt(g["QT"][hs, c, 64 * c:64 * c + 64], pQ[hs, hd * 128 + 64 * c:hd * 128 + 64 * c + 64],
                                  r_bd[hs, hd, t0 + 64 * c:t0 + 64 * c + 64], ALU.add)

                def rec(p):
                    g = G[p % 2]
                    t0 = p * 128
                    gsl = slice(t0, t0 + 128)
                    Z = g["Z"]
                    mk.mm(pY, zb, zb)
                    for hd in range(2):
                        hs = slice(64 * hd, 64 * hd + 64)
                        mk.mm(pY[:, hs], g["ArbT"][:, hd, :], Z[:, 1, hd, :], start=False, stop=False,
                              skip_group_check=True)
                        mk.mm(pY[:, hs], g["ArkT"][:, hd, :], TMav[:, p, 1, hs], start=False, stop=False,
                              skip_group_check=True)
                    for c in range(2):
                        ci = 2 * p + c
                        mk.mm(pY, g["QT"][:, c, :], Hb, start=False, stop=True, skip_group_check=True)
                        mk.mm(pHp, g["T1T"][:, c, :], Hb)
                        yield
                        mk.tt(ht1, pHp, H32, ALU.add)
                        mk.tt(ht1, ht1, g["G1"][:, c, :], ALU.add)
                        mk.ts(H32, ht1, DL[:, ci:ci + 1], None, op0=ALU.mult)
                        yield
                        mk.copy(Hb, H32, e="act")
                        yield
                    for hd in range(2):
                        ysl = pY[:, 64 * hd:64 * hd + 64]
                        mk.op("dve", lambda eng, o=g["st6"][:, hd, :], i_=ysl: eng.bn_stats(out=o.ap, in_=i_.ap),
                              reads=[ysl], writes=[g["st6"]])
                        mk.op("dve", lambda eng, o=g["mv"][:, hd, :], i_=g["st6"][:, hd, :]: eng.bn_aggr(out=o.ap, in_=i_.ap),
                              reads=[g["st6"]], writes=[g["mv"]])
                    yield
                    mk.act(g["rs"], g["mv"][:, :, 1], AF.Sqrt, bias=lneps)
                    yield
                    mk.recip(g["rs"], g["rs"])
                    for hd in range(2):
                        mk.ts(g["yn"][:, 64 * hd:64 * hd + 64], pY[:, 64 * hd:64 * hd + 64], g["mv"][:, hd, 0:1],
                              g["rs"][:, hd:hd + 1], op0=ALU.subtract, op1=ALU.mult)
                    yield
                    ptr = pTr.bitcast(BF16)
                    mk.transpose(ptr[:, 0:128], g["yn"], self.identb)
                    yield
                    mk.ts(g["ot"], ptr[:, 0:128], lngc[:, hb:hb + 1], lnbc[:, hb:hb + 1], op0=ALU.mult, op1=ALU.add)
                    mk.tt(g["ot"], g["ot"], bon[:, gsl], ALU.add, e="pool")
                    mk.tt(oacc[:, gsl], g["ot"], gT[:, gsl], ALU.mult, e="pool")

                def drive(gens):
                    gens = list(gens)
                    while gens:
                        for gg in list(gens):
                            try:
                                next(gg)
                            except StopIteration:
                                gens.remove(gg)

                NG = DBG.get("ngrp", 16)
                if NG:
                    drive([local(0)])
                for p in range(NG):
                    gens = [rec(p)]
                    if p + 1 < NG:
                        gens.append(local(p + 1))
                    drive(gens)
                mk.dma(V(oTd.ap[hb * 128:(hb + 1) * 128, tsl], oTd.bufs[hb]), oacc, q=DBG.get("st_q", "pool"))


Builder.rwkv = _rwkv


def _pool(self, uT, oT, P, layer):
    mk = self.mk
    ei = layer // 2
    with mk.scope():
        pwb = mk.sb([128, 4, 128], BF16, "pwb")
        mk.dma(pwb, P["pool_w"][ei].re("g c d -> c g d"), q="pool")
        psc = mk.sb([128, 4], F32, "psc")
        mk.dma(psc, P["pool_scale_col"][ei])
        A = [mk.sb([128, 16 + T], F32, "pA%d" % i) for i in range(2)]
        U0 = mk.sb([128, 16 + T], F32, "pU")
        for t_ in A + [U0]:
            mk.memset(t_[:, 0:16], 0.0)
        rcw = mk.sb([128, T], F32, "rcw")
        dT = mk.sb([128, T], BF16, "dT")
        tmp = mk.sb([128, T], F32, "ptmp")
        pob = [mk.sb([128, 512], BF16) for i in range(2)]
        for gi in range(4):
            win = 2 ** (gi + 1)
            mk.op("pool", lambda eng: eng.iota(rcw.ap, [[1, T]], base=1, channel_multiplier=0,
                                                allow_small_or_imprecise_dtypes=True), writes=[rcw])
            mk.ts(rcw, rcw, float(win), None, op0=ALU.min)
            mk.recip(rcw, rcw)
            for s in range(NSEQ if DBG.get("pool_stage", 9) >= 2 else 0):
                mk.dma(U0[:, 16:], V(uT.ap[(14 + gi) * 128:(15 + gi) * 128, s * T:(s + 1) * T], uT.bufs[14 + gi]))
                src = U0
                for lev in range(gi + 1):
                    sh = 2 ** lev
                    dst = A[lev % 2]
                    mk.tt(dst[:, 16:], src[:, 16:], src[:, 16 - sh:16 - sh + T], ALU.add, e=("pool" if lev % 2 else "dve"))
                    src = dst
                mk.tt(tmp, src[:, 16:], rcw, ALU.mult)
                mk.tt(dT, tmp, U0[:, 16:], ALU.subtract, e="pool")
                for ch in range(4 if DBG.get("pool_stage", 9) >= 3 else 0):
                    pb = self.bank[ch % 2]
                    mk.mm(pb, pwb[:, gi, :], dT[:, ch * 512:(ch + 1) * 512])
                    ob_ = pob[ch % 2]
                    if DBG.get("pool_stage", 9) >= 4:
                        mk.ts(ob_, pb, psc[:, gi:gi + 1], None, op0=ALU.mult)
                    if DBG.get("pool_stage", 9) >= 5:
                        mk.dma(V(oT.ap[(4 + gi) * 128:(5 + gi) * 128, s * T + ch * 512:s * T + (ch + 1) * 512],
                                 oT.bufs[4 + gi]), ob_, q=DBG.get("pool_q", "pool"))


Builder.pool = _pool


def _even_layer(self, xres_in, xres_out, layer, P, vfirst):
    mk = self.mk
    ei = layer // 2
    uT = self.scratch("uTe", [2304, NTOK], F32)
    self.proj_in(xres_in, P["norm_mix_g"][layer:layer + 1, :], P["even_w_in"][ei], 2304,
                 list(range(18)), uT, None, None)
    oT = self.scratch("oTd", [D, NTOK], BF16)
    if not DBG.get("no_rwkv"):
        self.rwkv(uT, oT, P, layer, vfirst)
    if not DBG.get("no_pool"):
        self.pool(uT, oT, P, layer)
    self.proj_out(xres_in, xres_out, oT, P["even_w_out"][ei])


Builder.even_layer = _even_layer


def host_even_params(inputs):
    f = lambda a: np.ascontiguousarray(a, dtype=np.float32)
    out = {}
    out["norm_mix_g"] = f(inputs["norm_mix_g"])
    out["even_w_in"] = f(inputs["even_w_in"])
    out["even_w_out"] = f(inputs["even_w_out"])
    n_even = inputs["even_w_in"].shape[0]
    out["rw_mu_col"] = f(np.transpose(inputs["rw_mu"].reshape(n_even, 14, 128), (0, 2, 1)))
    colp = [inputs["rw_w0"], inputs["rw_a0"], inputs["rw_k_k"], inputs["rw_k_a"],
            inputs["rw_r_k"].reshape(n_even, 512), inputs["rw_ln_g"], inputs["rw_ln_b"]]
    out["rw_cols"] = f(np.stack([np.transpose(c.reshape(n_even, 4, 128), (0, 2, 1)) for c in colp], axis=2))
    out["rw_w2a2"] = f(np.concatenate([inputs["rw_w2"], inputs["rw_a2"]], axis=1))
    out["rw_g2"] = f(inputs["rw_g2"])
    nv = inputs["rw_v0"].shape[0]
    out["rw_v0col"] = f(np.transpose(inputs["rw_v0"].reshape(nv, 4, 128), (0, 2, 1)))
    out["rw_v1"] = f(inputs["rw_v1"])
    out["rw_v2"] = f(inputs["rw_v2"])
    out["pool_w"] = f(inputs["pool_w"])
    out["pool_scale_col"] = f(np.transpose(inputs["pool_scale"].reshape(n_even, 4, 128), (0, 2, 1)))
    return out


def host_consts2():
    c = host_consts()
    blk = np.kron(np.eye(2, dtype=np.float32), np.ones((64, 64), np.float32))
    lo = np.tril(np.ones((64, 64), np.float32), -1)
    e2 = np.eye(2, dtype=np.float32)
    mLs = np.kron(e2, lo)
    mUs = np.kron(e2, lo.T)
    mUi = np.kron(e2, np.triu(np.ones((64, 64), np.float32)))
    c["c_blk"] = blk
    c["c_masks"] = np.stack([mLs, mUs, mUi]).astype(np.float32)
    return c


def mk_check(mk):
    val = {}
    pos = {e: 0 for e in ENGS}
    total = sum(len(mk.ops[e]) for e in ENGS)
    done = 0
    while done < total:
        prog = False
        for e in ENGS:
            lst = mk.ops[e]
            while pos[e] < len(lst):
                waits, fn, inc = lst[pos[e]]
                if all(val.get(id(s), 0) >= v for s, v in waits):
                    val[id(inc[0])] = val.get(id(inc[0]), 0) + inc[1]
                    pos[e] += 1
                    done += 1
                    prog = True
                else:
                    break
        if not prog:
            names = {id(v): k for k, v in mk.semobj.items()}
            for e in ENGS:
                if pos[e] < len(mk.ops[e]):
                    waits, fn, inc = mk.ops[e][pos[e]]
                    print("STUCK", e, pos[e], [(names.get(id(s)), v, val.get(id(s), 0)) for s, v in waits])
            return False
    return True


_PROG = {}


def host_params(inputs):
    hp = {}
    hp.update(host_even_params(inputs))
    hp.update(host_odd_params(inputs))
    hp.update(host_moe_params(inputs))
    hp.update(host_consts2())
    return hp


def build_program(shapes, n_layers=4):
    nc = bass.Bass("TRN2", target_bir_lowering=False)
    with ExitStack() as st:
        B = Builder(nc, st)
        mk = B.mk
        x_in = DR(mk, "x_in", [NTOK, D], F32, kind="ExternalInput")
        y = DR(mk, "y", [NTOK, D], F32, kind="ExternalOutput")
        P = {}
        for k, shp in shapes.items():
            if k in B.inp:
                continue
            P[k] = B.ext_in(k, list(shp))
        vf = B.scratch("vfirst", [512, NTOK], F32)
        for layer in range(n_layers):
            xin = x_in if layer == 0 else y
            if layer % 2 == 0:
                B.even_layer(xin, y, layer, P, vf)
            else:
                B.odd_layer(xin, y, layer, P)
            B.moe(y, layer, P)
        mk.emit()
    return nc


def kernel(**inputs):
    inputs = {k: np.asarray(v) for k, v in inputs.items()}
    hp = host_params(inputs)
    key = "full"
    if key not in _PROG:
        _PROG[key] = build_program({k: v.shape for k, v in hp.items()})
    nc = _PROG[key]
    x = np.ascontiguousarray(inputs["x"], dtype=np.float32)
    nb = x.shape[0]
    n_cores = 8
    per = nb // n_cores
    in_maps = []
    for c in range(n_cores):
        m = dict(hp)
        m["x_in"] = np.ascontiguousarray(x[c * per:(c + 1) * per].reshape(NTOK, D))
        in_maps.append(m)
    res = run_bass_kernel_spmd(nc, in_maps, core_ids=list(range(n_cores)))
    out = np.stack([np.asarray(r["y"], dtype=np.float32).reshape(per, T, D) for r in res.results], axis=0)
    return out.reshape(nb, T, D)
```

```python
import numpy as np
from contextlib import ExitStack
import concourse.bass as bass
import concourse.mybir as mybir
from concourse.bass_utils import run_bass_kernel_spmd

F32 = mybir.dt.float32
BF16 = mybir.dt.bfloat16
I32 = mybir.dt.int32
U32 = mybir.dt.uint32
AF = mybir.ActivationFunctionType
ALU = mybir.AluOpType
AX = mybir.AxisListType

SAME_ENGINE_SYNC = True
EPOCH = 20000
DBG = {}


class Buf:
    __slots__ = ("w", "r", "name")

    def __init__(self, name=""):
        self.w = None
        self.r = {}
        self.name = name


class V:
    __slots__ = ("ap", "buf")

    def __init__(self, ap, buf):
        self.ap = ap
        self.buf = buf

    def __getitem__(self, k):
        return V(self.ap[k], self.buf)

    def re(self, s, **kw):
        return V(self.ap.rearrange(s, **kw), self.buf)

    def bc(self, shape):
        return V(self.ap.to_broadcast(list(shape)), self.buf)

    def pbc(self, n):
        return V(self.ap.partition_broadcast(n), self.buf)

    def bitcast(self, dt):
        return V(self.ap.bitcast(dt), self.buf)

    def sub(self, buf):
        return V(self.ap, buf)

    @property
    def shape(self):
        return self.ap.shape


ENGS = ("pe", "act", "dve", "pool", "sp")


class MK:
    def __init__(self, nc, stack, n_dma_slots=8):
        self.nc = nc
        self.stack = stack
        self.ops = {e: [] for e in ENGS}
        self.root = stack
        self.esem = {}
        self.cnt = {e: 0 for e in ENGS}
        self.seen = {e: {} for e in ENGS}
        self.dma_slots = {}
        self.dma_n = {}
        for q in ("sp", "pool", "act"):
            self.dma_slots[q] = [stack.enter_context(nc.semaphore("dma_%s_%d" % (q, i)))
                                 for i in range(n_dma_slots)]
            self.dma_n[q] = 0
        self.semobj = {}
        self.n_ops = 0
        self.uid = 0

    def sb(self, shape, dtype, name=None):
        self.uid += 1
        name = name or "t%d" % self.uid
        t = self.stack.enter_context(self.nc.sbuf_tensor(name + "_%d" % self.uid, list(shape), dtype))
        return V(t[:], Buf(name))

    def ps(self, shape, dtype=F32, name=None):
        self.uid += 1
        name = name or "p%d" % self.uid
        t = self.stack.enter_context(self.nc.psum_tensor(name + "_%d" % self.uid, list(shape), dtype))
        return V(t[:], Buf(name))

    def dram(self, name, shape, dtype, kind="Internal"):
        t = self.nc.dram_tensor(name, list(shape), dtype, kind=kind)
        return V(t.ap(), Buf(name))

    def _tok_need(self, e, tok, waits):
        if tok is None:
            return
        semkey, val, src, is_dma = tok
        if src == e and not is_dma:
            if e == "pe" or not SAME_ENGINE_SYNC:
                return
        if self.seen[e].get(semkey, 0) >= val:
            return
        if waits.get(semkey, 0) < val:
            waits[semkey] = val

    def op(self, e, fn, reads=(), writes=(), dma=False):
        waits = {}
        pend = getattr(self, "pending", {}).pop(e, None)
        if pend:
            waits.update(pend)
        rb = [v.buf for v in reads if v is not None]
        wb = [v.buf for v in writes if v is not None]
        for b in rb:
            self._tok_need(e, b.w, waits)
        for b in wb:
            self._tok_need(e, b.w, waits)
            for t in b.r.values():
                self._tok_need(e, t, waits)
        if dma:
            slots = self.dma_slots[e]
            n = self.dma_n[e]
            self.dma_n[e] = n + 1
            sem = slots[n % len(slots)]
            val = 16 * (n // len(slots) + 1)
            semkey = ("d", e, n % len(slots))
            self.semobj[semkey] = sem
            if val > 16:
                if self.seen[e].get(semkey, 0) < val - 16 and waits.get(semkey, 0) < val - 16:
                    waits[semkey] = val - 16
            tok = (semkey, val, e, True)
            inc = (sem, 16)
        else:
            self.cnt[e] += 1
            ep = (self.cnt[e] - 1) // EPOCH
            semkey = ("c", e, ep)
            if semkey not in self.esem:
                self.esem[semkey] = self.root.enter_context(self.nc.semaphore("sem_%s_%d" % (e, ep)))
            self.semobj[semkey] = self.esem[semkey]
            tok = (semkey, (self.cnt[e] - 1) % EPOCH + 1, e, False)
            inc = (self.esem[semkey], 1)
        for k, v in waits.items():
            self.seen[e][k] = v
        for b in wb:
            b.w = tok
            b.r = {}
        for b in rb:
            old = b.r.get(tok[0])
            if old is None or old[1] < tok[1]:
                b.r[tok[0]] = tok
        self.ops[e].append(([(self.semobj[k], v) for k, v in waits.items()], fn, inc))
        self.n_ops += 1
        return tok

    def emit(self):
        nc = self.nc
        fin = []
        for q in ("sp", "pool", "act"):
            n = self.dma_n[q]
            slots = self.dma_slots[q]
            for i in range(min(n, len(slots))):
                uses = (n - i + len(slots) - 1) // len(slots)
                fin.append((slots[i], 16 * uses))
        for e in ("pe", "act", "dve", "pool"):
            if self.cnt[e]:
                ep = (self.cnt[e] - 1) // EPOCH
                fin.append((self.esem[("c", e, ep)], (self.cnt[e] - 1) % EPOCH + 1))
        with nc.Block() as block:
            def run(eng, lst, final=None):
                for waits, fn, inc in lst:
                    for s, v in waits:
                        eng.wait_ge(s, v)
                    fn(eng).then_inc(inc[0], inc[1])
                if final:
                    for s, v in final:
                        eng.wait_ge(s, v)

            @block.tensor
            def _(eng):
                run(eng, self.ops["pe"])

            @block.scalar
            def _(eng):
                run(eng, self.ops["act"])

            @block.vector
            def _(eng):
                run(eng, self.ops["dve"])

            @block.gpsimd
            def _(eng):
                run(eng, self.ops["pool"])

            @block.sync
            def _(eng):
                run(eng, self.ops["sp"], fin)

    def dma(self, out, in_, q="sp", **kw):
        return self.op(q, lambda eng: eng.dma_start(out=out.ap, in_=in_.ap, **kw),
                       reads=[in_], writes=[out], dma=True)

    def mm(self, out, lhsT, rhs, start=True, stop=True, **kw):
        return self.op("pe", lambda eng: eng.matmul(out.ap, lhsT.ap, rhs.ap, start=start, stop=stop, **kw),
                       reads=[lhsT, rhs], writes=[out])

    def transpose(self, out, in_, ident):
        return self.op("pe", lambda eng: eng.transpose(out.ap, in_.ap, ident.ap),
                       reads=[in_, ident], writes=[out])

    def act(self, out, in_, func, bias=None, scale=1.0, accum_out=None, e="act"):
        reads = [in_]
        kw = {}
        if isinstance(bias, V):
            reads.append(bias)
            kw["bias"] = bias.ap
        elif bias is not None:
            kw["bias"] = bias
        if isinstance(scale, V):
            reads.append(scale)
            kw["scale"] = scale.ap
        else:
            kw["scale"] = scale
        writes = [out]
        if accum_out is not None:
            writes.append(accum_out)
            kw["accum_out"] = accum_out.ap
        return self.op(e, lambda eng: eng.activation(out=out.ap, in_=in_.ap, func=func, **kw),
                       reads=reads, writes=writes)

    def tt(self, out, in0, in1, op, e="dve"):
        return self.op(e, lambda eng: eng.tensor_tensor(out=out.ap, in0=in0.ap, in1=in1.ap, op=op),
                       reads=[in0, in1], writes=[out])

    def ts(self, out, in0, s1, s2=None, op0=ALU.mult, op1=None, accum_out=None, e="dve"):
        reads = [in0]
        a1 = s1
        if isinstance(s1, V):
            reads.append(s1)
            a1 = s1.ap
        a2 = s2
        if isinstance(s2, V):
            reads.append(s2)
            a2 = s2.ap
        kw = {}
        if op1 is not None:
            kw["op1"] = op1
        writes = [out]
        if accum_out is not None:
            writes.append(accum_out)
            kw["accum_out"] = accum_out.ap
        return self.op(e, lambda eng: eng.tensor_scalar(out=out.ap, in0=in0.ap, scalar1=a1, scalar2=a2,
                                                        op0=op0, **kw),
                       reads=reads, writes=writes)

    def stt(self, out, in0, scalar, in1, op0, op1, e="dve"):
        reads = [in0, in1]
        a = scalar
        if isinstance(scalar, V):
            reads.append(scalar)
            a = scalar.ap
        return self.op(e, lambda eng: eng.scalar_tensor_tensor(out=out.ap, in0=in0.ap, scalar=a, in1=in1.ap,
                                                               op0=op0, op1=op1),
                       reads=reads, writes=[out])

    def copy(self, out, in_, e="dve"):
        if e == "act":
            return self.op(e, lambda eng: eng.copy(out=out.ap, in_=in_.ap), reads=[in_], writes=[out])
        return self.op(e, lambda eng: eng.tensor_copy(out=out.ap, in_=in_.ap), reads=[in_], writes=[out])

    def memset(self, out, val, e="pool"):
        return self.op(e, lambda eng: eng.memset(out.ap, val), writes=[out])

    def reduce(self, out, in_, op=ALU.add, axis=AX.X, e="dve"):
        return self.op(e, lambda eng: eng.tensor_reduce(out=out.ap, in_=in_.ap, axis=axis, op=op),
                       reads=[in_], writes=[out])

    def recip(self, out, in_):
        return self.op("dve", lambda eng: eng.reciprocal(out=out.ap, in_=in_.ap), reads=[in_], writes=[out])

    def scan(self, out, d0, d1, init, op0=ALU.mult, op1=ALU.add):
        reads = [d0, d1]
        a = init
        if isinstance(init, V):
            reads.append(init)
            a = init.ap
        return self.op("dve", lambda eng: eng.tensor_tensor_scan(out=out.ap, data0=d0.ap, data1=d1.ap,
                                                                 initial=a, op0=op0, op1=op1),
                       reads=reads, writes=[out])

    def barrier(self):
        fin = {}
        for q in ("sp", "pool", "act"):
            n = self.dma_n[q]
            slots = self.dma_slots[q]
            for i in range(min(n, len(slots))):
                uses = (n - i + len(slots) - 1) // len(slots)
                fin[("d", q, i)] = 16 * uses
        for e in ("pe", "act", "dve", "pool"):
            if self.cnt[e]:
                ep = (self.cnt[e] - 1) // EPOCH
                fin[("c", e, ep)] = (self.cnt[e] - 1) % EPOCH + 1
        for e in ENGS:
            waits = {}
            for k, v in fin.items():
                if k[0] == "c" and k[1] == e:
                    continue
                if self.seen[e].get(k, 0) < v:
                    waits[k] = v
                    self.seen[e][k] = v
            if waits:
                if e == "sp":
                    continue_fn = None
                self.pending = getattr(self, "pending", {})
                self.pending.setdefault(e, {}).update(waits)

    def scope(self):
        return _Scope(self)


class _Scope:
    def __init__(self, mk):
        self.mk = mk

    def __enter__(self):
        self.old = self.mk.stack
        self.st = ExitStack()
        self.mk.stack = self.st
        return self

    def __exit__(self, *a):
        self.mk.barrier()
        self.mk.stack = self.old
        self.st.close()
        return False


D = 1024
T = 2048
NSEQ = 2
NTOK = NSEQ * T
NT = NTOK // 128
CAP = 512
NE = 32
NSLOT = NE * CAP
NROW_TL = NSLOT + 128
RMS_EPS = 1e-6


class DR:
    def __init__(self, mk, name, shape, dtype, kind="Internal", rows_per=128):
        self.t = mk.nc.dram_tensor(name, list(shape), dtype, kind=kind)
        self.ap = self.t.ap()
        self.rows_per = rows_per
        n = (shape[0] + rows_per - 1) // rows_per
        self.bufs = [Buf("%s_%d" % (name, i)) for i in range(n)]
        self.whole = Buf(name)

    def rows(self, r0, n):
        assert r0 % self.rows_per == 0 and n <= self.rows_per
        return V(self.ap[r0:r0 + n], self.bufs[r0 // self.rows_per])

    def all(self):
        return [V(self.ap, b) for b in self.bufs]


class Builder:
    def __init__(self, nc, stack):
        self.nc = nc
        self.mk = MK(nc, stack)
        mk = self.mk
        self.inp = {}
        self.c_ident = self.ext_in("c_ident", [128, 128], F32)
        self.c_tri = self.ext_in("c_tri", [128, 128], F32)
        self.ext_in("c_blk", [128, 128], F32)
        self.ext_in("c_masks", [3, 128, 128], F32)
        self.ident32 = mk.sb([128, 128], F32, "ident32")
        self.identb = mk.sb([128, 128], BF16, "identb")
        self.trib = mk.sb([128, 128], BF16, "trib")
        self.onesb = mk.sb([128, 128], BF16, "onesb")
        mk.dma(self.ident32, self.c_ident)
        mk.dma(self.identb, self.c_ident, q="pool")
        mk.dma(self.trib, self.c_tri, q="pool")
        mk.memset(self.onesb, 1.0)
        self.eps_t = mk.sb([128, 1], F32, "eps_t")
        mk.memset(self.eps_t, RMS_EPS)
        self.psum_all = mk.ps([128, 4096], F32, "psum_all")
        self.bank = [V(self.psum_all.ap[:, i * 512:(i + 1) * 512], Buf("bank%d" % i)) for i in range(8)]

    def scratch(self, name, shape, dtype, rows_per=128):
        if not hasattr(self, "_scr"):
            self._scr = {}
        if name not in self._scr:
            self._scr[name] = DR(self.mk, name, shape, dtype, rows_per=rows_per)
        return self._scr[name]

    def ext_in(self, name, shape, dtype=F32):
        v = self.mk.dram(name, shape, dtype, kind="ExternalInput")
        self.inp[name] = v
        return v

    def rms_tile(self, xt, gbc, h32, eps=RMS_EPS, sq=None, small=None):
        mk = self.mk
        ss, rstd = small
        mk.act(sq, xt, AF.Square, accum_out=ss)
        mk.act(rstd, ss, AF.Sqrt, scale=1.0 / D, bias=self.eps_t)
        mk.recip(rstd, rstd)
        mk.stt(h32, xt, rstd, gbc, ALU.mult, ALU.mult)

    def moe(self, xres, layer, P):
        mk = self.mk
        with mk.scope():
            Hrows = self.scratch("hrows", [NTOK + 128, D], BF16)
            Ybuf = self.scratch("ybuf", [NROW_TL, D], F32)
            TokL = self.scratch("tokl", [NROW_TL, 8], I32, rows_per=NROW_TL)
            gbc = mk.sb([128, D], F32, "gbc")
            mk.dma(gbc, P["norm_ffn_g"][layer:layer + 1, :].pbc(128))
            wr = mk.sb([128, 8, 36], F32, "wr")
            mk.dma(wr, P["w_router"][layer].re("(k p) n -> p k n", p=128))
            brt = mk.sb([128, 36], F32, "brt")
            mk.dma(brt, P["b_router"][layer:layer + 1, :].pbc(128))
            maskall = mk.sb([128, NT, 32], BF16, "maskall")
            E1all = mk.sb([128, NT, 32], F32, "E1all")
            E2all = mk.sb([128, NT, 32], F32, "E2all")
            gate1 = mk.sb([128, NT], F32, "gate1")
            gate2 = mk.sb([128, NT], F32, "gate2")
            slot1 = mk.sb([128, NT], I32, "slot1")
            slot2 = mk.sb([128, NT], I32, "slot2")
            base = mk.sb([128, 32], F32, "base")
            lim = mk.sb([128, 32], F32, "lim")
            trash = mk.sb([128, 1], F32, "trash")
            zero_t = mk.sb([128, D], F32, "zero_t")
            sent = mk.sb([128, (NROW_TL // 128) * 8], I32, "sent")
            mk.op("pool", lambda eng: eng.iota(base.ap, [[CAP, 32]], base=-1, channel_multiplier=0,
                                                allow_small_or_imprecise_dtypes=True), writes=[base])
            mk.ts(lim, base, float(CAP) + 0.5, None, op0=ALU.add)
            mk.op("pool", lambda eng: eng.iota(trash.ap, [[0, 1]], base=NSLOT, channel_multiplier=1,
                                                allow_small_or_imprecise_dtypes=True), writes=[trash])
            mk.memset(zero_t, 0.0)
            mk.op("pool", lambda eng: eng.iota(sent.ap, [[0, (NROW_TL // 128) * 8]], base=NTOK,
                                                channel_multiplier=0), writes=[sent])
            mk.dma(V(TokL.ap.rearrange("(p r) c -> p (r c)", p=128), TokL.bufs[0]), sent)
            mk.dma(Hrows.rows(NTOK, 128), zero_t.bitcast(BF16)[:, 0:D])
            mk.dma(Ybuf.rows(NSLOT, 128), zero_t)

            xts = [mk.sb([128, D], F32, "xt%d" % i) for i in range(2)]
            sqs = [mk.sb([128, D], F32, "sq%d" % i) for i in range(2)]
            h32s = [mk.sb([128, D], F32, "h32%d" % i) for i in range(2)]
            hbs = [mk.sb([128, D], BF16, "hb%d" % i) for i in range(2)]
            hT32s = [mk.sb([128, 8, 128], F32, "hT32%d" % i) for i in range(2)]
            smalls = [(mk.sb([128, 1], F32), mk.sb([128, 1], F32)) for i in range(2)]
            lg_all = mk.sb([128, NT, 36], F32, "lg_all")
            for i in range(NT):
                b = i % 2
                xt, sq, h32, hb, hT32 = xts[b], sqs[b], h32s[b], hbs[b], hT32s[b]
                mk.dma(xt, xres.rows(i * 128, 128))
                self.rms_tile(xt, gbc, h32, sq=sq, small=smalls[b])
                mk.copy(hb, h32, e="pool")
                mk.dma(Hrows.rows(i * 128, 128), hb)
                for half in range(2):
                    pb = self.bank[2 * b + half]
                    for kk in range(4):
                        k = half * 4 + kk
                        mk.transpose(pb[:, kk * 128:(kk + 1) * 128], h32[:, k * 128:(k + 1) * 128], self.ident32)
                    mk.copy(hT32[:, half * 4:(half + 1) * 4, :].re("p k t -> p (k t)"), pb, e="act")
                pl = self.bank[4 + b]
                for k in range(8):
                    mk.mm(pl[:, 0:36], hT32[:, k, :], wr[:, k, :], start=(k == 0), stop=(k == 7))
                mk.tt(lg_all[:, i, :], pl[:, 0:36], brt, ALU.add)

            def bc(v, axis, shape):
                return V(v.ap.unsqueeze(axis).to_broadcast(list(shape)), v.buf)
            gl = lg_all[:, :, 0:4]
            el4 = lg_all[:, :, 4:36].re("p t (g e) -> p t g e", g=4)
            gmax = mk.sb([128, NT], F32)
            ohg = mk.sb([128, NT, 4], F32)
            eg = mk.sb([128, NT, 4], F32)
            gsum = mk.sb([128, NT], F32)
            tmp4 = mk.sb([128, NT, 4, 8], F32)
            sel = mk.sb([128, NT, 8], F32)
            sel2 = mk.sb([128, NT, 8], F32)
            m1 = mk.sb([128, NT], F32)
            m2 = mk.sb([128, NT], F32)
            oh1 = mk.sb([128, NT, 8], F32)
            oh2 = mk.sb([128, NT, 8], F32)
            mk.reduce(gmax, gl, op=ALU.max)
            mk.tt(ohg, gl, bc(gmax, 2, [128, NT, 4]), ALU.is_equal)
            mk.tt(eg, gl, bc(gmax, 2, [128, NT, 4]), ALU.subtract)
            mk.act(eg, eg, AF.Exp)
            mk.reduce(gsum, eg, op=ALU.add)
            mk.recip(gsum, gsum)
            mk.tt(tmp4, el4, bc(ohg, 3, [128, NT, 4, 8]), ALU.mult)
            mk.reduce(sel, tmp4.re("p t g e -> p t e g"), op=ALU.add)
            mk.reduce(m1, sel, op=ALU.max)
            mk.tt(oh1, sel, bc(m1, 2, [128, NT, 8]), ALU.is_equal)
            mk.stt(sel2, oh1, -1e30, sel, ALU.mult, ALU.add)
            mk.reduce(m2, sel2, op=ALU.max)
            mk.tt(oh2, sel2, bc(m2, 2, [128, NT, 8]), ALU.is_equal)
            mk.tt(m2, m2, m1, ALU.subtract)
            mk.act(m2, m2, AF.Exp)
            mk.ts(m2, m2, 1.0, None, op0=ALU.add)
            mk.recip(m2, m2)
            mk.tt(gate1, gsum, m2, ALU.mult)
            mk.tt(gate2, gsum, gate1, ALU.subtract)
            mk.tt(E1all.re("p t (g e) -> p t g e", g=4), bc(ohg, 3, [128, NT, 4, 8]), bc(oh1, 2, [128, NT, 4, 8]), ALU.mult)
            mk.tt(E2all.re("p t (g e) -> p t g e", g=4), bc(ohg, 3, [128, NT, 4, 8]), bc(oh2, 2, [128, NT, 4, 8]), ALU.mult)
            mk.tt(maskall, E1all, E2all, ALU.add)

            pos_ps = V(self.psum_all.ap[:, 0:1024], self.bank[0].buf)
            for i in range(NT):
                reg_ = V(self.psum_all.ap[:, i * 32:(i + 1) * 32], self.bank[i // 16].buf)
                for j in range(i):
                    mk.mm(reg_, self.onesb, maskall[:, j, :], start=(j == 0), stop=False)
                mk.mm(reg_, self.trib, maskall[:, i, :], start=(i == 0), stop=True)
            posf = mk.sb([128, NT, 32], F32, "posf")
            okm = mk.sb([128, NT, 32], F32, "okm")
            slf = mk.sb([128, NT], F32, "slf")
            mk.op("dve", lambda eng: eng.tensor_tensor(out=posf.ap, in0=pos_ps.ap.rearrange("p (t e) -> p t e", e=32),
                                                       in1=base.ap.unsqueeze(1).to_broadcast([128, NT, 32]), op=ALU.add),
                  reads=[self.bank[0], self.bank[1], base], writes=[posf])
            mk.tt(okm, posf, bc(lim, 1, [128, NT, 32]), ALU.is_lt)
            mk.ts(posf, posf, trash, None, op0=ALU.subtract)
            mk.tt(posf, posf, okm, ALU.mult)
            mk.ts(posf, posf, trash, None, op0=ALU.add)
            for Eall, slot in ((E1all, slot1), (E2all, slot2)):
                mk.tt(okm, posf, Eall, ALU.mult)
                mk.reduce(slf, okm, op=ALU.add)
                mk.copy(slot, slf)
            tokid = [mk.sb([128, 8], I32) for i in range(4)]
            sc_bufs = []
            init_v = V(TokL.ap, TokL.bufs[0])
            for i in range(NT):
                t_ = tokid[i % 4]
                mk.op("pool", lambda eng, t=t_, i=i: eng.iota(t.ap, [[0, 8]], base=i * 128,
                                                              channel_multiplier=1), writes=[t_])
                for slot in (slot1, slot2):
                    bf = Buf("tokl_sc")
                    sc_bufs.append(bf)
                    mk.op("pool", lambda eng, slot=slot, i=i, t=t_: eng.indirect_dma_start(
                        out=TokL.ap, out_offset=bass.IndirectOffsetOnAxis(ap=slot.ap[:, i:i + 1], axis=0),
                        in_=t.ap, in_offset=None),
                        reads=[slot, t_, init_v], writes=[V(TokL.ap, bf)], dma=True)
            tokl_all = [V(TokL.ap, bf) for bf in sc_bufs] + [init_v]

            NCT = CAP // 128
            wg = [mk.sb([128, 8, 512], BF16, "wg%d" % i) for i in range(2)]
            wu = [mk.sb([128, 8, 512], BF16, "wu%d" % i) for i in range(2)]
            wd = [mk.sb([128, 4, D], BF16, "wd%d" % i) for i in range(2)]
            idx = [mk.sb([128, 8], I32) for i in range(4)]
            xg = [mk.sb([128, D], BF16) for i in range(4)]
            xgT = [mk.sb([128, 8, CAP], BF16) for i in range(2)]
            hidT = [mk.sb([128, 4, CAP], BF16) for i in range(2)]
            sil = [mk.sb([128, CAP], F32) for i in range(2)]
            yrow = [mk.sb([128, D], F32) for i in range(2)]
            nslot = 0
            ny = 0
            for e in range(NE):
                b = e % 2
                mk.dma(wg[b], P["moe_w_gate"][layer, e].re("(k p) n -> p k n", p=128), q="pool")
                mk.dma(wu[b], P["moe_w_up"][layer, e].re("(k p) n -> p k n", p=128), q="pool")
                mk.dma(wd[b], P["moe_w_down"][layer, e].re("(k p) n -> p k n", p=128), q="pool")
                for j in range(NCT):
                    s = nslot % 4
                    nslot += 1
                    r0 = e * CAP + j * 128
                    mk.op("sp", lambda eng, s=s, r0=r0: eng.dma_start(out=idx[s].ap, in_=TokL.ap[r0:r0 + 128, :]),
                          reads=tokl_all, writes=[idx[s]], dma=True)
                    mk.op("pool", lambda eng, s=s: eng.indirect_dma_start(
                        out=xg[s].ap, out_offset=None, in_=Hrows.ap,
                        in_offset=bass.IndirectOffsetOnAxis(ap=idx[s].ap[:, 0:1], axis=0)),
                        reads=[idx[s]] + Hrows.all(), writes=[xg[s]], dma=True)
                    pb = self.bank[j % 2]
                    pbb = pb.bitcast(BF16)
                    for k in range(8):
                        mk.transpose(pbb[:, k * 128:(k + 1) * 128], xg[s][:, k * 128:(k + 1) * 128], self.identb)
                    mk.copy(xgT[b][:, :, j * 128:(j + 1) * 128], pbb.re("p (k t) -> p k t", k=8),
                            e=("act" if j % 2 else "dve"))
                for c in range(4):
                    pg = self.bank[2 + (c % 2)]
                    pu = self.bank[4 + (c % 2)]
                    for k in range(8):
                        mk.mm(pg, wg[b][:, k, c * 128:(c + 1) * 128], xgT[b][:, k, :], start=(k == 0), stop=(k == 7))
                    for k in range(8):
                        mk.mm(pu, wu[b][:, k, c * 128:(c + 1) * 128], xgT[b][:, k, :], start=(k == 0), stop=(k == 7))
                    mk.act(sil[c % 2], pg, AF.Silu)
                    mk.tt(hidT[b][:, c, :], sil[c % 2], pu, ALU.mult)
                for j in range(NCT):
                    yb = ny % 2
                    ny += 1
                    for half in range(2):
                        pd = self.bank[6 + half]
                        for c in range(4):
                            mk.mm(pd, hidT[b][:, c, j * 128:(j + 1) * 128], wd[b][:, c, half * 512:(half + 1) * 512],
                                  start=(c == 0), stop=(c == 3))
                        mk.copy(yrow[yb][:, half * 512:(half + 1) * 512], pd, e=("act" if half else "dve"))
                    mk.dma(Ybuf.rows(e * CAP + j * 128, 128), yrow[yb])

            y1 = [mk.sb([128, D], F32) for i in range(3)]
            y2 = [mk.sb([128, D], F32) for i in range(3)]
            xt3 = xts + [mk.sb([128, D], F32)]
            for i in range(NT):
                b = i % 3
                xt = xt3[b]
                mk.dma(xt, xres.rows(i * 128, 128))
                for slot, yy in ((slot1, y1[b]), (slot2, y2[b])):
                    mk.op("pool", lambda eng, slot=slot, yy=yy, i=i: eng.indirect_dma_start(
                        out=yy.ap, out_offset=None, in_=Ybuf.ap,
                        in_offset=bass.IndirectOffsetOnAxis(ap=slot.ap[:, i:i + 1], axis=0)),
                        reads=[slot] + Ybuf.all(), writes=[yy], dma=True)
                mk.stt(xt, y1[b], gate1[:, i:i + 1], xt, ALU.mult, ALU.add)
                mk.stt(xt, y2[b], gate2[:, i:i + 1], xt, ALU.mult, ALU.add)
                mk.dma(xres.rows(i * 128, 128), xt)


def host_consts():
    ident = np.eye(128, dtype=np.float32)
    tri = np.triu(np.ones((128, 128), np.float32))
    return {"c_ident": ident, "c_tri": tri}


MOE_KEYS = ("norm_ffn_g", "w_router", "b_router", "moe_w_gate", "moe_w_up", "moe_w_down")


def host_moe_params(inputs):
    out = {}
    out["norm_ffn_g"] = np.ascontiguousarray(inputs["norm_ffn_g"], dtype=np.float32)
    out["w_router"] = np.ascontiguousarray(
        np.concatenate([inputs["moe_w_group"], inputs["moe_w_expert"]], axis=-1), dtype=np.float32)
    out["b_router"] = np.ascontiguousarray(
        np.concatenate([inputs["moe_b_group"], inputs["moe_b_expert"]], axis=-1), dtype=np.float32)
    for k in ("moe_w_gate", "moe_w_up", "moe_w_down"):
        out[k] = np.ascontiguousarray(inputs[k], dtype=np.float32)
    return out


TWO_PI = 6.283185307179586
CW1 = 6.28125
CW2 = TWO_PI - CW1
PI_SAFE = 3.1415925


def _rr_sin(self, dst, X, tmpf, tmpi, phase=0.0, e="dve"):
    mk = self.mk
    mk.ts(tmpf, X, 1.0 / TWO_PI, 0.5 + phase / TWO_PI, op0=ALU.mult, op1=ALU.add, e=e)
    mk.copy(tmpi, tmpf, e=e)
    mk.copy(tmpf, tmpi, e=e)
    mk.stt(dst, tmpf, -CW1, X, ALU.mult, ALU.add)
    mk.stt(dst, tmpf, -CW2, dst, ALU.mult, ALU.add)
    if phase:
        mk.ts(dst, dst, phase, None, op0=ALU.add, e=e)
    mk.ts(tmpf, dst, -PI_SAFE, TWO_PI, op0=ALU.is_lt, op1=ALU.mult, e=e)
    mk.tt(dst, dst, tmpf, ALU.add, e=e)
    mk.ts(tmpf, dst, PI_SAFE, TWO_PI, op0=ALU.is_gt, op1=ALU.mult, e=e)
    mk.tt(dst, dst, tmpf, ALU.subtract, e=e)
    mk.ts(dst, dst, PI_SAFE, -PI_SAFE, op0=ALU.min, op1=ALU.max, e=e)
    mk.act(dst, dst, AF.Sin)


Builder.rr_sin = _rr_sin


def _proj_in(self, xres, g_row, W, ncols, fm_blocks, uT, tm_range, u_tm):
    mk = self.mk
    with mk.scope():
        gbc = mk.sb([128, D], F32, "gbc")
        mk.dma(gbc, g_row.pbc(128))
        Wb = mk.sb([128, 8, ncols], BF16, "Wb")
        for k in range(8):
            mk.dma(Wb[:, k, :], W[k * 128:(k + 1) * 128, :], q="pool")
        xts = [mk.sb([128, D], F32) for i in range(2)]
        sqs = [mk.sb([128, D], F32) for i in range(2)]
        hbs = [mk.sb([128, D], BF16) for i in range(2)]
        smalls = [(mk.sb([128, 1], F32), mk.sb([128, 1], F32)) for i in range(2)]
        hT = [mk.sb([128, 8, 512], BF16) for i in range(2)]
        ev = [mk.sb([128, 512], F32) for i in range(4)]
        nev = 0
        for gidx in range(NTOK // 512):
            hb_ = hT[gidx % 2]
            for tl in range(4):
                i = gidx * 4 + tl
                b = i % 2
                mk.dma(xts[b], xres.rows(i * 128, 128))
                self.rms_tile(xts[b], gbc, hbs[b], sq=sqs[b], small=smalls[b])
                pbb = self.bank[b].bitcast(BF16)
                for k in range(8):
                    mk.transpose(pbb[:, k * 128:(k + 1) * 128], hbs[b][:, k * 128:(k + 1) * 128], self.identb)
                mk.copy(hb_[:, :, tl * 128:(tl + 1) * 128], pbb.re("p (k t) -> p k t", k=8),
                        e=("act" if tl % 2 else "pool_never") if False else ("act" if tl % 2 else "dve"))
            for bi, cb in enumerate(fm_blocks):
                pb = self.bank[2 + (bi % 3)]
                for k in range(8):
                    mk.mm(pb, Wb[:, k, cb * 128:(cb + 1) * 128], hb_[:, k, :], start=(k == 0), stop=(k == 7))
                t = ev[nev % 4]
                mk.copy(t, pb, e=("act" if nev % 2 else "dve"))
                nev += 1
                mk.dma(V(uT.ap[bi * 128:(bi + 1) * 128, gidx * 512:(gidx + 1) * 512], uT.bufs[bi]), t)
            if tm_range is not None:
                c0, c1 = tm_range
                for tl in range(4):
                    i = gidx * 4 + tl
                    for cc in range(c0, c1, 512):
                        pb = self.bank[5 + (nev % 3)]
                        for k in range(8):
                            mk.mm(pb, hb_[:, k, tl * 128:(tl + 1) * 128], Wb[:, k, cc:cc + 512],
                                  start=(k == 0), stop=(k == 7))
                        t = ev[nev % 4]
                        mk.copy(t, pb, e=("act" if nev % 2 else "dve"))
                        nev += 1
                        mk.dma(V(u_tm.ap[i * 128:(i + 1) * 128, cc - c0:cc - c0 + 512], u_tm.bufs[i]), t)


Builder.proj_in = _proj_in


def _proj_out(self, xres_in, xres_out, oTd, Wout):
    mk = self.mk
    with mk.scope():
        Wb = mk.sb([128, 8, D], BF16, "Wob")
        for k in range(8):
            mk.dma(Wb[:, k, :], Wout[k * 128:(k + 1) * 128, :], q="pool")
        xts = [mk.sb([128, D], F32) for i in range(2)]
        ot = [mk.sb([128, 8, 512], BF16) for i in range(2)]
        for gi in range(NTOK // 512):
            o_ = ot[gi % 2]
            for k in range(8):
                mk.dma(o_[:, k, :], V(oTd.ap[k * 128:(k + 1) * 128, gi * 512:(gi + 1) * 512], oTd.bufs[k]))
            for tl in range(4):
                i = gi * 4 + tl
                b = i % 2
                mk.dma(xts[b], xres_in.rows(i * 128, 128))
                for half in range(2):
                    pb = self.bank[(i % 2) * 2 + half]
                    for k in range(8):
                        mk.mm(pb, o_[:, k, tl * 128:(tl + 1) * 128], Wb[:, k, half * 512:(half + 1) * 512],
                              start=(k == 0), stop=(k == 7))
                    mk.tt(xts[b][:, half * 512:(half + 1) * 512], xts[b][:, half * 512:(half + 1) * 512], pb, ALU.add)
                mk.dma(xres_out.rows(i * 128, 128), xts[b])


Builder.proj_out = _proj_out


def _attn(self, u_tm, oT, P, layer):
    import math
    mk = self.mk
    oi = layer // 2
    lam_init = 0.8 - 0.6 * math.exp(-0.3 * layer)
    with mk.scope():
        gqk = mk.sb([128, D], F32, "gqk")
        mk.dma(gqk, P["da_qk_gain"][oi:oi + 1, :].pbc(128))
        subg = mk.sb([128, 128], F32, "subg")
        mk.dma(subg, P["da_subln"][oi:oi + 1, :].pbc(128))
        mk.ts(subg, subg, 1.0 - lam_init, None, op0=ALU.mult)
        lamv = mk.sb([128, 4, 64], F32, "lamv")
        mk.dma(lamv.re("p a d -> p (a d)"), P["da_lam"][oi:oi + 1].re("o a d -> o (a d)").pbc(128))
        lt = mk.sb([128, 2, 64], F32)
        ls = mk.sb([128, 2], F32)
        mk.tt(lt[:, 0, :], lamv[:, 0, :], lamv[:, 1, :], ALU.mult)
        mk.tt(lt[:, 1, :], lamv[:, 2, :], lamv[:, 3, :], ALU.mult)
        mk.reduce(ls, lt, op=ALU.add)
        mk.act(ls, ls, AF.Exp)
        nlam = mk.sb([128, 1], F32, "nlam")
        mk.tt(nlam, ls[:, 1:2], ls[:, 0:1], ALU.subtract)
        mk.ts(nlam, nlam, -lam_init, None, op0=ALU.add)
        eps5 = mk.sb([128, 1], F32)
        mk.memset(eps5, 1e-5)
        nshift = mk.sb([128, 1], F32)
        mk.memset(nshift, -4.0)
        zb = mk.sb([128, 512], BF16, "zb")
        mk.memset(zb, 0.0)
        jf = mk.sb([128, 32], F32)
        mk.op("pool", lambda eng: eng.iota(jf.ap, [[1, 32]], base=0, channel_multiplier=0,
                                            allow_small_or_imprecise_dtypes=True), writes=[jf])
        mk.act(jf, jf, AF.Exp, scale=-math.log(10000.0) / 32.0)
        posf = mk.sb([128, 16], F32)
        mk.op("pool", lambda eng: eng.iota(posf.ap, [[128, 16]], base=0, channel_multiplier=1,
                                            allow_small_or_imprecise_dtypes=True), writes=[posf])
        ang = mk.sb([128, 16, 32], F32)
        mk.tt(ang, V(jf.ap.unsqueeze(1).to_broadcast([128, 16, 32]), jf.buf),
              V(posf.ap.unsqueeze(2).to_broadcast([128, 16, 32]), posf.buf), ALU.mult)
        sint = mk.sb([128, 16, 32], F32, "sint")
        cost = mk.sb([128, 16, 32], F32, "cost")
        tf = mk.sb([128, 16, 32], F32)
        ti = mk.sb([128, 16, 32], I32)
        self.rr_sin(sint, ang, tf, ti)
        self.rr_sin(cost, ang, tf, ti, phase=math.pi / 2)

        QT = mk.sb([128, 4, T], BF16, "QT")
        KT = mk.sb([128, 4, T], BF16, "KT")
        Vt = mk.sb([128, 16, 512], BF16, "Vt")
        qk = [mk.sb([128, 16, 2, 32], F32) for i in range(2)]
        sq = mk.sb([128, 16, 64], F32)
        ss = mk.sb([128, 16], F32)
        ta = mk.sb([128, 16, 32], F32)
        tb = mk.sb([128, 16, 32], F32)
        qr = [mk.sb([128, 16, 2, 32], BF16) for i in range(2)]
        pts = [mk.sb([128, 512], BF16) for i in range(3)]
        rls = [mk.sb([128, 8], F32) for i in range(2)]
        ob32 = [mk.sb([128, 128], F32) for i in range(2)]
        obb = [mk.sb([128, 128], BF16) for i in range(2)]
        junk = mk.sb([128, 128], F32)
        ss1 = [mk.sb([128, 1], F32) for i in range(2)]
        npt = 0
        nfin = 0
        ostage = [mk.sb([128, T], BF16, "ostage%d" % i) for i in range(2)]
        for s in range(NSEQ):
            mk.dma(Vt, V(u_tm.ap[s * T:(s + 1) * T, 1024:1536].rearrange("(i p) c -> p i c", p=128),
                         u_tm.whole), q="pool", )
            for i in range(16):
                b = i % 2
                row0 = s * T + i * 128
                q_ = qk[b]
                qf = q_.re("p g m d -> p (g m d)")
                mk.dma(qf, V(u_tm.ap[row0:row0 + 128, 0:1024], u_tm.bufs[row0 // 128]))
                mk.act(sq.re("p g d -> p (g d)"), qf, AF.Square)
                mk.reduce(ss, sq, op=ALU.add)
                mk.act(ss, ss, AF.Sqrt, scale=1.0 / 64.0, bias=self.eps_t)
                mk.recip(ss, ss)
                q3 = q_.re("p g m d -> p g (m d)")
                mk.tt(q3, q3, V(ss.ap.unsqueeze(2).to_broadcast([128, 16, 64]), ss.buf), ALU.mult)
                mk.tt(qf, qf, gqk, ALU.mult)
                cb_ = V(cost.ap[:, i, :].unsqueeze(1).to_broadcast([128, 16, 32]), cost.buf)
                sb_ = V(sint.ap[:, i, :].unsqueeze(1).to_broadcast([128, 16, 32]), sint.buf)
                x1 = q_[:, :, 0, :]
                x2 = q_[:, :, 1, :]
                mk.tt(ta, x1, cb_, ALU.mult)
                mk.tt(tb, x2, sb_, ALU.mult, e="pool")
                mk.tt(qr[b][:, :, 0, :], ta, tb, ALU.subtract)
                mk.tt(ta, x2, cb_, ALU.mult)
                mk.tt(tb, x1, sb_, ALU.mult, e="pool")
                mk.tt(qr[b][:, :, 1, :], ta, tb, ALU.add)
                qrf = qr[b].re("p g m d -> p (g m d)")
                pbb = self.bank[6 + b].bitcast(BF16)
                for k in range(8):
                    mk.transpose(pbb[:, k * 128:(k + 1) * 128], qrf[:, k * 128:(k + 1) * 128], self.identb)
                mk.copy(QT[:, :, i * 128:(i + 1) * 128], pbb[:, 0:512].re("p (h t) -> p h t", h=4), e="act")
                mk.copy(KT[:, :, i * 128:(i + 1) * 128], pbb[:, 512:1024].re("p (h t) -> p h t", h=4), e="act")
            units = [(h, qc) for h in range(4) for qc in range(4)]

            def osets(u):
                par = u % 2
                O = [self.bank[2], self.bank[3]] if par == 0 else [self.bank[6], self.bank[7]]
                Lb = V(self.bank[4].ap[:, 8 * par:8 * par + 8], self.bank[4].buf)
                return O, Lb

            def main(u):
                nonlocal npt
                h, qc = units[u]
                O, Lb = osets(u)
                mk.mm(O[0], zb[:, 0:128], zb)
                mk.mm(O[1], zb[:, 0:128], zb)
                mk.mm(Lb, zb[:, 0:128], zb[:, 0:8])
                steps = [(m, kt) for m in range(2) for kt in range(4 * qc + 4)]
                info = []

                def issue_S(i):
                    nonlocal npt
                    m, kt = steps[i]
                    q0 = max(kt * 128, qc * 512)
                    nq = (qc + 1) * 512 - q0
                    S = self.bank[npt % 2]
                    Pt = pts[npt % 3]
                    npt += 1
                    mk.mm(S[:, 0:nq], KT[m * 64:(m + 1) * 64, h, kt * 128:(kt + 1) * 128],
                          QT[m * 64:(m + 1) * 64, h, q0:q0 + nq])
                    info.append((S, Pt, q0, nq))

                issue_S(0)
                for i, (m, kt) in enumerate(steps):
                    if i + 1 < len(steps):
                        issue_S(i + 1)
                    S, Pt, q0, nq = info[i]
                    mk.act(Pt[:, 0:nq], S[:, 0:nq], AF.Exp, scale=0.125, bias=nshift)
                    if kt >= 4 * qc:
                        mk.tt(Pt[:, 0:128], Pt[:, 0:128], self.trib, ALU.mult, e="pool")
                    for qb in range(max(kt, 4 * qc), 4 * qc + 4):
                        ql = qb - 4 * qc
                        c0 = qb * 128 - q0
                        mk.mm(O[m][:, ql * 128:(ql + 1) * 128], Pt[:, c0:c0 + 128],
                              Vt[:, kt, h * 128:(h + 1) * 128], start=False, stop=(kt == qb),
                              skip_group_check=True)
                        mk.mm(Lb[:, m * 4 + ql:m * 4 + ql + 1], Pt[:, c0:c0 + 128], self.onesb[:, 0:1],
                              start=False, stop=(kt == qb), skip_group_check=True)

            def fin(u):
                nonlocal nfin
                h, qc = units[u]
                O, Lb = osets(u)
                rl = rls[u % 2]
                mk.recip(rl, Lb)
                mk.ts(rl[:, 4:8], rl[:, 4:8], nlam, None, op0=ALU.mult)
                for ql in range(4):
                    fb = nfin % 2
                    nfin += 1
                    o = ob32[fb]
                    mk.ts(o, O[0][:, ql * 128:(ql + 1) * 128], rl[:, ql:ql + 1], None, op0=ALU.mult)
                    mk.stt(o, O[1][:, ql * 128:(ql + 1) * 128], rl[:, 4 + ql:5 + ql], o, ALU.mult, ALU.add)
                    mk.act(junk, o, AF.Square, accum_out=ss1[fb])
                    mk.act(ss1[fb], ss1[fb], AF.Sqrt, scale=1.0 / 128.0, bias=eps5)
                    mk.recip(ss1[fb], ss1[fb])
                    mk.stt(obb[fb], o, ss1[fb], subg, ALU.mult, ALU.mult)
                    ptr = self.bank[5].bitcast(BF16)
                    mk.transpose(ptr[:, fb * 128:(fb + 1) * 128], obb[fb], self.identb)
                    t0 = (4 * qc + ql) * 128
                    mk.copy(ostage[h % 2][:, t0:t0 + 128], ptr[:, fb * 128:(fb + 1) * 128], e="act")
                if qc == 3:
                    mk.dma(V(oT.ap[h * 128:(h + 1) * 128, s * T:(s + 1) * T], oT.bufs[h]), ostage[h % 2])

            main(0)
            for u in range(len(units)):
                if u + 1 < len(units):
                    main(u + 1)
                fin(u)


Builder.attn = _attn


def _s5_params(self, a_re, a_im, lstep, shape, want_coef):
    import math
    mk = self.mk
    n = lambda: mk.sb(shape, F32)
    are, step, lr, th, rho = n(), n(), n(), n(), n()
    mk.ts(are, a_re, -1e-4, None, op0=ALU.min)
    mk.act(step, lstep, AF.Exp)
    mk.tt(lr, are, step, ALU.mult)
    mk.tt(th, a_im, step, ALU.mult)
    mk.act(rho, lr, AF.Exp)
    out = dict(rho=rho, th=th)
    if want_coef:
        sn, cs, tf, x, y, den, cre, cim = n(), n(), n(), n(), n(), n(), n(), n()
        ti = mk.sb(shape, I32)
        self.rr_sin(sn, th, tf, ti)
        self.rr_sin(cs, th, tf, ti, phase=math.pi / 2)
        mk.tt(x, rho, cs, ALU.mult)
        mk.ts(x, x, -1.0, None, op0=ALU.add)
        mk.tt(y, rho, sn, ALU.mult)
        mk.tt(den, are, are, ALU.mult)
        mk.tt(tf, a_im, a_im, ALU.mult)
        mk.tt(den, den, tf, ALU.add)
        mk.recip(den, den)
        mk.tt(cre, x, are, ALU.mult)
        mk.tt(tf, y, a_im, ALU.mult)
        mk.tt(cre, cre, tf, ALU.add)
        mk.tt(cre, cre, den, ALU.mult)
        mk.tt(cim, y, are, ALU.mult)
        mk.tt(tf, x, a_im, ALU.mult)
        mk.tt(cim, cim, tf, ALU.subtract)
        mk.tt(cim, cim, den, ALU.mult)
        out.update(cre=cre, cim=cim)
    return out


Builder.s5_params = _s5_params


def _s5(self, uT, oT, P, layer):
    import math
    mk = self.mk
    oi = layer // 2
    with mk.scope():
        bbr = mk.sb([128, 4, 128], BF16, "bbr")
        bbi = mk.sb([128, 4, 128], BF16, "bbi")
        rho = mk.sb([128, 16], F32, "rho16")
        theta = mk.sb([128, 16], F32, "th16")
        bfr = mk.sb([128, 16, 128], BF16, "bfr")
        bfi = mk.sb([128, 16, 128], BF16, "bfi")
        Cfr = mk.sb([128, 16, 128], BF16, "Cfr")
        Cfi = mk.sb([128, 16, 128], BF16, "Cfi")
        with mk.scope():
            rep = mk.sb([128, 3, 512], F32, "rep")
            mk.dma(rep, P["s5_rep"][oi].re("a p j s -> p a (j s)"))
            pr_ = self.s5_params(rep[:, 0, :], rep[:, 1, :], rep[:, 2, :], [128, 512], True)
            Bre = mk.sb([128, 512], F32)
            Bim = mk.sb([128, 512], F32)
            mk.dma(Bre, P["s5_bbd_re"][oi].re("p j s -> p (j s)"))
            mk.dma(Bim, P["s5_bbd_im"][oi].re("p j s -> p (j s)"))
            t1p = mk.sb([128, 512], F32)
            t2p = mk.sb([128, 512], F32)
            mk.tt(t1p, Bre, pr_["cre"], ALU.mult)
            mk.tt(t2p, Bim, pr_["cim"], ALU.mult)
            mk.tt(bbr.re("p j s -> p (j s)"), t1p, t2p, ALU.subtract)
            mk.tt(t1p, Bre, pr_["cim"], ALU.mult)
            mk.tt(t2p, Bim, pr_["cre"], ALU.mult)
            mk.tt(bbi.re("p j s -> p (j s)"), t1p, t2p, ALU.add)
            mk.memset(bfr, 0.0)
            mk.memset(bfi, 0.0)
            for q in range(4):
                for (src, dst) in ((bbr, bfr), (bbi, bfi)):
                    mk.copy(dst[32 * q:32 * q + 32].re("p (j q) s -> p j q s", q=4)[:, :, q, :],
                            src[32 * q:32 * q + 32, :, :], e="pool")
            st = mk.sb([128, 3, 16], F32, "st")
            mk.dma(st, P["s5_st"][oi].re("a p b -> p a b"))
            ps_ = self.s5_params(st[:, 0, :], st[:, 1, :], st[:, 2, :], [128, 16], False)
            mk.copy(rho, ps_["rho"])
            mk.copy(theta, ps_["th"])
        Cre = mk.sb([128, 16, 32], BF16, "Cre")
        nCim = mk.sb([128, 16, 32], BF16, "nCim")
        cim32 = mk.sb([128, 16, 32], F32)
        mk.dma(Cre, P["s5_cbd_re"][oi], q="pool")
        mk.dma(cim32, P["s5_cbd_im"][oi])
        mk.ts(nCim, cim32, -1.0, None, op0=ALU.mult)
        mk.memset(Cfr, 0.0)
        mk.memset(Cfi, 0.0)
        for q in range(4):
            for (src, dst) in ((Cre, Cfr), (nCim, Cfi)):
                mk.copy(dst.re("p (j q) c -> p j q c", q=4)[:, :, q, 32 * q:32 * q + 32],
                        src.re("p (j q) c -> p j q c", q=4)[:, :, q, :], e="pool")
        dcol = mk.sb([128, 4], F32, "dcol")
        mk.dma(dcol, P["s5_dcol"][oi])
        cbase = mk.sb([128, 16, 64], F32, "cbase")
        sbase = mk.sb([128, 16, 64], F32, "sbase")
        cstep = mk.sb([128, 16, 32], F32, "cstep")
        sstep = mk.sb([128, 16, 32], F32, "sstep")
        with mk.scope():
            rio = mk.sb([128, 64], F32)
            mk.op("pool", lambda eng: eng.iota(rio.ap, [[1, 64]], base=0, channel_multiplier=0,
                                                allow_small_or_imprecise_dtypes=True), writes=[rio])
            kio = mk.sb([128, 32], F32)
            mk.op("pool", lambda eng: eng.iota(kio.ap, [[64, 32]], base=0, channel_multiplier=0,
                                                allow_small_or_imprecise_dtypes=True), writes=[kio])
            angb = mk.sb([128, 16, 64], F32)
            angs = mk.sb([128, 16, 32], F32)
            mk.tt(angb, V(theta.ap.unsqueeze(2).to_broadcast([128, 16, 64]), theta.buf),
                  V(rio.ap.unsqueeze(1).to_broadcast([128, 16, 64]), rio.buf), ALU.mult)
            mk.tt(angs, V(theta.ap.unsqueeze(2).to_broadcast([128, 16, 32]), theta.buf),
                  V(kio.ap.unsqueeze(1).to_broadcast([128, 16, 32]), kio.buf), ALU.mult)
            tfb = mk.sb([128, 16, 64], F32)
            tib = mk.sb([128, 16, 64], I32)
            self.rr_sin(sbase, angb, tfb, tib)
            self.rr_sin(cbase, angb, tfb, tib, phase=math.pi / 2)
            self.rr_sin(sstep, angs, tfb[:, :, 0:32], tib[:, :, 0:32])
            self.rr_sin(cstep, angs, tfb[:, :, 0:32], tib[:, :, 0:32], phase=math.pi / 2)
        ubb = mk.sb([128, NTOK], BF16, "ubb")
        zTd = self.scratch("zTd", [512, NTOK], BF16)
        yT = mk.sb([128, NTOK], F32, "yT")
        sint2 = [mk.sb([128, T], F32, "sint%d" % i) for i in range(2)]
        cost2 = [mk.sb([128, T], F32, "cost%d" % i) for i in range(2)]
        gre2 = [mk.sb([128, T], F32, "gre%d" % i) for i in range(2)]
        gim2 = [mk.sb([128, T], F32, "gim%d" % i) for i in range(2)]
        wre2 = [mk.sb([128, T], F32, "wre%d" % i) for i in range(2)]
        wim2 = [mk.sb([128, T], F32, "wim%d" % i) for i in range(2)]
        xre2 = [mk.sb([128, T], BF16, "xre%d" % i) for i in range(2)]
        xim2 = [mk.sb([128, T], BF16, "xim%d" % i) for i in range(2)]
        tt1 = mk.sb([128, T], F32, "tt1")
        tt2 = mk.sb([128, T], F32, "tt2")
        bur_f = mk.sb([128, T], F32, "bur_f")
        bui_f = mk.sb([128, T], F32, "bui_f")
        ubb2 = [ubb, ubb]
        state = dict(nb=0)

        S5E = DBG.get('s5_eng', 'dve')

        def tables(sbi):
            sint, cost = sint2[sbi % 2], cost2[sbi % 2]
            gre, gim, wre, wim = gre2[0], gim2[0], wre2[0], wim2[0]
            cs_b = V(cstep.ap[:, sbi, :].unsqueeze(2).to_broadcast([128, 32, 64]), cstep.buf)
            ss_b = V(sstep.ap[:, sbi, :].unsqueeze(2).to_broadcast([128, 32, 64]), sstep.buf)
            cb_b = V(cbase.ap[:, sbi, :].unsqueeze(1).to_broadcast([128, 32, 64]), cbase.buf)
            sb_b = V(sbase.ap[:, sbi, :].unsqueeze(1).to_broadcast([128, 32, 64]), sbase.buf)
            v3_ = lambda t_: t_.re("p (k r) -> p k r", r=64)
            mk.tt(v3_(tt1), cs_b, cb_b, ALU.mult)
            mk.tt(v3_(tt2), ss_b, sb_b, ALU.mult, e=S5E)
            mk.tt(cost, tt1, tt2, ALU.subtract)
            mk.tt(v3_(tt1), ss_b, cb_b, ALU.mult, e=S5E)
            mk.tt(v3_(tt2), cs_b, sb_b, ALU.mult)
            mk.tt(sint, tt1, tt2, ALU.add, e=S5E)

        def multi(b0, nb_):
            return V(self.psum_all.ap[:, b0 * 512:(b0 + nb_) * 512], self.bank[b0].buf)

        def stageA(it):
            sbi, s = it // NSEQ, it % NSEQ
            cb = sbi // 4
            sint, cost = sint2[sbi % 2], cost2[sbi % 2]
            gre, gim, wre, wim = gre2[it % 2], gim2[it % 2], wre2[it % 2], wim2[it % 2]
            ub_ = ubb2[cb % 2]
            rho_b = V(rho.ap[:, sbi:sbi + 1].to_broadcast([128, T]), rho.buf)
            for ch in range(4):
                tok0 = s * T + ch * 512
                mk.mm(self.bank[ch], bfr[:, sbi, :], ub_[:, tok0:tok0 + 512])
            for ch in range(4):
                tok0 = s * T + ch * 512
                mk.mm(self.bank[4 + ch], bfi[:, sbi, :], ub_[:, tok0:tok0 + 512])
            rd_r = [self.bank[i] for i in range(1, 4)]
            rd_i = [self.bank[i] for i in range(5, 8)]
            src_r, src_i = multi(0, 4), multi(4, 4)
            mk.op("act", lambda eng: eng.copy(out=bur_f.ap, in_=src_r.ap), reads=[src_r] + rd_r, writes=[bur_f])
            mk.op("act", lambda eng: eng.copy(out=bui_f.ap, in_=src_i.ap), reads=[src_i] + rd_i, writes=[bui_f])
            mk.tt(tt1, bur_f, cost, ALU.mult)
            mk.tt(gre, bui_f, sint, ALU.mult, e=S5E)
            mk.tt(gre, gre, tt1, ALU.add)
            mk.tt(tt2, bui_f, cost, ALU.mult)
            mk.tt(gim, bur_f, sint, ALU.mult, e=S5E)
            mk.tt(gim, tt2, gim, ALU.subtract)
            mk.scan(wre, rho_b, gre, 0.0)
            mk.scan(wim, rho_b, gim, 0.0)

        def stageB(it):
            sbi, s = it // NSEQ, it % NSEQ
            cb, q = sbi // 4, sbi % 4
            sint, cost = sint2[sbi % 2], cost2[sbi % 2]
            gre, gim, wre, wim = gre2[it % 2], gim2[it % 2], wre2[it % 2], wim2[it % 2]
            xre, xim = xre2[it % 2], xim2[it % 2]
            mk.tt(gre, cost, wre, ALU.mult, e=S5E)
            mk.tt(gim, sint, wim, ALU.mult)
            mk.tt(xre, gre, gim, ALU.subtract)
            mk.tt(wre, sint, wre, ALU.mult)
            mk.tt(wim, cost, wim, ALU.mult, e=S5E)
            mk.tt(xim, wre, wim, ALU.add)
            for ch in range(4):
                sl = slice(ch * 512, (ch + 1) * 512)
                py = self.bank[ch]
                mk.mm(py, Cfr[:, sbi, :], xre[:, sl], start=True, stop=False)
                mk.mm(py, Cfi[:, sbi, :], xim[:, sl], start=False, stop=True)
            src_y = multi(0, 4)
            rd_y = [self.bank[i] for i in range(1, 4)]
            ysl = yT[:, s * T:(s + 1) * T]
            if q == 0:
                mk.op("act", lambda eng: eng.copy(out=ysl.ap, in_=src_y.ap), reads=[src_y] + rd_y, writes=[ysl])
            else:
                mk.op("act", lambda eng: eng.copy(out=bur_f.ap, in_=src_y.ap), reads=[src_y] + rd_y, writes=[bur_f])
                mk.tt(ysl, ysl, bur_f, ALU.add, e=S5E)

        def finish(cb):
            for s in range(NSEQ):
                ys = yT[:, s * T:(s + 1) * T]
                mk.dma(tt1, V(uT.ap[cb * 128:(cb + 1) * 128, s * T:(s + 1) * T], uT.bufs[cb]))
                mk.stt(ys, tt1, dcol[:, cb:cb + 1], ys, ALU.mult, ALU.add)
                mk.tt(tt2, ys, ys, ALU.mult, e="pool")
                mk.ts(tt2, tt2, 0.044715, 1.0, op0=ALU.mult, op1=ALU.add)
                mk.tt(tt2, tt2, ys, ALU.mult, e="pool")
                mk.act(tt2, tt2, AF.Sigmoid, scale=2.0 * math.sqrt(2.0 / math.pi))
                mk.tt(xre2[s % 2], ys, tt2, ALU.mult)
                mk.dma(V(zTd.ap[cb * 128:(cb + 1) * 128, s * T:(s + 1) * T], zTd.bufs[cb]), xre2[s % 2], q="pool")

        NIT = 16 * NSEQ
        for cb in range(4):
            if cb == 0:
                mk.dma(ubb2[0], V(uT.ap[0:128, :], uT.bufs[0]), q="pool")
                tables(0)
                stageA(0)
            for q in range(4):
                sbi = 4 * cb + q
                for s in range(NSEQ):
                    it = sbi * NSEQ + s
                    nxt = it + 1
                    if nxt < NIT and (nxt // NSEQ) // 4 == cb:
                        if nxt % NSEQ == 0:
                            tables(nxt // NSEQ)
                        stageA(nxt)
                    stageB(it)
            finish(cb)
            nxt = (4 * cb + 4) * NSEQ
            if nxt < NIT:
                mk.dma(ubb2[(cb + 1) % 2], V(uT.ap[(cb + 1) * 128:(cb + 2) * 128, :], uT.bufs[cb + 1]), q="pool")
                tables(nxt // NSEQ)
                stageA(nxt)
    with mk.scope():
        wgl = mk.sb([128, 4, 512], BF16, "wgl")
        for k in range(4):
            mk.dma(wgl[:, k, :], P["s5_w_glu"][oi, k * 128:(k + 1) * 128, :], q="pool")
        sg = [mk.sb([128, 512], F32) for i in range(4)]
        obuf = [mk.sb([128, 512], BF16) for i in range(4)]
        zc = [mk.sb([128, 4, 512], BF16, "zc%d" % i) for i in range(2)]
        for ch in range(NTOK // 512):
            sl = slice(ch * 512, (ch + 1) * 512)
            zch = zc[ch % 2]
            for k in range(4):
                mk.dma(zch[:, k, :], V(zTd.ap[k * 128:(k + 1) * 128, sl], zTd.bufs[k]))
            for cbo in range(4):
                pg = self.bank[cbo]
                for k in range(4):
                    mk.mm(pg, wgl[:, k, cbo * 128:(cbo + 1) * 128], zch[:, k, :], start=(k == 0), stop=(k == 3))
                mk.act(sg[cbo], pg, AF.Sigmoid)
            for cbo in range(4):
                ob_ = obuf[(ch * 4 + cbo) % 4]
                mk.tt(ob_, zch[:, cbo, :], sg[cbo], ALU.mult, e=("pool" if cbo % 2 else "dve"))
                mk.dma(V(oT.ap[(4 + cbo) * 128:(5 + cbo) * 128, sl], oT.bufs[4 + cbo]), ob_)


Builder.s5 = _s5


def _odd_layer(self, xres_in, xres_out, layer, P):
    mk = self.mk
    oi = layer // 2
    u_tm = self.scratch("u_tm", [NTOK, 1536], F32)
    uT = self.scratch("uTo", [512, NTOK], F32)
    self.proj_in(xres_in, P["norm_mix_g"][layer:layer + 1, :], P["odd_w_in"][oi], 2048,
                 [12, 13, 14, 15], uT, (0, 1536), u_tm)
    oT = self.scratch("oTd", [D, NTOK], BF16)
    if not DBG.get("no_attn"):
        self.attn(u_tm, oT, P, layer)
    if not DBG.get("no_s5"):
        self.s5(uT, oT, P, layer)
    self.proj_out(xres_in, xres_out, oT, P["odd_w_out"][oi])


Builder.odd_layer = _odd_layer


def host_odd_params(inputs):
    f = lambda a: np.ascontiguousarray(a, dtype=np.float32)
    out = {}
    out["norm_mix_g"] = f(inputs["norm_mix_g"])
    out["odd_w_in"] = f(inputs["odd_w_in"])
    out["odd_w_out"] = f(inputs["odd_w_out"])
    n_odd = inputs["odd_w_in"].shape[0]
    out["da_qk_gain"] = f(np.concatenate([np.tile(inputs["da_q_norm"], (1, 8)),
                                          np.tile(inputs["da_k_norm"], (1, 8))], axis=1))
    out["da_lam"] = f(np.stack([inputs["da_lam_q1"], inputs["da_lam_k1"],
                                inputs["da_lam_q2"], inputs["da_lam_k2"]], axis=1))
    out["da_subln"] = f(inputs["da_subln"])
    three = np.stack([inputs["s5_a_re"], inputs["s5_a_im"], inputs["s5_log_step"]], axis=1)
    t16 = three.reshape(n_odd, 3, 16, 128)
    rep = t16.reshape(n_odd, 3, 4, 4, 128)
    rep = np.transpose(rep, (0, 1, 3, 2, 4))
    rep = np.repeat(rep[:, :, :, None, :, :], 32, axis=3)
    out["s5_rep"] = f(rep.reshape(n_odd, 3, 128, 4, 128))
    out["s5_st"] = f(np.transpose(t16, (0, 1, 3, 2)))
    for nm, key in (("s5_bbd_re", "s5_b_re"), ("s5_bbd_im", "s5_b_im")):
        Bm = inputs[key]
        bd = np.zeros((n_odd, 4, 2, 16, 4, 2, 64), np.float32)
        for j in range(4):
            for q in range(4):
                for gl in range(2):
                    g = 2 * (4 * j + q) + gl
                    bd[:, q, gl, :, j, gl, :] = np.transpose(Bm[:, g], (0, 2, 1))
        out[nm] = f(bd.reshape(n_odd, 128, 4, 128))
    for nm, key in (("s5_cbd_re", "s5_c_re"), ("s5_cbd_im", "s5_c_im")):
        Cm = inputs[key]
        bd = np.zeros((n_odd, 2, 64, 16, 2, 16), np.float32)
        for sbi in range(16):
            for gl in range(2):
                bd[:, gl, :, sbi, gl, :] = np.transpose(Cm[:, 2 * sbi + gl], (0, 2, 1))
        out[nm] = f(bd.reshape(n_odd, 128, 16, 32))
    out["s5_dcol"] = f(np.transpose(inputs["s5_d"].reshape(n_odd, 4, 128), (0, 2, 1)))
    out["s5_w_glu"] = f(inputs["s5_w_glu"])
    return out


LCH = 64
NCH = T // LCH
DECAY_C = 0.6065306597126334


def _load_shift_mix(self, dst, uT, blk, s, mu_col, U, dtmp):
    mk = self.mk
    mk.dma(U[:, 1:T + 1], V(uT.ap[blk * 128:(blk + 1) * 128, s * T:(s + 1) * T], uT.bufs[blk]))
    mk.tt(dtmp, U[:, 0:T], U[:, 1:T + 1], ALU.subtract, e="pool")
    mk.stt(dst, dtmp, mu_col, U[:, 1:T + 1], ALU.mult, ALU.add)


Builder.load_shift_mix = _load_shift_mix


def _rwkv(self, uT, oTd, P, layer, vfirst):
    mk = self.mk
    ei = layer // 2
    has_vres = layer > 0
    with mk.scope():
        mu = mk.sb([128, 14], F32, "mu")
        mk.dma(mu, P["rw_mu_col"][ei])
        cols = mk.sb([128, 7, 4], F32, "cols")
        mk.dma(cols, P["rw_cols"][ei])
        w0c, a0c, kkc, kac, rkc, lngc, lnbc = [cols[:, i, :] for i in range(7)]
        w2a2 = mk.sb([128, 512], BF16, "w2a2")
        mk.dma(w2a2, P["rw_w2a2"][ei], q="pool")
        g2b = mk.sb([128, 512], BF16, "g2b")
        mk.dma(g2b, P["rw_g2"][ei], q="pool")
        blk = mk.sb([128, 128], BF16, "blk")
        mk.dma(blk, self.inp["c_blk"], q="pool")
        masks = mk.sb([128, 3, 128], BF16, "masks")
        mk.dma(masks, self.inp["c_masks"].re("a p c -> p a c"), q="pool")

        def mb(i):
            return V(masks.ap[:, i, :].unsqueeze(1).to_broadcast([128, 2, 128]), masks.buf)
        mLs, mUs, mUi = mb(0), mb(1), mb(2)
        rmask = mk.sb([128, T], BF16, "rmask")
        tB = mk.sb([128, T], F32, "tB")
        r32 = mk.sb([128, T], F32, "r32")
        mk.op("pool", lambda eng: eng.iota(tB.ap.rearrange("p (c l) -> p c l", l=LCH), [[0, NCH], [1, LCH]],
                                            base=0, channel_multiplier=0, allow_small_or_imprecise_dtypes=True),
              writes=[tB])
        mk.ts(rmask, tB, 1.0, None, op0=ALU.min)
        lneps = mk.sb([128, 1], F32)
        mk.memset(lneps, 64e-5)
        zb = mk.sb([128, 128], BF16, "zb")
        mk.memset(zb, 0.0)
        U = mk.sb([128, T + 1], F32, "U")
        mk.memset(U[:, 0:1], 0.0)
        dtmp = tB
        m_ = r32
        lr12 = mk.sb([128, T], BF16, "lr12")
        sdg = mk.sb([128, T], BF16, "sdg")
        tbf = mk.sb([128, T], BF16, "tbf")
        if has_vres:
            v0c = mk.sb([128, 4], F32, "v0c")
            mk.dma(v0c, P["rw_v0col"][ei - 1])
            v1b = mk.sb([128, 4, 32], BF16, "v1b")
            mk.dma(v1b, P["rw_v1"][ei - 1].re("(k p) n -> p k n", p=128), q="pool")
            v2b = mk.sb([32, 512], BF16, "v2b")
            mk.dma(v2b, P["rw_v2"][ei - 1], q="pool")
            t32b = mk.sb([32, T], BF16, "t32b")
        k32 = mk.sb([128, T], F32, "k32")
        a16 = mk.sb([128, T], BF16, "a16")
        lw = mk.sb([128, T], F32, "lw")
        cl = mk.sb([128, T], F32, "cl")
        v32 = cl
        tA = mk.sb([128, T], F32, "tA")
        gT = mk.sb([128, T], BF16, "gT")
        bon = mk.sb([128, T], BF16, "bon")
        aT_ = mk.sb([128, T], BF16, "aT_")
        bT_ = mk.sb([128, T], BF16, "bT_")
        kT_ = mk.sb([128, T], BF16, "kT_")
        vT_ = tbf
        a_bd = mk.sb([128, 2, T], BF16, "a_bd")
        b_bd = mk.sb([128, 2, T], BF16, "b_bd")
        r_bd = mk.sb([128, 2, T], BF16, "r_bd")
        for t_ in (a_bd, b_bd, r_bd):
            mk.memset(t_, 0.0)
        TMav = mk.sb([128, 16, 2, 128], BF16, "TMav")
        TMbk = mk.sb([128, 16, 2, 2, 128], BF16, "TMbk")
        mk.memset(TMbk, 0.0)
        DL = mk.sb([128, NCH], F32, "DL")
        H32 = mk.sb([128, 128], F32, "H32")
        Hb = mk.sb([128, 128], BF16, "Hb")
        ht1 = mk.sb([128, 128], F32, "ht1")
        oacc = mk.sb([128, T], BF16, "oacc")

        def grp():
            d = dict(X=[mk.sb([128, 2, 128], BF16) for _ in range(2)], XT=[mk.sb([128, 2, 128], BF16) for _ in range(2)],
                     AakT=mk.sb([128, 2, 128], BF16), ArbT=mk.sb([128, 2, 128], BF16), ArkT=mk.sb([128, 2, 128], BF16),
                     Z=mk.sb([128, 2, 2, 64], BF16), T1T=mk.sb([128, 2, 128], BF16), G1=mk.sb([128, 2, 128], F32),
                     QT=mk.sb([128, 2, 128], BF16), yn=mk.sb([128, 128], BF16), ot=mk.sb([128, 128], F32),
                     st6=mk.sb([128, 2, 6], F32), mv=mk.sb([128, 2, 2], F32), rs=mk.sb([128, 2], F32))
            mk.memset(d["T1T"], 0.0)
            mk.memset(d["G1"], 0.0)
            mk.memset(d["QT"], 0.0)
            return d
        NGS = DBG.get("rw_depth", 1) + 1
        G = [grp() for _ in range(NGS)]
        B_ = self.bank

        def reg(b, c0, c1):
            return V(B_[b].ap[:, c0:c1], B_[b].buf)
        pN, pNT = reg(0, 0, 256), reg(0, 256, 512)
        pAk, pRb = reg(1, 0, 256), reg(1, 256, 512)
        pRk, pZ2 = reg(2, 0, 256), reg(2, 256, 384)
        pZa = [reg(3, 0, 256), reg(3, 256, 512)]
        pX, pXT = reg(4, 0, 256), reg(4, 256, 512)
        pHp, pTr = reg(5, 0, 128), reg(5, 128, 256)
        pT, pG = reg(6, 0, 256), reg(6, 256, 512)
        pQ, pY = reg(3, 0, 256), reg(7, 0, 128)
        pbig = [B_[5], B_[6], B_[7]]

        def v3(t):
            return t.re("p (h c) -> p h c", h=2)

        for s in range(NSEQ):
            tsl = slice(s * T, (s + 1) * T)
            self.load_shift_mix(m_, uT, 12, s, mu[:, 12:13], U, dtmp)
            mk.act(lr12[0:64, :], m_[0:64, :], AF.Tanh)
            mk.copy(lr12[64:128, :], m_[64:128, :], e="pool")
            self.load_shift_mix(m_, uT, 13, s, mu[:, 13:14], U, dtmp)
            mk.act(sdg, m_, AF.Sigmoid)
            if has_vres:
                pv = [B_[i] for i in range(4)]
                for hb in range(4):
                    self.load_shift_mix(m_, uT, 8 + hb, s, mu[:, 8 + hb:9 + hb], U, dtmp)
                    mk.copy(tbf, m_, e="act")
                    for ch in range(4):
                        mk.mm(pv[ch][0:32, :], v1b[:, hb, :], tbf[:, ch * 512:(ch + 1) * 512],
                              start=(hb == 0), stop=(hb == 3))
                for ch in range(4):
                    mk.copy(t32b[:, ch * 512:(ch + 1) * 512], pv[ch][0:32, :], e="act")
            for hb in range(4):
                hsl = slice(hb * 128, (hb + 1) * 128)
                self.load_shift_mix(r32, uT, hb, s, mu[:, hb:hb + 1], U, dtmp)
                self.load_shift_mix(k32, uT, 4 + hb, s, mu[:, 4 + hb:5 + hb], U, dtmp)
                self.load_shift_mix(v32, uT, 8 + hb, s, mu[:, 8 + hb:9 + hb], U, dtmp)
                for ch in range(4):
                    csl = slice(ch * 512, (ch + 1) * 512)
                    pb = pbig[ch % 3]
                    mk.mm(pb, w2a2[0:64, hsl], lr12[0:64, csl])
                    mk.act(lw[:, csl], pb, AF.Sigmoid, bias=w0c[:, hb:hb + 1])
                    pb = pbig[(ch + 1) % 3]
                    mk.mm(pb, w2a2[64:128, hsl], lr12[64:128, csl])
                    mk.act(a16[:, csl], pb, AF.Sigmoid, bias=a0c[:, hb:hb + 1])
                    pb = pbig[(ch + 2) % 3]
                    mk.mm(pb, g2b[:, hsl], sdg[:, csl])
                    mk.copy(gT[:, csl], pb, e="act")
                    if has_vres:
                        pb = pbig[ch % 3]
                        mk.mm(pb, v2b[:, hsl], t32b[:, csl])
                        mk.act(tA[:, csl], pb, AF.Sigmoid, bias=v0c[:, hb:hb + 1])
                mk.ts(lw, lw, -DECAY_C, None, op0=ALU.mult)
                if has_vres:
                    mk.dma(tB, V(vfirst.ap[hb * 128:(hb + 1) * 128, tsl], vfirst.bufs[hb]))
                    mk.tt(tB, tB, v32, ALU.subtract, e="pool")
                    mk.tt(tB, tB, tA, ALU.mult, e="pool")
                    mk.tt(v32, v32, tB, ALU.add, e="pool")
                else:
                    mk.dma(V(vfirst.ap[hb * 128:(hb + 1) * 128, tsl], vfirst.bufs[hb]), v32, q=DBG.get("st_q", "pool"))
                mk.ts(tA, k32, kkc[:, hb:hb + 1], None, op0=ALU.mult)
                mk.tt(tbf, tA, tA, ALU.mult, e="pool")
                for ch in range(4):
                    csl = slice(ch * 512, (ch + 1) * 512)
                    pb = pbig[ch % 3]
                    mk.mm(pb, blk, tbf[:, csl])
                    mk.act(tB[:, csl], pb, AF.Sqrt)
                mk.ts(tB, tB, 1e-12, None, op0=ALU.max)
                mk.recip(tB, tB)
                mk.tt(tA, tA, tB, ALU.mult)
                mk.ts(tB, a16, -1.0, kac[:, hb:hb + 1], op0=ALU.add, op1=ALU.mult)
                mk.ts(tB, tB, 1.0, None, op0=ALU.add)
                mk.tt(k32, k32, tB, ALU.mult)
                mk.tt(tB, r32, k32, ALU.mult, e="pool")
                mk.ts(tbf, tB, rkc[:, hb:hb + 1], None, op0=ALU.mult)
                for ch in range(4):
                    csl = slice(ch * 512, (ch + 1) * 512)
                    pb = pbig[ch % 3]
                    mk.mm(pb, blk, tbf[:, csl])
                    mk.tt(bon[:, csl], pb, v32[:, csl], ALU.mult)
                mk.copy(vT_, v32, e="act")
                mk.scan(cl, rmask, lw, 0.0)
                mk.act(tB, cl, AF.Exp)
                mk.copy(DL, tB.re("p (c l) -> p c l", l=LCH)[:, :, LCH - 1], e="pool")
                mk.tt(r_bd[0:64, 0, :], r32[0:64, :], tB[0:64, :], ALU.mult)
                mk.tt(r_bd[64:128, 1, :], r32[64:128, :], tB[64:128, :], ALU.mult, e="pool")
                mk.tt(cl, cl, lw, ALU.subtract, e="pool")
                mk.act(tB, cl, AF.Exp)
                mk.tt(tB, tB, tA, ALU.mult)
                mk.ts(aT_, tB, -1.0, None, op0=ALU.mult)
                mk.copy(a_bd[0:64, 0, :], aT_[0:64, :], e="pool")
                mk.copy(a_bd[64:128, 1, :], aT_[64:128, :], e="act")
                mk.tt(cl, cl, lw, ALU.add, e="pool")
                mk.act(tB, cl, AF.Exp, scale=-1.0)
                mk.tt(kT_, k32, tB, ALU.mult)
                mk.tt(tA, tA, a16, ALU.mult, e="pool")
                mk.tt(bT_, tA, tB, ALU.mult)
                mk.copy(b_bd[0:64, 0, :], bT_[0:64, :], e="pool")
                mk.copy(b_bd[64:128, 1, :], bT_[64:128, :], e="act")
                for p in range(16):
                    ptm = B_[5 + p % 2].bitcast(BF16)
                    psl = slice(p * 128, (p + 1) * 128)
                    for qi, src in enumerate((aT_, vT_, bT_, kT_)):
                        mk.transpose(ptm[:, qi * 128:(qi + 1) * 128], src[:, psl], self.identb)
                    mk.copy(TMav[:, p, :, :], ptm[:, 0:256].re("p (q c) -> p q c", q=2), e="act")
                    for c in range(2):
                        mk.copy(TMbk[64 * c:64 * c + 64, p, c, :, :],
                                ptm[64 * c:64 * c + 64, 256:512].re("p (q c) -> p q c", q=2), e=("dve" if c else "act"))
                mk.memset(H32, 0.0)
                mk.memset(Hb, 0.0)
                def local(p):
                    g = G[p % NGS]
                    t0 = p * 128
                    gsl = slice(t0, t0 + 128)
                    mk.mm(pN, aT_[:, gsl], b_bd[:, :, gsl])
                    mk.mm(pNT, bT_[:, gsl], a_bd[:, :, gsl])
                    mk.mm(pAk, kT_[:, gsl], a_bd[:, :, gsl])
                    mk.mm(pRb, bT_[:, gsl], r_bd[:, :, gsl])
                    mk.mm(pRk, kT_[:, gsl], r_bd[:, :, gsl])
                    yield
                    mk.tt(g["X"][0], v3(pN), mLs, ALU.mult)
                    mk.tt(g["XT"][0], v3(pNT), mUs, ALU.mult)
                    mk.tt(g["AakT"], v3(pAk), mUs, ALU.mult)
                    mk.tt(g["ArbT"], v3(pRb), mUi, ALU.mult)
                    mk.tt(g["ArkT"], v3(pRk), mUi, ALU.mult)
                    yield
                    for hd in range(2):
                        mk.mm(pZ2[:, 64 * hd:64 * hd + 64], g["AakT"][:, hd, :], TMav[:, p, 1, 64 * hd:64 * hd + 64])
                    Z = g["Z"]
                    mk.copy(Z[:, 0, :, :], TMav[:, p, 0, :].re("p (h j) -> p h j", h=2), e="pool")
                    yield
                    mk.copy(Z[:, 1, :, :], pZ2.re("p (h i) -> p h i", h=2), e="act")
                    yield
                    for lev in range(6):
                        X, XT = g["X"][lev % 2], g["XT"][lev % 2]
                        pz = pZa[lev % 2]
                        for hd in range(2):
                            mk.mm(pz[:, hd * 128:(hd + 1) * 128], XT[:, hd, :], Z[:, :, hd, :])
                        if lev < 5:
                            Xn, XTn = g["X"][(lev + 1) % 2], g["XT"][(lev + 1) % 2]
                            for hd in range(2):
                                mk.mm(pX[:, hd * 128:(hd + 1) * 128], XT[:, hd, :], X[:, hd, :])
                                mk.mm(pXT[:, hd * 128:(hd + 1) * 128], X[:, hd, :], XT[:, hd, :])
                        yield
                        if lev < 5:
                            mk.copy(Xn.re("p h c -> p (h c)"), pX, e="act")
                            mk.copy(XTn.re("p h c -> p (h c)"), pXT, e="act")
                        mk.tt(Z, Z, pz.re("p (h w c) -> p w h c", h=2, w=2), ALU.add)
                        yield
                    Wb_ = Z[:, 0, :, :].re("p h j -> p (h j)")
                    Ub_ = Z[:, 1, :, :].re("p h j -> p (h j)")
                    mk.mm(pT, Wb_, TMbk[:, p, :, 0, :])
                    for c in range(2):
                        mk.mm(pG[:, 128 * c:128 * c + 128], TMbk[:, p, c, 0, :], Ub_, start=True, stop=False)
                        mk.mm(pG[:, 128 * c:128 * c + 128], TMbk[:, p, c, 1, :], TMav[:, p, 1, :],
                              start=False, stop=True)
                    mk.mm(pQ, Wb_, g["ArbT"].re("p h t -> p (h t)"))
                    yield
                    for hd in range(2):
                        hs = slice(64 * hd, 64 * hd + 64)
                        mk.copy(g["T1T"][hs, :, hs], pT[hs, :].re("p (c h j) -> p c h j", c=2, h=2)[:, :, hd, :], e="act")
                        mk.copy(g["G1"][hs, :, hs], pG[hs, :].re("p (c h i) -> p c h i", c=2, h=2)[:, :, hd, :], e="act")
                        for c in range(2):
                            mk.tt(g["QT"][hs, c, 64 * c:64 * c + 64], pQ[hs, hd * 128 + 64 * c:hd * 128 + 64 * c + 64],
                                  r_bd[hs, hd, t0 + 64 * c:t0 + 64 * c + 64], ALU.add)

                def rec(p):
                    g = G[p % NGS]
                    t0 = p * 128
                    gsl = slice(t0, t0 + 128)
                    Z = g["Z"]
                    mk.mm(pY, zb, zb)
                    for hd in range(2):
                        hs = slice(64 * hd, 64 * hd + 64)
                        mk.mm(pY[:, hs], g["ArbT"][:, hd, :], Z[:, 1, hd, :], start=False, stop=False,
                              skip_group_check=True)
                        mk.mm(pY[:, hs], g["ArkT"][:, hd, :], TMav[:, p, 1, hs], start=False, stop=False,
                              skip_group_check=True)
                    for c in range(2):
                        ci = 2 * p + c
                        mk.mm(pY, g["QT"][:, c, :], Hb, start=False, stop=True, skip_group_check=True)
                        mk.mm(pHp, g["T1T"][:, c, :], Hb)
                        yield
                        mk.tt(ht1, pHp, H32, ALU.add)
                        mk.tt(ht1, ht1, g["G1"][:, c, :], ALU.add)
                        mk.ts(H32, ht1, DL[:, ci:ci + 1], None, op0=ALU.mult)
                        yield
                        mk.copy(Hb, H32, e="act")
                        yield
                    for hd in range(2):
                        ysl = pY[:, 64 * hd:64 * hd + 64]
                        mk.op("dve", lambda eng, o=g["st6"][:, hd, :], i_=ysl: eng.bn_stats(out=o.ap, in_=i_.ap),
                              reads=[ysl], writes=[g["st6"]])
                        mk.op("dve", lambda eng, o=g["mv"][:, hd, :], i_=g["st6"][:, hd, :]: eng.bn_aggr(out=o.ap, in_=i_.ap),
                              reads=[g["st6"]], writes=[g["mv"]])
                    yield
                    mk.act(g["rs"], g["mv"][:, :, 1], AF.Sqrt, bias=lneps)
                    yield
                    mk.recip(g["rs"], g["rs"])
                    for hd in range(2):
                        mk.ts(g["yn"][:, 64 * hd:64 * hd + 64], pY[:, 64 * hd:64 * hd + 64], g["mv"][:, hd, 0:1],
                              g["rs"][:, hd:hd + 1], op0=ALU.subtract, op1=ALU.mult)
                    yield
                    ptr = pTr.bitcast(BF16)
                    mk.transpose(ptr[:, 0:128], g["yn"], self.identb)
                    yield
                    mk.ts(g["ot"], ptr[:, 0:128], lngc[:, hb:hb + 1], lnbc[:, hb:hb + 1], op0=ALU.mult, op1=ALU.add)
                    mk.tt(g["ot"], g["ot"], bon[:, gsl], ALU.add, e="pool")
                    mk.tt(oacc[:, gsl], g["ot"], gT[:, gsl], ALU.mult, e="pool")

                def drive(gens):
                    gens = list(gens)
                    while gens:
                        for gg in list(gens):
                            try:
                                next(gg)
                            except StopIteration:
                                gens.remove(gg)

                NG = DBG.get("ngrp", 16)
                DEPTH = DBG.get("rw_depth", 1)
                started = {}

                def get_local(p):
                    if p not in started:
                        started[p] = [local(p), False]
                    return started[p]

                def step(ent):
                    if ent[1]:
                        return
                    try:
                        next(ent[0])
                    except StopIteration:
                        ent[1] = True

                if NG:
                    ent = get_local(0)
                    while not ent[1]:
                        step(ent)
                for p in range(NG):
                    r = [rec(p), False]
                    need = get_local(p + 1) if p + 1 < NG else None
                    extra = [get_local(p + k) for k in range(2, DEPTH + 1) if p + k < NG]
                    while not r[1] or (need is not None and not need[1]):
                        step(r)
                        if need is not None:
                            step(need)
                        for e_ in extra:
                            step(e_)
                mk.dma(V(oTd.ap[hb * 128:(hb + 1) * 128, tsl], oTd.bufs[hb]), oacc, q=DBG.get("st_q", "pool"))


Builder.rwkv = _rwkv


def _pool(self, uT, oT, P, layer):
    mk = self.mk
    ei = layer // 2
    with mk.scope():
        pwb = mk.sb([128, 4, 128], BF16, "pwb")
        mk.dma(pwb, P["pool_w"][ei].re("g c d -> c g d"), q="pool")
        psc = mk.sb([128, 4], F32, "psc")
        mk.dma(psc, P["pool_scale_col"][ei])
        A = [mk.sb([128, 16 + T], F32, "pA%d" % i) for i in range(2)]
        U0 = mk.sb([128, 16 + T], F32, "pU")
        for t_ in A + [U0]:
            mk.memset(t_[:, 0:16], 0.0)
        rcw = mk.sb([128, T], F32, "rcw")
        dT = mk.sb([128, T], BF16, "dT")
        tmp = mk.sb([128, T], F32, "ptmp")
        pob = [mk.sb([128, 512], BF16) for i in range(2)]
        for gi in range(4):
            win = 2 ** (gi + 1)
            mk.op("pool", lambda eng: eng.iota(rcw.ap, [[1, T]], base=1, channel_multiplier=0,
                                                allow_small_or_imprecise_dtypes=True), writes=[rcw])
            mk.ts(rcw, rcw, float(win), None, op0=ALU.min)
            mk.recip(rcw, rcw)
            for s in range(NSEQ if DBG.get("pool_stage", 9) >= 2 else 0):
                mk.dma(U0[:, 16:], V(uT.ap[(14 + gi) * 128:(15 + gi) * 128, s * T:(s + 1) * T], uT.bufs[14 + gi]))
                src = U0
                for lev in range(gi + 1):
                    sh = 2 ** lev
                    dst = A[lev % 2]
                    mk.tt(dst[:, 16:], src[:, 16:], src[:, 16 - sh:16 - sh + T], ALU.add, e=("pool" if lev % 2 else "dve"))
                    src = dst
                mk.tt(tmp, src[:, 16:], rcw, ALU.mult)
                mk.tt(dT, tmp, U0[:, 16:], ALU.subtract, e="pool")
                for ch in range(4 if DBG.get("pool_stage", 9) >= 3 else 0):
                    pb = self.bank[ch % 2]
                    mk.mm(pb, pwb[:, gi, :], dT[:, ch * 512:(ch + 1) * 512])
                    ob_ = pob[ch % 2]
                    if DBG.get("pool_stage", 9) >= 4:
                        mk.ts(ob_, pb, psc[:, gi:gi + 1], None, op0=ALU.mult)
                    if DBG.get("pool_stage", 9) >= 5:
                        mk.dma(V(oT.ap[(4 + gi) * 128:(5 + gi) * 128, s * T + ch * 512:s * T + (ch + 1) * 512],
                                 oT.bufs[4 + gi]), ob_, q=DBG.get("pool_q", "pool"))


Builder.pool = _pool


def _even_layer(self, xres_in, xres_out, layer, P, vfirst):
    mk = self.mk
    ei = layer // 2
    uT = self.scratch("uTe", [2304, NTOK], F32)
    self.proj_in(xres_in, P["norm_mix_g"][layer:layer + 1, :], P["even_w_in"][ei], 2304,
                 list(range(18)), uT, None, None)
    oT = self.scratch("oTd", [D, NTOK], BF16)
    if not DBG.get("no_rwkv"):
        self.rwkv(uT, oT, P, layer, vfirst)
    if not DBG.get("no_pool"):
        self.pool(uT, oT, P, layer)
    self.proj_out(xres_in, xres_out, oT, P["even_w_out"][ei])


Builder.even_layer = _even_layer


def host_even_params(inputs):
    f = lambda a: np.ascontiguousarray(a, dtype=np.float32)
    out = {}
    out["norm_mix_g"] = f(inputs["norm_mix_g"])
    out["even_w_in"] = f(inputs["even_w_in"])
    out["even_w_out"] = f(inputs["even_w_out"])
    n_even = inputs["even_w_in"].shape[0]
    out["rw_mu_col"] = f(np.transpose(inputs["rw_mu"].reshape(n_even, 14, 128), (0, 2, 1)))
    colp = [inputs["rw_w0"], inputs["rw_a0"], inputs["rw_k_k"], inputs["rw_k_a"],
            inputs["rw_r_k"].reshape(n_even, 512), inputs["rw_ln_g"], inputs["rw_ln_b"]]
    out["rw_cols"] = f(np.stack([np.transpose(c.reshape(n_even, 4, 128), (0, 2, 1)) for c in colp], axis=2))
    out["rw_w2a2"] = f(np.concatenate([inputs["rw_w2"], inputs["rw_a2"]], axis=1))
    out["rw_g2"] = f(inputs["rw_g2"])
    nv = inputs["rw_v0"].shape[0]
    out["rw_v0col"] = f(np.transpose(inputs["rw_v0"].reshape(nv, 4, 128), (0, 2, 1)))
    out["rw_v1"] = f(inputs["rw_v1"])
    out["rw_v2"] = f(inputs["rw_v2"])
    out["pool_w"] = f(inputs["pool_w"])
    out["pool_scale_col"] = f(np.transpose(inputs["pool_scale"].reshape(n_even, 4, 128), (0, 2, 1)))
    return out


def host_consts2():
    c = host_consts()
    blk = np.kron(np.eye(2, dtype=np.float32), np.ones((64, 64), np.float32))
    lo = np.tril(np.ones((64, 64), np.float32), -1)
    e2 = np.eye(2, dtype=np.float32)
    mLs = np.kron(e2, lo)
    mUs = np.kron(e2, lo.T)
    mUi = np.kron(e2, np.triu(np.ones((64, 64), np.float32)))
    c["c_blk"] = blk
    c["c_masks"] = np.stack([mLs, mUs, mUi]).astype(np.float32)
    return c


def mk_check(mk):
    val = {}
    pos = {e: 0 for e in ENGS}
    total = sum(len(mk.ops[e]) for e in ENGS)
    done = 0
    while done < total:
        prog = False
        for e in ENGS:
            lst = mk.ops[e]
            while pos[e] < len(lst):
                waits, fn, inc = lst[pos[e]]
                if all(val.get(id(s), 0) >= v for s, v in waits):
                    val[id(inc[0])] = val.get(id(inc[0]), 0) + inc[1]
                    pos[e] += 1
                    done += 1
                    prog = True
                else:
                    break
        if not prog:
            names = {id(v): k for k, v in mk.semobj.items()}
            for e in ENGS:
                if pos[e] < len(mk.ops[e]):
                    waits, fn, inc = mk.ops[e][pos[e]]
                    print("STUCK", e, pos[e], [(names.get(id(s)), v, val.get(id(s), 0)) for s, v in waits])
            return False
    return True


_PROG = {}


def host_params(inputs):
    hp = {}
    hp.update(host_even_params(inputs))
    hp.update(host_odd_params(inputs))
    hp.update(host_moe_params(inputs))
    hp.update(host_consts2())
    return hp


def build_program(shapes, n_layers=4):
    nc = bass.Bass("TRN2", target_bir_lowering=False)
    with ExitStack() as st:
        B = Builder(nc, st)
        mk = B.mk
        x_in = DR(mk, "x_in", [NTOK, D], F32, kind="ExternalInput")
        y = DR(mk, "y", [NTOK, D], F32, kind="ExternalOutput")
        P = {}
        for k, shp in shapes.items():
            if k in B.inp:
                continue
            P[k] = B.ext_in(k, list(shp))
        vf = B.scratch("vfirst", [512, NTOK], F32)
        for layer in range(n_layers):
            xin = x_in if layer == 0 else y
            if layer % 2 == 0:
                B.even_layer(xin, y, layer, P, vf)
            else:
                B.odd_layer(xin, y, layer, P)
            B.moe(y, layer, P)
        mk.emit()
    return nc


def kernel(**inputs):
    inputs = {k: np.asarray(v) for k, v in inputs.items()}
    hp = host_params(inputs)
    key = "full"
    if key not in _PROG:
        _PROG[key] = build_program({k: v.shape for k, v in hp.items()})
    nc = _PROG[key]
    x = np.ascontiguousarray(inputs["x"], dtype=np.float32)
    nb = x.shape[0]
    n_cores = 8
    per = nb // n_cores
    in_maps = []
    for c in range(n_cores):
        m = dict(hp)
        m["x_in"] = np.ascontiguousarray(x[c * per:(c + 1) * per].reshape(NTOK, D))
        in_maps.append(m)
    res = run_bass_kernel_spmd(nc, in_maps, core_ids=list(range(n_cores)))
    out = np.stack([np.asarray(r["y"], dtype=np.float32).reshape(per, T, D) for r in res.results], axis=0)
    return out.reshape(nb, T, D)
```

```python
import numpy as np
from contextlib import ExitStack
import concourse.bass as bass
import concourse.mybir as mybir
from concourse.bass_utils import run_bass_kernel_spmd

F32 = mybir.dt.float32
BF16 = mybir.dt.bfloat16
I32 = mybir.dt.int32
U32 = mybir.dt.uint32
AF = mybir.ActivationFunctionType
ALU = mybir.AluOpType
AX = mybir.AxisListType

SAME_ENGINE_SYNC = True
EPOCH = 20000
DBG = {}


class Buf:
    __slots__ = ("w", "r", "name")

    def __init__(self, name=""):
        self.w = None
        self.r = {}
        self.name = name


class V:
    __slots__ = ("ap", "buf")

    def __init__(self, ap, buf):
        self.ap = ap
        self.buf = buf

    def __getitem__(self, k):
        return V(self.ap[k], self.buf)

    def re(self, s, **kw):
        return V(self.ap.rearrange(s, **kw), self.buf)

    def bc(self, shape):
        return V(self.ap.to_broadcast(list(shape)), self.buf)

    def pbc(self, n):
        return V(self.ap.partition_broadcast(n), self.buf)

    def bitcast(self, dt):
        return V(self.ap.bitcast(dt), self.buf)

    def sub(self, buf):
        return V(self.ap, buf)

    @property
    def shape(self):
        return self.ap.shape


ENGS = ("pe", "act", "dve", "pool", "sp")


class MK:
    def __init__(self, nc, stack, n_dma_slots=8):
        self.nc = nc
        self.stack = stack
        self.ops = {e: [] for e in ENGS}
        self.root = stack
        self.esem = {}
        self.cnt = {e: 0 for e in ENGS}
        self.seen = {e: {} for e in ENGS}
        self.dma_slots = {}
        self.dma_n = {}
        for q in ("sp", "pool", "act"):
            self.dma_slots[q] = [stack.enter_context(nc.semaphore("dma_%s_%d" % (q, i)))
                                 for i in range(n_dma_slots)]
            self.dma_n[q] = 0
        self.semobj = {}
        self.n_ops = 0
        self.uid = 0

    def sb(self, shape, dtype, name=None):
        self.uid += 1
        name = name or "t%d" % self.uid
        t = self.stack.enter_context(self.nc.sbuf_tensor(name + "_%d" % self.uid, list(shape), dtype))
        return V(t[:], Buf(name))

    def ps(self, shape, dtype=F32, name=None):
        self.uid += 1
        name = name or "p%d" % self.uid
        t = self.stack.enter_context(self.nc.psum_tensor(name + "_%d" % self.uid, list(shape), dtype))
        return V(t[:], Buf(name))

    def dram(self, name, shape, dtype, kind="Internal"):
        t = self.nc.dram_tensor(name, list(shape), dtype, kind=kind)
        return V(t.ap(), Buf(name))

    def _tok_need(self, e, tok, waits):
        if tok is None:
            return
        semkey, val, src, is_dma = tok
        if src == e and not is_dma:
            if e == "pe" or not SAME_ENGINE_SYNC:
                return
        if self.seen[e].get(semkey, 0) >= val:
            return
        if waits.get(semkey, 0) < val:
            waits[semkey] = val

    def op(self, e, fn, reads=(), writes=(), dma=False):
        waits = {}
        pend = getattr(self, "pending", {}).pop(e, None)
        if pend:
            waits.update(pend)
        rb = [v.buf for v in reads if v is not None]
        wb = [v.buf for v in writes if v is not None]
        for b in rb:
            self._tok_need(e, b.w, waits)
        for b in wb:
            self._tok_need(e, b.w, waits)
            for t in b.r.values():
                self._tok_need(e, t, waits)
        if dma:
            slots = self.dma_slots[e]
            n = self.dma_n[e]
            self.dma_n[e] = n + 1
            sem = slots[n % len(slots)]
            val = 16 * (n // len(slots) + 1)
            semkey = ("d", e, n % len(slots))
            self.semobj[semkey] = sem
            if val > 16:
                if self.seen[e].get(semkey, 0) < val - 16 and waits.get(semkey, 0) < val - 16:
                    waits[semkey] = val - 16
            tok = (semkey, val, e, True)
            inc = (sem, 16)
        else:
            self.cnt[e] += 1
            ep = (self.cnt[e] - 1) // EPOCH
            semkey = ("c", e, ep)
            if semkey not in self.esem:
                self.esem[semkey] = self.root.enter_context(self.nc.semaphore("sem_%s_%d" % (e, ep)))
            self.semobj[semkey] = self.esem[semkey]
            tok = (semkey, (self.cnt[e] - 1) % EPOCH + 1, e, False)
            inc = (self.esem[semkey], 1)
        for k, v in waits.items():
            self.seen[e][k] = v
        for b in wb:
            b.w = tok
            b.r = {}
        for b in rb:
            old = b.r.get(tok[0])
            if old is None or old[1] < tok[1]:
                b.r[tok[0]] = tok
        self.ops[e].append(([(self.semobj[k], v) for k, v in waits.items()], fn, inc))
        self.n_ops += 1
        return tok

    def emit(self):
        nc = self.nc
        fin = []
        for q in ("sp", "pool", "act"):
            n = self.dma_n[q]
            slots = self.dma_slots[q]
            for i in range(min(n, len(slots))):
                uses = (n - i + len(slots) - 1) // len(slots)
                fin.append((slots[i], 16 * uses))
        for e in ("pe", "act", "dve", "pool"):
            if self.cnt[e]:
                ep = (self.cnt[e] - 1) // EPOCH
                fin.append((self.esem[("c", e, ep)], (self.cnt[e] - 1) % EPOCH + 1))
        with nc.Block() as block:
            def run(eng, lst, final=None):
                for waits, fn, inc in lst:
                    for s, v in waits:
                        eng.wait_ge(s, v)
                    fn(eng).then_inc(inc[0], inc[1])
                if final:
                    for s, v in final:
                        eng.wait_ge(s, v)

            @block.tensor
            def _(eng):
                run(eng, self.ops["pe"])

            @block.scalar
            def _(eng):
                run(eng, self.ops["act"])

            @block.vector
            def _(eng):
                run(eng, self.ops["dve"])

            @block.gpsimd
            def _(eng):
                run(eng, self.ops["pool"])

            @block.sync
            def _(eng):
                run(eng, self.ops["sp"], fin)

    def dma(self, out, in_, q="sp", **kw):
        return self.op(q, lambda eng: eng.dma_start(out=out.ap, in_=in_.ap, **kw),
                       reads=[in_], writes=[out], dma=True)

    def mm(self, out, lhsT, rhs, start=True, stop=True, **kw):
        return self.op("pe", lambda eng: eng.matmul(out.ap, lhsT.ap, rhs.ap, start=start, stop=stop, **kw),
                       reads=[lhsT, rhs], writes=[out])

    def transpose(self, out, in_, ident):
        return self.op("pe", lambda eng: eng.transpose(out.ap, in_.ap, ident.ap),
                       reads=[in_, ident], writes=[out])

    def act(self, out, in_, func, bias=None, scale=1.0, accum_out=None, e="act"):
        reads = [in_]
        kw = {}
        if isinstance(bias, V):
            reads.append(bias)
            kw["bias"] = bias.ap
        elif bias is not None:
            kw["bias"] = bias
        if isinstance(scale, V):
            reads.append(scale)
            kw["scale"] = scale.ap
        else:
            kw["scale"] = scale
        writes = [out]
        if accum_out is not None:
            writes.append(accum_out)
            kw["accum_out"] = accum_out.ap
        return self.op(e, lambda eng: eng.activation(out=out.ap, in_=in_.ap, func=func, **kw),
                       reads=reads, writes=writes)

    def tt(self, out, in0, in1, op, e="dve"):
        return self.op(e, lambda eng: eng.tensor_tensor(out=out.ap, in0=in0.ap, in1=in1.ap, op=op),
                       reads=[in0, in1], writes=[out])

    def ts(self, out, in0, s1, s2=None, op0=ALU.mult, op1=None, accum_out=None, e="dve"):
        reads = [in0]
        a1 = s1
        if isinstance(s1, V):
            reads.append(s1)
            a1 = s1.ap
        a2 = s2
        if isinstance(s2, V):
            reads.append(s2)
            a2 = s2.ap
        kw = {}
        if op1 is not None:
            kw["op1"] = op1
        writes = [out]
        if accum_out is not None:
            writes.append(accum_out)
            kw["accum_out"] = accum_out.ap
        return self.op(e, lambda eng: eng.tensor_scalar(out=out.ap, in0=in0.ap, scalar1=a1, scalar2=a2,
                                                        op0=op0, **kw),
                       reads=reads, writes=writes)

    def stt(self, out, in0, scalar, in1, op0, op1, e="dve"):
        reads = [in0, in1]
        a = scalar
        if isinstance(scalar, V):
            reads.append(scalar)
            a = scalar.ap
        return self.op(e, lambda eng: eng.scalar_tensor_tensor(out=out.ap, in0=in0.ap, scalar=a, in1=in1.ap,
                                                               op0=op0, op1=op1),
                       reads=reads, writes=[out])

    def copy(self, out, in_, e="dve"):
        if e == "act":
            return self.op(e, lambda eng: eng.copy(out=out.ap, in_=in_.ap), reads=[in_], writes=[out])
        return self.op(e, lambda eng: eng.tensor_copy(out=out.ap, in_=in_.ap), reads=[in_], writes=[out])

    def memset(self, out, val, e="pool"):
        return self.op(e, lambda eng: eng.memset(out.ap, val), writes=[out])

    def reduce(self, out, in_, op=ALU.add, axis=AX.X, e="dve"):
        return self.op(e, lambda eng: eng.tensor_reduce(out=out.ap, in_=in_.ap, axis=axis, op=op),
                       reads=[in_], writes=[out])

    def recip(self, out, in_):
        return self.op("dve", lambda eng: eng.reciprocal(out=out.ap, in_=in_.ap), reads=[in_], writes=[out])

    def scan(self, out, d0, d1, init, op0=ALU.mult, op1=ALU.add):
        reads = [d0, d1]
        a = init
        if isinstance(init, V):
            reads.append(init)
            a = init.ap
        return self.op("dve", lambda eng: eng.tensor_tensor_scan(out=out.ap, data0=d0.ap, data1=d1.ap,
                                                                 initial=a, op0=op0, op1=op1),
                       reads=reads, writes=[out])

    def barrier(self):
        fin = {}
        for q in ("sp", "pool", "act"):
            n = self.dma_n[q]
            slots = self.dma_slots[q]
            for i in range(min(n, len(slots))):
                uses = (n - i + len(slots) - 1) // len(slots)
                fin[("d", q, i)] = 16 * uses
        for e in ("pe", "act", "dve", "pool"):
            if self.cnt[e]:
                ep = (self.cnt[e] - 1) // EPOCH
                fin[("c", e, ep)] = (self.cnt[e] - 1) % EPOCH + 1
        for e in ENGS:
            waits = {}
            for k, v in fin.items():
                if k[0] == "c" and k[1] == e:
                    continue
                if self.seen[e].get(k, 0) < v:
                    waits[k] = v
                    self.seen[e][k] = v
            if waits:
                if e == "sp":
                    continue_fn = None
                self.pending = getattr(self, "pending", {})
                self.pending.setdefault(e, {}).update(waits)

    def scope(self):
        return _Scope(self)


class _Scope:
    def __init__(self, mk):
        self.mk = mk

    def __enter__(self):
        self.old = self.mk.stack
        self.st = ExitStack()
        self.mk.stack = self.st
        return self

    def __exit__(self, *a):
        self.mk.barrier()
        self.mk.stack = self.old
        self.st.close()
        return False


D = 1024
T = 2048
NSEQ = 2
NTOK = NSEQ * T
NT = NTOK // 128
CAP = 512
NE = 32
NSLOT = NE * CAP
NROW_TL = NSLOT + 128
RMS_EPS = 1e-6


class DR:
    def __init__(self, mk, name, shape, dtype, kind="Internal", rows_per=128):
        self.t = mk.nc.dram_tensor(name, list(shape), dtype, kind=kind)
        self.ap = self.t.ap()
        self.rows_per = rows_per
        n = (shape[0] + rows_per - 1) // rows_per
        self.bufs = [Buf("%s_%d" % (name, i)) for i in range(n)]
        self.whole = Buf(name)

    def rows(self, r0, n):
        assert r0 % self.rows_per == 0 and n <= self.rows_per
        return V(self.ap[r0:r0 + n], self.bufs[r0 // self.rows_per])

    def all(self):
        return [V(self.ap, b) for b in self.bufs]


class Builder:
    def __init__(self, nc, stack):
        self.nc = nc
        self.mk = MK(nc, stack)
        mk = self.mk
        self.inp = {}
        self.c_ident = self.ext_in("c_ident", [128, 128], F32)
        self.c_tri = self.ext_in("c_tri", [128, 128], F32)
        self.ext_in("c_blk", [128, 128], F32)
        self.ext_in("c_masks", [3, 128, 128], F32)
        self.ident32 = mk.sb([128, 128], F32, "ident32")
        self.identb = mk.sb([128, 128], BF16, "identb")
        self.trib = mk.sb([128, 128], BF16, "trib")
        self.onesb = mk.sb([128, 128], BF16, "onesb")
        mk.dma(self.ident32, self.c_ident)
        mk.dma(self.identb, self.c_ident, q="pool")
        mk.dma(self.trib, self.c_tri, q="pool")
        mk.memset(self.onesb, 1.0)
        self.eps_t = mk.sb([128, 1], F32, "eps_t")
        mk.memset(self.eps_t, RMS_EPS)
        self.psum_all = mk.ps([128, 4096], F32, "psum_all")
        self.bank = [V(self.psum_all.ap[:, i * 512:(i + 1) * 512], Buf("bank%d" % i)) for i in range(8)]

    def scratch(self, name, shape, dtype, rows_per=128):
        if not hasattr(self, "_scr"):
            self._scr = {}
        if name not in self._scr:
            self._scr[name] = DR(self.mk, name, shape, dtype, rows_per=rows_per)
        return self._scr[name]

    def ext_in(self, name, shape, dtype=F32):
        v = self.mk.dram(name, shape, dtype, kind="ExternalInput")
        self.inp[name] = v
        return v

    def rms_tile(self, xt, gbc, h32, eps=RMS_EPS, sq=None, small=None):
        mk = self.mk
        ss, rstd = small
        mk.act(sq, xt, AF.Square, accum_out=ss)
        mk.act(rstd, ss, AF.Sqrt, scale=1.0 / D, bias=self.eps_t)
        mk.recip(rstd, rstd)
        mk.stt(h32, xt, rstd, gbc, ALU.mult, ALU.mult)

    def moe(self, xres, layer, P):
        mk = self.mk
        with mk.scope():
            Hrows = self.scratch("hrows", [NTOK + 128, D], BF16)
            Ybuf = self.scratch("ybuf", [NROW_TL, D], F32)
            TokL = self.scratch("tokl", [NROW_TL, 8], I32, rows_per=NROW_TL)
            gbc = mk.sb([128, D], F32, "gbc")
            mk.dma(gbc, P["norm_ffn_g"][layer:layer + 1, :].pbc(128))
            wr = mk.sb([128, 8, 36], F32, "wr")
            mk.dma(wr, P["w_router"][layer].re("(k p) n -> p k n", p=128))
            brt = mk.sb([128, 36], F32, "brt")
            mk.dma(brt, P["b_router"][layer:layer + 1, :].pbc(128))
            maskall = mk.sb([128, NT, 32], BF16, "maskall")
            E1all = mk.sb([128, NT, 32], F32, "E1all")
            E2all = mk.sb([128, NT, 32], F32, "E2all")
            gate1 = mk.sb([128, NT], F32, "gate1")
            gate2 = mk.sb([128, NT], F32, "gate2")
            slot1 = mk.sb([128, NT], I32, "slot1")
            slot2 = mk.sb([128, NT], I32, "slot2")
            base = mk.sb([128, 32], F32, "base")
            lim = mk.sb([128, 32], F32, "lim")
            trash = mk.sb([128, 1], F32, "trash")
            zero_t = mk.sb([128, D], F32, "zero_t")
            sent = mk.sb([128, (NROW_TL // 128) * 8], I32, "sent")
            mk.op("pool", lambda eng: eng.iota(base.ap, [[CAP, 32]], base=-1, channel_multiplier=0,
                                                allow_small_or_imprecise_dtypes=True), writes=[base])
            mk.ts(lim, base, float(CAP) + 0.5, None, op0=ALU.add)
            mk.op("pool", lambda eng: eng.iota(trash.ap, [[0, 1]], base=NSLOT, channel_multiplier=1,
                                                allow_small_or_imprecise_dtypes=True), writes=[trash])
            mk.memset(zero_t, 0.0)
            mk.op("pool", lambda eng: eng.iota(sent.ap, [[0, (NROW_TL // 128) * 8]], base=NTOK,
                                                channel_multiplier=0), writes=[sent])
            mk.dma(V(TokL.ap.rearrange("(p r) c -> p (r c)", p=128), TokL.bufs[0]), sent)
            mk.dma(Hrows.rows(NTOK, 128), zero_t.bitcast(BF16)[:, 0:D])
            mk.dma(Ybuf.rows(NSLOT, 128), zero_t)

            xts = [mk.sb([128, D], F32, "xt%d" % i) for i in range(2)]
            sqs = [mk.sb([128, D], F32, "sq%d" % i) for i in range(2)]
            h32s = [mk.sb([128, D], F32, "h32%d" % i) for i in range(2)]
            hbs = [mk.sb([128, D], BF16, "hb%d" % i) for i in range(2)]
            hT32s = [mk.sb([128, 8, 128], F32, "hT32%d" % i) for i in range(2)]
            smalls = [(mk.sb([128, 1], F32), mk.sb([128, 1], F32)) for i in range(2)]
            lg_all = mk.sb([128, NT, 36], F32, "lg_all")
            for i in range(NT):
                b = i % 2
                xt, sq, h32, hb, hT32 = xts[b], sqs[b], h32s[b], hbs[b], hT32s[b]
                mk.dma(xt, xres.rows(i * 128, 128))
                self.rms_tile(xt, gbc, h32, sq=sq, small=smalls[b])
                mk.copy(hb, h32, e="pool")
                mk.dma(Hrows.rows(i * 128, 128), hb)
                for half in range(2):
                    pb = self.bank[2 * b + half]
                    for kk in range(4):
                        k = half * 4 + kk
                        mk.transpose(pb[:, kk * 128:(kk + 1) * 128], h32[:, k * 128:(k + 1) * 128], self.ident32)
                    mk.copy(hT32[:, half * 4:(half + 1) * 4, :].re("p k t -> p (k t)"), pb, e="act")
                pl = self.bank[4 + b]
                for k in range(8):
                    mk.mm(pl[:, 0:36], hT32[:, k, :], wr[:, k, :], start=(k == 0), stop=(k == 7))
                mk.tt(lg_all[:, i, :], pl[:, 0:36], brt, ALU.add)

            def bc(v, axis, shape):
                return V(v.ap.unsqueeze(axis).to_broadcast(list(shape)), v.buf)
            gl = lg_all[:, :, 0:4]
            el4 = lg_all[:, :, 4:36].re("p t (g e) -> p t g e", g=4)
            gmax = mk.sb([128, NT], F32)
            ohg = mk.sb([128, NT, 4], F32)
            eg = mk.sb([128, NT, 4], F32)
            gsum = mk.sb([128, NT], F32)
            tmp4 = mk.sb([128, NT, 4, 8], F32)
            sel = mk.sb([128, NT, 8], F32)
            sel2 = mk.sb([128, NT, 8], F32)
            m1 = mk.sb([128, NT], F32)
            m2 = mk.sb([128, NT], F32)
            oh1 = mk.sb([128, NT, 8], F32)
            oh2 = mk.sb([128, NT, 8], F32)
            mk.reduce(gmax, gl, op=ALU.max)
            mk.tt(ohg, gl, bc(gmax, 2, [128, NT, 4]), ALU.is_equal)
            mk.tt(eg, gl, bc(gmax, 2, [128, NT, 4]), ALU.subtract)
            mk.act(eg, eg, AF.Exp)
            mk.reduce(gsum, eg, op=ALU.add)
            mk.recip(gsum, gsum)
            mk.tt(tmp4, el4, bc(ohg, 3, [128, NT, 4, 8]), ALU.mult)
            mk.reduce(sel, tmp4.re("p t g e -> p t e g"), op=ALU.add)
            mk.reduce(m1, sel, op=ALU.max)
            mk.tt(oh1, sel, bc(m1, 2, [128, NT, 8]), ALU.is_equal)
            mk.stt(sel2, oh1, -1e30, sel, ALU.mult, ALU.add)
            mk.reduce(m2, sel2, op=ALU.max)
            mk.tt(oh2, sel2, bc(m2, 2, [128, NT, 8]), ALU.is_equal)
            mk.tt(m2, m2, m1, ALU.subtract)
            mk.act(m2, m2, AF.Exp)
            mk.ts(m2, m2, 1.0, None, op0=ALU.add)
            mk.recip(m2, m2)
            mk.tt(gate1, gsum, m2, ALU.mult)
            mk.tt(gate2, gsum, gate1, ALU.subtract)
            mk.tt(E1all.re("p t (g e) -> p t g e", g=4), bc(ohg, 3, [128, NT, 4, 8]), bc(oh1, 2, [128, NT, 4, 8]), ALU.mult)
            mk.tt(E2all.re("p t (g e) -> p t g e", g=4), bc(ohg, 3, [128, NT, 4, 8]), bc(oh2, 2, [128, NT, 4, 8]), ALU.mult)
            mk.tt(maskall, E1all, E2all, ALU.add)

            pos_ps = V(self.psum_all.ap[:, 0:1024], self.bank[0].buf)
            for i in range(NT):
                reg_ = V(self.psum_all.ap[:, i * 32:(i + 1) * 32], self.bank[i // 16].buf)
                for j in range(i):
                    mk.mm(reg_, self.onesb, maskall[:, j, :], start=(j == 0), stop=False)
                mk.mm(reg_, self.trib, maskall[:, i, :], start=(i == 0), stop=True)
            posf = mk.sb([128, NT, 32], F32, "posf")
            okm = mk.sb([128, NT, 32], F32, "okm")
            slf = mk.sb([128, NT], F32, "slf")
            mk.op("dve", lambda eng: eng.tensor_tensor(out=posf.ap, in0=pos_ps.ap.rearrange("p (t e) -> p t e", e=32),
                                                       in1=base.ap.unsqueeze(1).to_broadcast([128, NT, 32]), op=ALU.add),
                  reads=[self.bank[0], self.bank[1], base], writes=[posf])
            mk.tt(okm, posf, bc(lim, 1, [128, NT, 32]), ALU.is_lt)
            mk.ts(posf, posf, trash, None, op0=ALU.subtract)
            mk.tt(posf, posf, okm, ALU.mult)
            mk.ts(posf, posf, trash, None, op0=ALU.add)
            for Eall, slot in ((E1all, slot1), (E2all, slot2)):
                mk.tt(okm, posf, Eall, ALU.mult)
                mk.reduce(slf, okm, op=ALU.add)
                mk.copy(slot, slf)
            tokid = [mk.sb([128, 8], I32) for i in range(4)]
            sc_bufs = []
            init_v = V(TokL.ap, TokL.bufs[0])
            for i in range(NT):
                t_ = tokid[i % 4]
                mk.op("pool", lambda eng, t=t_, i=i: eng.iota(t.ap, [[0, 8]], base=i * 128,
                                                              channel_multiplier=1), writes=[t_])
                for slot in (slot1, slot2):
                    bf = Buf("tokl_sc")
                    sc_bufs.append(bf)
                    mk.op("pool", lambda eng, slot=slot, i=i, t=t_: eng.indirect_dma_start(
                        out=TokL.ap, out_offset=bass.IndirectOffsetOnAxis(ap=slot.ap[:, i:i + 1], axis=0),
                        in_=t.ap, in_offset=None),
                        reads=[slot, t_, init_v], writes=[V(TokL.ap, bf)], dma=True)
            tokl_all = [V(TokL.ap, bf) for bf in sc_bufs] + [init_v]

            NCT = CAP // 128
            wg = [mk.sb([128, 8, 512], BF16, "wg%d" % i) for i in range(2)]
            wu = [mk.sb([128, 8, 512], BF16, "wu%d" % i) for i in range(2)]
            wd = [mk.sb([128, 4, D], BF16, "wd%d" % i) for i in range(2)]
            idx = [mk.sb([128, 8], I32) for i in range(4)]
            xg = [mk.sb([128, D], BF16) for i in range(4)]
            xgT = [mk.sb([128, 8, CAP], BF16) for i in range(2)]
            hidT = [mk.sb([128, 4, CAP], BF16) for i in range(2)]
            sil = [mk.sb([128, CAP], F32) for i in range(2)]
            yrow = [mk.sb([128, D], F32) for i in range(2)]
            nslot = 0
            ny = 0
            for e in range(NE):
                b = e % 2
                mk.dma(wg[b], P["moe_w_gate"][layer, e].re("(k p) n -> p k n", p=128), q="pool")
                mk.dma(wu[b], P["moe_w_up"][layer, e].re("(k p) n -> p k n", p=128), q="pool")
                mk.dma(wd[b], P["moe_w_down"][layer, e].re("(k p) n -> p k n", p=128), q="pool")
                for j in range(NCT):
                    s = nslot % 4
                    nslot += 1
                    r0 = e * CAP + j * 128
                    mk.op("sp", lambda eng, s=s, r0=r0: eng.dma_start(out=idx[s].ap, in_=TokL.ap[r0:r0 + 128, :]),
                          reads=tokl_all, writes=[idx[s]], dma=True)
                    mk.op("pool", lambda eng, s=s: eng.indirect_dma_start(
                        out=xg[s].ap, out_offset=None, in_=Hrows.ap,
                        in_offset=bass.IndirectOffsetOnAxis(ap=idx[s].ap[:, 0:1], axis=0)),
                        reads=[idx[s]] + Hrows.all(), writes=[xg[s]], dma=True)
                    pb = self.bank[j % 2]
                    pbb = pb.bitcast(BF16)
                    for k in range(8):
                        mk.transpose(pbb[:, k * 128:(k + 1) * 128], xg[s][:, k * 128:(k + 1) * 128], self.identb)
                    mk.copy(xgT[b][:, :, j * 128:(j + 1) * 128], pbb.re("p (k t) -> p k t", k=8),
                            e=("act" if j % 2 else "dve"))
                for c in range(4):
                    pg = self.bank[2 + (c % 2)]
                    pu = self.bank[4 + (c % 2)]
                    for k in range(8):
                        mk.mm(pg, wg[b][:, k, c * 128:(c + 1) * 128], xgT[b][:, k, :], start=(k == 0), stop=(k == 7))
                    for k in range(8):
                        mk.mm(pu, wu[b][:, k, c * 128:(c + 1) * 128], xgT[b][:, k, :], start=(k == 0), stop=(k == 7))
                    mk.act(sil[c % 2], pg, AF.Silu)
                    mk.tt(hidT[b][:, c, :], sil[c % 2], pu, ALU.mult)
                for j in range(NCT):
                    yb = ny % 2
                    ny += 1
                    for half in range(2):
                        pd = self.bank[6 + half]
                        for c in range(4):
                            mk.mm(pd, hidT[b][:, c, j * 128:(j + 1) * 128], wd[b][:, c, half * 512:(half + 1) * 512],
                                  start=(c == 0), stop=(c == 3))
                        mk.copy(yrow[yb][:, half * 512:(half + 1) * 512], pd, e=("act" if half else "dve"))
                    mk.dma(Ybuf.rows(e * CAP + j * 128, 128), yrow[yb])

            y1 = [mk.sb([128, D], F32) for i in range(3)]
            y2 = [mk.sb([128, D], F32) for i in range(3)]
            xt3 = xts + [mk.sb([128, D], F32)]
            for i in range(NT):
                b = i % 3
                xt = xt3[b]
                mk.dma(xt, xres.rows(i * 128, 128))
                for slot, yy in ((slot1, y1[b]), (slot2, y2[b])):
                    mk.op("pool", lambda eng, slot=slot, yy=yy, i=i: eng.indirect_dma_start(
                        out=yy.ap, out_offset=None, in_=Ybuf.ap,
                        in_offset=bass.IndirectOffsetOnAxis(ap=slot.ap[:, i:i + 1], axis=0)),
                        reads=[slot] + Ybuf.all(), writes=[yy], dma=True)
                mk.stt(xt, y1[b], gate1[:, i:i + 1], xt, ALU.mult, ALU.add)
                mk.stt(xt, y2[b], gate2[:, i:i + 1], xt, ALU.mult, ALU.add)
                mk.dma(xres.rows(i * 128, 128), xt)


def host_consts():
    ident = np.eye(128, dtype=np.float32)
    tri = np.triu(np.ones((128, 128), np.float32))
    return {"c_ident": ident, "c_tri": tri}


MOE_KEYS = ("norm_ffn_g", "w_router", "b_router", "moe_w_gate", "moe_w_up", "moe_w_down")


def host_moe_params(inputs):
    out = {}
    out["norm_ffn_g"] = np.ascontiguousarray(inputs["norm_ffn_g"], dtype=np.float32)
    out["w_router"] = np.ascontiguousarray(
        np.concatenate([inputs["moe_w_group"], inputs["moe_w_expert"]], axis=-1), dtype=np.float32)
    out["b_router"] = np.ascontiguousarray(
        np.concatenate([inputs["moe_b_group"], inputs["moe_b_expert"]], axis=-1), dtype=np.float32)
    for k in ("moe_w_gate", "moe_w_up", "moe_w_down"):
        out[k] = np.ascontiguousarray(inputs[k], dtype=np.float32)
    return out


TWO_PI = 6.283185307179586
CW1 = 6.28125
CW2 = TWO_PI - CW1
PI_SAFE = 3.1415925


def _rr_sin(self, dst, X, tmpf, tmpi, phase=0.0, e="dve"):
    mk = self.mk
    mk.ts(tmpf, X, 1.0 / TWO_PI, 0.5 + phase / TWO_PI, op0=ALU.mult, op1=ALU.add, e=e)
    mk.copy(tmpi, tmpf, e=e)
    mk.copy(tmpf, tmpi, e=e)
    mk.stt(dst, tmpf, -CW1, X, ALU.mult, ALU.add)
    mk.stt(dst, tmpf, -CW2, dst, ALU.mult, ALU.add)
    if phase:
        mk.ts(dst, dst, phase, None, op0=ALU.add, e=e)
    mk.ts(tmpf, dst, -PI_SAFE, TWO_PI, op0=ALU.is_lt, op1=ALU.mult, e=e)
    mk.tt(dst, dst, tmpf, ALU.add, e=e)
    mk.ts(tmpf, dst, PI_SAFE, TWO_PI, op0=ALU.is_gt, op1=ALU.mult, e=e)
    mk.tt(dst, dst, tmpf, ALU.subtract, e=e)
    mk.ts(dst, dst, PI_SAFE, -PI_SAFE, op0=ALU.min, op1=ALU.max, e=e)
    mk.act(dst, dst, AF.Sin)


Builder.rr_sin = _rr_sin


def _proj_in(self, xres, g_row, W, ncols, fm_blocks, uT, tm_range, u_tm):
    mk = self.mk
    with mk.scope():
        gbc = mk.sb([128, D], F32, "gbc")
        mk.dma(gbc, g_row.pbc(128))
        Wb = mk.sb([128, 8, ncols], BF16, "Wb")
        for k in range(8):
            mk.dma(Wb[:, k, :], W[k * 128:(k + 1) * 128, :], q="pool")
        xts = [mk.sb([128, D], F32) for i in range(2)]
        sqs = [mk.sb([128, D], F32) for i in range(2)]
        hbs = [mk.sb([128, D], BF16) for i in range(2)]
        smalls = [(mk.sb([128, 1], F32), mk.sb([128, 1], F32)) for i in range(2)]
        hT = [mk.sb([128, 8, 512], BF16) for i in range(2)]
        ev = [mk.sb([128, 512], F32) for i in range(4)]
        nev = 0
        for gidx in range(NTOK // 512):
            hb_ = hT[gidx % 2]
            for tl in range(4):
                i = gidx * 4 + tl
                b = i % 2
                mk.dma(xts[b], xres.rows(i * 128, 128))
                self.rms_tile(xts[b], gbc, hbs[b], sq=sqs[b], small=smalls[b])
                pbb = self.bank[b].bitcast(BF16)
                for k in range(8):
                    mk.transpose(pbb[:, k * 128:(k + 1) * 128], hbs[b][:, k * 128:(k + 1) * 128], self.identb)
                mk.copy(hb_[:, :, tl * 128:(tl + 1) * 128], pbb.re("p (k t) -> p k t", k=8),
                        e=("act" if tl % 2 else "pool_never") if False else ("act" if tl % 2 else "dve"))
            for bi, cb in enumerate(fm_blocks):
                pb = self.bank[2 + (bi % 3)]
                for k in range(8):
                    mk.mm(pb, Wb[:, k, cb * 128:(cb + 1) * 128], hb_[:, k, :], start=(k == 0), stop=(k == 7))
                t = ev[nev % 4]
                mk.copy(t, pb, e=("act" if nev % 2 else "dve"))
                nev += 1
                mk.dma(V(uT.ap[bi * 128:(bi + 1) * 128, gidx * 512:(gidx + 1) * 512], uT.bufs[bi]), t)
            if tm_range is not None:
                c0, c1 = tm_range
                for tl in range(4):
                    i = gidx * 4 + tl
                    for cc in range(c0, c1, 512):
                        pb = self.bank[5 + (nev % 3)]
                        for k in range(8):
                            mk.mm(pb, hb_[:, k, tl * 128:(tl + 1) * 128], Wb[:, k, cc:cc + 512],
                                  start=(k == 0), stop=(k == 7))
                        t = ev[nev % 4]
                        mk.copy(t, pb, e=("act" if nev % 2 else "dve"))
                        nev += 1
                        mk.dma(V(u_tm.ap[i * 128:(i + 1) * 128, cc - c0:cc - c0 + 512], u_tm.bufs[i]), t)


Builder.proj_in = _proj_in


def _proj_out(self, xres_in, xres_out, oTd, Wout):
    mk = self.mk
    with mk.scope():
        Wb = mk.sb([128, 8, D], BF16, "Wob")
        for k in range(8):
            mk.dma(Wb[:, k, :], Wout[k * 128:(k + 1) * 128, :], q="pool")
        xts = [mk.sb([128, D], F32) for i in range(2)]
        ot = [mk.sb([128, 8, 512], BF16) for i in range(2)]
        for gi in range(NTOK // 512):
            o_ = ot[gi % 2]
            for k in range(8):
                mk.dma(o_[:, k, :], V(oTd.ap[k * 128:(k + 1) * 128, gi * 512:(gi + 1) * 512], oTd.bufs[k]))
            for tl in range(4):
                i = gi * 4 + tl
                b = i % 2
                mk.dma(xts[b], xres_in.rows(i * 128, 128))
                for half in range(2):
                    pb = self.bank[(i % 2) * 2 + half]
                    for k in range(8):
                        mk.mm(pb, o_[:, k, tl * 128:(tl + 1) * 128], Wb[:, k, half * 512:(half + 1) * 512],
                              start=(k == 0), stop=(k == 7))
                    mk.tt(xts[b][:, half * 512:(half + 1) * 512], xts[b][:, half * 512:(half + 1) * 512], pb, ALU.add)
                mk.dma(xres_out.rows(i * 128, 128), xts[b])


Builder.proj_out = _proj_out


def _attn(self, u_tm, oT, P, layer):
    import math
    mk = self.mk
    oi = layer // 2
    lam_init = 0.8 - 0.6 * math.exp(-0.3 * layer)
    with mk.scope():
        gqk = mk.sb([128, D], F32, "gqk")
        mk.dma(gqk, P["da_qk_gain"][oi:oi + 1, :].pbc(128))
        subg = mk.sb([128, 128], F32, "subg")
        mk.dma(subg, P["da_subln"][oi:oi + 1, :].pbc(128))
        mk.ts(subg, subg, 1.0 - lam_init, None, op0=ALU.mult)
        lamv = mk.sb([128, 4, 64], F32, "lamv")
        mk.dma(lamv.re("p a d -> p (a d)"), P["da_lam"][oi:oi + 1].re("o a d -> o (a d)").pbc(128))
        lt = mk.sb([128, 2, 64], F32)
        ls = mk.sb([128, 2], F32)
        mk.tt(lt[:, 0, :], lamv[:, 0, :], lamv[:, 1, :], ALU.mult)
        mk.tt(lt[:, 1, :], lamv[:, 2, :], lamv[:, 3, :], ALU.mult)
        mk.reduce(ls, lt, op=ALU.add)
        mk.act(ls, ls, AF.Exp)
        nlam = mk.sb([128, 1], F32, "nlam")
        mk.tt(nlam, ls[:, 1:2], ls[:, 0:1], ALU.subtract)
        mk.ts(nlam, nlam, -lam_init, None, op0=ALU.add)
        eps5 = mk.sb([128, 1], F32)
        mk.memset(eps5, 1e-5)
        nshift = mk.sb([128, 1], F32)
        mk.memset(nshift, -4.0)
        zb = mk.sb([128, 512], BF16, "zb")
        mk.memset(zb, 0.0)
        jf = mk.sb([128, 32], F32)
        mk.op("pool", lambda eng: eng.iota(jf.ap, [[1, 32]], base=0, channel_multiplier=0,
                                            allow_small_or_imprecise_dtypes=True), writes=[jf])
        mk.act(jf, jf, AF.Exp, scale=-math.log(10000.0) / 32.0)
        posf = mk.sb([128, 16], F32)
        mk.op("pool", lambda eng: eng.iota(posf.ap, [[128, 16]], base=0, channel_multiplier=1,
                                            allow_small_or_imprecise_dtypes=True), writes=[posf])
        ang = mk.sb([128, 16, 32], F32)
        mk.tt(ang, V(jf.ap.unsqueeze(1).to_broadcast([128, 16, 32]), jf.buf),
              V(posf.ap.unsqueeze(2).to_broadcast([128, 16, 32]), posf.buf), ALU.mult)
        sint = mk.sb([128, 16, 32], F32, "sint")
        cost = mk.sb([128, 16, 32], F32, "cost")
        tf = mk.sb([128, 16, 32], F32)
        ti = mk.sb([128, 16, 32], I32)
        self.rr_sin(sint, ang, tf, ti)
        self.rr_sin(cost, ang, tf, ti, phase=math.pi / 2)

        QT = mk.sb([128, 4, T], BF16, "QT")
        KT = mk.sb([128, 4, T], BF16, "KT")
        Vt = mk.sb([128, 16, 512], BF16, "Vt")
        qk = [mk.sb([128, 16, 2, 32], F32) for i in range(2)]
        sq = mk.sb([128, 16, 64], F32)
        ss = mk.sb([128, 16], F32)
        ta = mk.sb([128, 16, 32], F32)
        tb = mk.sb([128, 16, 32], F32)
        qr = [mk.sb([128, 16, 2, 32], BF16) for i in range(2)]
        pts = [mk.sb([128, 512], BF16) for i in range(3)]
        rls = [mk.sb([128, 8], F32) for i in range(2)]
        ob32 = [mk.sb([128, 128], F32) for i in range(2)]
        obb = [mk.sb([128, 128], BF16) for i in range(2)]
        junk = mk.sb([128, 128], F32)
        ss1 = [mk.sb([128, 1], F32) for i in range(2)]
        npt = 0
        nfin = 0
        ostage = [mk.sb([128, T], BF16, "ostage%d" % i) for i in range(2)]
        for s in range(NSEQ):
            mk.dma(Vt, V(u_tm.ap[s * T:(s + 1) * T, 1024:1536].rearrange("(i p) c -> p i c", p=128),
                         u_tm.whole), q="pool", )
            for i in range(16):
                b = i % 2
                row0 = s * T + i * 128
                q_ = qk[b]
                qf = q_.re("p g m d -> p (g m d)")
                mk.dma(qf, V(u_tm.ap[row0:row0 + 128, 0:1024], u_tm.bufs[row0 // 128]))
                mk.act(sq.re("p g d -> p (g d)"), qf, AF.Square)
                mk.reduce(ss, sq, op=ALU.add)
                mk.act(ss, ss, AF.Sqrt, scale=1.0 / 64.0, bias=self.eps_t)
                mk.recip(ss, ss)
                q3 = q_.re("p g m d -> p g (m d)")
                mk.tt(q3, q3, V(ss.ap.unsqueeze(2).to_broadcast([128, 16, 64]), ss.buf), ALU.mult)
                mk.tt(qf, qf, gqk, ALU.mult)
                cb_ = V(cost.ap[:, i, :].unsqueeze(1).to_broadcast([128, 16, 32]), cost.buf)
                sb_ = V(sint.ap[:, i, :].unsqueeze(1).to_broadcast([128, 16, 32]), sint.buf)
                x1 = q_[:, :, 0, :]
                x2 = q_[:, :, 1, :]
                mk.tt(ta, x1, cb_, ALU.mult)
                mk.tt(tb, x2, sb_, ALU.mult, e="pool")
                mk.tt(qr[b][:, :, 0, :], ta, tb, ALU.subtract)
                mk.tt(ta, x2, cb_, ALU.mult)
                mk.tt(tb, x1, sb_, ALU.mult, e="pool")
                mk.tt(qr[b][:, :, 1, :], ta, tb, ALU.add)
                qrf = qr[b].re("p g m d -> p (g m d)")
                pbb = self.bank[6 + b].bitcast(BF16)
                for k in range(8):
                    mk.transpose(pbb[:, k * 128:(k + 1) * 128], qrf[:, k * 128:(k + 1) * 128], self.identb)
                mk.copy(QT[:, :, i * 128:(i + 1) * 128], pbb[:, 0:512].re("p (h t) -> p h t", h=4), e="act")
                mk.copy(KT[:, :, i * 128:(i + 1) * 128], pbb[:, 512:1024].re("p (h t) -> p h t", h=4), e="act")
            units = [(h, qc) for h in range(4) for qc in range(4)]

            def osets(u):
                par = u % 2
                O = [self.bank[2], self.bank[3]] if par == 0 else [self.bank[6], self.bank[7]]
                Lb = V(self.bank[4].ap[:, 8 * par:8 * par + 8], self.bank[4].buf)
                return O, Lb

            def main(u):
                nonlocal npt
                h, qc = units[u]
                O, Lb = osets(u)
                mk.mm(O[0], zb[:, 0:128], zb)
                mk.mm(O[1], zb[:, 0:128], zb)
                mk.mm(Lb, zb[:, 0:128], zb[:, 0:8])
                steps = [(m, kt) for m in range(2) for kt in range(4 * qc + 4)]
                info = []

                def issue_S(i):
                    nonlocal npt
                    m, kt = steps[i]
                    q0 = max(kt * 128, qc * 512)
                    nq = (qc + 1) * 512 - q0
                    S = self.bank[npt % 2]
                    Pt = pts[npt % 3]
                    npt += 1
                    mk.mm(S[:, 0:nq], KT[m * 64:(m + 1) * 64, h, kt * 128:(kt + 1) * 128],
                          QT[m * 64:(m + 1) * 64, h, q0:q0 + nq])
                    info.append((S, Pt, q0, nq))

                issue_S(0)
                for i, (m, kt) in enumerate(steps):
                    if i + 1 < len(steps):
                        issue_S(i + 1)
                    S, Pt, q0, nq = info[i]
                    mk.act(Pt[:, 0:nq], S[:, 0:nq], AF.Exp, scale=0.125, bias=nshift)
                    if kt >= 4 * qc:
                        mk.tt(Pt[:, 0:128], Pt[:, 0:128], self.trib, ALU.mult, e="pool")
                    for qb in range(max(kt, 4 * qc), 4 * qc + 4):
                        ql = qb - 4 * qc
                        c0 = qb * 128 - q0
                        mk.mm(O[m][:, ql * 128:(ql + 1) * 128], Pt[:, c0:c0 + 128],
                              Vt[:, kt, h * 128:(h + 1) * 128], start=False, stop=(kt == qb),
                              skip_group_check=True)
                        mk.mm(Lb[:, m * 4 + ql:m * 4 + ql + 1], Pt[:, c0:c0 + 128], self.onesb[:, 0:1],
                              start=False, stop=(kt == qb), skip_group_check=True)

            def fin(u):
                nonlocal nfin
                h, qc = units[u]
                O, Lb = osets(u)
                rl = rls[u % 2]
                mk.recip(rl, Lb)
                mk.ts(rl[:, 4:8], rl[:, 4:8], nlam, None, op0=ALU.mult)
                for ql in range(4):
                    fb = nfin % 2
                    nfin += 1
                    o = ob32[fb]
                    mk.ts(o, O[0][:, ql * 128:(ql + 1) * 128], rl[:, ql:ql + 1], None, op0=ALU.mult)
                    mk.stt(o, O[1][:, ql * 128:(ql + 1) * 128], rl[:, 4 + ql:5 + ql], o, ALU.mult, ALU.add)
                    mk.act(junk, o, AF.Square, accum_out=ss1[fb])
                    mk.act(ss1[fb], ss1[fb], AF.Sqrt, scale=1.0 / 128.0, bias=eps5)
                    mk.recip(ss1[fb], ss1[fb])
                    mk.stt(obb[fb], o, ss1[fb], subg, ALU.mult, ALU.mult)
                    ptr = self.bank[5].bitcast(BF16)
                    mk.transpose(ptr[:, fb * 128:(fb + 1) * 128], obb[fb], self.identb)
                    t0 = (4 * qc + ql) * 128
                    mk.copy(ostage[h % 2][:, t0:t0 + 128], ptr[:, fb * 128:(fb + 1) * 128], e="act")
                if qc == 3:
                    mk.dma(V(oT.ap[h * 128:(h + 1) * 128, s * T:(s + 1) * T], oT.bufs[h]), ostage[h % 2])

            main(0)
            for u in range(len(units)):
                if u + 1 < len(units):
                    main(u + 1)
                fin(u)


Builder.attn = _attn


def _s5_params(self, a_re, a_im, lstep, shape, want_coef):
    import math
    mk = self.mk
    n = lambda: mk.sb(shape, F32)
    are, step, lr, th, rho = n(), n(), n(), n(), n()
    mk.ts(are, a_re, -1e-4, None, op0=ALU.min)
    mk.act(step, lstep, AF.Exp)
    mk.tt(lr, are, step, ALU.mult)
    mk.tt(th, a_im, step, ALU.mult)
    mk.act(rho, lr, AF.Exp)
    out = dict(rho=rho, th=th)
    if want_coef:
        sn, cs, tf, x, y, den, cre, cim = n(), n(), n(), n(), n(), n(), n(), n()
        ti = mk.sb(shape, I32)
        self.rr_sin(sn, th, tf, ti)
        self.rr_sin(cs, th, tf, ti, phase=math.pi / 2)
        mk.tt(x, rho, cs, ALU.mult)
        mk.ts(x, x, -1.0, None, op0=ALU.add)
        mk.tt(y, rho, sn, ALU.mult)
        mk.tt(den, are, are, ALU.mult)
        mk.tt(tf, a_im, a_im, ALU.mult)
        mk.tt(den, den, tf, ALU.add)
        mk.recip(den, den)
        mk.tt(cre, x, are, ALU.mult)
        mk.tt(tf, y, a_im, ALU.mult)
        mk.tt(cre, cre, tf, ALU.add)
        mk.tt(cre, cre, den, ALU.mult)
        mk.tt(cim, y, are, ALU.mult)
        mk.tt(tf, x, a_im, ALU.mult)
        mk.tt(cim, cim, tf, ALU.subtract)
        mk.tt(cim, cim, den, ALU.mult)
        out.update(cre=cre, cim=cim)
    return out


Builder.s5_params = _s5_params


def _s5(self, uT, oT, P, layer):
    import math
    mk = self.mk
    oi = layer // 2
    with mk.scope():
        bbr = mk.sb([128, 4, 128], BF16, "bbr")
        bbi = mk.sb([128, 4, 128], BF16, "bbi")
        rho = mk.sb([128, 16], F32, "rho16")
        theta = mk.sb([128, 16], F32, "th16")
        bfr = mk.sb([128, 16, 128], BF16, "bfr")
        bfi = mk.sb([128, 16, 128], BF16, "bfi")
        Cfr = mk.sb([128, 16, 128], BF16, "Cfr")
        Cfi = mk.sb([128, 16, 128], BF16, "Cfi")
        with mk.scope():
            rep = mk.sb([128, 3, 512], F32, "rep")
            mk.dma(rep, P["s5_rep"][oi].re("a p j s -> p a (j s)"))
            pr_ = self.s5_params(rep[:, 0, :], rep[:, 1, :], rep[:, 2, :], [128, 512], True)
            Bre = mk.sb([128, 512], F32)
            Bim = mk.sb([128, 512], F32)
            mk.dma(Bre, P["s5_bbd_re"][oi].re("p j s -> p (j s)"))
            mk.dma(Bim, P["s5_bbd_im"][oi].re("p j s -> p (j s)"))
            t1p = mk.sb([128, 512], F32)
            t2p = mk.sb([128, 512], F32)
            mk.tt(t1p, Bre, pr_["cre"], ALU.mult)
            mk.tt(t2p, Bim, pr_["cim"], ALU.mult)
            mk.tt(bbr.re("p j s -> p (j s)"), t1p, t2p, ALU.subtract)
            mk.tt(t1p, Bre, pr_["cim"], ALU.mult)
            mk.tt(t2p, Bim, pr_["cre"], ALU.mult)
            mk.tt(bbi.re("p j s -> p (j s)"), t1p, t2p, ALU.add)
            mk.memset(bfr, 0.0)
            mk.memset(bfi, 0.0)
            for q in range(4):
                for (src, dst) in ((bbr, bfr), (bbi, bfi)):
                    mk.copy(dst[32 * q:32 * q + 32].re("p (j q) s -> p j q s", q=4)[:, :, q, :],
                            src[32 * q:32 * q + 32, :, :], e="pool")
            st = mk.sb([128, 3, 16], F32, "st")
            mk.dma(st, P["s5_st"][oi].re("a p b -> p a b"))
            ps_ = self.s5_params(st[:, 0, :], st[:, 1, :], st[:, 2, :], [128, 16], False)
            mk.copy(rho, ps_["rho"])
            mk.copy(theta, ps_["th"])
        Cre = mk.sb([128, 16, 32], BF16, "Cre")
        nCim = mk.sb([128, 16, 32], BF16, "nCim")
        cim32 = mk.sb([128, 16, 32], F32)
        mk.dma(Cre, P["s5_cbd_re"][oi], q="pool")
        mk.dma(cim32, P["s5_cbd_im"][oi])
        mk.ts(nCim, cim32, -1.0, None, op0=ALU.mult)
        mk.memset(Cfr, 0.0)
        mk.memset(Cfi, 0.0)
        for q in range(4):
            for (src, dst) in ((Cre, Cfr), (nCim, Cfi)):
                mk.copy(dst.re("p (j q) c -> p j q c", q=4)[:, :, q, 32 * q:32 * q + 32],
                        src.re("p (j q) c -> p j q c", q=4)[:, :, q, :], e="pool")
        dcol = mk.sb([128, 4], F32, "dcol")
        mk.dma(dcol, P["s5_dcol"][oi])
        cbase = mk.sb([128, 16, 64], F32, "cbase")
        sbase = mk.sb([128, 16, 64], F32, "sbase")
        cstep = mk.sb([128, 16, 32], F32, "cstep")
        sstep = mk.sb([128, 16, 32], F32, "sstep")
        with mk.scope():
            rio = mk.sb([128, 64], F32)
            mk.op("pool", lambda eng: eng.iota(rio.ap, [[1, 64]], base=0, channel_multiplier=0,
                                                allow_small_or_imprecise_dtypes=True), writes=[rio])
            kio = mk.sb([128, 32], F32)
            mk.op("pool", lambda eng: eng.iota(kio.ap, [[64, 32]], base=0, channel_multiplier=0,
                                                allow_small_or_imprecise_dtypes=True), writes=[kio])
            angb = mk.sb([128, 16, 64], F32)
            angs = mk.sb([128, 16, 32], F32)
            mk.tt(angb, V(theta.ap.unsqueeze(2).to_broadcast([128, 16, 64]), theta.buf),
                  V(rio.ap.unsqueeze(1).to_broadcast([128, 16, 64]), rio.buf), ALU.mult)
            mk.tt(angs, V(theta.ap.unsqueeze(2).to_broadcast([128, 16, 32]), theta.buf),
                  V(kio.ap.unsqueeze(1).to_broadcast([128, 16, 32]), kio.buf), ALU.mult)
            tfb = mk.sb([128, 16, 64], F32)
            tib = mk.sb([128, 16, 64], I32)
            self.rr_sin(sbase, angb, tfb, tib)
            self.rr_sin(cbase, angb, tfb, tib, phase=math.pi / 2)
            self.rr_sin(sstep, angs, tfb[:, :, 0:32], tib[:, :, 0:32])
            self.rr_sin(cstep, angs, tfb[:, :, 0:32], tib[:, :, 0:32], phase=math.pi / 2)
        ubb = mk.sb([128, NTOK], BF16, "ubb")
        zTd = self.scratch("zTd", [512, NTOK], BF16)
        yT = mk.sb([128, NTOK], F32, "yT")
        sint2 = [mk.sb([128, T], F32, "sint%d" % i) for i in range(2)]
        cost2 = [mk.sb([128, T], F32, "cost%d" % i) for i in range(2)]
        gre2 = [mk.sb([128, T], F32, "gre%d" % i) for i in range(2)]
        gim2 = [mk.sb([128, T], F32, "gim%d" % i) for i in range(2)]
        wre2 = [mk.sb([128, T], F32, "wre%d" % i) for i in range(2)]
        wim2 = [mk.sb([128, T], F32, "wim%d" % i) for i in range(2)]
        xre2 = [mk.sb([128, T], BF16, "xre%d" % i) for i in range(2)]
        xim2 = [mk.sb([128, T], BF16, "xim%d" % i) for i in range(2)]
        tt1 = mk.sb([128, T], F32, "tt1")
        tt2 = mk.sb([128, T], F32, "tt2")
        bur_f = mk.sb([128, T], F32, "bur_f")
        bui_f = mk.sb([128, T], F32, "bui_f")
        ubb2 = [ubb, ubb]
        state = dict(nb=0)

        S5E = DBG.get('s5_eng', 'dve')

        def tables(sbi):
            sint, cost = sint2[sbi % 2], cost2[sbi % 2]
            gre, gim, wre, wim = gre2[0], gim2[0], wre2[0], wim2[0]
            cs_b = V(cstep.ap[:, sbi, :].unsqueeze(2).to_broadcast([128, 32, 64]), cstep.buf)
            ss_b = V(sstep.ap[:, sbi, :].unsqueeze(2).to_broadcast([128, 32, 64]), sstep.buf)
            cb_b = V(cbase.ap[:, sbi, :].unsqueeze(1).to_broadcast([128, 32, 64]), cbase.buf)
            sb_b = V(sbase.ap[:, sbi, :].unsqueeze(1).to_broadcast([128, 32, 64]), sbase.buf)
            v3_ = lambda t_: t_.re("p (k r) -> p k r", r=64)
            mk.tt(v3_(tt1), cs_b, cb_b, ALU.mult)
            mk.tt(v3_(tt2), ss_b, sb_b, ALU.mult, e=S5E)
            mk.tt(cost, tt1, tt2, ALU.subtract)
            mk.tt(v3_(tt1), ss_b, cb_b, ALU.mult, e=S5E)
            mk.tt(v3_(tt2), cs_b, sb_b, ALU.mult)
            mk.tt(sint, tt1, tt2, ALU.add, e=S5E)

        def multi(b0, nb_):
            return V(self.psum_all.ap[:, b0 * 512:(b0 + nb_) * 512], self.bank[b0].buf)

        def stageA(it):
            sbi, s = it // NSEQ, it % NSEQ
            cb = sbi // 4
            sint, cost = sint2[sbi % 2], cost2[sbi % 2]
            gre, gim, wre, wim = gre2[it % 2], gim2[it % 2], wre2[it % 2], wim2[it % 2]
            ub_ = ubb2[cb % 2]
            rho_b = V(rho.ap[:, sbi:sbi + 1].to_broadcast([128, T]), rho.buf)
            for ch in range(4):
                tok0 = s * T + ch * 512
                mk.mm(self.bank[ch], bfr[:, sbi, :], ub_[:, tok0:tok0 + 512])
            for ch in range(4):
                tok0 = s * T + ch * 512
                mk.mm(self.bank[4 + ch], bfi[:, sbi, :], ub_[:, tok0:tok0 + 512])
            rd_r = [self.bank[i] for i in range(1, 4)]
            rd_i = [self.bank[i] for i in range(5, 8)]
            src_r, src_i = multi(0, 4), multi(4, 4)
            mk.op("act", lambda eng: eng.copy(out=bur_f.ap, in_=src_r.ap), reads=[src_r] + rd_r, writes=[bur_f])
            mk.op("act", lambda eng: eng.copy(out=bui_f.ap, in_=src_i.ap), reads=[src_i] + rd_i, writes=[bui_f])
            mk.tt(tt1, bur_f, cost, ALU.mult)
            mk.tt(gre, bui_f, sint, ALU.mult, e=S5E)
            mk.tt(gre, gre, tt1, ALU.add)
            mk.tt(tt2, bui_f, cost, ALU.mult)
            mk.tt(gim, bur_f, sint, ALU.mult, e=S5E)
            mk.tt(gim, tt2, gim, ALU.subtract)
            mk.scan(wre, rho_b, gre, 0.0)
            mk.scan(wim, rho_b, gim, 0.0)

        def stageB(it):
            sbi, s = it // NSEQ, it % NSEQ
            cb, q = sbi // 4, sbi % 4
            sint, cost = sint2[sbi % 2], cost2[sbi % 2]
            gre, gim, wre, wim = gre2[it % 2], gim2[it % 2], wre2[it % 2], wim2[it % 2]
            xre, xim = xre2[it % 2], xim2[it % 2]
            mk.tt(gre, cost, wre, ALU.mult, e=S5E)
            mk.tt(gim, sint, wim, ALU.mult)
            mk.tt(xre, gre, gim, ALU.subtract)
            mk.tt(wre, sint, wre, ALU.mult)
            mk.tt(wim, cost, wim, ALU.mult, e=S5E)
            mk.tt(xim, wre, wim, ALU.add)
            for ch in range(4):
                sl = slice(ch * 512, (ch + 1) * 512)
                py = self.bank[ch]
                mk.mm(py, Cfr[:, sbi, :], xre[:, sl], start=True, stop=False)
                mk.mm(py, Cfi[:, sbi, :], xim[:, sl], start=False, stop=True)
            src_y = multi(0, 4)
            rd_y = [self.bank[i] for i in range(1, 4)]
            ysl = yT[:, s * T:(s + 1) * T]
            if q == 0:
                mk.op("act", lambda eng: eng.copy(out=ysl.ap, in_=src_y.ap), reads=[src_y] + rd_y, writes=[ysl])
            else:
                mk.op("act", lambda eng: eng.copy(out=bur_f.ap, in_=src_y.ap), reads=[src_y] + rd_y, writes=[bur_f])
                mk.tt(ysl, ysl, bur_f, ALU.add, e=S5E)

        def finish(cb):
            for s in range(NSEQ):
                ys = yT[:, s * T:(s + 1) * T]
                mk.dma(tt1, V(uT.ap[cb * 128:(cb + 1) * 128, s * T:(s + 1) * T], uT.bufs[cb]))
                mk.stt(ys, tt1, dcol[:, cb:cb + 1], ys, ALU.mult, ALU.add)
                mk.tt(tt2, ys, ys, ALU.mult, e="pool")
                mk.ts(tt2, tt2, 0.044715, 1.0, op0=ALU.mult, op1=ALU.add)
                mk.tt(tt2, tt2, ys, ALU.mult, e="pool")
                mk.act(tt2, tt2, AF.Sigmoid, scale=2.0 * math.sqrt(2.0 / math.pi))
                mk.tt(xre2[s % 2], ys, tt2, ALU.mult)
                mk.dma(V(zTd.ap[cb * 128:(cb + 1) * 128, s * T:(s + 1) * T], zTd.bufs[cb]), xre2[s % 2], q="pool")

        NIT = 16 * NSEQ
        for cb in range(4):
            if cb == 0:
                mk.dma(ubb2[0], V(uT.ap[0:128, :], uT.bufs[0]), q="pool")
                tables(0)
                stageA(0)
            for q in range(4):
                sbi = 4 * cb + q
                for s in range(NSEQ):
                    it = sbi * NSEQ + s
                    nxt = it + 1
                    if nxt < NIT and (nxt // NSEQ) // 4 == cb:
                        if nxt % NSEQ == 0:
                            tables(nxt // NSEQ)
                        stageA(nxt)
                    stageB(it)
            finish(cb)
            nxt = (4 * cb + 4) * NSEQ
            if nxt < NIT:
                mk.dma(ubb2[(cb + 1) % 2], V(uT.ap[(cb + 1) * 128:(cb + 2) * 128, :], uT.bufs[cb + 1]), q="pool")
                tables(nxt // NSEQ)
                stageA(nxt)
    with mk.scope():
        wgl = mk.sb([128, 4, 512], BF16, "wgl")
        for k in range(4):
            mk.dma(wgl[:, k, :], P["s5_w_glu"][oi, k * 128:(k + 1) * 128, :], q="pool")
        sg = [mk.sb([128, 512], F32) for i in range(4)]
        obuf = [mk.sb([128, 512], BF16) for i in range(4)]
        zc = [mk.sb([128, 4, 512], BF16, "zc%d" % i) for i in range(2)]
        for ch in range(NTOK // 512):
            sl = slice(ch * 512, (ch + 1) * 512)
            zch = zc[ch % 2]
            for k in range(4):
                mk.dma(zch[:, k, :], V(zTd.ap[k * 128:(k + 1) * 128, sl], zTd.bufs[k]))
            for cbo in range(4):
                pg = self.bank[cbo]
                for k in range(4):
                    mk.mm(pg, wgl[:, k, cbo * 128:(cbo + 1) * 128], zch[:, k, :], start=(k == 0), stop=(k == 3))
                mk.act(sg[cbo], pg, AF.Sigmoid)
            for cbo in range(4):
                ob_ = obuf[(ch * 4 + cbo) % 4]
                mk.tt(ob_, zch[:, cbo, :], sg[cbo], ALU.mult, e=("pool" if cbo % 2 else "dve"))
                mk.dma(V(oT.ap[(4 + cbo) * 128:(5 + cbo) * 128, sl], oT.bufs[4 + cbo]), ob_)


Builder.s5 = _s5


def _odd_layer(self, xres_in, xres_out, layer, P):
    mk = self.mk
    oi = layer // 2
    u_tm = self.scratch("u_tm", [NTOK, 1536], F32)
    uT = self.scratch("uTo", [512, NTOK], F32)
    self.proj_in(xres_in, P["norm_mix_g"][layer:layer + 1, :], P["odd_w_in"][oi], 2048,
                 [12, 13, 14, 15], uT, (0, 1536), u_tm)
    oT = self.scratch("oTd", [D, NTOK], BF16)
    if not DBG.get("no_attn"):
        self.attn(u_tm, oT, P, layer)
    if not DBG.get("no_s5"):
        self.s5(uT, oT, P, layer)
    self.proj_out(xres_in, xres_out, oT, P["odd_w_out"][oi])


Builder.odd_layer = _odd_layer


def host_odd_params(inputs):
    f = lambda a: np.ascontiguousarray(a, dtype=np.float32)
    out = {}
    out["norm_mix_g"] = f(inputs["norm_mix_g"])
    out["odd_w_in"] = f(inputs["odd_w_in"])
    out["odd_w_out"] = f(inputs["odd_w_out"])
    n_odd = inputs["odd_w_in"].shape[0]
    out["da_qk_gain"] = f(np.concatenate([np.tile(inputs["da_q_norm"], (1, 8)),
                                          np.tile(inputs["da_k_norm"], (1, 8))], axis=1))
    out["da_lam"] = f(np.stack([inputs["da_lam_q1"], inputs["da_lam_k1"],
                                inputs["da_lam_q2"], inputs["da_lam_k2"]], axis=1))
    out["da_subln"] = f(inputs["da_subln"])
    three = np.stack([inputs["s5_a_re"], inputs["s5_a_im"], inputs["s5_log_step"]], axis=1)
    t16 = three.reshape(n_odd, 3, 16, 128)
    rep = t16.reshape(n_odd, 3, 4, 4, 128)
    rep = np.transpose(rep, (0, 1, 3, 2, 4))
    rep = np.repeat(rep[:, :, :, None, :, :], 32, axis=3)
    out["s5_rep"] = f(rep.reshape(n_odd, 3, 128, 4, 128))
    out["s5_st"] = f(np.transpose(t16, (0, 1, 3, 2)))
    for nm, key in (("s5_bbd_re", "s5_b_re"), ("s5_bbd_im", "s5_b_im")):
        Bm = inputs[key]
        bd = np.zeros((n_odd, 4, 2, 16, 4, 2, 64), np.float32)
        for j in range(4):
            for q in range(4):
                for gl in range(2):
                    g = 2 * (4 * j + q) + gl
                    bd[:, q, gl, :, j, gl, :] = np.transpose(Bm[:, g], (0, 2, 1))
        out[nm] = f(bd.reshape(n_odd, 128, 4, 128))
    for nm, key in (("s5_cbd_re", "s5_c_re"), ("s5_cbd_im", "s5_c_im")):
        Cm = inputs[key]
        bd = np.zeros((n_odd, 2, 64, 16, 2, 16), np.float32)
        for sbi in range(16):
            for gl in range(2):
                bd[:, gl, :, sbi, gl, :] = np.transpose(Cm[:, 2 * sbi + gl], (0, 2, 1))
        out[nm] = f(bd.reshape(n_odd, 128, 16, 32))
    out["s5_dcol"] = f(np.transpose(inputs["s5_d"].reshape(n_odd, 4, 128), (0, 2, 1)))
    out["s5_w_glu"] = f(inputs["s5_w_glu"])
    return out


LCH = 64
NCH = T // LCH
DECAY_C = 0.6065306597126334


def _load_shift_mix(self, dst, uT, blk, s, mu_col, U, dtmp):
    mk = self.mk
    mk.dma(U[:, 1:T + 1], V(uT.ap[blk * 128:(blk + 1) * 128, s * T:(s + 1) * T], uT.bufs[blk]))
    mk.tt(dtmp, U[:, 0:T], U[:, 1:T + 1], ALU.subtract, e=DBG.get("rw_eng", "dve"))
    mk.stt(dst, dtmp, mu_col, U[:, 1:T + 1], ALU.mult, ALU.add)


Builder.load_shift_mix = _load_shift_mix


def _rwkv(self, uT, oTd, P, layer, vfirst):
    mk = self.mk
    ei = layer // 2
    has_vres = layer > 0
    with mk.scope():
        mu = mk.sb([128, 14], F32, "mu")
        mk.dma(mu, P["rw_mu_col"][ei])
        cols = mk.sb([128, 7, 4], F32, "cols")
        mk.dma(cols, P["rw_cols"][ei])
        w0c, a0c, kkc, kac, rkc, lngc, lnbc = [cols[:, i, :] for i in range(7)]
        w2a2 = mk.sb([128, 512], BF16, "w2a2")
        mk.dma(w2a2, P["rw_w2a2"][ei], q="pool")
        g2b = mk.sb([128, 512], BF16, "g2b")
        mk.dma(g2b, P["rw_g2"][ei], q="pool")
        blk = mk.sb([128, 128], BF16, "blk")
        mk.dma(blk, self.inp["c_blk"], q="pool")
        masks = mk.sb([128, 3, 128], BF16, "masks")
        mk.dma(masks, self.inp["c_masks"].re("a p c -> p a c"), q="pool")

        def mb(i):
            return V(masks.ap[:, i, :].unsqueeze(1).to_broadcast([128, 2, 128]), masks.buf)
        mLs, mUs, mUi = mb(0), mb(1), mb(2)
        rmask = mk.sb([128, T], BF16, "rmask")
        tB = mk.sb([128, T], F32, "tB")
        r32 = mk.sb([128, T], F32, "r32")
        mk.op("pool", lambda eng: eng.iota(tB.ap.rearrange("p (c l) -> p c l", l=LCH), [[0, NCH], [1, LCH]],
                                            base=0, channel_multiplier=0, allow_small_or_imprecise_dtypes=True),
              writes=[tB])
        mk.ts(rmask, tB, 1.0, None, op0=ALU.min)
        lneps = mk.sb([128, 1], F32)
        mk.memset(lneps, 64e-5)
        zb = mk.sb([128, 128], BF16, "zb")
        mk.memset(zb, 0.0)
        U = mk.sb([128, T + 1], F32, "U")
        mk.memset(U[:, 0:1], 0.0)
        dtmp = tB
        m_ = r32
        lr12 = mk.sb([128, T], BF16, "lr12")
        sdg = mk.sb([128, T], BF16, "sdg")
        tbf = mk.sb([128, T], BF16, "tbf")
        if has_vres:
            v0c = mk.sb([128, 4], F32, "v0c")
            mk.dma(v0c, P["rw_v0col"][ei - 1])
            v1b = mk.sb([128, 4, 32], BF16, "v1b")
            mk.dma(v1b, P["rw_v1"][ei - 1].re("(k p) n -> p k n", p=128), q="pool")
            v2b = mk.sb([32, 512], BF16, "v2b")
            mk.dma(v2b, P["rw_v2"][ei - 1], q="pool")
            t32b = mk.sb([32, T], BF16, "t32b")
        k32 = mk.sb([128, T], F32, "k32")
        a16 = mk.sb([128, T], BF16, "a16")
        lw = mk.sb([128, T], F32, "lw")
        cl = mk.sb([128, T], F32, "cl")
        v32 = cl
        tA = mk.sb([128, T], F32, "tA")
        gT = mk.sb([128, T], BF16, "gT")
        bon = mk.sb([128, T], BF16, "bon")
        aT_ = mk.sb([128, T], BF16, "aT_")
        bT_ = mk.sb([128, T], BF16, "bT_")
        kT_ = mk.sb([128, T], BF16, "kT_")
        vT_ = tbf
        a_bd = mk.sb([128, 2, T], BF16, "a_bd")
        b_bd = mk.sb([128, 2, T], BF16, "b_bd")
        r_bd = mk.sb([128, 2, T], BF16, "r_bd")
        for t_ in (a_bd, b_bd, r_bd):
            mk.memset(t_, 0.0)
        TMav = mk.sb([128, 16, 2, 128], BF16, "TMav")
        TMbk = mk.sb([128, 16, 2, 2, 128], BF16, "TMbk")
        mk.memset(TMbk, 0.0)
        DL = mk.sb([128, NCH], F32, "DL")
        H32 = mk.sb([128, 128], F32, "H32")
        Hb = mk.sb([128, 128], BF16, "Hb")
        ht1 = mk.sb([128, 128], F32, "ht1")
        oacc = mk.sb([128, T], BF16, "oacc")

        def grp():
            d = dict(X=[mk.sb([128, 2, 128], BF16) for _ in range(2)], XT=[mk.sb([128, 2, 128], BF16) for _ in range(2)],
                     AakT=mk.sb([128, 2, 128], BF16), ArbT=mk.sb([128, 2, 128], BF16), ArkT=mk.sb([128, 2, 128], BF16),
                     Z=mk.sb([128, 2, 2, 64], BF16), T1T=mk.sb([128, 2, 128], BF16), G1=mk.sb([128, 2, 128], F32),
                     QT=mk.sb([128, 2, 128], BF16), yn=mk.sb([128, 128], BF16), ot=mk.sb([128, 128], F32),
                     st6=mk.sb([128, 2, 6], F32), mv=mk.sb([128, 2, 2], F32), rs=mk.sb([128, 2], F32))
            mk.memset(d["T1T"], 0.0)
            mk.memset(d["G1"], 0.0)
            mk.memset(d["QT"], 0.0)
            return d
        NGS = DBG.get("rw_depth", 1) + 1
        G = [grp() for _ in range(NGS)]
        B_ = self.bank

        def reg(b, c0, c1):
            return V(B_[b].ap[:, c0:c1], B_[b].buf)
        pN, pNT = reg(0, 0, 256), reg(0, 256, 512)
        pAk, pRb = reg(1, 0, 256), reg(1, 256, 512)
        pRk, pZ2 = reg(2, 0, 256), reg(2, 256, 384)
        pZa = [reg(3, 0, 256), reg(3, 256, 512)]
        pX, pXT = reg(4, 0, 256), reg(4, 256, 512)
        pHp, pTr = reg(5, 0, 128), reg(5, 128, 256)
        pT, pG = reg(6, 0, 256), reg(6, 256, 512)
        pQ, pY = reg(3, 0, 256), reg(7, 0, 128)
        pbig = [B_[5], B_[6], B_[7]]

        def v3(t):
            return t.re("p (h c) -> p h c", h=2)

        for s in range(NSEQ):
            tsl = slice(s * T, (s + 1) * T)
            self.load_shift_mix(m_, uT, 12, s, mu[:, 12:13], U, dtmp)
            mk.act(lr12[0:64, :], m_[0:64, :], AF.Tanh)
            mk.copy(lr12[64:128, :], m_[64:128, :], e=DBG.get("rw_eng", "dve"))
            self.load_shift_mix(m_, uT, 13, s, mu[:, 13:14], U, dtmp)
            mk.act(sdg, m_, AF.Sigmoid)
            if has_vres:
                pv = [B_[i] for i in range(4)]
                for hb in range(4):
                    self.load_shift_mix(m_, uT, 8 + hb, s, mu[:, 8 + hb:9 + hb], U, dtmp)
                    mk.copy(tbf, m_, e="act")
                    for ch in range(4):
                        mk.mm(pv[ch][0:32, :], v1b[:, hb, :], tbf[:, ch * 512:(ch + 1) * 512],
                              start=(hb == 0), stop=(hb == 3))
                for ch in range(4):
                    mk.copy(t32b[:, ch * 512:(ch + 1) * 512], pv[ch][0:32, :], e="act")
            for hb in range(4):
                hsl = slice(hb * 128, (hb + 1) * 128)
                self.load_shift_mix(r32, uT, hb, s, mu[:, hb:hb + 1], U, dtmp)
                self.load_shift_mix(k32, uT, 4 + hb, s, mu[:, 4 + hb:5 + hb], U, dtmp)
                self.load_shift_mix(v32, uT, 8 + hb, s, mu[:, 8 + hb:9 + hb], U, dtmp)
                for ch in range(4):
                    csl = slice(ch * 512, (ch + 1) * 512)
                    pb = pbig[ch % 3]
                    mk.mm(pb, w2a2[0:64, hsl], lr12[0:64, csl])
                    mk.act(lw[:, csl], pb, AF.Sigmoid, bias=w0c[:, hb:hb + 1])
                    pb = pbig[(ch + 1) % 3]
                    mk.mm(pb, w2a2[64:128, hsl], lr12[64:128, csl])
                    mk.act(a16[:, csl], pb, AF.Sigmoid, bias=a0c[:, hb:hb + 1])
                    pb = pbig[(ch + 2) % 3]
                    mk.mm(pb, g2b[:, hsl], sdg[:, csl])
                    mk.copy(gT[:, csl], pb, e="act")
                    if has_vres:
                        pb = pbig[ch % 3]
                        mk.mm(pb, v2b[:, hsl], t32b[:, csl])
                        mk.act(tA[:, csl], pb, AF.Sigmoid, bias=v0c[:, hb:hb + 1])
                mk.ts(lw, lw, -DECAY_C, None, op0=ALU.mult)
                if has_vres:
                    mk.dma(tB, V(vfirst.ap[hb * 128:(hb + 1) * 128, tsl], vfirst.bufs[hb]))
                    mk.tt(tB, tB, v32, ALU.subtract, e=DBG.get("rw_eng", "dve"))
                    mk.tt(tB, tB, tA, ALU.mult, e=DBG.get("rw_eng", "dve"))
                    mk.tt(v32, v32, tB, ALU.add, e=DBG.get("rw_eng", "dve"))
                else:
                    mk.dma(V(vfirst.ap[hb * 128:(hb + 1) * 128, tsl], vfirst.bufs[hb]), v32, q=DBG.get("st_q", "pool"))
                mk.ts(tA, k32, kkc[:, hb:hb + 1], None, op0=ALU.mult)
                mk.tt(tbf, tA, tA, ALU.mult, e=DBG.get("rw_eng", "dve"))
                for ch in range(4):
                    csl = slice(ch * 512, (ch + 1) * 512)
                    pb = pbig[ch % 3]
                    mk.mm(pb, blk, tbf[:, csl])
                    mk.act(tB[:, csl], pb, AF.Sqrt)
                mk.ts(tB, tB, 1e-12, None, op0=ALU.max)
                mk.recip(tB, tB)
                mk.tt(tA, tA, tB, ALU.mult)
                mk.ts(tB, a16, -1.0, kac[:, hb:hb + 1], op0=ALU.add, op1=ALU.mult)
                mk.ts(tB, tB, 1.0, None, op0=ALU.add)
                mk.tt(k32, k32, tB, ALU.mult)
                mk.tt(tB, r32, k32, ALU.mult, e=DBG.get("rw_eng", "dve"))
                mk.ts(tbf, tB, rkc[:, hb:hb + 1], None, op0=ALU.mult)
                for ch in range(4):
                    csl = slice(ch * 512, (ch + 1) * 512)
                    pb = pbig[ch % 3]
                    mk.mm(pb, blk, tbf[:, csl])
                    mk.tt(bon[:, csl], pb, v32[:, csl], ALU.mult)
                mk.copy(vT_, v32, e="act")
                mk.scan(cl, rmask, lw, 0.0)
                mk.act(tB, cl, AF.Exp)
                mk.copy(DL, tB.re("p (c l) -> p c l", l=LCH)[:, :, LCH - 1], e=DBG.get("rw_eng", "dve"))
                mk.tt(r_bd[0:64, 0, :], r32[0:64, :], tB[0:64, :], ALU.mult)
                mk.tt(r_bd[64:128, 1, :], r32[64:128, :], tB[64:128, :], ALU.mult, e=DBG.get("rw_eng", "dve"))
                mk.tt(cl, cl, lw, ALU.subtract, e=DBG.get("rw_eng", "dve"))
                mk.act(tB, cl, AF.Exp)
                mk.tt(tB, tB, tA, ALU.mult)
                mk.ts(aT_, tB, -1.0, None, op0=ALU.mult)
                mk.copy(a_bd[0:64, 0, :], aT_[0:64, :], e=DBG.get("rw_eng", "dve"))
                mk.copy(a_bd[64:128, 1, :], aT_[64:128, :], e="act")
                mk.tt(cl, cl, lw, ALU.add, e=DBG.get("rw_eng", "dve"))
                mk.act(tB, cl, AF.Exp, scale=-1.0)
                mk.tt(kT_, k32, tB, ALU.mult)
                mk.tt(tA, tA, a16, ALU.mult, e=DBG.get("rw_eng", "dve"))
                mk.tt(bT_, tA, tB, ALU.mult)
                mk.copy(b_bd[0:64, 0, :], bT_[0:64, :], e=DBG.get("rw_eng", "dve"))
                mk.copy(b_bd[64:128, 1, :], bT_[64:128, :], e="act")
                for p in range(16):
                    ptm = B_[5 + p % 2].bitcast(BF16)
                    psl = slice(p * 128, (p + 1) * 128)
                    for qi, src in enumerate((aT_, vT_, bT_, kT_)):
                        mk.transpose(ptm[:, qi * 128:(qi + 1) * 128], src[:, psl], self.identb)
                    mk.copy(TMav[:, p, :, :], ptm[:, 0:256].re("p (q c) -> p q c", q=2), e="act")
                    for c in range(2):
                        mk.copy(TMbk[64 * c:64 * c + 64, p, c, :, :],
                                ptm[64 * c:64 * c + 64, 256:512].re("p (q c) -> p q c", q=2), e=("dve" if c else "act"))
                mk.memset(H32, 0.0)
                mk.memset(Hb, 0.0)
                def local(p):
                    g = G[p % NGS]
                    t0 = p * 128
                    gsl = slice(t0, t0 + 128)
                    mk.mm(pN, aT_[:, gsl], b_bd[:, :, gsl])
                    mk.mm(pNT, bT_[:, gsl], a_bd[:, :, gsl])
                    mk.mm(pAk, kT_[:, gsl], a_bd[:, :, gsl])
                    mk.mm(pRb, bT_[:, gsl], r_bd[:, :, gsl])
                    mk.mm(pRk, kT_[:, gsl], r_bd[:, :, gsl])
                    yield
                    mk.tt(g["X"][0], v3(pN), mLs, ALU.mult)
                    mk.tt(g["XT"][0], v3(pNT), mUs, ALU.mult)
                    mk.tt(g["AakT"], v3(pAk), mUs, ALU.mult)
                    mk.tt(g["ArbT"], v3(pRb), mUi, ALU.mult)
                    mk.tt(g["ArkT"], v3(pRk), mUi, ALU.mult)
                    yield
                    for hd in range(2):
                        mk.mm(pZ2[:, 64 * hd:64 * hd + 64], g["AakT"][:, hd, :], TMav[:, p, 1, 64 * hd:64 * hd + 64])
                    Z = g["Z"]
                    mk.copy(Z[:, 0, :, :], TMav[:, p, 0, :].re("p (h j) -> p h j", h=2), e=DBG.get("rw_eng", "dve"))
                    yield
                    mk.copy(Z[:, 1, :, :], pZ2.re("p (h i) -> p h i", h=2), e="act")
                    yield
                    for lev in range(6):
                        X, XT = g["X"][lev % 2], g["XT"][lev % 2]
                        pz = pZa[lev % 2]
                        for hd in range(2):
                            mk.mm(pz[:, hd * 128:(hd + 1) * 128], XT[:, hd, :], Z[:, :, hd, :])
                        if lev < 5:
                            Xn, XTn = g["X"][(lev + 1) % 2], g["XT"][(lev + 1) % 2]
                            for hd in range(2):
                                mk.mm(pX[:, hd * 128:(hd + 1) * 128], XT[:, hd, :], X[:, hd, :])
                                mk.mm(pXT[:, hd * 128:(hd + 1) * 128], X[:, hd, :], XT[:, hd, :])
                        yield
                        if lev < 5:
                            mk.copy(Xn.re("p h c -> p (h c)"), pX, e="act")
                            mk.copy(XTn.re("p h c -> p (h c)"), pXT, e="act")
                        mk.tt(Z, Z, pz.re("p (h w c) -> p w h c", h=2, w=2), ALU.add)
                        yield
                    Wb_ = Z[:, 0, :, :].re("p h j -> p (h j)")
                    Ub_ = Z[:, 1, :, :].re("p h j -> p (h j)")
                    mk.mm(pT, Wb_, TMbk[:, p, :, 0, :])
                    for c in range(2):
                        mk.mm(pG[:, 128 * c:128 * c + 128], TMbk[:, p, c, 0, :], Ub_, start=True, stop=False)
                        mk.mm(pG[:, 128 * c:128 * c + 128], TMbk[:, p, c, 1, :], TMav[:, p, 1, :],
                              start=False, stop=True)
                    mk.mm(pQ, Wb_, g["ArbT"].re("p h t -> p (h t)"))
                    yield
                    for hd in range(2):
                        hs = slice(64 * hd, 64 * hd + 64)
                        mk.copy(g["T1T"][hs, :, hs], pT[hs, :].re("p (c h j) -> p c h j", c=2, h=2)[:, :, hd, :], e="act")
                        mk.copy(g["G1"][hs, :, hs], pG[hs, :].re("p (c h i) -> p c h i", c=2, h=2)[:, :, hd, :], e="act")
                        for c in range(2):
                            mk.tt(g["QT"][hs, c, 64 * c:64 * c + 64], pQ[hs, hd * 128 + 64 * c:hd * 128 + 64 * c + 64],
                                  r_bd[hs, hd, t0 + 64 * c:t0 + 64 * c + 64], ALU.add)

                def rec(p):
                    g = G[p % NGS]
                    t0 = p * 128
                    gsl = slice(t0, t0 + 128)
                    Z = g["Z"]
                    mk.mm(pY, zb, zb)
                    for hd in range(2):
                        hs = slice(64 * hd, 64 * hd + 64)
                        mk.mm(pY[:, hs], g["ArbT"][:, hd, :], Z[:, 1, hd, :], start=False, stop=False,
                              skip_group_check=True)
                        mk.mm(pY[:, hs], g["ArkT"][:, hd, :], TMav[:, p, 1, hs], start=False, stop=False,
                              skip_group_check=True)
                    for c in range(2):
                        ci = 2 * p + c
                        mk.mm(pY, g["QT"][:, c, :], Hb, start=False, stop=True, skip_group_check=True)
                        mk.mm(pHp, g["T1T"][:, c, :], Hb)
                        yield
                        mk.tt(ht1, pHp, H32, ALU.add)
                        mk.tt(ht1, ht1, g["G1"][:, c, :], ALU.add)
                        mk.ts(H32, ht1, DL[:, ci:ci + 1], None, op0=ALU.mult)
                        yield
                        mk.copy(Hb, H32, e="act")
                        yield
                    for hd in range(2):
                        ysl = pY[:, 64 * hd:64 * hd + 64]
                        mk.op("dve", lambda eng, o=g["st6"][:, hd, :], i_=ysl: eng.bn_stats(out=o.ap, in_=i_.ap),
                              reads=[ysl], writes=[g["st6"]])
                        mk.op("dve", lambda eng, o=g["mv"][:, hd, :], i_=g["st6"][:, hd, :]: eng.bn_aggr(out=o.ap, in_=i_.ap),
                              reads=[g["st6"]], writes=[g["mv"]])
                    yield
                    mk.act(g["rs"], g["mv"][:, :, 1], AF.Sqrt, bias=lneps)
                    yield
                    mk.recip(g["rs"], g["rs"])
                    for hd in range(2):
                        mk.ts(g["yn"][:, 64 * hd:64 * hd + 64], pY[:, 64 * hd:64 * hd + 64], g["mv"][:, hd, 0:1],
                              g["rs"][:, hd:hd + 1], op0=ALU.subtract, op1=ALU.mult)
                    yield
                    ptr = pTr.bitcast(BF16)
                    mk.transpose(ptr[:, 0:128], g["yn"], self.identb)
                    yield
                    mk.ts(g["ot"], ptr[:, 0:128], lngc[:, hb:hb + 1], lnbc[:, hb:hb + 1], op0=ALU.mult, op1=ALU.add)
                    mk.tt(g["ot"], g["ot"], bon[:, gsl], ALU.add, e=DBG.get("rw_eng", "dve"))
                    mk.tt(oacc[:, gsl], g["ot"], gT[:, gsl], ALU.mult, e=DBG.get("rw_eng", "dve"))

                def drive(gens):
                    gens = list(gens)
                    while gens:
                        for gg in list(gens):
                            try:
                                next(gg)
                            except StopIteration:
                                gens.remove(gg)

                NG = DBG.get("ngrp", 16)
                DEPTH = DBG.get("rw_depth", 1)
                started = {}

                def get_local(p):
                    if p not in started:
                        started[p] = [local(p), False]
                    return started[p]

                def step(ent):
                    if ent[1]:
                        return
                    try:
                        next(ent[0])
                    except StopIteration:
                        ent[1] = True

                if NG:
                    ent = get_local(0)
                    while not ent[1]:
                        step(ent)
                for p in range(NG):
                    r = [rec(p), False]
                    need = get_local(p + 1) if p + 1 < NG else None
                    extra = [get_local(p + k) for k in range(2, DEPTH + 1) if p + k < NG]
                    while not r[1] or (need is not None and not need[1]):
                        step(r)
                        if need is not None:
                            step(need)
                        for e_ in extra:
                            step(e_)
                mk.dma(V(oTd.ap[hb * 128:(hb + 1) * 128, tsl], oTd.bufs[hb]), oacc, q=DBG.get("st_q", "pool"))


Builder.rwkv = _rwkv


def _pool(self, uT, oT, P, layer):
    mk = self.mk
    ei = layer // 2
    with mk.scope():
        pwb = mk.sb([128, 4, 128], BF16, "pwb")
        mk.dma(pwb, P["pool_w"][ei].re("g c d -> c g d"), q="pool")
        psc = mk.sb([128, 4], F32, "psc")
        mk.dma(psc, P["pool_scale_col"][ei])
        A = [mk.sb([128, 16 + T], F32, "pA%d" % i) for i in range(2)]
        U0 = mk.sb([128, 16 + T], F32, "pU")
        for t_ in A + [U0]:
            mk.memset(t_[:, 0:16], 0.0)
        rcw = mk.sb([128, T], F32, "rcw")
        dT = mk.sb([128, T], BF16, "dT")
        tmp = mk.sb([128, T], F32, "ptmp")
        pob = [mk.sb([128, 512], BF16) for i in range(2)]
        for gi in range(4):
            win = 2 ** (gi + 1)
            mk.op("pool", lambda eng: eng.iota(rcw.ap, [[1, T]], base=1, channel_multiplier=0,
                                                allow_small_or_imprecise_dtypes=True), writes=[rcw])
            mk.ts(rcw, rcw, float(win), None, op0=ALU.min)
            mk.recip(rcw, rcw)
            for s in range(NSEQ if DBG.get("pool_stage", 9) >= 2 else 0):
                mk.dma(U0[:, 16:], V(uT.ap[(14 + gi) * 128:(15 + gi) * 128, s * T:(s + 1) * T], uT.bufs[14 + gi]))
                src = U0
                for lev in range(gi + 1):
                    sh = 2 ** lev
                    dst = A[lev % 2]
                    mk.tt(dst[:, 16:], src[:, 16:], src[:, 16 - sh:16 - sh + T], ALU.add, e=("pool" if lev % 2 else "dve"))
                    src = dst
                mk.tt(tmp, src[:, 16:], rcw, ALU.mult)
                mk.tt(dT, tmp, U0[:, 16:], ALU.subtract, e="pool")
                for ch in range(4 if DBG.get("pool_stage", 9) >= 3 else 0):
                    pb = self.bank[ch % 2]
                    mk.mm(pb, pwb[:, gi, :], dT[:, ch * 512:(ch + 1) * 512])
                    ob_ = pob[ch % 2]
                    if DBG.get("pool_stage", 9) >= 4:
                        mk.ts(ob_, pb, psc[:, gi:gi + 1], None, op0=ALU.mult)
                    if DBG.get("pool_stage", 9) >= 5:
                        mk.dma(V(oT.ap[(4 + gi) * 128:(5 + gi) * 128, s * T + ch * 512:s * T + (ch + 1) * 512],
                                 oT.bufs[4 + gi]), ob_, q=DBG.get("pool_q", "pool"))


Builder.pool = _pool


def _even_layer(self, xres_in, xres_out, layer, P, vfirst):
    mk = self.mk
    ei = layer // 2
    uT = self.scratch("uTe", [2304, NTOK], F32)
    self.proj_in(xres_in, P["norm_mix_g"][layer:layer + 1, :], P["even_w_in"][ei], 2304,
                 list(range(18)), uT, None, None)
    oT = self.scratch("oTd", [D, NTOK], BF16)
    if not DBG.get("no_rwkv"):
        self.rwkv(uT, oT, P, layer, vfirst)
    if not DBG.get("no_pool"):
        self.pool(uT, oT, P, layer)
    self.proj_out(xres_in, xres_out, oT, P["even_w_out"][ei])


Builder.even_layer = _even_layer


def host_even_params(inputs):
    f = lambda a: np.ascontiguousarray(a, dtype=np.float32)
    out = {}
    out["norm_mix_g"] = f(inputs["norm_mix_g"])
    out["even_w_in"] = f(inputs["even_w_in"])
    out["even_w_out"] = f(inputs["even_w_out"])
    n_even = inputs["even_w_in"].shape[0]
    out["rw_mu_col"] = f(np.transpose(inputs["rw_mu"].reshape(n_even, 14, 128), (0, 2, 1)))
    colp = [inputs["rw_w0"], inputs["rw_a0"], inputs["rw_k_k"], inputs["rw_k_a"],
            inputs["rw_r_k"].reshape(n_even, 512), inputs["rw_ln_g"], inputs["rw_ln_b"]]
    out["rw_cols"] = f(np.stack([np.transpose(c.reshape(n_even, 4, 128), (0, 2, 1)) for c in colp], axis=2))
    out["rw_w2a2"] = f(np.concatenate([inputs["rw_w2"], inputs["rw_a2"]], axis=1))
    out["rw_g2"] = f(inputs["rw_g2"])
    nv = inputs["rw_v0"].shape[0]
    out["rw_v0col"] = f(np.transpose(inputs["rw_v0"].reshape(nv, 4, 128), (0, 2, 1)))
    out["rw_v1"] = f(inputs["rw_v1"])
    out["rw_v2"] = f(inputs["rw_v2"])
    out["pool_w"] = f(inputs["pool_w"])
    out["pool_scale_col"] = f(np.transpose(inputs["pool_scale"].reshape(n_even, 4, 128), (0, 2, 1)))
    return out


def host_consts2():
    c = host_consts()
    blk = np.kron(np.eye(2, dtype=np.float32), np.ones((64, 64), np.float32))
    lo = np.tril(np.ones((64, 64), np.float32), -1)
    e2 = np.eye(2, dtype=np.float32)
    mLs = np.kron(e2, lo)
    mUs = np.kron(e2, lo.T)
    mUi = np.kron(e2, np.triu(np.ones((64, 64), np.float32)))
    c["c_blk"] = blk
    c["c_masks"] = np.stack([mLs, mUs, mUi]).astype(np.float32)
    return c


def mk_check(mk):
    val = {}
    pos = {e: 0 for e in ENGS}
    total = sum(len(mk.ops[e]) for e in ENGS)
    done = 0
    while done < total:
        prog = False
        for e in ENGS:
            lst = mk.ops[e]
            while pos[e] < len(lst):
                waits, fn, inc = lst[pos[e]]
                if all(val.get(id(s), 0) >= v for s, v in waits):
                    val[id(inc[0])] = val.get(id(inc[0]), 0) + inc[1]
                    pos[e] += 1
                    done += 1
                    prog = True
                else:
                    break
        if not prog:
            names = {id(v): k for k, v in mk.semobj.items()}
            for e in ENGS:
                if pos[e] < len(mk.ops[e]):
                    waits, fn, inc = mk.ops[e][pos[e]]
                    print("STUCK", e, pos[e], [(names.get(id(s)), v, val.get(id(s), 0)) for s, v in waits])
            return False
    return True


_PROG = {}


def host_params(inputs):
    hp = {}
    hp.update(host_even_params(inputs))
    hp.update(host_odd_params(inputs))
    hp.update(host_moe_params(inputs))
    hp.update(host_consts2())
    return hp


def build_program(shapes, n_layers=4):
    nc = bass.Bass("TRN2", target_bir_lowering=False)
    with ExitStack() as st:
        B = Builder(nc, st)
        mk = B.mk
        x_in = DR(mk, "x_in", [NTOK, D], F32, kind="ExternalInput")
        y = DR(mk, "y", [NTOK, D], F32, kind="ExternalOutput")
        P = {}
        for k, shp in shapes.items():
            if k in B.inp:
                continue
            P[k] = B.ext_in(k, list(shp))
        vf = B.scratch("vfirst", [512, NTOK], F32)
        for layer in range(n_layers):
            xin = x_in if layer == 0 else y
            if layer % 2 == 0:
                B.even_layer(xin, y, layer, P, vf)
            else:
                B.odd_layer(xin, y, layer, P)
            B.moe(y, layer, P)
        mk.emit()
    return nc


def kernel(**inputs):
    inputs = {k: np.asarray(v) for k, v in inputs.items()}
    hp = host_params(inputs)
    key = "full"
    if key not in _PROG:
        _PROG[key] = build_program({k: v.shape for k, v in hp.items()})
    nc = _PROG[key]
    x = np.ascontiguousarray(inputs["x"], dtype=np.float32)
    nb = x.shape[0]
    n_cores = 8
    per = nb // n_cores
    in_maps = []
    for c in range(n_cores):
        m = dict(hp)
        m["x_in"] = np.ascontiguousarray(x[c * per:(c + 1) * per].reshape(NTOK, D))
        in_maps.append(m)
    res = run_bass_kernel_spmd(nc, in_maps, core_ids=list(range(n_cores)))
    out = np.stack([np.asarray(r["y"], dtype=np.float32).reshape(per, T, D) for r in res.results], axis=0)
    return out.reshape(nb, T, D)
```

```python
import numpy as np
from contextlib import ExitStack
import concourse.bass as bass
import concourse.mybir as mybir
from concourse.bass_utils import run_bass_kernel_spmd

F32 = mybir.dt.float32
BF16 = mybir.dt.bfloat16
I32 = mybir.dt.int32
U32 = mybir.dt.uint32
AF = mybir.ActivationFunctionType
ALU = mybir.AluOpType
AX = mybir.AxisListType

SAME_ENGINE_SYNC = True
EPOCH = 20000
DBG = {}


class Buf:
    __slots__ = ("w", "r", "name")

    def __init__(self, name=""):
        self.w = None
        self.r = {}
        self.name = name


class V:
    __slots__ = ("ap", "buf")

    def __init__(self, ap, buf):
        self.ap = ap
        self.buf = buf

    def __getitem__(self, k):
        return V(self.ap[k], self.buf)

    def re(self, s, **kw):
        return V(self.ap.rearrange(s, **kw), self.buf)

    def bc(self, shape):
        return V(self.ap.to_broadcast(list(shape)), self.buf)

    def pbc(self, n):
        return V(self.ap.partition_broadcast(n), self.buf)

    def bitcast(self, dt):
        return V(self.ap.bitcast(dt), self.buf)

    def sub(self, buf):
        return V(self.ap, buf)

    @property
    def shape(self):
        return self.ap.shape


ENGS = ("pe", "act", "dve", "pool", "sp")


class MK:
    def __init__(self, nc, stack, n_dma_slots=8):
        self.nc = nc
        self.stack = stack
        self.ops = {e: [] for e in ENGS}
        self.root = stack
        self.esem = {}
        self.cnt = {e: 0 for e in ENGS}
        self.seen = {e: {} for e in ENGS}
        self.dma_slots = {}
        self.dma_n = {}
        for q in ("sp", "pool", "act"):
            self.dma_slots[q] = [stack.enter_context(nc.semaphore("dma_%s_%d" % (q, i)))
                                 for i in range(n_dma_slots)]
            self.dma_n[q] = 0
        self.semobj = {}
        self.n_ops = 0
        self.uid = 0

    def sb(self, shape, dtype, name=None):
        self.uid += 1
        name = name or "t%d" % self.uid
        t = self.stack.enter_context(self.nc.sbuf_tensor(name + "_%d" % self.uid, list(shape), dtype))
        return V(t[:], Buf(name))

    def ps(self, shape, dtype=F32, name=None):
        self.uid += 1
        name = name or "p%d" % self.uid
        t = self.stack.enter_context(self.nc.psum_tensor(name + "_%d" % self.uid, list(shape), dtype))
        return V(t[:], Buf(name))

    def dram(self, name, shape, dtype, kind="Internal"):
        t = self.nc.dram_tensor(name, list(shape), dtype, kind=kind)
        return V(t.ap(), Buf(name))

    def _tok_need(self, e, tok, waits):
        if tok is None:
            return
        semkey, val, src, is_dma = tok
        if src == e and not is_dma:
            if e == "pe" or not SAME_ENGINE_SYNC:
                return
        if self.seen[e].get(semkey, 0) >= val:
            return
        if waits.get(semkey, 0) < val:
            waits[semkey] = val

    def op(self, e, fn, reads=(), writes=(), dma=False):
        waits = {}
        pend = getattr(self, "pending", {}).pop(e, None)
        if pend:
            waits.update(pend)
        rb = [v.buf for v in reads if v is not None]
        wb = [v.buf for v in writes if v is not None]
        for b in rb:
            self._tok_need(e, b.w, waits)
        for b in wb:
            self._tok_need(e, b.w, waits)
            for t in b.r.values():
                self._tok_need(e, t, waits)
        if dma:
            slots = self.dma_slots[e]
            n = self.dma_n[e]
            self.dma_n[e] = n + 1
            sem = slots[n % len(slots)]
            val = 16 * (n // len(slots) + 1)
            semkey = ("d", e, n % len(slots))
            self.semobj[semkey] = sem
            if val > 16:
                if self.seen[e].get(semkey, 0) < val - 16 and waits.get(semkey, 0) < val - 16:
                    waits[semkey] = val - 16
            tok = (semkey, val, e, True)
            inc = (sem, 16)
        else:
            self.cnt[e] += 1
            ep = (self.cnt[e] - 1) // EPOCH
            semkey = ("c", e, ep)
            if semkey not in self.esem:
                self.esem[semkey] = self.root.enter_context(self.nc.semaphore("sem_%s_%d" % (e, ep)))
            self.semobj[semkey] = self.esem[semkey]
            tok = (semkey, (self.cnt[e] - 1) % EPOCH + 1, e, False)
            inc = (self.esem[semkey], 1)
        for k, v in waits.items():
            self.seen[e][k] = v
        for b in wb:
            b.w = tok
            b.r = {}
        for b in rb:
            old = b.r.get(tok[0])
            if old is None or old[1] < tok[1]:
                b.r[tok[0]] = tok
        self.ops[e].append(([(self.semobj[k], v) for k, v in waits.items()], fn, inc))
        self.n_ops += 1
        return tok

    def emit(self):
        nc = self.nc
        fin = []
        for q in ("sp", "pool", "act"):
            n = self.dma_n[q]
            slots = self.dma_slots[q]
            for i in range(min(n, len(slots))):
                uses = (n - i + len(slots) - 1) // len(slots)
                fin.append((slots[i], 16 * uses))
        for e in ("pe", "act", "dve", "pool"):
            if self.cnt[e]:
                ep = (self.cnt[e] - 1) // EPOCH
                fin.append((self.esem[("c", e, ep)], (self.cnt[e] - 1) % EPOCH + 1))
        with nc.Block() as block:
            def run(eng, lst, final=None):
                for waits, fn, inc in lst:
                    for s, v in waits:
                        eng.wait_ge(s, v)
                    fn(eng).then_inc(inc[0], inc[1])
                if final:
                    for s, v in final:
                        eng.wait_ge(s, v)

            @block.tensor
            def _(eng):
                run(eng, self.ops["pe"])

            @block.scalar
            def _(eng):
                run(eng, self.ops["act"])

            @block.vector
            def _(eng):
                run(eng, self.ops["dve"])

            @block.gpsimd
            def _(eng):
                run(eng, self.ops["pool"])

            @block.sync
            def _(eng):
                run(eng, self.ops["sp"], fin)

    def dma(self, out, in_, q="sp", **kw):
        return self.op(q, lambda eng: eng.dma_start(out=out.ap, in_=in_.ap, **kw),
                       reads=[in_], writes=[out], dma=True)

    def mm(self, out, lhsT, rhs, start=True, stop=True, **kw):
        return self.op("pe", lambda eng: eng.matmul(out.ap, lhsT.ap, rhs.ap, start=start, stop=stop, **kw),
                       reads=[lhsT, rhs], writes=[out])

    def transpose(self, out, in_, ident):
        return self.op("pe", lambda eng: eng.transpose(out.ap, in_.ap, ident.ap),
                       reads=[in_, ident], writes=[out])

    def act(self, out, in_, func, bias=None, scale=1.0, accum_out=None, e="act"):
        reads = [in_]
        kw = {}
        if isinstance(bias, V):
            reads.append(bias)
            kw["bias"] = bias.ap
        elif bias is not None:
            kw["bias"] = bias
        if isinstance(scale, V):
            reads.append(scale)
            kw["scale"] = scale.ap
        else:
            kw["scale"] = scale
        writes = [out]
        if accum_out is not None:
            writes.append(accum_out)
            kw["accum_out"] = accum_out.ap
        return self.op(e, lambda eng: eng.activation(out=out.ap, in_=in_.ap, func=func, **kw),
                       reads=reads, writes=writes)

    def tt(self, out, in0, in1, op, e="dve"):
        return self.op(e, lambda eng: eng.tensor_tensor(out=out.ap, in0=in0.ap, in1=in1.ap, op=op),
                       reads=[in0, in1], writes=[out])

    def ts(self, out, in0, s1, s2=None, op0=ALU.mult, op1=None, accum_out=None, e="dve"):
        reads = [in0]
        a1 = s1
        if isinstance(s1, V):
            reads.append(s1)
            a1 = s1.ap
        a2 = s2
        if isinstance(s2, V):
            reads.append(s2)
            a2 = s2.ap
        kw = {}
        if op1 is not None:
            kw["op1"] = op1
        writes = [out]
        if accum_out is not None:
            writes.append(accum_out)
            kw["accum_out"] = accum_out.ap
        return self.op(e, lambda eng: eng.tensor_scalar(out=out.ap, in0=in0.ap, scalar1=a1, scalar2=a2,
                                                        op0=op0, **kw),
                       reads=reads, writes=writes)

    def stt(self, out, in0, scalar, in1, op0, op1, e="dve"):
        reads = [in0, in1]
        a = scalar
        if isinstance(scalar, V):
            reads.append(scalar)
            a = scalar.ap
        return self.op(e, lambda eng: eng.scalar_tensor_tensor(out=out.ap, in0=in0.ap, scalar=a, in1=in1.ap,
                                                               op0=op0, op1=op1),
                       reads=reads, writes=[out])

    def copy(self, out, in_, e="dve"):
        if e == "act":
            return self.op(e, lambda eng: eng.copy(out=out.ap, in_=in_.ap), reads=[in_], writes=[out])
        return self.op(e, lambda eng: eng.tensor_copy(out=out.ap, in_=in_.ap), reads=[in_], writes=[out])

    def memset(self, out, val, e="pool"):
        return self.op(e, lambda eng: eng.memset(out.ap, val), writes=[out])

    def reduce(self, out, in_, op=ALU.add, axis=AX.X, e="dve"):
        return self.op(e, lambda eng: eng.tensor_reduce(out=out.ap, in_=in_.ap, axis=axis, op=op),
                       reads=[in_], writes=[out])

    def recip(self, out, in_):
        return self.op("dve", lambda eng: eng.reciprocal(out=out.ap, in_=in_.ap), reads=[in_], writes=[out])

    def scan(self, out, d0, d1, init, op0=ALU.mult, op1=ALU.add):
        reads = [d0, d1]
        a = init
        if isinstance(init, V):
            reads.append(init)
            a = init.ap
        return self.op("dve", lambda eng: eng.tensor_tensor_scan(out=out.ap, data0=d0.ap, data1=d1.ap,
                                                                 initial=a, op0=op0, op1=op1),
                       reads=reads, writes=[out])

    def barrier(self):
        fin = {}
        for q in ("sp", "pool", "act"):
            n = self.dma_n[q]
            slots = self.dma_slots[q]
            for i in range(min(n, len(slots))):
                uses = (n - i + len(slots) - 1) // len(slots)
                fin[("d", q, i)] = 16 * uses
        for e in ("pe", "act", "dve", "pool"):
            if self.cnt[e]:
                ep = (self.cnt[e] - 1) // EPOCH
                fin[("c", e, ep)] = (self.cnt[e] - 1) % EPOCH + 1
        for e in ENGS:
            waits = {}
            for k, v in fin.items():
                if k[0] == "c" and k[1] == e:
                    continue
                if self.seen[e].get(k, 0) < v:
                    waits[k] = v
                    self.seen[e][k] = v
            if waits:
                if e == "sp":
                    continue_fn = None
                self.pending = getattr(self, "pending", {})
                self.pending.setdefault(e, {}).update(waits)

    def scope(self):
        return _Scope(self)


class _Scope:
    def __init__(self, mk):
        self.mk = mk

    def __enter__(self):
        self.old = self.mk.stack
        self.st = ExitStack()
        self.mk.stack = self.st
        return self

    def __exit__(self, *a):
        self.mk.barrier()
        self.mk.stack = self.old
        self.st.close()
        return False


D = 1024
T = 2048
NSEQ = 2
NTOK = NSEQ * T
NT = NTOK // 128
CAP = 512
NE = 32
NSLOT = NE * CAP
NROW_TL = NSLOT + 128
RMS_EPS = 1e-6


class DR:
    def __init__(self, mk, name, shape, dtype, kind="Internal", rows_per=128):
        self.t = mk.nc.dram_tensor(name, list(shape), dtype, kind=kind)
        self.ap = self.t.ap()
        self.rows_per = rows_per
        n = (shape[0] + rows_per - 1) // rows_per
        self.bufs = [Buf("%s_%d" % (name, i)) for i in range(n)]
        self.whole = Buf(name)

    def rows(self, r0, n):
        assert r0 % self.rows_per == 0 and n <= self.rows_per
        return V(self.ap[r0:r0 + n], self.bufs[r0 // self.rows_per])

    def all(self):
        return [V(self.ap, b) for b in self.bufs]


class Builder:
    def __init__(self, nc, stack):
        self.nc = nc
        self.mk = MK(nc, stack)
        mk = self.mk
        self.inp = {}
        self.c_ident = self.ext_in("c_ident", [128, 128], F32)
        self.c_tri = self.ext_in("c_tri", [128, 128], F32)
        self.ext_in("c_blk", [128, 128], F32)
        self.ext_in("c_masks", [3, 128, 128], F32)
        self.ident32 = mk.sb([128, 128], F32, "ident32")
        self.identb = mk.sb([128, 128], BF16, "identb")
        self.trib = mk.sb([128, 128], BF16, "trib")
        self.onesb = mk.sb([128, 128], BF16, "onesb")
        mk.dma(self.ident32, self.c_ident)
        mk.dma(self.identb, self.c_ident, q="pool")
        mk.dma(self.trib, self.c_tri, q="pool")
        mk.memset(self.onesb, 1.0)
        self.eps_t = mk.sb([128, 1], F32, "eps_t")
        mk.memset(self.eps_t, RMS_EPS)
        self.psum_all = mk.ps([128, 4096], F32, "psum_all")
        self.bank = [V(self.psum_all.ap[:, i * 512:(i + 1) * 512], Buf("bank%d" % i)) for i in range(8)]

    def scratch(self, name, shape, dtype, rows_per=128):
        if not hasattr(self, "_scr"):
            self._scr = {}
        if name not in self._scr:
            self._scr[name] = DR(self.mk, name, shape, dtype, rows_per=rows_per)
        return self._scr[name]

    def ext_in(self, name, shape, dtype=F32):
        v = self.mk.dram(name, shape, dtype, kind="ExternalInput")
        self.inp[name] = v
        return v

    def rms_tile(self, xt, gbc, h32, eps=RMS_EPS, sq=None, small=None):
        mk = self.mk
        ss, rstd = small
        mk.act(sq, xt, AF.Square, accum_out=ss)
        mk.act(rstd, ss, AF.Sqrt, scale=1.0 / D, bias=self.eps_t)
        mk.recip(rstd, rstd)
        mk.stt(h32, xt, rstd, gbc, ALU.mult, ALU.mult)

    def moe(self, xres, layer, P):
        mk = self.mk
        with mk.scope():
            Hrows = self.scratch("hrows", [NTOK + 128, D], BF16)
            Ybuf = self.scratch("ybuf", [NROW_TL, D], F32)
            TokL = self.scratch("tokl", [NROW_TL, 8], I32, rows_per=NROW_TL)
            gbc = mk.sb([128, D], F32, "gbc")
            mk.dma(gbc, P["norm_ffn_g"][layer:layer + 1, :].pbc(128))
            wr = mk.sb([128, 8, 36], F32, "wr")
            mk.dma(wr, P["w_router"][layer].re("(k p) n -> p k n", p=128))
            brt = mk.sb([128, 36], F32, "brt")
            mk.dma(brt, P["b_router"][layer:layer + 1, :].pbc(128))
            maskall = mk.sb([128, NT, 32], BF16, "maskall")
            E1all = mk.sb([128, NT, 32], F32, "E1all")
            E2all = mk.sb([128, NT, 32], F32, "E2all")
            gate1 = mk.sb([128, NT], F32, "gate1")
            gate2 = mk.sb([128, NT], F32, "gate2")
            slot1 = mk.sb([128, NT], I32, "slot1")
            slot2 = mk.sb([128, NT], I32, "slot2")
            base = mk.sb([128, 32], F32, "base")
            lim = mk.sb([128, 32], F32, "lim")
            trash = mk.sb([128, 1], F32, "trash")
            zero_t = mk.sb([128, D], F32, "zero_t")
            sent = mk.sb([128, (NROW_TL // 128) * 8], I32, "sent")
            mk.op("pool", lambda eng: eng.iota(base.ap, [[CAP, 32]], base=-1, channel_multiplier=0,
                                                allow_small_or_imprecise_dtypes=True), writes=[base])
            mk.ts(lim, base, float(CAP) + 0.5, None, op0=ALU.add)
            mk.op("pool", lambda eng: eng.iota(trash.ap, [[0, 1]], base=NSLOT, channel_multiplier=1,
                                                allow_small_or_imprecise_dtypes=True), writes=[trash])
            mk.memset(zero_t, 0.0)
            mk.op("pool", lambda eng: eng.iota(sent.ap, [[0, (NROW_TL // 128) * 8]], base=NTOK,
                                                channel_multiplier=0), writes=[sent])
            mk.dma(V(TokL.ap.rearrange("(p r) c -> p (r c)", p=128), TokL.bufs[0]), sent)
            mk.dma(Hrows.rows(NTOK, 128), zero_t.bitcast(BF16)[:, 0:D])
            mk.dma(Ybuf.rows(NSLOT, 128), zero_t)

            xts = [mk.sb([128, D], F32, "xt%d" % i) for i in range(2)]
            sqs = [mk.sb([128, D], F32, "sq%d" % i) for i in range(2)]
            h32s = [mk.sb([128, D], F32, "h32%d" % i) for i in range(2)]
            hbs = [mk.sb([128, D], BF16, "hb%d" % i) for i in range(2)]
            hT32s = [mk.sb([128, 8, 128], F32, "hT32%d" % i) for i in range(2)]
            smalls = [(mk.sb([128, 1], F32), mk.sb([128, 1], F32)) for i in range(2)]
            lg_all = mk.sb([128, NT, 36], F32, "lg_all")
            for i in range(NT):
                b = i % 2
                xt, sq, h32, hb, hT32 = xts[b], sqs[b], h32s[b], hbs[b], hT32s[b]
                mk.dma(xt, xres.rows(i * 128, 128))
                self.rms_tile(xt, gbc, h32, sq=sq, small=smalls[b])
                mk.copy(hb, h32, e="act")
                mk.dma(Hrows.rows(i * 128, 128), hb)
                for half in range(2):
                    pb = self.bank[2 * b + half]
                    for kk in range(4):
                        k = half * 4 + kk
                        mk.transpose(pb[:, kk * 128:(kk + 1) * 128], h32[:, k * 128:(k + 1) * 128], self.ident32)
                    mk.copy(hT32[:, half * 4:(half + 1) * 4, :].re("p k t -> p (k t)"), pb, e="act")
                pl = self.bank[4 + b]
                for k in range(8):
                    mk.mm(pl[:, 0:36], hT32[:, k, :], wr[:, k, :], start=(k == 0), stop=(k == 7))
                mk.tt(lg_all[:, i, :], pl[:, 0:36], brt, ALU.add)

            def bc(v, axis, shape):
                return V(v.ap.unsqueeze(axis).to_broadcast(list(shape)), v.buf)
            gl = lg_all[:, :, 0:4]
            el4 = lg_all[:, :, 4:36].re("p t (g e) -> p t g e", g=4)
            gmax = mk.sb([128, NT], F32)
            ohg = mk.sb([128, NT, 4], F32)
            eg = mk.sb([128, NT, 4], F32)
            gsum = mk.sb([128, NT], F32)
            tmp4 = mk.sb([128, NT, 4, 8], F32)
            sel = mk.sb([128, NT, 8], F32)
            sel2 = mk.sb([128, NT, 8], F32)
            m1 = mk.sb([128, NT], F32)
            m2 = mk.sb([128, NT], F32)
            oh1 = mk.sb([128, NT, 8], F32)
            oh2 = mk.sb([128, NT, 8], F32)
            mk.reduce(gmax, gl, op=ALU.max)
            mk.tt(ohg, gl, bc(gmax, 2, [128, NT, 4]), ALU.is_equal)
            mk.tt(eg, gl, bc(gmax, 2, [128, NT, 4]), ALU.subtract)
            mk.act(eg, eg, AF.Exp)
            mk.reduce(gsum, eg, op=ALU.add)
            mk.recip(gsum, gsum)
            mk.tt(tmp4, el4, bc(ohg, 3, [128, NT, 4, 8]), ALU.mult)
            mk.reduce(sel, tmp4.re("p t g e -> p t e g"), op=ALU.add)
            mk.reduce(m1, sel, op=ALU.max)
            mk.tt(oh1, sel, bc(m1, 2, [128, NT, 8]), ALU.is_equal)
            mk.stt(sel2, oh1, -1e30, sel, ALU.mult, ALU.add)
            mk.reduce(m2, sel2, op=ALU.max)
            mk.tt(oh2, sel2, bc(m2, 2, [128, NT, 8]), ALU.is_equal)
            mk.tt(m2, m2, m1, ALU.subtract)
            mk.act(m2, m2, AF.Exp)
            mk.ts(m2, m2, 1.0, None, op0=ALU.add)
            mk.recip(m2, m2)
            mk.tt(gate1, gsum, m2, ALU.mult)
            mk.tt(gate2, gsum, gate1, ALU.subtract)
            mk.tt(E1all.re("p t (g e) -> p t g e", g=4), bc(ohg, 3, [128, NT, 4, 8]), bc(oh1, 2, [128, NT, 4, 8]), ALU.mult)
            mk.tt(E2all.re("p t (g e) -> p t g e", g=4), bc(ohg, 3, [128, NT, 4, 8]), bc(oh2, 2, [128, NT, 4, 8]), ALU.mult)
            mk.tt(maskall, E1all, E2all, ALU.add)

            pos_ps = V(self.psum_all.ap[:, 0:1024], self.bank[0].buf)
            for i in range(NT):
                reg_ = V(self.psum_all.ap[:, i * 32:(i + 1) * 32], self.bank[i // 16].buf)
                for j in range(i):
                    mk.mm(reg_, self.onesb, maskall[:, j, :], start=(j == 0), stop=False)
                mk.mm(reg_, self.trib, maskall[:, i, :], start=(i == 0), stop=True)
            posf = mk.sb([128, NT, 32], F32, "posf")
            okm = mk.sb([128, NT, 32], F32, "okm")
            slf = mk.sb([128, NT], F32, "slf")
            mk.op("dve", lambda eng: eng.tensor_tensor(out=posf.ap, in0=pos_ps.ap.rearrange("p (t e) -> p t e", e=32),
                                                       in1=base.ap.unsqueeze(1).to_broadcast([128, NT, 32]), op=ALU.add),
                  reads=[self.bank[0], self.bank[1], base], writes=[posf])
            mk.tt(okm, posf, bc(lim, 1, [128, NT, 32]), ALU.is_lt)
            mk.ts(posf, posf, trash, None, op0=ALU.subtract)
            mk.tt(posf, posf, okm, ALU.mult)
            mk.ts(posf, posf, trash, None, op0=ALU.add)
            for Eall, slot in ((E1all, slot1), (E2all, slot2)):
                mk.tt(okm, posf, Eall, ALU.mult)
                mk.reduce(slf, okm, op=ALU.add)
                mk.copy(slot, slf)
            tokid = [mk.sb([128, 8], I32) for i in range(4)]
            sc_bufs = []
            init_v = V(TokL.ap, TokL.bufs[0])
            for i in range(NT):
                t_ = tokid[i % 4]
                mk.op("pool", lambda eng, t=t_, i=i: eng.iota(t.ap, [[0, 8]], base=i * 128,
                                                              channel_multiplier=1), writes=[t_])
                for slot in (slot1, slot2):
                    bf = Buf("tokl_sc")
                    sc_bufs.append(bf)
                    mk.op("pool", lambda eng, slot=slot, i=i, t=t_: eng.indirect_dma_start(
                        out=TokL.ap, out_offset=bass.IndirectOffsetOnAxis(ap=slot.ap[:, i:i + 1], axis=0),
                        in_=t.ap, in_offset=None),
                        reads=[slot, t_, init_v], writes=[V(TokL.ap, bf)], dma=True)
            tokl_all = [V(TokL.ap, bf) for bf in sc_bufs] + [init_v]

            NCT = CAP // 128
            wg = [mk.sb([128, 8, 512], BF16, "wg%d" % i) for i in range(2)]
            wu = [mk.sb([128, 8, 512], BF16, "wu%d" % i) for i in range(2)]
            wd = [mk.sb([128, 4, D], BF16, "wd%d" % i) for i in range(2)]
            idx = [mk.sb([128, 8], I32) for i in range(4)]
            xg = [mk.sb([128, D], BF16) for i in range(4)]
            xgT = [mk.sb([128, 8, CAP], BF16) for i in range(2)]
            hidT = [mk.sb([128, 4, CAP], BF16) for i in range(2)]
            sil = [mk.sb([128, CAP], F32) for i in range(2)]
            yrow = [mk.sb([128, D], F32) for i in range(2)]
            nslot = 0
            ny = 0
            for e in range(NE):
                b = e % 2
                mk.dma(wg[b], P["moe_w_gate"][layer, e].re("(k p) n -> p k n", p=128), q="pool")
                mk.dma(wu[b], P["moe_w_up"][layer, e].re("(k p) n -> p k n", p=128), q="pool")
                mk.dma(wd[b], P["moe_w_down"][layer, e].re("(k p) n -> p k n", p=128), q="pool")
                for j in range(NCT):
                    s = nslot % 4
                    nslot += 1
                    r0 = e * CAP + j * 128
                    mk.op("sp", lambda eng, s=s, r0=r0: eng.dma_start(out=idx[s].ap, in_=TokL.ap[r0:r0 + 128, :]),
                          reads=tokl_all, writes=[idx[s]], dma=True)
                    mk.op("pool", lambda eng, s=s: eng.indirect_dma_start(
                        out=xg[s].ap, out_offset=None, in_=Hrows.ap,
                        in_offset=bass.IndirectOffsetOnAxis(ap=idx[s].ap[:, 0:1], axis=0)),
                        reads=[idx[s]] + Hrows.all(), writes=[xg[s]], dma=True)
                    pb = self.bank[j % 2]
                    pbb = pb.bitcast(BF16)
                    for k in range(8):
                        mk.transpose(pbb[:, k * 128:(k + 1) * 128], xg[s][:, k * 128:(k + 1) * 128], self.identb)
                    mk.copy(xgT[b][:, :, j * 128:(j + 1) * 128], pbb.re("p (k t) -> p k t", k=8),
                            e=("act" if j % 2 else "dve"))
                for c in range(4):
                    pg = self.bank[2 + (c % 2)]
                    pu = self.bank[4 + (c % 2)]
                    for k in range(8):
                        mk.mm(pg, wg[b][:, k, c * 128:(c + 1) * 128], xgT[b][:, k, :], start=(k == 0), stop=(k == 7))
                    for k in range(8):
                        mk.mm(pu, wu[b][:, k, c * 128:(c + 1) * 128], xgT[b][:, k, :], start=(k == 0), stop=(k == 7))
                    mk.act(sil[c % 2], pg, AF.Silu)
                    mk.tt(hidT[b][:, c, :], sil[c % 2], pu, ALU.mult)
                for j in range(NCT):
                    yb = ny % 2
                    ny += 1
                    for half in range(2):
                        pd = self.bank[6 + half]
                        for c in range(4):
                            mk.mm(pd, hidT[b][:, c, j * 128:(j + 1) * 128], wd[b][:, c, half * 512:(half + 1) * 512],
                                  start=(c == 0), stop=(c == 3))
                        mk.copy(yrow[yb][:, half * 512:(half + 1) * 512], pd, e=("act" if half else "dve"))
                    mk.dma(Ybuf.rows(e * CAP + j * 128, 128), yrow[yb])

            y1 = [mk.sb([128, D], F32) for i in range(3)]
            y2 = [mk.sb([128, D], F32) for i in range(3)]
            xt3 = xts + [mk.sb([128, D], F32)]
            for i in range(NT):
                b = i % 3
                xt = xt3[b]
                mk.dma(xt, xres.rows(i * 128, 128))
                for slot, yy in ((slot1, y1[b]), (slot2, y2[b])):
                    mk.op("pool", lambda eng, slot=slot, yy=yy, i=i: eng.indirect_dma_start(
                        out=yy.ap, out_offset=None, in_=Ybuf.ap,
                        in_offset=bass.IndirectOffsetOnAxis(ap=slot.ap[:, i:i + 1], axis=0)),
                        reads=[slot] + Ybuf.all(), writes=[yy], dma=True)
                mk.stt(xt, y1[b], gate1[:, i:i + 1], xt, ALU.mult, ALU.add)
                mk.stt(xt, y2[b], gate2[:, i:i + 1], xt, ALU.mult, ALU.add)
                mk.dma(xres.rows(i * 128, 128), xt)


def host_consts():
    ident = np.eye(128, dtype=np.float32)
    tri = np.triu(np.ones((128, 128), np.float32))
    return {"c_ident": ident, "c_tri": tri}


MOE_KEYS = ("norm_ffn_g", "w_router", "b_router", "moe_w_gate", "moe_w_up", "moe_w_down")


def host_moe_params(inputs):
    out = {}
    out["norm_ffn_g"] = np.ascontiguousarray(inputs["norm_ffn_g"], dtype=np.float32)
    out["w_router"] = np.ascontiguousarray(
        np.concatenate([inputs["moe_w_group"], inputs["moe_w_expert"]], axis=-1), dtype=np.float32)
    out["b_router"] = np.ascontiguousarray(
        np.concatenate([inputs["moe_b_group"], inputs["moe_b_expert"]], axis=-1), dtype=np.float32)
    for k in ("moe_w_gate", "moe_w_up", "moe_w_down"):
        out[k] = np.ascontiguousarray(inputs[k], dtype=np.float32)
    return out


TWO_PI = 6.283185307179586
CW1 = 6.28125
CW2 = TWO_PI - CW1
PI_SAFE = 3.1415925


def _rr_sin(self, dst, X, tmpf, tmpi, phase=0.0, e="dve"):
    mk = self.mk
    mk.ts(tmpf, X, 1.0 / TWO_PI, 0.5 + phase / TWO_PI, op0=ALU.mult, op1=ALU.add, e=e)
    mk.copy(tmpi, tmpf, e=e)
    mk.copy(tmpf, tmpi, e=e)
    mk.stt(dst, tmpf, -CW1, X, ALU.mult, ALU.add)
    mk.stt(dst, tmpf, -CW2, dst, ALU.mult, ALU.add)
    if phase:
        mk.ts(dst, dst, phase, None, op0=ALU.add, e=e)
    mk.ts(tmpf, dst, -PI_SAFE, TWO_PI, op0=ALU.is_lt, op1=ALU.mult, e=e)
    mk.tt(dst, dst, tmpf, ALU.add, e=e)
    mk.ts(tmpf, dst, PI_SAFE, TWO_PI, op0=ALU.is_gt, op1=ALU.mult, e=e)
    mk.tt(dst, dst, tmpf, ALU.subtract, e=e)
    mk.ts(dst, dst, PI_SAFE, -PI_SAFE, op0=ALU.min, op1=ALU.max, e=e)
    mk.act(dst, dst, AF.Sin)


Builder.rr_sin = _rr_sin


def _proj_in(self, xres, g_row, W, ncols, fm_blocks, uT, tm_range, u_tm):
    mk = self.mk
    with mk.scope():
        gbc = mk.sb([128, D], F32, "gbc")
        mk.dma(gbc, g_row.pbc(128))
        Wb = mk.sb([128, 8, ncols], BF16, "Wb")
        for k in range(8):
            mk.dma(Wb[:, k, :], W[k * 128:(k + 1) * 128, :], q="pool")
        xts = [mk.sb([128, D], F32) for i in range(2)]
        sqs = [mk.sb([128, D], F32) for i in range(2)]
        hbs = [mk.sb([128, D], BF16) for i in range(2)]
        smalls = [(mk.sb([128, 1], F32), mk.sb([128, 1], F32)) for i in range(2)]
        hT = [mk.sb([128, 8, 512], BF16) for i in range(2)]
        ev = [mk.sb([128, 512], F32) for i in range(4)]
        nev = 0
        for gidx in range(NTOK // 512):
            hb_ = hT[gidx % 2]
            for tl in range(4):
                i = gidx * 4 + tl
                b = i % 2
                mk.dma(xts[b], xres.rows(i * 128, 128))
                self.rms_tile(xts[b], gbc, hbs[b], sq=sqs[b], small=smalls[b])
                pbb = self.bank[b].bitcast(BF16)
                for k in range(8):
                    mk.transpose(pbb[:, k * 128:(k + 1) * 128], hbs[b][:, k * 128:(k + 1) * 128], self.identb)
                mk.copy(hb_[:, :, tl * 128:(tl + 1) * 128], pbb.re("p (k t) -> p k t", k=8),
                        e=("act" if tl % 2 else "pool_never") if False else ("act" if tl % 2 else "dve"))
            for bi, cb in enumerate(fm_blocks):
                pb = self.bank[2 + (bi % 3)]
                for k in range(8):
                    mk.mm(pb, Wb[:, k, cb * 128:(cb + 1) * 128], hb_[:, k, :], start=(k == 0), stop=(k == 7))
                t = ev[nev % 4]
                mk.copy(t, pb, e=("act" if nev % 2 else "dve"))
                nev += 1
                mk.dma(V(uT.ap[bi * 128:(bi + 1) * 128, gidx * 512:(gidx + 1) * 512], uT.bufs[bi]), t)
            if tm_range is not None:
                c0, c1 = tm_range
                for tl in range(4):
                    i = gidx * 4 + tl
                    for cc in range(c0, c1, 512):
                        pb = self.bank[5 + (nev % 3)]
                        for k in range(8):
                            mk.mm(pb, hb_[:, k, tl * 128:(tl + 1) * 128], Wb[:, k, cc:cc + 512],
                                  start=(k == 0), stop=(k == 7))
                        t = ev[nev % 4]
                        mk.copy(t, pb, e=("act" if nev % 2 else "dve"))
                        nev += 1
                        mk.dma(V(u_tm.ap[i * 128:(i + 1) * 128, cc - c0:cc - c0 + 512], u_tm.bufs[i]), t)


Builder.proj_in = _proj_in


def _proj_out(self, xres_in, xres_out, oTd, Wout):
    mk = self.mk
    with mk.scope():
        Wb = mk.sb([128, 8, D], BF16, "Wob")
        for k in range(8):
            mk.dma(Wb[:, k, :], Wout[k * 128:(k + 1) * 128, :], q="pool")
        xts = [mk.sb([128, D], F32) for i in range(2)]
        ot = [mk.sb([128, 8, 512], BF16) for i in range(2)]
        for gi in range(NTOK // 512):
            o_ = ot[gi % 2]
            for k in range(8):
                mk.dma(o_[:, k, :], V(oTd.ap[k * 128:(k + 1) * 128, gi * 512:(gi + 1) * 512], oTd.bufs[k]))
            for tl in range(4):
                i = gi * 4 + tl
                b = i % 2
                mk.dma(xts[b], xres_in.rows(i * 128, 128))
                for half in range(2):
                    pb = self.bank[(i % 2) * 2 + half]
                    for k in range(8):
                        mk.mm(pb, o_[:, k, tl * 128:(tl + 1) * 128], Wb[:, k, half * 512:(half + 1) * 512],
                              start=(k == 0), stop=(k == 7))
                    mk.tt(xts[b][:, half * 512:(half + 1) * 512], xts[b][:, half * 512:(half + 1) * 512], pb, ALU.add)
                mk.dma(xres_out.rows(i * 128, 128), xts[b])


Builder.proj_out = _proj_out


def _attn(self, u_tm, oT, P, layer):
    import math
    mk = self.mk
    oi = layer // 2
    lam_init = 0.8 - 0.6 * math.exp(-0.3 * layer)
    with mk.scope():
        gqk = mk.sb([128, D], F32, "gqk")
        mk.dma(gqk, P["da_qk_gain"][oi:oi + 1, :].pbc(128))
        subg = mk.sb([128, 128], F32, "subg")
        mk.dma(subg, P["da_subln"][oi:oi + 1, :].pbc(128))
        mk.ts(subg, subg, 1.0 - lam_init, None, op0=ALU.mult)
        lamv = mk.sb([128, 4, 64], F32, "lamv")
        mk.dma(lamv.re("p a d -> p (a d)"), P["da_lam"][oi:oi + 1].re("o a d -> o (a d)").pbc(128))
        lt = mk.sb([128, 2, 64], F32)
        ls = mk.sb([128, 2], F32)
        mk.tt(lt[:, 0, :], lamv[:, 0, :], lamv[:, 1, :], ALU.mult)
        mk.tt(lt[:, 1, :], lamv[:, 2, :], lamv[:, 3, :], ALU.mult)
        mk.reduce(ls, lt, op=ALU.add)
        mk.act(ls, ls, AF.Exp)
        nlam = mk.sb([128, 1], F32, "nlam")
        mk.tt(nlam, ls[:, 1:2], ls[:, 0:1], ALU.subtract)
        mk.ts(nlam, nlam, -lam_init, None, op0=ALU.add)
        eps5 = mk.sb([128, 1], F32)
        mk.memset(eps5, 1e-5)
        nshift = mk.sb([128, 1], F32)
        mk.memset(nshift, -4.0)
        zb = mk.sb([128, 512], BF16, "zb")
        mk.memset(zb, 0.0)
        jf = mk.sb([128, 32], F32)
        mk.op("pool", lambda eng: eng.iota(jf.ap, [[1, 32]], base=0, channel_multiplier=0,
                                            allow_small_or_imprecise_dtypes=True), writes=[jf])
        mk.act(jf, jf, AF.Exp, scale=-math.log(10000.0) / 32.0)
        posf = mk.sb([128, 16], F32)
        mk.op("pool", lambda eng: eng.iota(posf.ap, [[128, 16]], base=0, channel_multiplier=1,
                                            allow_small_or_imprecise_dtypes=True), writes=[posf])
        ang = mk.sb([128, 16, 32], F32)
        mk.tt(ang, V(jf.ap.unsqueeze(1).to_broadcast([128, 16, 32]), jf.buf),
              V(posf.ap.unsqueeze(2).to_broadcast([128, 16, 32]), posf.buf), ALU.mult)
        sint = mk.sb([128, 16, 32], F32, "sint")
        cost = mk.sb([128, 16, 32], F32, "cost")
        tf = mk.sb([128, 16, 32], F32)
        ti = mk.sb([128, 16, 32], I32)
        self.rr_sin(sint, ang, tf, ti)
        self.rr_sin(cost, ang, tf, ti, phase=math.pi / 2)

        QT = mk.sb([128, 4, T], BF16, "QT")
        KT = mk.sb([128, 4, T], BF16, "KT")
        Vt = mk.sb([128, 16, 512], BF16, "Vt")
        qk = [mk.sb([128, 16, 2, 32], F32) for i in range(2)]
        sq = mk.sb([128, 16, 64], F32)
        ss = mk.sb([128, 16], F32)
        ta = mk.sb([128, 16, 32], F32)
        tb = mk.sb([128, 16, 32], F32)
        qr = [mk.sb([128, 16, 2, 32], BF16) for i in range(2)]
        pts = [mk.sb([128, 512], BF16) for i in range(3)]
        rls = [mk.sb([128, 8], F32) for i in range(2)]
        ob32 = [mk.sb([128, 128], F32) for i in range(2)]
        obb = [mk.sb([128, 128], BF16) for i in range(2)]
        junk = mk.sb([128, 128], F32)
        ss1 = [mk.sb([128, 1], F32) for i in range(2)]
        npt = 0
        nfin = 0
        ostage = [mk.sb([128, T], BF16, "ostage%d" % i) for i in range(2)]
        for s in range(NSEQ):
            mk.dma(Vt, V(u_tm.ap[s * T:(s + 1) * T, 1024:1536].rearrange("(i p) c -> p i c", p=128),
                         u_tm.whole), q="pool", )
            for i in range(16):
                b = i % 2
                row0 = s * T + i * 128
                q_ = qk[b]
                qf = q_.re("p g m d -> p (g m d)")
                mk.dma(qf, V(u_tm.ap[row0:row0 + 128, 0:1024], u_tm.bufs[row0 // 128]))
                mk.act(sq.re("p g d -> p (g d)"), qf, AF.Square)
                mk.reduce(ss, sq, op=ALU.add)
                mk.act(ss, ss, AF.Sqrt, scale=1.0 / 64.0, bias=self.eps_t)
                mk.recip(ss, ss)
                q3 = q_.re("p g m d -> p g (m d)")
                mk.tt(q3, q3, V(ss.ap.unsqueeze(2).to_broadcast([128, 16, 64]), ss.buf), ALU.mult)
                mk.tt(qf, qf, gqk, ALU.mult)
                cb_ = V(cost.ap[:, i, :].unsqueeze(1).to_broadcast([128, 16, 32]), cost.buf)
                sb_ = V(sint.ap[:, i, :].unsqueeze(1).to_broadcast([128, 16, 32]), sint.buf)
                x1 = q_[:, :, 0, :]
                x2 = q_[:, :, 1, :]
                mk.tt(ta, x1, cb_, ALU.mult)
                mk.tt(tb, x2, sb_, ALU.mult, e=DBG.get("at_eng", "dve"))
                mk.tt(qr[b][:, :, 0, :], ta, tb, ALU.subtract)
                mk.tt(ta, x2, cb_, ALU.mult)
                mk.tt(tb, x1, sb_, ALU.mult, e=DBG.get("at_eng", "dve"))
                mk.tt(qr[b][:, :, 1, :], ta, tb, ALU.add)
                qrf = qr[b].re("p g m d -> p (g m d)")
                pbb = self.bank[6 + b].bitcast(BF16)
                for k in range(8):
                    mk.transpose(pbb[:, k * 128:(k + 1) * 128], qrf[:, k * 128:(k + 1) * 128], self.identb)
                mk.copy(QT[:, :, i * 128:(i + 1) * 128], pbb[:, 0:512].re("p (h t) -> p h t", h=4), e="act")
                mk.copy(KT[:, :, i * 128:(i + 1) * 128], pbb[:, 512:1024].re("p (h t) -> p h t", h=4), e="act")
            units = [(h, qc) for h in range(4) for qc in range(4)]

            def osets(u):
                par = u % 2
                O = [self.bank[2], self.bank[3]] if par == 0 else [self.bank[6], self.bank[7]]
                Lb = V(self.bank[4].ap[:, 8 * par:8 * par + 8], self.bank[4].buf)
                return O, Lb

            def main(u):
                nonlocal npt
                h, qc = units[u]
                O, Lb = osets(u)
                mk.mm(O[0], zb[:, 0:128], zb)
                mk.mm(O[1], zb[:, 0:128], zb)
                mk.mm(Lb, zb[:, 0:128], zb[:, 0:8])
                steps = [(m, kt) for m in range(2) for kt in range(4 * qc + 4)]
                info = []

                def issue_S(i):
                    nonlocal npt
                    m, kt = steps[i]
                    q0 = max(kt * 128, qc * 512)
                    nq = (qc + 1) * 512 - q0
                    S = self.bank[npt % 2]
                    Pt = pts[npt % 3]
                    npt += 1
                    mk.mm(S[:, 0:nq], KT[m * 64:(m + 1) * 64, h, kt * 128:(kt + 1) * 128],
                          QT[m * 64:(m + 1) * 64, h, q0:q0 + nq])
                    info.append((S, Pt, q0, nq))

                issue_S(0)
                for i, (m, kt) in enumerate(steps):
                    if i + 1 < len(steps):
                        issue_S(i + 1)
                    S, Pt, q0, nq = info[i]
                    mk.act(Pt[:, 0:nq], S[:, 0:nq], AF.Exp, scale=0.125, bias=nshift)
                    if kt >= 4 * qc:
                        mk.tt(Pt[:, 0:128], Pt[:, 0:128], self.trib, ALU.mult, e="pool")
                    for qb in range(max(kt, 4 * qc), 4 * qc + 4):
                        ql = qb - 4 * qc
                        c0 = qb * 128 - q0
                        mk.mm(O[m][:, ql * 128:(ql + 1) * 128], Pt[:, c0:c0 + 128],
                              Vt[:, kt, h * 128:(h + 1) * 128], start=False, stop=(kt == qb),
                              skip_group_check=True)
                        mk.mm(Lb[:, m * 4 + ql:m * 4 + ql + 1], Pt[:, c0:c0 + 128], self.onesb[:, 0:1],
                              start=False, stop=(kt == qb), skip_group_check=True)

            def fin(u):
                nonlocal nfin
                h, qc = units[u]
                O, Lb = osets(u)
                rl = rls[u % 2]
                mk.recip(rl, Lb)
                mk.ts(rl[:, 4:8], rl[:, 4:8], nlam, None, op0=ALU.mult)
                for ql in range(4):
                    fb = nfin % 2
                    nfin += 1
                    o = ob32[fb]
                    mk.ts(o, O[0][:, ql * 128:(ql + 1) * 128], rl[:, ql:ql + 1], None, op0=ALU.mult)
                    mk.stt(o, O[1][:, ql * 128:(ql + 1) * 128], rl[:, 4 + ql:5 + ql], o, ALU.mult, ALU.add)
                    mk.act(junk, o, AF.Square, accum_out=ss1[fb])
                    mk.act(ss1[fb], ss1[fb], AF.Sqrt, scale=1.0 / 128.0, bias=eps5)
                    mk.recip(ss1[fb], ss1[fb])
                    mk.stt(obb[fb], o, ss1[fb], subg, ALU.mult, ALU.mult)
                    ptr = self.bank[5].bitcast(BF16)
                    mk.transpose(ptr[:, fb * 128:(fb + 1) * 128], obb[fb], self.identb)
                    t0 = (4 * qc + ql) * 128
                    mk.copy(ostage[h % 2][:, t0:t0 + 128], ptr[:, fb * 128:(fb + 1) * 128], e="act")
                if qc == 3:
                    mk.dma(V(oT.ap[h * 128:(h + 1) * 128, s * T:(s + 1) * T], oT.bufs[h]), ostage[h % 2])

            main(0)
            for u in range(len(units)):
                if u + 1 < len(units):
                    main(u + 1)
                fin(u)


Builder.attn = _attn


def _s5_params(self, a_re, a_im, lstep, shape, want_coef):
    import math
    mk = self.mk
    n = lambda: mk.sb(shape, F32)
    are, step, lr, th, rho = n(), n(), n(), n(), n()
    mk.ts(are, a_re, -1e-4, None, op0=ALU.min)
    mk.act(step, lstep, AF.Exp)
    mk.tt(lr, are, step, ALU.mult)
    mk.tt(th, a_im, step, ALU.mult)
    mk.act(rho, lr, AF.Exp)
    out = dict(rho=rho, th=th)
    if want_coef:
        sn, cs, tf, x, y, den, cre, cim = n(), n(), n(), n(), n(), n(), n(), n()
        ti = mk.sb(shape, I32)
        self.rr_sin(sn, th, tf, ti)
        self.rr_sin(cs, th, tf, ti, phase=math.pi / 2)
        mk.tt(x, rho, cs, ALU.mult)
        mk.ts(x, x, -1.0, None, op0=ALU.add)
        mk.tt(y, rho, sn, ALU.mult)
        mk.tt(den, are, are, ALU.mult)
        mk.tt(tf, a_im, a_im, ALU.mult)
        mk.tt(den, den, tf, ALU.add)
        mk.recip(den, den)
        mk.tt(cre, x, are, ALU.mult)
        mk.tt(tf, y, a_im, ALU.mult)
        mk.tt(cre, cre, tf, ALU.add)
        mk.tt(cre, cre, den, ALU.mult)
        mk.tt(cim, y, are, ALU.mult)
        mk.tt(tf, x, a_im, ALU.mult)
        mk.tt(cim, cim, tf, ALU.subtract)
        mk.tt(cim, cim, den, ALU.mult)
        out.update(cre=cre, cim=cim)
    return out


Builder.s5_params = _s5_params


def _s5(self, uT, oT, P, layer):
    import math
    mk = self.mk
    oi = layer // 2
    with mk.scope():
        bbr = mk.sb([128, 4, 128], BF16, "bbr")
        bbi = mk.sb([128, 4, 128], BF16, "bbi")
        rho = mk.sb([128, 16], F32, "rho16")
        theta = mk.sb([128, 16], F32, "th16")
        bfr = mk.sb([128, 16, 128], BF16, "bfr")
        bfi = mk.sb([128, 16, 128], BF16, "bfi")
        Cfr = mk.sb([128, 16, 128], BF16, "Cfr")
        Cfi = mk.sb([128, 16, 128], BF16, "Cfi")
        with mk.scope():
            rep = mk.sb([128, 3, 512], F32, "rep")
            mk.dma(rep, P["s5_rep"][oi].re("a p j s -> p a (j s)"))
            pr_ = self.s5_params(rep[:, 0, :], rep[:, 1, :], rep[:, 2, :], [128, 512], True)
            Bre = mk.sb([128, 512], F32)
            Bim = mk.sb([128, 512], F32)
            mk.dma(Bre, P["s5_bbd_re"][oi].re("p j s -> p (j s)"))
            mk.dma(Bim, P["s5_bbd_im"][oi].re("p j s -> p (j s)"))
            t1p = mk.sb([128, 512], F32)
            t2p = mk.sb([128, 512], F32)
            mk.tt(t1p, Bre, pr_["cre"], ALU.mult)
            mk.tt(t2p, Bim, pr_["cim"], ALU.mult)
            mk.tt(bbr.re("p j s -> p (j s)"), t1p, t2p, ALU.subtract)
            mk.tt(t1p, Bre, pr_["cim"], ALU.mult)
            mk.tt(t2p, Bim, pr_["cre"], ALU.mult)
            mk.tt(bbi.re("p j s -> p (j s)"), t1p, t2p, ALU.add)
            mk.memset(bfr, 0.0)
            mk.memset(bfi, 0.0)
            for q in range(4):
                for (src, dst) in ((bbr, bfr), (bbi, bfi)):
                    mk.copy(dst[32 * q:32 * q + 32].re("p (j q) s -> p j q s", q=4)[:, :, q, :],
                            src[32 * q:32 * q + 32, :, :], e="pool")
            st = mk.sb([128, 3, 16], F32, "st")
            mk.dma(st, P["s5_st"][oi].re("a p b -> p a b"))
            ps_ = self.s5_params(st[:, 0, :], st[:, 1, :], st[:, 2, :], [128, 16], False)
            mk.copy(rho, ps_["rho"])
            mk.copy(theta, ps_["th"])
        Cre = mk.sb([128, 16, 32], BF16, "Cre")
        nCim = mk.sb([128, 16, 32], BF16, "nCim")
        cim32 = mk.sb([128, 16, 32], F32)
        mk.dma(Cre, P["s5_cbd_re"][oi], q="pool")
        mk.dma(cim32, P["s5_cbd_im"][oi])
        mk.ts(nCim, cim32, -1.0, None, op0=ALU.mult)
        mk.memset(Cfr, 0.0)
        mk.memset(Cfi, 0.0)
        for q in range(4):
            for (src, dst) in ((Cre, Cfr), (nCim, Cfi)):
                mk.copy(dst.re("p (j q) c -> p j q c", q=4)[:, :, q, 32 * q:32 * q + 32],
                        src.re("p (j q) c -> p j q c", q=4)[:, :, q, :], e="pool")
        dcol = mk.sb([128, 4], F32, "dcol")
        mk.dma(dcol, P["s5_dcol"][oi])
        cbase = mk.sb([128, 16, 64], F32, "cbase")
        sbase = mk.sb([128, 16, 64], F32, "sbase")
        cstep = mk.sb([128, 16, 32], F32, "cstep")
        sstep = mk.sb([128, 16, 32], F32, "sstep")
        with mk.scope():
            rio = mk.sb([128, 64], F32)
            mk.op("pool", lambda eng: eng.iota(rio.ap, [[1, 64]], base=0, channel_multiplier=0,
                                                allow_small_or_imprecise_dtypes=True), writes=[rio])
            kio = mk.sb([128, 32], F32)
            mk.op("pool", lambda eng: eng.iota(kio.ap, [[64, 32]], base=0, channel_multiplier=0,
                                                allow_small_or_imprecise_dtypes=True), writes=[kio])
            angb = mk.sb([128, 16, 64], F32)
            angs = mk.sb([128, 16, 32], F32)
            mk.tt(angb, V(theta.ap.unsqueeze(2).to_broadcast([128, 16, 64]), theta.buf),
                  V(rio.ap.unsqueeze(1).to_broadcast([128, 16, 64]), rio.buf), ALU.mult)
            mk.tt(angs, V(theta.ap.unsqueeze(2).to_broadcast([128, 16, 32]), theta.buf),
                  V(kio.ap.unsqueeze(1).to_broadcast([128, 16, 32]), kio.buf), ALU.mult)
            tfb = mk.sb([128, 16, 64], F32)
            tib = mk.sb([128, 16, 64], I32)
            self.rr_sin(sbase, angb, tfb, tib)
            self.rr_sin(cbase, angb, tfb, tib, phase=math.pi / 2)
            self.rr_sin(sstep, angs, tfb[:, :, 0:32], tib[:, :, 0:32])
            self.rr_sin(cstep, angs, tfb[:, :, 0:32], tib[:, :, 0:32], phase=math.pi / 2)
        ubb = mk.sb([128, NTOK], BF16, "ubb")
        zTd = self.scratch("zTd", [512, NTOK], BF16)
        yT = mk.sb([128, NTOK], F32, "yT")
        sint2 = [mk.sb([128, T], F32, "sint%d" % i) for i in range(2)]
        cost2 = [mk.sb([128, T], F32, "cost%d" % i) for i in range(2)]
        gre2 = [mk.sb([128, T], F32, "gre%d" % i) for i in range(2)]
        gim2 = [mk.sb([128, T], F32, "gim%d" % i) for i in range(2)]
        wre2 = [mk.sb([128, T], F32, "wre%d" % i) for i in range(2)]
        wim2 = [mk.sb([128, T], F32, "wim%d" % i) for i in range(2)]
        xre2 = [mk.sb([128, T], BF16, "xre%d" % i) for i in range(2)]
        xim2 = [mk.sb([128, T], BF16, "xim%d" % i) for i in range(2)]
        tt1 = mk.sb([128, T], F32, "tt1")
        tt2 = mk.sb([128, T], F32, "tt2")
        bur_f = mk.sb([128, T], F32, "bur_f")
        bui_f = mk.sb([128, T], F32, "bui_f")
        ubb2 = [ubb, ubb]
        state = dict(nb=0)

        S5E = DBG.get('s5_eng', 'dve')

        def tables(sbi):
            sint, cost = sint2[sbi % 2], cost2[sbi % 2]
            gre, gim, wre, wim = gre2[0], gim2[0], wre2[0], wim2[0]
            cs_b = V(cstep.ap[:, sbi, :].unsqueeze(2).to_broadcast([128, 32, 64]), cstep.buf)
            ss_b = V(sstep.ap[:, sbi, :].unsqueeze(2).to_broadcast([128, 32, 64]), sstep.buf)
            cb_b = V(cbase.ap[:, sbi, :].unsqueeze(1).to_broadcast([128, 32, 64]), cbase.buf)
            sb_b = V(sbase.ap[:, sbi, :].unsqueeze(1).to_broadcast([128, 32, 64]), sbase.buf)
            v3_ = lambda t_: t_.re("p (k r) -> p k r", r=64)
            mk.tt(v3_(tt1), cs_b, cb_b, ALU.mult)
            mk.tt(v3_(tt2), ss_b, sb_b, ALU.mult, e=S5E)
            mk.tt(cost, tt1, tt2, ALU.subtract)
            mk.tt(v3_(tt1), ss_b, cb_b, ALU.mult, e=S5E)
            mk.tt(v3_(tt2), cs_b, sb_b, ALU.mult)
            mk.tt(sint, tt1, tt2, ALU.add, e=S5E)

        def multi(b0, nb_):
            return V(self.psum_all.ap[:, b0 * 512:(b0 + nb_) * 512], self.bank[b0].buf)

        def stageA(it):
            sbi, s = it // NSEQ, it % NSEQ
            cb = sbi // 4
            sint, cost = sint2[sbi % 2], cost2[sbi % 2]
            gre, gim, wre, wim = gre2[it % 2], gim2[it % 2], wre2[it % 2], wim2[it % 2]
            ub_ = ubb2[cb % 2]
            rho_b = V(rho.ap[:, sbi:sbi + 1].to_broadcast([128, T]), rho.buf)
            for ch in range(4):
                tok0 = s * T + ch * 512
                mk.mm(self.bank[ch], bfr[:, sbi, :], ub_[:, tok0:tok0 + 512])
            for ch in range(4):
                tok0 = s * T + ch * 512
                mk.mm(self.bank[4 + ch], bfi[:, sbi, :], ub_[:, tok0:tok0 + 512])
            rd_r = [self.bank[i] for i in range(1, 4)]
            rd_i = [self.bank[i] for i in range(5, 8)]
            src_r, src_i = multi(0, 4), multi(4, 4)
            mk.op("act", lambda eng: eng.copy(out=bur_f.ap, in_=src_r.ap), reads=[src_r] + rd_r, writes=[bur_f])
            mk.op("act", lambda eng: eng.copy(out=bui_f.ap, in_=src_i.ap), reads=[src_i] + rd_i, writes=[bui_f])
            mk.tt(tt1, bur_f, cost, ALU.mult)
            mk.tt(gre, bui_f, sint, ALU.mult, e=S5E)
            mk.tt(gre, gre, tt1, ALU.add)
            mk.tt(tt2, bui_f, cost, ALU.mult)
            mk.tt(gim, bur_f, sint, ALU.mult, e=S5E)
            mk.tt(gim, tt2, gim, ALU.subtract)
            mk.scan(wre, rho_b, gre, 0.0)
            mk.scan(wim, rho_b, gim, 0.0)

        def stageB(it):
            sbi, s = it // NSEQ, it % NSEQ
            cb, q = sbi // 4, sbi % 4
            sint, cost = sint2[sbi % 2], cost2[sbi % 2]
            gre, gim, wre, wim = gre2[it % 2], gim2[it % 2], wre2[it % 2], wim2[it % 2]
            xre, xim = xre2[it % 2], xim2[it % 2]
            mk.tt(gre, cost, wre, ALU.mult, e=S5E)
            mk.tt(gim, sint, wim, ALU.mult)
            mk.tt(xre, gre, gim, ALU.subtract)
            mk.tt(wre, sint, wre, ALU.mult)
            mk.tt(wim, cost, wim, ALU.mult, e=S5E)
            mk.tt(xim, wre, wim, ALU.add)
            for ch in range(4):
                sl = slice(ch * 512, (ch + 1) * 512)
                py = self.bank[ch]
                mk.mm(py, Cfr[:, sbi, :], xre[:, sl], start=True, stop=False)
                mk.mm(py, Cfi[:, sbi, :], xim[:, sl], start=False, stop=True)
            src_y = multi(0, 4)
            rd_y = [self.bank[i] for i in range(1, 4)]
            ysl = yT[:, s * T:(s + 1) * T]
            if q == 0:
                mk.op("act", lambda eng: eng.copy(out=ysl.ap, in_=src_y.ap), reads=[src_y] + rd_y, writes=[ysl])
            else:
                mk.op("act", lambda eng: eng.copy(out=bur_f.ap, in_=src_y.ap), reads=[src_y] + rd_y, writes=[bur_f])
                mk.tt(ysl, ysl, bur_f, ALU.add, e=S5E)

        def finish(cb):
            for s in range(NSEQ):
                ys = yT[:, s * T:(s + 1) * T]
                mk.dma(tt1, V(uT.ap[cb * 128:(cb + 1) * 128, s * T:(s + 1) * T], uT.bufs[cb]))
                mk.stt(ys, tt1, dcol[:, cb:cb + 1], ys, ALU.mult, ALU.add)
                mk.tt(tt2, ys, ys, ALU.mult, e="pool")
                mk.ts(tt2, tt2, 0.044715, 1.0, op0=ALU.mult, op1=ALU.add)
                mk.tt(tt2, tt2, ys, ALU.mult, e="pool")
                mk.act(tt2, tt2, AF.Sigmoid, scale=2.0 * math.sqrt(2.0 / math.pi))
                mk.tt(xre2[s % 2], ys, tt2, ALU.mult)
                mk.dma(V(zTd.ap[cb * 128:(cb + 1) * 128, s * T:(s + 1) * T], zTd.bufs[cb]), xre2[s % 2], q="pool")

        NIT = 16 * NSEQ
        for cb in range(4):
            if cb == 0:
                mk.dma(ubb2[0], V(uT.ap[0:128, :], uT.bufs[0]), q="pool")
                tables(0)
                stageA(0)
            for q in range(4):
                sbi = 4 * cb + q
                for s in range(NSEQ):
                    it = sbi * NSEQ + s
                    nxt = it + 1
                    if nxt < NIT and (nxt // NSEQ) // 4 == cb:
                        if nxt % NSEQ == 0:
                            tables(nxt // NSEQ)
                        stageA(nxt)
                    stageB(it)
            finish(cb)
            nxt = (4 * cb + 4) * NSEQ
            if nxt < NIT:
                mk.dma(ubb2[(cb + 1) % 2], V(uT.ap[(cb + 1) * 128:(cb + 2) * 128, :], uT.bufs[cb + 1]), q="pool")
                tables(nxt // NSEQ)
                stageA(nxt)
    with mk.scope():
        wgl = mk.sb([128, 4, 512], BF16, "wgl")
        for k in range(4):
            mk.dma(wgl[:, k, :], P["s5_w_glu"][oi, k * 128:(k + 1) * 128, :], q="pool")
        sg = [mk.sb([128, 512], F32) for i in range(4)]
        obuf = [mk.sb([128, 512], BF16) for i in range(4)]
        zc = [mk.sb([128, 4, 512], BF16, "zc%d" % i) for i in range(2)]
        for ch in range(NTOK // 512):
            sl = slice(ch * 512, (ch + 1) * 512)
            zch = zc[ch % 2]
            for k in range(4):
                mk.dma(zch[:, k, :], V(zTd.ap[k * 128:(k + 1) * 128, sl], zTd.bufs[k]))
            for cbo in range(4):
                pg = self.bank[cbo]
                for k in range(4):
                    mk.mm(pg, wgl[:, k, cbo * 128:(cbo + 1) * 128], zch[:, k, :], start=(k == 0), stop=(k == 3))
                mk.act(sg[cbo], pg, AF.Sigmoid)
            for cbo in range(4):
                ob_ = obuf[(ch * 4 + cbo) % 4]
                mk.tt(ob_, zch[:, cbo, :], sg[cbo], ALU.mult, e=("pool" if cbo % 2 else "dve"))
                mk.dma(V(oT.ap[(4 + cbo) * 128:(5 + cbo) * 128, sl], oT.bufs[4 + cbo]), ob_)


Builder.s5 = _s5


def _odd_layer(self, xres_in, xres_out, layer, P):
    mk = self.mk
    oi = layer // 2
    u_tm = self.scratch("u_tm", [NTOK, 1536], F32)
    uT = self.scratch("uTo", [512, NTOK], F32)
    self.proj_in(xres_in, P["norm_mix_g"][layer:layer + 1, :], P["odd_w_in"][oi], 2048,
                 [12, 13, 14, 15], uT, (0, 1536), u_tm)
    oT = self.scratch("oTd", [D, NTOK], BF16)
    if not DBG.get("no_attn"):
        self.attn(u_tm, oT, P, layer)
    if not DBG.get("no_s5"):
        self.s5(uT, oT, P, layer)
    self.proj_out(xres_in, xres_out, oT, P["odd_w_out"][oi])


Builder.odd_layer = _odd_layer


def host_odd_params(inputs):
    f = lambda a: np.ascontiguousarray(a, dtype=np.float32)
    out = {}
    out["norm_mix_g"] = f(inputs["norm_mix_g"])
    out["odd_w_in"] = f(inputs["odd_w_in"])
    out["odd_w_out"] = f(inputs["odd_w_out"])
    n_odd = inputs["odd_w_in"].shape[0]
    out["da_qk_gain"] = f(np.concatenate([np.tile(inputs["da_q_norm"], (1, 8)),
                                          np.tile(inputs["da_k_norm"], (1, 8))], axis=1))
    out["da_lam"] = f(np.stack([inputs["da_lam_q1"], inputs["da_lam_k1"],
                                inputs["da_lam_q2"], inputs["da_lam_k2"]], axis=1))
    out["da_subln"] = f(inputs["da_subln"])
    three = np.stack([inputs["s5_a_re"], inputs["s5_a_im"], inputs["s5_log_step"]], axis=1)
    t16 = three.reshape(n_odd, 3, 16, 128)
    rep = t16.reshape(n_odd, 3, 4, 4, 128)
    rep = np.transpose(rep, (0, 1, 3, 2, 4))
    rep = np.repeat(rep[:, :, :, None, :, :], 32, axis=3)
    out["s5_rep"] = f(rep.reshape(n_odd, 3, 128, 4, 128))
    out["s5_st"] = f(np.transpose(t16, (0, 1, 3, 2)))
    for nm, key in (("s5_bbd_re", "s5_b_re"), ("s5_bbd_im", "s5_b_im")):
        Bm = inputs[key]
        bd = np.zeros((n_odd, 4, 2, 16, 4, 2, 64), np.float32)
        for j in range(4):
            for q in range(4):
                for gl in range(2):
                    g = 2 * (4 * j + q) + gl
                    bd[:, q, gl, :, j, gl, :] = np.transpose(Bm[:, g], (0, 2, 1))
        out[nm] = f(bd.reshape(n_odd, 128, 4, 128))
    for nm, key in (("s5_cbd_re", "s5_c_re"), ("s5_cbd_im", "s5_c_im")):
        Cm = inputs[key]
        bd = np.zeros((n_odd, 2, 64, 16, 2, 16), np.float32)
        for sbi in range(16):
            for gl in range(2):
                bd[:, gl, :, sbi, gl, :] = np.transpose(Cm[:, 2 * sbi + gl], (0, 2, 1))
        out[nm] = f(bd.reshape(n_odd, 128, 16, 32))
    out["s5_dcol"] = f(np.transpose(inputs["s5_d"].reshape(n_odd, 4, 128), (0, 2, 1)))
    out["s5_w_glu"] = f(inputs["s5_w_glu"])
    return out


LCH = 64
NCH = T // LCH
DECAY_C = 0.6065306597126334


def _load_shift_mix(self, dst, uT, blk, s, mu_col, U, dtmp):
    mk = self.mk
    mk.dma(U[:, 1:T + 1], V(uT.ap[blk * 128:(blk + 1) * 128, s * T:(s + 1) * T], uT.bufs[blk]))
    mk.tt(dtmp, U[:, 0:T], U[:, 1:T + 1], ALU.subtract, e=DBG.get("rw_eng", "dve"))
    mk.stt(dst, dtmp, mu_col, U[:, 1:T + 1], ALU.mult, ALU.add)


Builder.load_shift_mix = _load_shift_mix


def _rwkv(self, uT, oTd, P, layer, vfirst):
    mk = self.mk
    ei = layer // 2
    has_vres = layer > 0
    with mk.scope():
        mu = mk.sb([128, 14], F32, "mu")
        mk.dma(mu, P["rw_mu_col"][ei])
        cols = mk.sb([128, 7, 4], F32, "cols")
        mk.dma(cols, P["rw_cols"][ei])
        w0c, a0c, kkc, kac, rkc, lngc, lnbc = [cols[:, i, :] for i in range(7)]
        w2a2 = mk.sb([128, 512], BF16, "w2a2")
        mk.dma(w2a2, P["rw_w2a2"][ei], q="pool")
        g2b = mk.sb([128, 512], BF16, "g2b")
        mk.dma(g2b, P["rw_g2"][ei], q="pool")
        blk = mk.sb([128, 128], BF16, "blk")
        mk.dma(blk, self.inp["c_blk"], q="pool")
        masks = mk.sb([128, 3, 128], BF16, "masks")
        mk.dma(masks, self.inp["c_masks"].re("a p c -> p a c"), q="pool")

        def mb(i):
            return V(masks.ap[:, i, :].unsqueeze(1).to_broadcast([128, 2, 128]), masks.buf)
        mLs, mUs, mUi = mb(0), mb(1), mb(2)
        rmask = mk.sb([128, T], BF16, "rmask")
        tB = mk.sb([128, T], F32, "tB")
        r32 = mk.sb([128, T], F32, "r32")
        mk.op("pool", lambda eng: eng.iota(tB.ap.rearrange("p (c l) -> p c l", l=LCH), [[0, NCH], [1, LCH]],
                                            base=0, channel_multiplier=0, allow_small_or_imprecise_dtypes=True),
              writes=[tB])
        mk.ts(rmask, tB, 1.0, None, op0=ALU.min)
        lneps = mk.sb([128, 1], F32)
        mk.memset(lneps, 64e-5)
        zb = mk.sb([128, 128], BF16, "zb")
        mk.memset(zb, 0.0)
        U = mk.sb([128, T + 1], F32, "U")
        mk.memset(U[:, 0:1], 0.0)
        dtmp = tB
        m_ = r32
        lr12 = mk.sb([128, T], BF16, "lr12")
        sdg = mk.sb([128, T], BF16, "sdg")
        tbf = mk.sb([128, T], BF16, "tbf")
        if has_vres:
            v0c = mk.sb([128, 4], F32, "v0c")
            mk.dma(v0c, P["rw_v0col"][ei - 1])
            v1b = mk.sb([128, 4, 32], BF16, "v1b")
            mk.dma(v1b, P["rw_v1"][ei - 1].re("(k p) n -> p k n", p=128), q="pool")
            v2b = mk.sb([32, 512], BF16, "v2b")
            mk.dma(v2b, P["rw_v2"][ei - 1], q="pool")
            t32b = mk.sb([32, T], BF16, "t32b")
        k32 = mk.sb([128, T], F32, "k32")
        a16 = mk.sb([128, T], BF16, "a16")
        lw = mk.sb([128, T], F32, "lw")
        cl = mk.sb([128, T], F32, "cl")
        v32 = cl
        tA = mk.sb([128, T], F32, "tA")
        gT = mk.sb([128, T], BF16, "gT")
        bon = mk.sb([128, T], BF16, "bon")
        aT_ = mk.sb([128, T], BF16, "aT_")
        bT_ = mk.sb([128, T], BF16, "bT_")
        kT_ = mk.sb([128, T], BF16, "kT_")
        vT_ = tbf
        a_bd = mk.sb([128, 2, T], BF16, "a_bd")
        b_bd = mk.sb([128, 2, T], BF16, "b_bd")
        r_bd = mk.sb([128, 2, T], BF16, "r_bd")
        for t_ in (a_bd, b_bd, r_bd):
            mk.memset(t_, 0.0)
        TMav = mk.sb([128, 16, 2, 128], BF16, "TMav")
        TMbk = mk.sb([128, 16, 2, 2, 128], BF16, "TMbk")
        mk.memset(TMbk, 0.0)
        DL = mk.sb([128, NCH], F32, "DL")
        H32 = mk.sb([128, 128], F32, "H32")
        Hb = mk.sb([128, 128], BF16, "Hb")
        ht1 = mk.sb([128, 128], F32, "ht1")
        oacc = mk.sb([128, T], BF16, "oacc")

        def grp():
            d = dict(X=[mk.sb([128, 2, 128], BF16) for _ in range(2)], XT=[mk.sb([128, 2, 128], BF16) for _ in range(2)],
                     AakT=mk.sb([128, 2, 128], BF16), ArbT=mk.sb([128, 2, 128], BF16), ArkT=mk.sb([128, 2, 128], BF16),
                     Z=mk.sb([128, 2, 2, 64], BF16), T1T=mk.sb([128, 2, 128], BF16), G1=mk.sb([128, 2, 128], F32),
                     QT=mk.sb([128, 2, 128], BF16), yn=mk.sb([128, 128], BF16), ot=mk.sb([128, 128], F32),
                     st6=mk.sb([128, 2, 6], F32), mv=mk.sb([128, 2, 2], F32), rs=mk.sb([128, 2], F32))
            mk.memset(d["T1T"], 0.0)
            mk.memset(d["G1"], 0.0)
            mk.memset(d["QT"], 0.0)
            return d
        NGS = DBG.get("rw_depth", 1) + 1
        G = [grp() for _ in range(NGS)]
        B_ = self.bank

        def reg(b, c0, c1):
            return V(B_[b].ap[:, c0:c1], B_[b].buf)
        pN, pNT = reg(0, 0, 256), reg(0, 256, 512)
        pAk, pRb = reg(1, 0, 256), reg(1, 256, 512)
        pRk, pZ2 = reg(2, 0, 256), reg(2, 256, 384)
        pZa = [reg(3, 0, 256), reg(3, 256, 512)]
        pX, pXT = reg(4, 0, 256), reg(4, 256, 512)
        pHp, pTr = reg(5, 0, 128), reg(5, 128, 256)
        pT, pG = reg(6, 0, 256), reg(6, 256, 512)
        pQ, pY = reg(3, 0, 256), reg(7, 0, 128)
        pbig = [B_[5], B_[6], B_[7]]

        def v3(t):
            return t.re("p (h c) -> p h c", h=2)

        for s in range(NSEQ):
            tsl = slice(s * T, (s + 1) * T)
            self.load_shift_mix(m_, uT, 12, s, mu[:, 12:13], U, dtmp)
            mk.act(lr12[0:64, :], m_[0:64, :], AF.Tanh)
            mk.copy(lr12[64:128, :], m_[64:128, :], e=DBG.get("rw_eng", "dve"))
            self.load_shift_mix(m_, uT, 13, s, mu[:, 13:14], U, dtmp)
            mk.act(sdg, m_, AF.Sigmoid)
            if has_vres:
                pv = [B_[i] for i in range(4)]
                for hb in range(4):
                    self.load_shift_mix(m_, uT, 8 + hb, s, mu[:, 8 + hb:9 + hb], U, dtmp)
                    mk.copy(tbf, m_, e="act")
                    for ch in range(4):
                        mk.mm(pv[ch][0:32, :], v1b[:, hb, :], tbf[:, ch * 512:(ch + 1) * 512],
                              start=(hb == 0), stop=(hb == 3))
                for ch in range(4):
                    mk.copy(t32b[:, ch * 512:(ch + 1) * 512], pv[ch][0:32, :], e="act")
            for hb in range(4):
                hsl = slice(hb * 128, (hb + 1) * 128)
                self.load_shift_mix(r32, uT, hb, s, mu[:, hb:hb + 1], U, dtmp)
                self.load_shift_mix(k32, uT, 4 + hb, s, mu[:, 4 + hb:5 + hb], U, dtmp)
                self.load_shift_mix(v32, uT, 8 + hb, s, mu[:, 8 + hb:9 + hb], U, dtmp)
                for ch in range(4):
                    csl = slice(ch * 512, (ch + 1) * 512)
                    pb = pbig[ch % 3]
                    mk.mm(pb, w2a2[0:64, hsl], lr12[0:64, csl])
                    mk.act(lw[:, csl], pb, AF.Sigmoid, bias=w0c[:, hb:hb + 1])
                    pb = pbig[(ch + 1) % 3]
                    mk.mm(pb, w2a2[64:128, hsl], lr12[64:128, csl])
                    mk.act(a16[:, csl], pb, AF.Sigmoid, bias=a0c[:, hb:hb + 1])
                    pb = pbig[(ch + 2) % 3]
                    mk.mm(pb, g2b[:, hsl], sdg[:, csl])
                    mk.copy(gT[:, csl], pb, e="act")
                    if has_vres:
                        pb = pbig[ch % 3]
                        mk.mm(pb, v2b[:, hsl], t32b[:, csl])
                        mk.act(tA[:, csl], pb, AF.Sigmoid, bias=v0c[:, hb:hb + 1])
                mk.ts(lw, lw, -DECAY_C, None, op0=ALU.mult)
                if has_vres:
                    mk.dma(tB, V(vfirst.ap[hb * 128:(hb + 1) * 128, tsl], vfirst.bufs[hb]))
                    mk.tt(tB, tB, v32, ALU.subtract, e=DBG.get("rw_eng", "dve"))
                    mk.tt(tB, tB, tA, ALU.mult, e=DBG.get("rw_eng", "dve"))
                    mk.tt(v32, v32, tB, ALU.add, e=DBG.get("rw_eng", "dve"))
                else:
                    mk.dma(V(vfirst.ap[hb * 128:(hb + 1) * 128, tsl], vfirst.bufs[hb]), v32, q=DBG.get("st_q", "pool"))
                mk.ts(tA, k32, kkc[:, hb:hb + 1], None, op0=ALU.mult)
                mk.tt(tbf, tA, tA, ALU.mult, e=DBG.get("rw_eng", "dve"))
                for ch in range(4):
                    csl = slice(ch * 512, (ch + 1) * 512)
                    pb = pbig[ch % 3]
                    mk.mm(pb, blk, tbf[:, csl])
                    mk.act(tB[:, csl], pb, AF.Sqrt)
                mk.ts(tB, tB, 1e-12, None, op0=ALU.max)
                mk.recip(tB, tB)
                mk.tt(tA, tA, tB, ALU.mult)
                mk.ts(tB, a16, -1.0, kac[:, hb:hb + 1], op0=ALU.add, op1=ALU.mult)
                mk.ts(tB, tB, 1.0, None, op0=ALU.add)
                mk.tt(k32, k32, tB, ALU.mult)
                mk.tt(tB, r32, k32, ALU.mult, e=DBG.get("rw_eng", "dve"))
                mk.ts(tbf, tB, rkc[:, hb:hb + 1], None, op0=ALU.mult)
                for ch in range(4):
                    csl = slice(ch * 512, (ch + 1) * 512)
                    pb = pbig[ch % 3]
                    mk.mm(pb, blk, tbf[:, csl])
                    mk.tt(bon[:, csl], pb, v32[:, csl], ALU.mult)
                mk.copy(vT_, v32, e="act")
                mk.scan(cl, rmask, lw, 0.0)
                mk.act(tB, cl, AF.Exp)
                mk.copy(DL, tB.re("p (c l) -> p c l", l=LCH)[:, :, LCH - 1], e=DBG.get("rw_eng", "dve"))
                mk.tt(r_bd[0:64, 0, :], r32[0:64, :], tB[0:64, :], ALU.mult)
                mk.tt(r_bd[64:128, 1, :], r32[64:128, :], tB[64:128, :], ALU.mult, e=DBG.get("rw_eng", "dve"))
                mk.tt(cl, cl, lw, ALU.subtract, e=DBG.get("rw_eng", "dve"))
                mk.act(tB, cl, AF.Exp)
                mk.tt(tB, tB, tA, ALU.mult)
                mk.ts(aT_, tB, -1.0, None, op0=ALU.mult)
                mk.copy(a_bd[0:64, 0, :], aT_[0:64, :], e=DBG.get("rw_eng", "dve"))
                mk.copy(a_bd[64:128, 1, :], aT_[64:128, :], e="act")
                mk.tt(cl, cl, lw, ALU.add, e=DBG.get("rw_eng", "dve"))
                mk.act(tB, cl, AF.Exp, scale=-1.0)
                mk.tt(kT_, k32, tB, ALU.mult)
                mk.tt(tA, tA, a16, ALU.mult, e=DBG.get("rw_eng", "dve"))
                mk.tt(bT_, tA, tB, ALU.mult)
                mk.copy(b_bd[0:64, 0, :], bT_[0:64, :], e=DBG.get("rw_eng", "dve"))
                mk.copy(b_bd[64:128, 1, :], bT_[64:128, :], e="act")
                for p in range(16):
                    ptm = B_[5 + p % 2].bitcast(BF16)
                    psl = slice(p * 128, (p + 1) * 128)
                    for qi, src in enumerate((aT_, vT_, bT_, kT_)):
                        mk.transpose(ptm[:, qi * 128:(qi + 1) * 128], src[:, psl], self.identb)
                    mk.copy(TMav[:, p, :, :], ptm[:, 0:256].re("p (q c) -> p q c", q=2), e="act")
                    for c in range(2):
                        mk.copy(TMbk[64 * c:64 * c + 64, p, c, :, :],
                                ptm[64 * c:64 * c + 64, 256:512].re("p (q c) -> p q c", q=2), e=("dve" if c else "act"))
                mk.memset(H32, 0.0)
                mk.memset(Hb, 0.0)
                def local(p):
                    g = G[p % NGS]
                    t0 = p * 128
                    gsl = slice(t0, t0 + 128)
                    mk.mm(pN, aT_[:, gsl], b_bd[:, :, gsl])
                    mk.mm(pNT, bT_[:, gsl], a_bd[:, :, gsl])
                    mk.mm(pAk, kT_[:, gsl], a_bd[:, :, gsl])
                    mk.mm(pRb, bT_[:, gsl], r_bd[:, :, gsl])
                    mk.mm(pRk, kT_[:, gsl], r_bd[:, :, gsl])
                    yield
                    mk.tt(g["X"][0], v3(pN), mLs, ALU.mult)
                    mk.tt(g["XT"][0], v3(pNT), mUs, ALU.mult)
                    mk.tt(g["AakT"], v3(pAk), mUs, ALU.mult)
                    mk.tt(g["ArbT"], v3(pRb), mUi, ALU.mult)
                    mk.tt(g["ArkT"], v3(pRk), mUi, ALU.mult)
                    yield
                    for hd in range(2):
                        mk.mm(pZ2[:, 64 * hd:64 * hd + 64], g["AakT"][:, hd, :], TMav[:, p, 1, 64 * hd:64 * hd + 64])
                    Z = g["Z"]
                    mk.copy(Z[:, 0, :, :], TMav[:, p, 0, :].re("p (h j) -> p h j", h=2), e=DBG.get("rw_eng", "dve"))
                    yield
                    mk.copy(Z[:, 1, :, :], pZ2.re("p (h i) -> p h i", h=2), e="act")
                    yield
                    for lev in range(6):
                        X, XT = g["X"][lev % 2], g["XT"][lev % 2]
                        pz = pZa[lev % 2]
                        for hd in range(2):
                            mk.mm(pz[:, hd * 128:(hd + 1) * 128], XT[:, hd, :], Z[:, :, hd, :])
                        if lev < 5:
                            Xn, XTn = g["X"][(lev + 1) % 2], g["XT"][(lev + 1) % 2]
                            for hd in range(2):
                                mk.mm(pX[:, hd * 128:(hd + 1) * 128], XT[:, hd, :], X[:, hd, :])
                                mk.mm(pXT[:, hd * 128:(hd + 1) * 128], X[:, hd, :], XT[:, hd, :])
                        yield
                        if lev < 5:
                            mk.copy(Xn.re("p h c -> p (h c)"), pX, e="act")
                            mk.copy(XTn.re("p h c -> p (h c)"), pXT, e="act")
                        mk.tt(Z, Z, pz.re("p (h w c) -> p w h c", h=2, w=2), ALU.add)
                        yield
                    Wb_ = Z[:, 0, :, :].re("p h j -> p (h j)")
                    Ub_ = Z[:, 1, :, :].re("p h j -> p (h j)")
                    mk.mm(pT, Wb_, TMbk[:, p, :, 0, :])
                    for c in range(2):
                        mk.mm(pG[:, 128 * c:128 * c + 128], TMbk[:, p, c, 0, :], Ub_, start=True, stop=False)
                        mk.mm(pG[:, 128 * c:128 * c + 128], TMbk[:, p, c, 1, :], TMav[:, p, 1, :],
                              start=False, stop=True)
                    mk.mm(pQ, Wb_, g["ArbT"].re("p h t -> p (h t)"))
                    yield
                    for hd in range(2):
                        hs = slice(64 * hd, 64 * hd + 64)
                        mk.copy(g["T1T"][hs, :, hs], pT[hs, :].re("p (c h j) -> p c h j", c=2, h=2)[:, :, hd, :], e="act")
                        mk.copy(g["G1"][hs, :, hs], pG[hs, :].re("p (c h i) -> p c h i", c=2, h=2)[:, :, hd, :], e="act")
                        for c in range(2):
                            mk.tt(g["QT"][hs, c, 64 * c:64 * c + 64], pQ[hs, hd * 128 + 64 * c:hd * 128 + 64 * c + 64],
                                  r_bd[hs, hd, t0 + 64 * c:t0 + 64 * c + 64], ALU.add)

                def rec(p):
                    g = G[p % NGS]
                    t0 = p * 128
                    gsl = slice(t0, t0 + 128)
                    Z = g["Z"]
                    mk.mm(pY, zb, zb)
                    for hd in range(2):
                        hs = slice(64 * hd, 64 * hd + 64)
                        mk.mm(pY[:, hs], g["ArbT"][:, hd, :], Z[:, 1, hd, :], start=False, stop=False,
                              skip_group_check=True)
                        mk.mm(pY[:, hs], g["ArkT"][:, hd, :], TMav[:, p, 1, hs], start=False, stop=False,
                              skip_group_check=True)
                    for c in range(2):
                        ci = 2 * p + c
                        mk.mm(pY, g["QT"][:, c, :], Hb, start=False, stop=True, skip_group_check=True)
                        mk.mm(pHp, g["T1T"][:, c, :], Hb)
                        yield
                        mk.tt(ht1, pHp, H32, ALU.add)
                        mk.tt(ht1, ht1, g["G1"][:, c, :], ALU.add)
                        mk.ts(H32, ht1, DL[:, ci:ci + 1], None, op0=ALU.mult)
                        yield
                        mk.copy(Hb, H32, e="act")
                        yield
                    for hd in range(2):
                        ysl = pY[:, 64 * hd:64 * hd + 64]
                        mk.op("dve", lambda eng, o=g["st6"][:, hd, :], i_=ysl: eng.bn_stats(out=o.ap, in_=i_.ap),
                              reads=[ysl], writes=[g["st6"]])
                        mk.op("dve", lambda eng, o=g["mv"][:, hd, :], i_=g["st6"][:, hd, :]: eng.bn_aggr(out=o.ap, in_=i_.ap),
                              reads=[g["st6"]], writes=[g["mv"]])
                    yield
                    mk.act(g["rs"], g["mv"][:, :, 1], AF.Sqrt, bias=lneps)
                    yield
                    mk.recip(g["rs"], g["rs"])
                    for hd in range(2):
                        mk.ts(g["yn"][:, 64 * hd:64 * hd + 64], pY[:, 64 * hd:64 * hd + 64], g["mv"][:, hd, 0:1],
                              g["rs"][:, hd:hd + 1], op0=ALU.subtract, op1=ALU.mult)
                    yield
                    ptr = pTr.bitcast(BF16)
                    mk.transpose(ptr[:, 0:128], g["yn"], self.identb)
                    yield
                    mk.ts(g["ot"], ptr[:, 0:128], lngc[:, hb:hb + 1], lnbc[:, hb:hb + 1], op0=ALU.mult, op1=ALU.add)
                    mk.tt(g["ot"], g["ot"], bon[:, gsl], ALU.add, e=DBG.get("rw_eng", "dve"))
                    mk.tt(oacc[:, gsl], g["ot"], gT[:, gsl], ALU.mult, e=DBG.get("rw_eng", "dve"))

                def drive(gens):
                    gens = list(gens)
                    while gens:
                        for gg in list(gens):
                            try:
                                next(gg)
                            except StopIteration:
                                gens.remove(gg)

                NG = DBG.get("ngrp", 16)
                DEPTH = DBG.get("rw_depth", 1)
                started = {}

                def get_local(p):
                    if p not in started:
                        started[p] = [local(p), False]
                    return started[p]

                def step(ent):
                    if ent[1]:
                        return
                    try:
                        next(ent[0])
                    except StopIteration:
                        ent[1] = True

                if NG:
                    ent = get_local(0)
                    while not ent[1]:
                        step(ent)
                for p in range(NG):
                    r = [rec(p), False]
                    need = get_local(p + 1) if p + 1 < NG else None
                    extra = [get_local(p + k) for k in range(2, DEPTH + 1) if p + k < NG]
                    while not r[1] or (need is not None and not need[1]):
                        step(r)
                        if need is not None:
                            step(need)
                        for e_ in extra:
                            step(e_)
                mk.dma(V(oTd.ap[hb * 128:(hb + 1) * 128, tsl], oTd.bufs[hb]), oacc, q=DBG.get("st_q", "pool"))


Builder.rwkv = _rwkv


def _pool(self, uT, oT, P, layer):
    mk = self.mk
    ei = layer // 2
    with mk.scope():
        pwb = mk.sb([128, 4, 128], BF16, "pwb")
        mk.dma(pwb, P["pool_w"][ei].re("g c d -> c g d"), q="pool")
        psc = mk.sb([128, 4], F32, "psc")
        mk.dma(psc, P["pool_scale_col"][ei])
        A = [mk.sb([128, 16 + T], F32, "pA%d" % i) for i in range(2)]
        U0 = mk.sb([128, 16 + T], F32, "pU")
        for t_ in A + [U0]:
            mk.memset(t_[:, 0:16], 0.0)
        rcw = mk.sb([128, T], F32, "rcw")
        dT = mk.sb([128, T], BF16, "dT")
        tmp = mk.sb([128, T], F32, "ptmp")
        pob = [mk.sb([128, 512], BF16) for i in range(2)]
        for gi in range(4):
            win = 2 ** (gi + 1)
            mk.op("pool", lambda eng: eng.iota(rcw.ap, [[1, T]], base=1, channel_multiplier=0,
                                                allow_small_or_imprecise_dtypes=True), writes=[rcw])
            mk.ts(rcw, rcw, float(win), None, op0=ALU.min)
            mk.recip(rcw, rcw)
            for s in range(NSEQ if DBG.get("pool_stage", 9) >= 2 else 0):
                mk.dma(U0[:, 16:], V(uT.ap[(14 + gi) * 128:(15 + gi) * 128, s * T:(s + 1) * T], uT.bufs[14 + gi]))
                src = U0
                for lev in range(gi + 1):
                    sh = 2 ** lev
                    dst = A[lev % 2]
                    mk.tt(dst[:, 16:], src[:, 16:], src[:, 16 - sh:16 - sh + T], ALU.add, e=("dve" if DBG.get("pm_eng", "dve") == "dve" else ("pool" if lev % 2 else "dve")))
                    src = dst
                mk.tt(tmp, src[:, 16:], rcw, ALU.mult)
                mk.tt(dT, tmp, U0[:, 16:], ALU.subtract, e=DBG.get("pm_eng", "dve"))
                for ch in range(4 if DBG.get("pool_stage", 9) >= 3 else 0):
                    pb = self.bank[ch % 2]
                    mk.mm(pb, pwb[:, gi, :], dT[:, ch * 512:(ch + 1) * 512])
                    ob_ = pob[ch % 2]
                    if DBG.get("pool_stage", 9) >= 4:
                        mk.ts(ob_, pb, psc[:, gi:gi + 1], None, op0=ALU.mult)
                    if DBG.get("pool_stage", 9) >= 5:
                        mk.dma(V(oT.ap[(4 + gi) * 128:(5 + gi) * 128, s * T + ch * 512:s * T + (ch + 1) * 512],
                                 oT.bufs[4 + gi]), ob_, q=DBG.get("pool_q", "pool"))


Builder.pool = _pool


def _even_layer(self, xres_in, xres_out, layer, P, vfirst):
    mk = self.mk
    ei = layer // 2
    uT = self.scratch("uTe", [2304, NTOK], F32)
    self.proj_in(xres_in, P["norm_mix_g"][layer:layer + 1, :], P["even_w_in"][ei], 2304,
                 list(range(18)), uT, None, None)
    oT = self.scratch("oTd", [D, NTOK], BF16)
    if not DBG.get("no_rwkv"):
        self.rwkv(uT, oT, P, layer, vfirst)
    if not DBG.get("no_pool"):
        self.pool(uT, oT, P, layer)
    self.proj_out(xres_in, xres_out, oT, P["even_w_out"][ei])


Builder.even_layer = _even_layer


def host_even_params(inputs):
    f = lambda a: np.ascontiguousarray(a, dtype=np.float32)
    out = {}
    out["norm_mix_g"] = f(inputs["norm_mix_g"])
    out["even_w_in"] = f(inputs["even_w_in"])
    out["even_w_out"] = f(inputs["even_w_out"])
    n_even = inputs["even_w_in"].shape[0]
    out["rw_mu_col"] = f(np.transpose(inputs["rw_mu"].reshape(n_even, 14, 128), (0, 2, 1)))
    colp = [inputs["rw_w0"], inputs["rw_a0"], inputs["rw_k_k"], inputs["rw_k_a"],
            inputs["rw_r_k"].reshape(n_even, 512), inputs["rw_ln_g"], inputs["rw_ln_b"]]
    out["rw_cols"] = f(np.stack([np.transpose(c.reshape(n_even, 4, 128), (0, 2, 1)) for c in colp], axis=2))
    out["rw_w2a2"] = f(np.concatenate([inputs["rw_w2"], inputs["rw_a2"]], axis=1))
    out["rw_g2"] = f(inputs["rw_g2"])
    nv = inputs["rw_v0"].shape[0]
    out["rw_v0col"] = f(np.transpose(inputs["rw_v0"].reshape(nv, 4, 128), (0, 2, 1)))
    out["rw_v1"] = f(inputs["rw_v1"])
    out["rw_v2"] = f(inputs["rw_v2"])
    out["pool_w"] = f(inputs["pool_w"])
    out["pool_scale_col"] = f(np.transpose(inputs["pool_scale"].reshape(n_even, 4, 128), (0, 2, 1)))
    return out


def host_consts2():
    c = host_consts()
    blk = np.kron(np.eye(2, dtype=np.float32), np.ones((64, 64), np.float32))
    lo = np.tril(np.ones((64, 64), np.float32), -1)
    e2 = np.eye(2, dtype=np.float32)
    mLs = np.kron(e2, lo)
    mUs = np.kron(e2, lo.T)
    mUi = np.kron(e2, np.triu(np.ones((64, 64), np.float32)))
    c["c_blk"] = blk
    c["c_masks"] = np.stack([mLs, mUs, mUi]).astype(np.float32)
    return c


def mk_check(mk):
    val = {}
    pos = {e: 0 for e in ENGS}
    total = sum(len(mk.ops[e]) for e in ENGS)
    done = 0
    while done < total:
        prog = False
        for e in ENGS:
            lst = mk.ops[e]
            while pos[e] < len(lst):
                waits, fn, inc = lst[pos[e]]
                if all(val.get(id(s), 0) >= v for s, v in waits):
                    val[id(inc[0])] = val.get(id(inc[0]), 0) + inc[1]
                    pos[e] += 1
                    done += 1
                    prog = True
                else:
                    break
        if not prog:
            names = {id(v): k for k, v in mk.semobj.items()}
            for e in ENGS:
                if pos[e] < len(mk.ops[e]):
                    waits, fn, inc = mk.ops[e][pos[e]]
                    print("STUCK", e, pos[e], [(names.get(id(s)), v, val.get(id(s), 0)) for s, v in waits])
            return False
    return True


_PROG = {}


def host_params(inputs):
    hp = {}
    hp.update(host_even_params(inputs))
    hp.update(host_odd_params(inputs))
    hp.update(host_moe_params(inputs))
    hp.update(host_consts2())
    return hp


def build_program(shapes, n_layers=4):
    nc = bass.Bass("TRN2", target_bir_lowering=False)
    with ExitStack() as st:
        B = Builder(nc, st)
        mk = B.mk
        x_in = DR(mk, "x_in", [NTOK, D], F32, kind="ExternalInput")
        y = DR(mk, "y", [NTOK, D], F32, kind="ExternalOutput")
        P = {}
        for k, shp in shapes.items():
            if k in B.inp:
                continue
            P[k] = B.ext_in(k, list(shp))
        vf = B.scratch("vfirst", [512, NTOK], F32)
        for layer in range(n_layers):
            xin = x_in if layer == 0 else y
            if layer % 2 == 0:
                B.even_layer(xin, y, layer, P, vf)
            else:
                B.odd_layer(xin, y, layer, P)
            B.moe(y, layer, P)
        mk.emit()
    return nc


def kernel(**inputs):
    inputs = {k: np.asarray(v) for k, v in inputs.items()}
    hp = host_params(inputs)
    key = "full"
    if key not in _PROG:
        _PROG[key] = build_program({k: v.shape for k, v in hp.items()})
    nc = _PROG[key]
    x = np.ascontiguousarray(inputs["x"], dtype=np.float32)
    nb = x.shape[0]
    n_cores = 8
    per = nb // n_cores
    in_maps = []
    for c in range(n_cores):
        m = dict(hp)
        m["x_in"] = np.ascontiguousarray(x[c * per:(c + 1) * per].reshape(NTOK, D))
        in_maps.append(m)
    res = run_bass_kernel_spmd(nc, in_maps, core_ids=list(range(n_cores)))
    out = np.stack([np.asarray(r["y"], dtype=np.float32).reshape(per, T, D) for r in res.results], axis=0)
    return out.reshape(nb, T, D)
```

```python
import numpy as np
from contextlib import ExitStack
import concourse.bass as bass
import concourse.mybir as mybir
from concourse.bass_utils import run_bass_kernel_spmd

F32 = mybir.dt.float32
BF16 = mybir.dt.bfloat16
I32 = mybir.dt.int32
U32 = mybir.dt.uint32
AF = mybir.ActivationFunctionType
ALU = mybir.AluOpType
AX = mybir.AxisListType

SAME_ENGINE_SYNC = True
EPOCH = 20000
DBG = {}


class Buf:
    __slots__ = ("w", "r", "name")

    def __init__(self, name=""):
        self.w = None
        self.r = {}
        self.name = name


class V:
    __slots__ = ("ap", "buf")

    def __init__(self, ap, buf):
        self.ap = ap
        self.buf = buf

    def __getitem__(self, k):
        return V(self.ap[k], self.buf)

    def re(self, s, **kw):
        return V(self.ap.rearrange(s, **kw), self.buf)

    def bc(self, shape):
        return V(self.ap.to_broadcast(list(shape)), self.buf)

    def pbc(self, n):
        return V(self.ap.partition_broadcast(n), self.buf)

    def bitcast(self, dt):
        return V(self.ap.bitcast(dt), self.buf)

    def sub(self, buf):
        return V(self.ap, buf)

    @property
    def shape(self):
        return self.ap.shape


ENGS = ("pe", "act", "dve", "pool", "sp")


class MK:
    def __init__(self, nc, stack, n_dma_slots=8):
        self.nc = nc
        self.stack = stack
        self.ops = {e: [] for e in ENGS}
        self.root = stack
        self.esem = {}
        self.cnt = {e: 0 for e in ENGS}
        self.seen = {e: {} for e in ENGS}
        self.dma_slots = {}
        self.dma_n = {}
        for q in ("sp", "pool", "act"):
            self.dma_slots[q] = [stack.enter_context(nc.semaphore("dma_%s_%d" % (q, i)))
                                 for i in range(n_dma_slots)]
            self.dma_n[q] = 0
        self.semobj = {}
        self.n_ops = 0
        self.uid = 0

    def sb(self, shape, dtype, name=None):
        self.uid += 1
        name = name or "t%d" % self.uid
        t = self.stack.enter_context(self.nc.sbuf_tensor(name + "_%d" % self.uid, list(shape), dtype))
        return V(t[:], Buf(name))

    def ps(self, shape, dtype=F32, name=None):
        self.uid += 1
        name = name or "p%d" % self.uid
        t = self.stack.enter_context(self.nc.psum_tensor(name + "_%d" % self.uid, list(shape), dtype))
        return V(t[:], Buf(name))

    def dram(self, name, shape, dtype, kind="Internal"):
        t = self.nc.dram_tensor(name, list(shape), dtype, kind=kind)
        return V(t.ap(), Buf(name))

    def _tok_need(self, e, tok, waits):
        if tok is None:
            return
        semkey, val, src, is_dma = tok
        if src == e and not is_dma:
            if e == "pe" or not SAME_ENGINE_SYNC:
                return
        if self.seen[e].get(semkey, 0) >= val:
            return
        if waits.get(semkey, 0) < val:
            waits[semkey] = val

    def op(self, e, fn, reads=(), writes=(), dma=False):
        waits = {}
        pend = getattr(self, "pending", {}).pop(e, None)
        if pend:
            waits.update(pend)
        rb = [v.buf for v in reads if v is not None]
        wb = [v.buf for v in writes if v is not None]
        for b in rb:
            self._tok_need(e, b.w, waits)
        for b in wb:
            self._tok_need(e, b.w, waits)
            for t in b.r.values():
                self._tok_need(e, t, waits)
        if dma:
            slots = self.dma_slots[e]
            n = self.dma_n[e]
            self.dma_n[e] = n + 1
            sem = slots[n % len(slots)]
            val = 16 * (n // len(slots) + 1)
            semkey = ("d", e, n % len(slots))
            self.semobj[semkey] = sem
            if val > 16:
                if self.seen[e].get(semkey, 0) < val - 16 and waits.get(semkey, 0) < val - 16:
                    waits[semkey] = val - 16
            tok = (semkey, val, e, True)
            inc = (sem, 16)
        else:
            self.cnt[e] += 1
            ep = (self.cnt[e] - 1) // EPOCH
            semkey = ("c", e, ep)
            if semkey not in self.esem:
                self.esem[semkey] = self.root.enter_context(self.nc.semaphore("sem_%s_%d" % (e, ep)))
            self.semobj[semkey] = self.esem[semkey]
            tok = (semkey, (self.cnt[e] - 1) % EPOCH + 1, e, False)
            inc = (self.esem[semkey], 1)
        for k, v in waits.items():
            self.seen[e][k] = v
        for b in wb:
            b.w = tok
            b.r = {}
        for b in rb:
            old = b.r.get(tok[0])
            if old is None or old[1] < tok[1]:
                b.r[tok[0]] = tok
        self.ops[e].append(([(self.semobj[k], v) for k, v in waits.items()], fn, inc))
        self.n_ops += 1
        return tok

    def emit(self):
        nc = self.nc
        fin = []
        for q in ("sp", "pool", "act"):
            n = self.dma_n[q]
            slots = self.dma_slots[q]
            for i in range(min(n, len(slots))):
                uses = (n - i + len(slots) - 1) // len(slots)
                fin.append((slots[i], 16 * uses))
        for e in ("pe", "act", "dve", "pool"):
            if self.cnt[e]:
                ep = (self.cnt[e] - 1) // EPOCH
                fin.append((self.esem[("c", e, ep)], (self.cnt[e] - 1) % EPOCH + 1))
        with nc.Block() as block:
            def run(eng, lst, final=None):
                for waits, fn, inc in lst:
                    for s, v in waits:
                        eng.wait_ge(s, v)
                    fn(eng).then_inc(inc[0], inc[1])
                if final:
                    for s, v in final:
                        eng.wait_ge(s, v)

            @block.tensor
            def _(eng):
                run(eng, self.ops["pe"])

            @block.scalar
            def _(eng):
                run(eng, self.ops["act"])

            @block.vector
            def _(eng):
                run(eng, self.ops["dve"])

            @block.gpsimd
            def _(eng):
                run(eng, self.ops["pool"])

            @block.sync
            def _(eng):
                run(eng, self.ops["sp"], fin)

    def dma(self, out, in_, q="sp", **kw):
        return self.op(q, lambda eng: eng.dma_start(out=out.ap, in_=in_.ap, **kw),
                       reads=[in_], writes=[out], dma=True)

    def mm(self, out, lhsT, rhs, start=True, stop=True, **kw):
        return self.op("pe", lambda eng: eng.matmul(out.ap, lhsT.ap, rhs.ap, start=start, stop=stop, **kw),
                       reads=[lhsT, rhs], writes=[out])

    def transpose(self, out, in_, ident):
        return self.op("pe", lambda eng: eng.transpose(out.ap, in_.ap, ident.ap),
                       reads=[in_, ident], writes=[out])

    def act(self, out, in_, func, bias=None, scale=1.0, accum_out=None, e="act"):
        reads = [in_]
        kw = {}
        if isinstance(bias, V):
            reads.append(bias)
            kw["bias"] = bias.ap
        elif bias is not None:
            kw["bias"] = bias
        if isinstance(scale, V):
            reads.append(scale)
            kw["scale"] = scale.ap
        else:
            kw["scale"] = scale
        writes = [out]
        if accum_out is not None:
            writes.append(accum_out)
            kw["accum_out"] = accum_out.ap
        return self.op(e, lambda eng: eng.activation(out=out.ap, in_=in_.ap, func=func, **kw),
                       reads=reads, writes=writes)

    def tt(self, out, in0, in1, op, e="dve"):
        return self.op(e, lambda eng: eng.tensor_tensor(out=out.ap, in0=in0.ap, in1=in1.ap, op=op),
                       reads=[in0, in1], writes=[out])

    def ts(self, out, in0, s1, s2=None, op0=ALU.mult, op1=None, accum_out=None, e="dve"):
        reads = [in0]
        a1 = s1
        if isinstance(s1, V):
            reads.append(s1)
            a1 = s1.ap
        a2 = s2
        if isinstance(s2, V):
            reads.append(s2)
            a2 = s2.ap
        kw = {}
        if op1 is not None:
            kw["op1"] = op1
        writes = [out]
        if accum_out is not None:
            writes.append(accum_out)
            kw["accum_out"] = accum_out.ap
        return self.op(e, lambda eng: eng.tensor_scalar(out=out.ap, in0=in0.ap, scalar1=a1, scalar2=a2,
                                                        op0=op0, **kw),
                       reads=reads, writes=writes)

    def stt(self, out, in0, scalar, in1, op0, op1, e="dve"):
        reads = [in0, in1]
        a = scalar
        if isinstance(scalar, V):
            reads.append(scalar)
            a = scalar.ap
        return self.op(e, lambda eng: eng.scalar_tensor_tensor(out=out.ap, in0=in0.ap, scalar=a, in1=in1.ap,
                                                               op0=op0, op1=op1),
                       reads=reads, writes=[out])

    def copy(self, out, in_, e="dve"):
        if e == "act":
            return self.op(e, lambda eng: eng.copy(out=out.ap, in_=in_.ap), reads=[in_], writes=[out])
        return self.op(e, lambda eng: eng.tensor_copy(out=out.ap, in_=in_.ap), reads=[in_], writes=[out])

    def memset(self, out, val, e="pool"):
        return self.op(e, lambda eng: eng.memset(out.ap, val), writes=[out])

    def reduce(self, out, in_, op=ALU.add, axis=AX.X, e="dve"):
        return self.op(e, lambda eng: eng.tensor_reduce(out=out.ap, in_=in_.ap, axis=axis, op=op),
                       reads=[in_], writes=[out])

    def recip(self, out, in_):
        return self.op("dve", lambda eng: eng.reciprocal(out=out.ap, in_=in_.ap), reads=[in_], writes=[out])

    def scan(self, out, d0, d1, init, op0=ALU.mult, op1=ALU.add):
        reads = [d0, d1]
        a = init
        if isinstance(init, V):
            reads.append(init)
            a = init.ap
        return self.op("dve", lambda eng: eng.tensor_tensor_scan(out=out.ap, data0=d0.ap, data1=d1.ap,
                                                                 initial=a, op0=op0, op1=op1),
                       reads=reads, writes=[out])

    def barrier(self):
        fin = {}
        for q in ("sp", "pool", "act"):
            n = self.dma_n[q]
            slots = self.dma_slots[q]
            for i in range(min(n, len(slots))):
                uses = (n - i + len(slots) - 1) // len(slots)
                fin[("d", q, i)] = 16 * uses
        for e in ("pe", "act", "dve", "pool"):
            if self.cnt[e]:
                ep = (self.cnt[e] - 1) // EPOCH
                fin[("c", e, ep)] = (self.cnt[e] - 1) % EPOCH + 1
        for e in ENGS:
            waits = {}
            for k, v in fin.items():
                if k[0] == "c" and k[1] == e:
                    continue
                if self.seen[e].get(k, 0) < v:
                    waits[k] = v
                    self.seen[e][k] = v
            if waits:
                if e == "sp":
                    continue_fn = None
                self.pending = getattr(self, "pending", {})
                self.pending.setdefault(e, {}).update(waits)

    def scope(self):
        return _Scope(self)


class _Scope:
    def __init__(self, mk):
        self.mk = mk

    def __enter__(self):
        self.old = self.mk.stack
        self.st = ExitStack()
        self.mk.stack = self.st
        return self

    def __exit__(self, *a):
        self.mk.barrier()
        self.mk.stack = self.old
        self.st.close()
        return False


D = 1024
T = 2048
NSEQ = 2
NTOK = NSEQ * T
NT = NTOK // 128
CAP = 512
NE = 32
NSLOT = NE * CAP
NROW_TL = NSLOT + 128
RMS_EPS = 1e-6


class DR:
    def __init__(self, mk, name, shape, dtype, kind="Internal", rows_per=128):
        self.t = mk.nc.dram_tensor(name, list(shape), dtype, kind=kind)
        self.ap = self.t.ap()
        self.rows_per = rows_per
        n = (shape[0] + rows_per - 1) // rows_per
        self.bufs = [Buf("%s_%d" % (name, i)) for i in range(n)]
        self.whole = Buf(name)

    def rows(self, r0, n):
        assert r0 % self.rows_per == 0 and n <= self.rows_per
        return V(self.ap[r0:r0 + n], self.bufs[r0 // self.rows_per])

    def all(self):
        return [V(self.ap, b) for b in self.bufs]


class Builder:
    def __init__(self, nc, stack):
        self.nc = nc
        self.mk = MK(nc, stack)
        mk = self.mk
        self.inp = {}
        self.c_ident = self.ext_in("c_ident", [128, 128], F32)
        self.c_tri = self.ext_in("c_tri", [128, 128], F32)
        self.ext_in("c_blk", [128, 128], F32)
        self.ext_in("c_masks", [3, 128, 128], F32)
        self.ident32 = mk.sb([128, 128], F32, "ident32")
        self.identb = mk.sb([128, 128], BF16, "identb")
        self.trib = mk.sb([128, 128], BF16, "trib")
        self.onesb = mk.sb([128, 128], BF16, "onesb")
        mk.dma(self.ident32, self.c_ident)
        mk.dma(self.identb, self.c_ident, q="pool")
        mk.dma(self.trib, self.c_tri, q="pool")
        mk.memset(self.onesb, 1.0)
        self.eps_t = mk.sb([128, 1], F32, "eps_t")
        mk.memset(self.eps_t, RMS_EPS)
        self.psum_all = mk.ps([128, 4096], F32, "psum_all")
        self.bank = [V(self.psum_all.ap[:, i * 512:(i + 1) * 512], Buf("bank%d" % i)) for i in range(8)]

    def scratch(self, name, shape, dtype, rows_per=128):
        if not hasattr(self, "_scr"):
            self._scr = {}
        if name not in self._scr:
            self._scr[name] = DR(self.mk, name, shape, dtype, rows_per=rows_per)
        return self._scr[name]

    def ext_in(self, name, shape, dtype=F32):
        v = self.mk.dram(name, shape, dtype, kind="ExternalInput")
        self.inp[name] = v
        return v

    def rms_tile(self, xt, gbc, h32, eps=RMS_EPS, sq=None, small=None):
        mk = self.mk
        ss, rstd = small
        mk.act(sq, xt, AF.Square, accum_out=ss)
        mk.act(rstd, ss, AF.Sqrt, scale=1.0 / D, bias=self.eps_t)
        mk.recip(rstd, rstd)
        mk.stt(h32, xt, rstd, gbc, ALU.mult, ALU.mult)

    def moe(self, xres, layer, P):
        mk = self.mk
        with mk.scope():
            Hrows = self.scratch("hrows", [NTOK + 128, D], BF16)
            Ybuf = self.scratch("ybuf", [NROW_TL, D], F32)
            TokL = self.scratch("tokl", [NROW_TL, 8], I32, rows_per=NROW_TL)
            gbc = mk.sb([128, D], F32, "gbc")
            mk.dma(gbc, P["norm_ffn_g"][layer:layer + 1, :].pbc(128))
            wr = mk.sb([128, 8, 36], F32, "wr")
            mk.dma(wr, P["w_router"][layer].re("(k p) n -> p k n", p=128))
            brt = mk.sb([128, 36], F32, "brt")
            mk.dma(brt, P["b_router"][layer:layer + 1, :].pbc(128))
            maskall = mk.sb([128, NT, 32], BF16, "maskall")
            E1all = mk.sb([128, NT, 32], F32, "E1all")
            E2all = mk.sb([128, NT, 32], F32, "E2all")
            gate1 = mk.sb([128, NT], F32, "gate1")
            gate2 = mk.sb([128, NT], F32, "gate2")
            slot1 = mk.sb([128, NT], I32, "slot1")
            slot2 = mk.sb([128, NT], I32, "slot2")
            base = mk.sb([128, 32], F32, "base")
            lim = mk.sb([128, 32], F32, "lim")
            trash = mk.sb([128, 1], F32, "trash")
            zero_t = mk.sb([128, D], F32, "zero_t")
            sent = mk.sb([128, (NROW_TL // 128) * 8], I32, "sent")
            mk.op("pool", lambda eng: eng.iota(base.ap, [[CAP, 32]], base=-1, channel_multiplier=0,
                                                allow_small_or_imprecise_dtypes=True), writes=[base])
            mk.ts(lim, base, float(CAP) + 0.5, None, op0=ALU.add)
            mk.op("pool", lambda eng: eng.iota(trash.ap, [[0, 1]], base=NSLOT, channel_multiplier=1,
                                                allow_small_or_imprecise_dtypes=True), writes=[trash])
            mk.memset(zero_t, 0.0)
            mk.op("pool", lambda eng: eng.iota(sent.ap, [[0, (NROW_TL // 128) * 8]], base=NTOK,
                                                channel_multiplier=0), writes=[sent])
            mk.dma(V(TokL.ap.rearrange("(p r) c -> p (r c)", p=128), TokL.bufs[0]), sent)
            mk.dma(Hrows.rows(NTOK, 128), zero_t.bitcast(BF16)[:, 0:D])
            mk.dma(Ybuf.rows(NSLOT, 128), zero_t)

            xts = [mk.sb([128, D], F32, "xt%d" % i) for i in range(2)]
            sqs = [mk.sb([128, D], F32, "sq%d" % i) for i in range(2)]
            h32s = [mk.sb([128, D], F32, "h32%d" % i) for i in range(2)]
            hbs = [mk.sb([128, D], BF16, "hb%d" % i) for i in range(2)]
            hT32s = [mk.sb([128, 8, 128], F32, "hT32%d" % i) for i in range(2)]
            smalls = [(mk.sb([128, 1], F32), mk.sb([128, 1], F32)) for i in range(2)]
            lg_all = mk.sb([128, NT, 36], F32, "lg_all")
            for i in range(NT):
                b = i % 2
                xt, sq, h32, hb, hT32 = xts[b], sqs[b], h32s[b], hbs[b], hT32s[b]
                mk.dma(xt, xres.rows(i * 128, 128))
                self.rms_tile(xt, gbc, h32, sq=sq, small=smalls[b])
                mk.copy(hb, h32, e="act")
                mk.dma(Hrows.rows(i * 128, 128), hb)
                for half in range(2):
                    pb = self.bank[2 * b + half]
                    for kk in range(4):
                        k = half * 4 + kk
                        mk.transpose(pb[:, kk * 128:(kk + 1) * 128], h32[:, k * 128:(k + 1) * 128], self.ident32)
                    mk.copy(hT32[:, half * 4:(half + 1) * 4, :].re("p k t -> p (k t)"), pb, e="act")
                pl = self.bank[4 + b]
                for k in range(8):
                    mk.mm(pl[:, 0:36], hT32[:, k, :], wr[:, k, :], start=(k == 0), stop=(k == 7))
                mk.tt(lg_all[:, i, :], pl[:, 0:36], brt, ALU.add)

            def bc(v, axis, shape):
                return V(v.ap.unsqueeze(axis).to_broadcast(list(shape)), v.buf)
            gl = lg_all[:, :, 0:4]
            el4 = lg_all[:, :, 4:36].re("p t (g e) -> p t g e", g=4)
            gmax = mk.sb([128, NT], F32)
            ohg = mk.sb([128, NT, 4], F32)
            eg = mk.sb([128, NT, 4], F32)
            gsum = mk.sb([128, NT], F32)
            tmp4 = mk.sb([128, NT, 4, 8], F32)
            sel = mk.sb([128, NT, 8], F32)
            sel2 = mk.sb([128, NT, 8], F32)
            m1 = mk.sb([128, NT], F32)
            m2 = mk.sb([128, NT], F32)
            oh1 = mk.sb([128, NT, 8], F32)
            oh2 = mk.sb([128, NT, 8], F32)
            mk.reduce(gmax, gl, op=ALU.max)
            mk.tt(ohg, gl, bc(gmax, 2, [128, NT, 4]), ALU.is_equal)
            mk.tt(eg, gl, bc(gmax, 2, [128, NT, 4]), ALU.subtract)
            mk.act(eg, eg, AF.Exp)
            mk.reduce(gsum, eg, op=ALU.add)
            mk.recip(gsum, gsum)
            mk.tt(tmp4, el4, bc(ohg, 3, [128, NT, 4, 8]), ALU.mult)
            mk.reduce(sel, tmp4.re("p t g e -> p t e g"), op=ALU.add)
            mk.reduce(m1, sel, op=ALU.max)
            mk.tt(oh1, sel, bc(m1, 2, [128, NT, 8]), ALU.is_equal)
            mk.stt(sel2, oh1, -1e30, sel, ALU.mult, ALU.add)
            mk.reduce(m2, sel2, op=ALU.max)
            mk.tt(oh2, sel2, bc(m2, 2, [128, NT, 8]), ALU.is_equal)
            mk.tt(m2, m2, m1, ALU.subtract)
            mk.act(m2, m2, AF.Exp)
            mk.ts(m2, m2, 1.0, None, op0=ALU.add)
            mk.recip(m2, m2)
            mk.tt(gate1, gsum, m2, ALU.mult)
            mk.tt(gate2, gsum, gate1, ALU.subtract)
            mk.tt(E1all.re("p t (g e) -> p t g e", g=4), bc(ohg, 3, [128, NT, 4, 8]), bc(oh1, 2, [128, NT, 4, 8]), ALU.mult)
            mk.tt(E2all.re("p t (g e) -> p t g e", g=4), bc(ohg, 3, [128, NT, 4, 8]), bc(oh2, 2, [128, NT, 4, 8]), ALU.mult)
            mk.tt(maskall, E1all, E2all, ALU.add)

            pos_ps = V(self.psum_all.ap[:, 0:1024], self.bank[0].buf)
            for i in range(NT):
                reg_ = V(self.psum_all.ap[:, i * 32:(i + 1) * 32], self.bank[i // 16].buf)
                for j in range(i):
                    mk.mm(reg_, self.onesb, maskall[:, j, :], start=(j == 0), stop=False)
                mk.mm(reg_, self.trib, maskall[:, i, :], start=(i == 0), stop=True)
            posf = mk.sb([128, NT, 32], F32, "posf")
            okm = mk.sb([128, NT, 32], F32, "okm")
            slf = mk.sb([128, NT], F32, "slf")
            mk.op("dve", lambda eng: eng.tensor_tensor(out=posf.ap, in0=pos_ps.ap.rearrange("p (t e) -> p t e", e=32),
                                                       in1=base.ap.unsqueeze(1).to_broadcast([128, NT, 32]), op=ALU.add),
                  reads=[self.bank[0], self.bank[1], base], writes=[posf])
            mk.tt(okm, posf, bc(lim, 1, [128, NT, 32]), ALU.is_lt)
            mk.ts(posf, posf, trash, None, op0=ALU.subtract)
            mk.tt(posf, posf, okm, ALU.mult)
            mk.ts(posf, posf, trash, None, op0=ALU.add)
            for Eall, slot in ((E1all, slot1), (E2all, slot2)):
                mk.tt(okm, posf, Eall, ALU.mult)
                mk.reduce(slf, okm, op=ALU.add)
                mk.copy(slot, slf)
            tokid = [mk.sb([128, 8], I32) for i in range(4)]
            sc_bufs = []
            init_v = V(TokL.ap, TokL.bufs[0])
            for i in range(NT):
                t_ = tokid[i % 4]
                mk.op("pool", lambda eng, t=t_, i=i: eng.iota(t.ap, [[0, 8]], base=i * 128,
                                                              channel_multiplier=1), writes=[t_])
                for slot in (slot1, slot2):
                    bf = Buf("tokl_sc")
                    sc_bufs.append(bf)
                    mk.op("pool", lambda eng, slot=slot, i=i, t=t_: eng.indirect_dma_start(
                        out=TokL.ap, out_offset=bass.IndirectOffsetOnAxis(ap=slot.ap[:, i:i + 1], axis=0),
                        in_=t.ap, in_offset=None),
                        reads=[slot, t_, init_v], writes=[V(TokL.ap, bf)], dma=True)
            tokl_all = [V(TokL.ap, bf) for bf in sc_bufs] + [init_v]

            NCT = CAP // 128
            wg = [mk.sb([128, 8, 512], BF16, "wg%d" % i) for i in range(2)]
            wu = [mk.sb([128, 8, 512], BF16, "wu%d" % i) for i in range(2)]
            wd = [mk.sb([128, 4, D], BF16, "wd%d" % i) for i in range(2)]
            idx = [mk.sb([128, 8], I32) for i in range(4)]
            xg = [mk.sb([128, D], BF16) for i in range(4)]
            xgT = [mk.sb([128, 8, CAP], BF16) for i in range(2)]
            hidT = [mk.sb([128, 4, CAP], BF16) for i in range(2)]
            sil = [mk.sb([128, CAP], F32) for i in range(2)]
            yrow = [mk.sb([128, D], F32) for i in range(2)]
            nslot = 0
            ny = 0
            for e in range(NE):
                b = e % 2
                mk.dma(wg[b], P["moe_w_gate"][layer, e].re("(k p) n -> p k n", p=128), q="pool")
                mk.dma(wu[b], P["moe_w_up"][layer, e].re("(k p) n -> p k n", p=128), q="pool")
                mk.dma(wd[b], P["moe_w_down"][layer, e].re("(k p) n -> p k n", p=128), q="pool")
                for j in range(NCT):
                    s = nslot % 4
                    nslot += 1
                    r0 = e * CAP + j * 128
                    mk.op("sp", lambda eng, s=s, r0=r0: eng.dma_start(out=idx[s].ap, in_=TokL.ap[r0:r0 + 128, :]),
                          reads=tokl_all, writes=[idx[s]], dma=True)
                    mk.op("pool", lambda eng, s=s: eng.indirect_dma_start(
                        out=xg[s].ap, out_offset=None, in_=Hrows.ap,
                        in_offset=bass.IndirectOffsetOnAxis(ap=idx[s].ap[:, 0:1], axis=0)),
                        reads=[idx[s]] + Hrows.all(), writes=[xg[s]], dma=True)
                    pb = self.bank[j % 2]
                    pbb = pb.bitcast(BF16)
                    for k in range(8):
                        mk.transpose(pbb[:, k * 128:(k + 1) * 128], xg[s][:, k * 128:(k + 1) * 128], self.identb)
                    mk.copy(xgT[b][:, :, j * 128:(j + 1) * 128], pbb.re("p (k t) -> p k t", k=8),
                            e=("act" if j % 2 else "dve"))
                for c in range(4):
                    pg = self.bank[2 + (c % 2)]
                    pu = self.bank[4 + (c % 2)]
                    for k in range(8):
                        mk.mm(pg, wg[b][:, k, c * 128:(c + 1) * 128], xgT[b][:, k, :], start=(k == 0), stop=(k == 7))
                    for k in range(8):
                        mk.mm(pu, wu[b][:, k, c * 128:(c + 1) * 128], xgT[b][:, k, :], start=(k == 0), stop=(k == 7))
                    mk.act(sil[c % 2], pg, AF.Silu)
                    mk.tt(hidT[b][:, c, :], sil[c % 2], pu, ALU.mult)
                for j in range(NCT):
                    yb = ny % 2
                    ny += 1
                    for half in range(2):
                        pd = self.bank[6 + half]
                        for c in range(4):
                            mk.mm(pd, hidT[b][:, c, j * 128:(j + 1) * 128], wd[b][:, c, half * 512:(half + 1) * 512],
                                  start=(c == 0), stop=(c == 3))
                        mk.copy(yrow[yb][:, half * 512:(half + 1) * 512], pd, e=("act" if half else "dve"))
                    mk.dma(Ybuf.rows(e * CAP + j * 128, 128), yrow[yb])

            y1 = [mk.sb([128, D], F32) for i in range(3)]
            y2 = [mk.sb([128, D], F32) for i in range(3)]
            xt3 = xts + [mk.sb([128, D], F32)]
            for i in range(NT):
                b = i % 3
                xt = xt3[b]
                mk.dma(xt, xres.rows(i * 128, 128))
                for slot, yy in ((slot1, y1[b]), (slot2, y2[b])):
                    mk.op("pool", lambda eng, slot=slot, yy=yy, i=i: eng.indirect_dma_start(
                        out=yy.ap, out_offset=None, in_=Ybuf.ap,
                        in_offset=bass.IndirectOffsetOnAxis(ap=slot.ap[:, i:i + 1], axis=0)),
                        reads=[slot] + Ybuf.all(), writes=[yy], dma=True)
                mk.stt(xt, y1[b], gate1[:, i:i + 1], xt, ALU.mult, ALU.add)
                mk.stt(xt, y2[b], gate2[:, i:i + 1], xt, ALU.mult, ALU.add)
                mk.dma(xres.rows(i * 128, 128), xt)


def host_consts():
    ident = np.eye(128, dtype=np.float32)
    tri = np.triu(np.ones((128, 128), np.float32))
    return {"c_ident": ident, "c_tri": tri}


MOE_KEYS = ("norm_ffn_g", "w_router", "b_router", "moe_w_gate", "moe_w_up", "moe_w_down")


def host_moe_params(inputs):
    out = {}
    out["norm_ffn_g"] = np.ascontiguousarray(inputs["norm_ffn_g"], dtype=np.float32)
    out["w_router"] = np.ascontiguousarray(
        np.concatenate([inputs["moe_w_group"], inputs["moe_w_expert"]], axis=-1), dtype=np.float32)
    out["b_router"] = np.ascontiguousarray(
        np.concatenate([inputs["moe_b_group"], inputs["moe_b_expert"]], axis=-1), dtype=np.float32)
    for k in ("moe_w_gate", "moe_w_up", "moe_w_down"):
        out[k] = np.ascontiguousarray(inputs[k], dtype=np.float32)
    return out


TWO_PI = 6.283185307179586
CW1 = 6.28125
CW2 = TWO_PI - CW1
PI_SAFE = 3.1415925


def _rr_sin(self, dst, X, tmpf, tmpi, phase=0.0, e="dve"):
    mk = self.mk
    mk.ts(tmpf, X, 1.0 / TWO_PI, 0.5 + phase / TWO_PI, op0=ALU.mult, op1=ALU.add, e=e)
    mk.copy(tmpi, tmpf, e=e)
    mk.copy(tmpf, tmpi, e=e)
    mk.stt(dst, tmpf, -CW1, X, ALU.mult, ALU.add)
    mk.stt(dst, tmpf, -CW2, dst, ALU.mult, ALU.add)
    if phase:
        mk.ts(dst, dst, phase, None, op0=ALU.add, e=e)
    mk.ts(tmpf, dst, -PI_SAFE, TWO_PI, op0=ALU.is_lt, op1=ALU.mult, e=e)
    mk.tt(dst, dst, tmpf, ALU.add, e=e)
    mk.ts(tmpf, dst, PI_SAFE, TWO_PI, op0=ALU.is_gt, op1=ALU.mult, e=e)
    mk.tt(dst, dst, tmpf, ALU.subtract, e=e)
    mk.ts(dst, dst, PI_SAFE, -PI_SAFE, op0=ALU.min, op1=ALU.max, e=e)
    mk.act(dst, dst, AF.Sin)


Builder.rr_sin = _rr_sin


def _proj_in(self, xres, g_row, W, ncols, fm_blocks, uT, tm_range, u_tm):
    mk = self.mk
    with mk.scope():
        gbc = mk.sb([128, D], F32, "gbc")
        mk.dma(gbc, g_row.pbc(128))
        Wb = mk.sb([128, 8, ncols], BF16, "Wb")
        for k in range(8):
            mk.dma(Wb[:, k, :], W[k * 128:(k + 1) * 128, :], q="pool")
        xts = [mk.sb([128, D], F32) for i in range(2)]
        sqs = [mk.sb([128, D], F32) for i in range(2)]
        hbs = [mk.sb([128, D], BF16) for i in range(2)]
        smalls = [(mk.sb([128, 1], F32), mk.sb([128, 1], F32)) for i in range(2)]
        hT = [mk.sb([128, 8, 512], BF16) for i in range(2)]
        ev = [mk.sb([128, 512], F32) for i in range(4)]
        nev = 0
        for gidx in range(NTOK // 512):
            hb_ = hT[gidx % 2]
            for tl in range(4):
                i = gidx * 4 + tl
                b = i % 2
                mk.dma(xts[b], xres.rows(i * 128, 128))
                self.rms_tile(xts[b], gbc, hbs[b], sq=sqs[b], small=smalls[b])
                pbb = self.bank[b].bitcast(BF16)
                for k in range(8):
                    mk.transpose(pbb[:, k * 128:(k + 1) * 128], hbs[b][:, k * 128:(k + 1) * 128], self.identb)
                mk.copy(hb_[:, :, tl * 128:(tl + 1) * 128], pbb.re("p (k t) -> p k t", k=8),
                        e=("act" if tl % 2 else "pool_never") if False else ("act" if tl % 2 else "dve"))
            for bi, cb in enumerate(fm_blocks):
                pb = self.bank[2 + (bi % 3)]
                for k in range(8):
                    mk.mm(pb, Wb[:, k, cb * 128:(cb + 1) * 128], hb_[:, k, :], start=(k == 0), stop=(k == 7))
                t = ev[nev % 4]
                mk.copy(t, pb, e=("act" if nev % 2 else "dve"))
                nev += 1
                mk.dma(V(uT.ap[bi * 128:(bi + 1) * 128, gidx * 512:(gidx + 1) * 512], uT.bufs[bi]), t)
            if tm_range is not None:
                c0, c1 = tm_range
                for tl in range(4):
                    i = gidx * 4 + tl
                    for cc in range(c0, c1, 512):
                        pb = self.bank[5 + (nev % 3)]
                        for k in range(8):
                            mk.mm(pb, hb_[:, k, tl * 128:(tl + 1) * 128], Wb[:, k, cc:cc + 512],
                                  start=(k == 0), stop=(k == 7))
                        t = ev[nev % 4]
                        mk.copy(t, pb, e=("act" if nev % 2 else "dve"))
                        nev += 1
                        mk.dma(V(u_tm.ap[i * 128:(i + 1) * 128, cc - c0:cc - c0 + 512], u_tm.bufs[i]), t)


Builder.proj_in = _proj_in


def _proj_out(self, xres_in, xres_out, oTd, Wout):
    mk = self.mk
    with mk.scope():
        Wb = mk.sb([128, 8, D], BF16, "Wob")
        for k in range(8):
            mk.dma(Wb[:, k, :], Wout[k * 128:(k + 1) * 128, :], q="pool")
        xts = [mk.sb([128, D], F32) for i in range(2)]
        ot = [mk.sb([128, 8, 512], BF16) for i in range(2)]
        for gi in range(NTOK // 512):
            o_ = ot[gi % 2]
            for k in range(8):
                mk.dma(o_[:, k, :], V(oTd.ap[k * 128:(k + 1) * 128, gi * 512:(gi + 1) * 512], oTd.bufs[k]))
            for tl in range(4):
                i = gi * 4 + tl
                b = i % 2
                mk.dma(xts[b], xres_in.rows(i * 128, 128))
                for half in range(2):
                    pb = self.bank[(i % 2) * 2 + half]
                    for k in range(8):
                        mk.mm(pb, o_[:, k, tl * 128:(tl + 1) * 128], Wb[:, k, half * 512:(half + 1) * 512],
                              start=(k == 0), stop=(k == 7))
                    mk.tt(xts[b][:, half * 512:(half + 1) * 512], xts[b][:, half * 512:(half + 1) * 512], pb, ALU.add)
                mk.dma(xres_out.rows(i * 128, 128), xts[b])


Builder.proj_out = _proj_out


def _attn(self, u_tm, oT, P, layer):
    import math
    mk = self.mk
    oi = layer // 2
    lam_init = 0.8 - 0.6 * math.exp(-0.3 * layer)
    with mk.scope():
        gqk = mk.sb([128, D], F32, "gqk")
        mk.dma(gqk, P["da_qk_gain"][oi:oi + 1, :].pbc(128))
        subg = mk.sb([128, 128], F32, "subg")
        mk.dma(subg, P["da_subln"][oi:oi + 1, :].pbc(128))
        mk.ts(subg, subg, 1.0 - lam_init, None, op0=ALU.mult)
        lamv = mk.sb([128, 4, 64], F32, "lamv")
        mk.dma(lamv.re("p a d -> p (a d)"), P["da_lam"][oi:oi + 1].re("o a d -> o (a d)").pbc(128))
        lt = mk.sb([128, 2, 64], F32)
        ls = mk.sb([128, 2], F32)
        mk.tt(lt[:, 0, :], lamv[:, 0, :], lamv[:, 1, :], ALU.mult)
        mk.tt(lt[:, 1, :], lamv[:, 2, :], lamv[:, 3, :], ALU.mult)
        mk.reduce(ls, lt, op=ALU.add)
        mk.act(ls, ls, AF.Exp)
        nlam = mk.sb([128, 1], F32, "nlam")
        mk.tt(nlam, ls[:, 1:2], ls[:, 0:1], ALU.subtract)
        mk.ts(nlam, nlam, -lam_init, None, op0=ALU.add)
        eps5 = mk.sb([128, 1], F32)
        mk.memset(eps5, 1e-5)
        nshift = mk.sb([128, 1], F32)
        mk.memset(nshift, -4.0)
        zb = mk.sb([128, 512], BF16, "zb")
        mk.memset(zb, 0.0)
        jf = mk.sb([128, 32], F32)
        mk.op("pool", lambda eng: eng.iota(jf.ap, [[1, 32]], base=0, channel_multiplier=0,
                                            allow_small_or_imprecise_dtypes=True), writes=[jf])
        mk.act(jf, jf, AF.Exp, scale=-math.log(10000.0) / 32.0)
        posf = mk.sb([128, 16], F32)
        mk.op("pool", lambda eng: eng.iota(posf.ap, [[128, 16]], base=0, channel_multiplier=1,
                                            allow_small_or_imprecise_dtypes=True), writes=[posf])
        ang = mk.sb([128, 16, 32], F32)
        mk.tt(ang, V(jf.ap.unsqueeze(1).to_broadcast([128, 16, 32]), jf.buf),
              V(posf.ap.unsqueeze(2).to_broadcast([128, 16, 32]), posf.buf), ALU.mult)
        sint = mk.sb([128, 16, 32], F32, "sint")
        cost = mk.sb([128, 16, 32], F32, "cost")
        tf = mk.sb([128, 16, 32], F32)
        ti = mk.sb([128, 16, 32], I32)
        self.rr_sin(sint, ang, tf, ti)
        self.rr_sin(cost, ang, tf, ti, phase=math.pi / 2)

        QT = mk.sb([128, 4, T], BF16, "QT")
        KT = mk.sb([128, 4, T], BF16, "KT")
        Vt = mk.sb([128, 16, 512], BF16, "Vt")
        qk = [mk.sb([128, 16, 2, 32], F32) for i in range(2)]
        sq = mk.sb([128, 16, 64], F32)
        ss = mk.sb([128, 16], F32)
        ta = mk.sb([128, 16, 32], F32)
        tb = mk.sb([128, 16, 32], F32)
        qr = [mk.sb([128, 16, 2, 32], BF16) for i in range(2)]
        pts = [mk.sb([128, 512], BF16) for i in range(3)]
        rls = [mk.sb([128, 8], F32) for i in range(2)]
        ob32 = [mk.sb([128, 128], F32) for i in range(2)]
        obb = [mk.sb([128, 128], BF16) for i in range(2)]
        junk = mk.sb([128, 128], F32)
        ss1 = [mk.sb([128, 1], F32) for i in range(2)]
        npt = 0
        nfin = 0
        ostage = [mk.sb([128, T], BF16, "ostage%d" % i) for i in range(2)]
        for s in range(NSEQ):
            mk.dma(Vt, V(u_tm.ap[s * T:(s + 1) * T, 1024:1536].rearrange("(i p) c -> p i c", p=128),
                         u_tm.whole), q="pool", )
            for i in range(16):
                b = i % 2
                row0 = s * T + i * 128
                q_ = qk[b]
                qf = q_.re("p g m d -> p (g m d)")
                mk.dma(qf, V(u_tm.ap[row0:row0 + 128, 0:1024], u_tm.bufs[row0 // 128]))
                mk.act(sq.re("p g d -> p (g d)"), qf, AF.Square)
                mk.reduce(ss, sq, op=ALU.add)
                mk.act(ss, ss, AF.Sqrt, scale=1.0 / 64.0, bias=self.eps_t)
                mk.recip(ss, ss)
                q3 = q_.re("p g m d -> p g (m d)")
                mk.tt(q3, q3, V(ss.ap.unsqueeze(2).to_broadcast([128, 16, 64]), ss.buf), ALU.mult)
                mk.tt(qf, qf, gqk, ALU.mult)
                cb_ = V(cost.ap[:, i, :].unsqueeze(1).to_broadcast([128, 16, 32]), cost.buf)
                sb_ = V(sint.ap[:, i, :].unsqueeze(1).to_broadcast([128, 16, 32]), sint.buf)
                x1 = q_[:, :, 0, :]
                x2 = q_[:, :, 1, :]
                mk.tt(ta, x1, cb_, ALU.mult)
                mk.tt(tb, x2, sb_, ALU.mult, e=DBG.get("at_eng", "dve"))
                mk.tt(qr[b][:, :, 0, :], ta, tb, ALU.subtract)
                mk.tt(ta, x2, cb_, ALU.mult)
                mk.tt(tb, x1, sb_, ALU.mult, e=DBG.get("at_eng", "dve"))
                mk.tt(qr[b][:, :, 1, :], ta, tb, ALU.add)
                qrf = qr[b].re("p g m d -> p (g m d)")
                pbb = self.bank[6 + b].bitcast(BF16)
                for k in range(8):
                    mk.transpose(pbb[:, k * 128:(k + 1) * 128], qrf[:, k * 128:(k + 1) * 128], self.identb)
                mk.copy(QT[:, :, i * 128:(i + 1) * 128], pbb[:, 0:512].re("p (h t) -> p h t", h=4), e="act")
                mk.copy(KT[:, :, i * 128:(i + 1) * 128], pbb[:, 512:1024].re("p (h t) -> p h t", h=4), e="act")
            units = [(h, qc) for h in range(4) for qc in range(4)]

            def osets(u):
                par = u % 2
                O = [self.bank[2], self.bank[3]] if par == 0 else [self.bank[6], self.bank[7]]
                Lb = V(self.bank[4].ap[:, 8 * par:8 * par + 8], self.bank[4].buf)
                return O, Lb

            def main(u):
                nonlocal npt
                h, qc = units[u]
                O, Lb = osets(u)
                mk.mm(O[0], zb[:, 0:128], zb)
                mk.mm(O[1], zb[:, 0:128], zb)
                mk.mm(Lb, zb[:, 0:128], zb[:, 0:8])
                steps = [(m, kt) for m in range(2) for kt in range(4 * qc + 4)]
                info = []

                def issue_S(i):
                    nonlocal npt
                    m, kt = steps[i]
                    q0 = max(kt * 128, qc * 512)
                    nq = (qc + 1) * 512 - q0
                    S = self.bank[npt % 2]
                    Pt = pts[npt % 3]
                    npt += 1
                    mk.mm(S[:, 0:nq], KT[m * 64:(m + 1) * 64, h, kt * 128:(kt + 1) * 128],
                          QT[m * 64:(m + 1) * 64, h, q0:q0 + nq])
                    info.append((S, Pt, q0, nq))

                issue_S(0)
                for i, (m, kt) in enumerate(steps):
                    if i + 1 < len(steps):
                        issue_S(i + 1)
                    S, Pt, q0, nq = info[i]
                    mk.act(Pt[:, 0:nq], S[:, 0:nq], AF.Exp, scale=0.125, bias=nshift)
                    if kt >= 4 * qc:
                        mk.tt(Pt[:, 0:128], Pt[:, 0:128], self.trib, ALU.mult, e="dve")
                    for qb in range(max(kt, 4 * qc), 4 * qc + 4):
                        ql = qb - 4 * qc
                        c0 = qb * 128 - q0
                        mk.mm(O[m][:, ql * 128:(ql + 1) * 128], Pt[:, c0:c0 + 128],
                              Vt[:, kt, h * 128:(h + 1) * 128], start=False, stop=(kt == qb),
                              skip_group_check=True)
                        mk.mm(Lb[:, m * 4 + ql:m * 4 + ql + 1], Pt[:, c0:c0 + 128], self.onesb[:, 0:1],
                              start=False, stop=(kt == qb), skip_group_check=True)

            def fin(u):
                nonlocal nfin
                h, qc = units[u]
                O, Lb = osets(u)
                rl = rls[u % 2]
                mk.recip(rl, Lb)
                mk.ts(rl[:, 4:8], rl[:, 4:8], nlam, None, op0=ALU.mult)
                for ql in range(4):
                    fb = nfin % 2
                    nfin += 1
                    o = ob32[fb]
                    mk.ts(o, O[0][:, ql * 128:(ql + 1) * 128], rl[:, ql:ql + 1], None, op0=ALU.mult)
                    mk.stt(o, O[1][:, ql * 128:(ql + 1) * 128], rl[:, 4 + ql:5 + ql], o, ALU.mult, ALU.add)
                    mk.act(junk, o, AF.Square, accum_out=ss1[fb])
                    mk.act(ss1[fb], ss1[fb], AF.Sqrt, scale=1.0 / 128.0, bias=eps5)
                    mk.recip(ss1[fb], ss1[fb])
                    mk.stt(obb[fb], o, ss1[fb], subg, ALU.mult, ALU.mult)
                    ptr = self.bank[5].bitcast(BF16)
                    mk.transpose(ptr[:, fb * 128:(fb + 1) * 128], obb[fb], self.identb)
                    t0 = (4 * qc + ql) * 128
                    mk.copy(ostage[h % 2][:, t0:t0 + 128], ptr[:, fb * 128:(fb + 1) * 128], e="act")
                if qc == 3:
                    mk.dma(V(oT.ap[h * 128:(h + 1) * 128, s * T:(s + 1) * T], oT.bufs[h]), ostage[h % 2])

            main(0)
            for u in range(len(units)):
                if u + 1 < len(units):
                    main(u + 1)
                fin(u)


Builder.attn = _attn


def _s5_params(self, a_re, a_im, lstep, shape, want_coef):
    import math
    mk = self.mk
    n = lambda: mk.sb(shape, F32)
    are, step, lr, th, rho = n(), n(), n(), n(), n()
    mk.ts(are, a_re, -1e-4, None, op0=ALU.min)
    mk.act(step, lstep, AF.Exp)
    mk.tt(lr, are, step, ALU.mult)
    mk.tt(th, a_im, step, ALU.mult)
    mk.act(rho, lr, AF.Exp)
    out = dict(rho=rho, th=th)
    if want_coef:
        sn, cs, tf, x, y, den, cre, cim = n(), n(), n(), n(), n(), n(), n(), n()
        ti = mk.sb(shape, I32)
        self.rr_sin(sn, th, tf, ti)
        self.rr_sin(cs, th, tf, ti, phase=math.pi / 2)
        mk.tt(x, rho, cs, ALU.mult)
        mk.ts(x, x, -1.0, None, op0=ALU.add)
        mk.tt(y, rho, sn, ALU.mult)
        mk.tt(den, are, are, ALU.mult)
        mk.tt(tf, a_im, a_im, ALU.mult)
        mk.tt(den, den, tf, ALU.add)
        mk.recip(den, den)
        mk.tt(cre, x, are, ALU.mult)
        mk.tt(tf, y, a_im, ALU.mult)
        mk.tt(cre, cre, tf, ALU.add)
        mk.tt(cre, cre, den, ALU.mult)
        mk.tt(cim, y, are, ALU.mult)
        mk.tt(tf, x, a_im, ALU.mult)
        mk.tt(cim, cim, tf, ALU.subtract)
        mk.tt(cim, cim, den, ALU.mult)
        out.update(cre=cre, cim=cim)
    return out


Builder.s5_params = _s5_params


def _s5(self, uT, oT, P, layer):
    import math
    mk = self.mk
    oi = layer // 2
    with mk.scope():
        bbr = mk.sb([128, 4, 128], BF16, "bbr")
        bbi = mk.sb([128, 4, 128], BF16, "bbi")
        rho = mk.sb([128, 16], F32, "rho16")
        theta = mk.sb([128, 16], F32, "th16")
        bfr = mk.sb([128, 16, 128], BF16, "bfr")
        bfi = mk.sb([128, 16, 128], BF16, "bfi")
        Cfr = mk.sb([128, 16, 128], BF16, "Cfr")
        Cfi = mk.sb([128, 16, 128], BF16, "Cfi")
        with mk.scope():
            rep = mk.sb([128, 3, 512], F32, "rep")
            mk.dma(rep, P["s5_rep"][oi].re("a p j s -> p a (j s)"))
            pr_ = self.s5_params(rep[:, 0, :], rep[:, 1, :], rep[:, 2, :], [128, 512], True)
            Bre = mk.sb([128, 512], F32)
            Bim = mk.sb([128, 512], F32)
            mk.dma(Bre, P["s5_bbd_re"][oi].re("p j s -> p (j s)"))
            mk.dma(Bim, P["s5_bbd_im"][oi].re("p j s -> p (j s)"))
            t1p = mk.sb([128, 512], F32)
            t2p = mk.sb([128, 512], F32)
            mk.tt(t1p, Bre, pr_["cre"], ALU.mult)
            mk.tt(t2p, Bim, pr_["cim"], ALU.mult)
            mk.tt(bbr.re("p j s -> p (j s)"), t1p, t2p, ALU.subtract)
            mk.tt(t1p, Bre, pr_["cim"], ALU.mult)
            mk.tt(t2p, Bim, pr_["cre"], ALU.mult)
            mk.tt(bbi.re("p j s -> p (j s)"), t1p, t2p, ALU.add)
            mk.memset(bfr, 0.0)
            mk.memset(bfi, 0.0)
            for q in range(4):
                for (src, dst) in ((bbr, bfr), (bbi, bfi)):
                    mk.copy(dst[32 * q:32 * q + 32].re("p (j q) s -> p j q s", q=4)[:, :, q, :],
                            src[32 * q:32 * q + 32, :, :], e="pool")
            st = mk.sb([128, 3, 16], F32, "st")
            mk.dma(st, P["s5_st"][oi].re("a p b -> p a b"))
            ps_ = self.s5_params(st[:, 0, :], st[:, 1, :], st[:, 2, :], [128, 16], False)
            mk.copy(rho, ps_["rho"])
            mk.copy(theta, ps_["th"])
        Cre = mk.sb([128, 16, 32], BF16, "Cre")
        nCim = mk.sb([128, 16, 32], BF16, "nCim")
        cim32 = mk.sb([128, 16, 32], F32)
        mk.dma(Cre, P["s5_cbd_re"][oi], q="pool")
        mk.dma(cim32, P["s5_cbd_im"][oi])
        mk.ts(nCim, cim32, -1.0, None, op0=ALU.mult)
        mk.memset(Cfr, 0.0)
        mk.memset(Cfi, 0.0)
        for q in range(4):
            for (src, dst) in ((Cre, Cfr), (nCim, Cfi)):
                mk.copy(dst.re("p (j q) c -> p j q c", q=4)[:, :, q, 32 * q:32 * q + 32],
                        src.re("p (j q) c -> p j q c", q=4)[:, :, q, :], e="pool")
        dcol = mk.sb([128, 4], F32, "dcol")
        mk.dma(dcol, P["s5_dcol"][oi])
        cbase = mk.sb([128, 16, 64], F32, "cbase")
        sbase = mk.sb([128, 16, 64], F32, "sbase")
        cstep = mk.sb([128, 16, 32], F32, "cstep")
        sstep = mk.sb([128, 16, 32], F32, "sstep")
        with mk.scope():
            rio = mk.sb([128, 64], F32)
            mk.op("pool", lambda eng: eng.iota(rio.ap, [[1, 64]], base=0, channel_multiplier=0,
                                                allow_small_or_imprecise_dtypes=True), writes=[rio])
            kio = mk.sb([128, 32], F32)
            mk.op("pool", lambda eng: eng.iota(kio.ap, [[64, 32]], base=0, channel_multiplier=0,
                                                allow_small_or_imprecise_dtypes=True), writes=[kio])
            angb = mk.sb([128, 16, 64], F32)
            angs = mk.sb([128, 16, 32], F32)
            mk.tt(angb, V(theta.ap.unsqueeze(2).to_broadcast([128, 16, 64]), theta.buf),
                  V(rio.ap.unsqueeze(1).to_broadcast([128, 16, 64]), rio.buf), ALU.mult)
            mk.tt(angs, V(theta.ap.unsqueeze(2).to_broadcast([128, 16, 32]), theta.buf),
                  V(kio.ap.unsqueeze(1).to_broadcast([128, 16, 32]), kio.buf), ALU.mult)
            tfb = mk.sb([128, 16, 64], F32)
            tib = mk.sb([128, 16, 64], I32)
            self.rr_sin(sbase, angb, tfb, tib)
            self.rr_sin(cbase, angb, tfb, tib, phase=math.pi / 2)
            self.rr_sin(sstep, angs, tfb[:, :, 0:32], tib[:, :, 0:32])
            self.rr_sin(cstep, angs, tfb[:, :, 0:32], tib[:, :, 0:32], phase=math.pi / 2)
        ubb = mk.sb([128, NTOK], BF16, "ubb")
        zTd = self.scratch("zTd", [512, NTOK], BF16)
        yT = mk.sb([128, NTOK], F32, "yT")
        sint2 = [mk.sb([128, T], F32, "sint%d" % i) for i in range(2)]
        cost2 = [mk.sb([128, T], F32, "cost%d" % i) for i in range(2)]
        gre2 = [mk.sb([128, T], F32, "gre%d" % i) for i in range(2)]
        gim2 = [mk.sb([128, T], F32, "gim%d" % i) for i in range(2)]
        wre2 = [mk.sb([128, T], F32, "wre%d" % i) for i in range(2)]
        wim2 = [mk.sb([128, T], F32, "wim%d" % i) for i in range(2)]
        xre2 = [mk.sb([128, T], BF16, "xre%d" % i) for i in range(2)]
        xim2 = [mk.sb([128, T], BF16, "xim%d" % i) for i in range(2)]
        tt1 = mk.sb([128, T], F32, "tt1")
        tt2 = mk.sb([128, T], F32, "tt2")
        bur_f = mk.sb([128, T], F32, "bur_f")
        bui_f = mk.sb([128, T], F32, "bui_f")
        ubb2 = [ubb, ubb]
        state = dict(nb=0)

        S5E = DBG.get('s5_eng', 'dve')

        def tables(sbi):
            sint, cost = sint2[sbi % 2], cost2[sbi % 2]
            gre, gim, wre, wim = gre2[0], gim2[0], wre2[0], wim2[0]
            cs_b = V(cstep.ap[:, sbi, :].unsqueeze(2).to_broadcast([128, 32, 64]), cstep.buf)
            ss_b = V(sstep.ap[:, sbi, :].unsqueeze(2).to_broadcast([128, 32, 64]), sstep.buf)
            cb_b = V(cbase.ap[:, sbi, :].unsqueeze(1).to_broadcast([128, 32, 64]), cbase.buf)
            sb_b = V(sbase.ap[:, sbi, :].unsqueeze(1).to_broadcast([128, 32, 64]), sbase.buf)
            v3_ = lambda t_: t_.re("p (k r) -> p k r", r=64)
            mk.tt(v3_(tt1), cs_b, cb_b, ALU.mult)
            mk.tt(v3_(tt2), ss_b, sb_b, ALU.mult, e=S5E)
            mk.tt(cost, tt1, tt2, ALU.subtract)
            mk.tt(v3_(tt1), ss_b, cb_b, ALU.mult, e=S5E)
            mk.tt(v3_(tt2), cs_b, sb_b, ALU.mult)
            mk.tt(sint, tt1, tt2, ALU.add, e=S5E)

        def multi(b0, nb_):
            return V(self.psum_all.ap[:, b0 * 512:(b0 + nb_) * 512], self.bank[b0].buf)

        def stageA(it):
            sbi, s = it // NSEQ, it % NSEQ
            cb = sbi // 4
            sint, cost = sint2[sbi % 2], cost2[sbi % 2]
            gre, gim, wre, wim = gre2[it % 2], gim2[it % 2], wre2[it % 2], wim2[it % 2]
            ub_ = ubb2[cb % 2]
            rho_b = V(rho.ap[:, sbi:sbi + 1].to_broadcast([128, T]), rho.buf)
            for ch in range(4):
                tok0 = s * T + ch * 512
                mk.mm(self.bank[ch], bfr[:, sbi, :], ub_[:, tok0:tok0 + 512])
            for ch in range(4):
                tok0 = s * T + ch * 512
                mk.mm(self.bank[4 + ch], bfi[:, sbi, :], ub_[:, tok0:tok0 + 512])
            rd_r = [self.bank[i] for i in range(1, 4)]
            rd_i = [self.bank[i] for i in range(5, 8)]
            src_r, src_i = multi(0, 4), multi(4, 4)
            mk.op("act", lambda eng: eng.copy(out=bur_f.ap, in_=src_r.ap), reads=[src_r] + rd_r, writes=[bur_f])
            mk.op("act", lambda eng: eng.copy(out=bui_f.ap, in_=src_i.ap), reads=[src_i] + rd_i, writes=[bui_f])
            mk.tt(tt1, bur_f, cost, ALU.mult)
            mk.tt(gre, bui_f, sint, ALU.mult, e=S5E)
            mk.tt(gre, gre, tt1, ALU.add)
            mk.tt(tt2, bui_f, cost, ALU.mult)
            mk.tt(gim, bur_f, sint, ALU.mult, e=S5E)
            mk.tt(gim, tt2, gim, ALU.subtract)
            mk.scan(wre, rho_b, gre, 0.0)
            mk.scan(wim, rho_b, gim, 0.0)

        def stageB(it):
            sbi, s = it // NSEQ, it % NSEQ
            cb, q = sbi // 4, sbi % 4
            sint, cost = sint2[sbi % 2], cost2[sbi % 2]
            gre, gim, wre, wim = gre2[it % 2], gim2[it % 2], wre2[it % 2], wim2[it % 2]
            xre, xim = xre2[it % 2], xim2[it % 2]
            mk.tt(gre, cost, wre, ALU.mult, e=S5E)
            mk.tt(gim, sint, wim, ALU.mult)
            mk.tt(xre, gre, gim, ALU.subtract)
            mk.tt(wre, sint, wre, ALU.mult)
            mk.tt(wim, cost, wim, ALU.mult, e=S5E)
            mk.tt(xim, wre, wim, ALU.add)
            for ch in range(4):
                sl = slice(ch * 512, (ch + 1) * 512)
                py = self.bank[ch]
                mk.mm(py, Cfr[:, sbi, :], xre[:, sl], start=True, stop=False)
                mk.mm(py, Cfi[:, sbi, :], xim[:, sl], start=False, stop=True)
            src_y = multi(0, 4)
            rd_y = [self.bank[i] for i in range(1, 4)]
            ysl = yT[:, s * T:(s + 1) * T]
            if q == 0:
                mk.op("act", lambda eng: eng.copy(out=ysl.ap, in_=src_y.ap), reads=[src_y] + rd_y, writes=[ysl])
            else:
                mk.op("act", lambda eng: eng.copy(out=bur_f.ap, in_=src_y.ap), reads=[src_y] + rd_y, writes=[bur_f])
                mk.tt(ysl, ysl, bur_f, ALU.add, e=S5E)

        def finish(cb):
            for s in range(NSEQ):
                ys = yT[:, s * T:(s + 1) * T]
                mk.dma(tt1, V(uT.ap[cb * 128:(cb + 1) * 128, s * T:(s + 1) * T], uT.bufs[cb]))
                mk.stt(ys, tt1, dcol[:, cb:cb + 1], ys, ALU.mult, ALU.add)
                mk.tt(tt2, ys, ys, ALU.mult, e="pool")
                mk.ts(tt2, tt2, 0.044715, 1.0, op0=ALU.mult, op1=ALU.add)
                mk.tt(tt2, tt2, ys, ALU.mult, e="pool")
                mk.act(tt2, tt2, AF.Sigmoid, scale=2.0 * math.sqrt(2.0 / math.pi))
                mk.tt(xre2[s % 2], ys, tt2, ALU.mult)
                mk.dma(V(zTd.ap[cb * 128:(cb + 1) * 128, s * T:(s + 1) * T], zTd.bufs[cb]), xre2[s % 2], q="pool")

        NIT = 16 * NSEQ
        for cb in range(4):
            if cb == 0:
                mk.dma(ubb2[0], V(uT.ap[0:128, :], uT.bufs[0]), q="pool")
                tables(0)
                stageA(0)
            for q in range(4):
                sbi = 4 * cb + q
                for s in range(NSEQ):
                    it = sbi * NSEQ + s
                    nxt = it + 1
                    if nxt < NIT and (nxt // NSEQ) // 4 == cb:
                        if nxt % NSEQ == 0:
                            tables(nxt // NSEQ)
                        stageA(nxt)
                    stageB(it)
            finish(cb)
            nxt = (4 * cb + 4) * NSEQ
            if nxt < NIT:
                mk.dma(ubb2[(cb + 1) % 2], V(uT.ap[(cb + 1) * 128:(cb + 2) * 128, :], uT.bufs[cb + 1]), q="pool")
                tables(nxt // NSEQ)
                stageA(nxt)
    with mk.scope():
        wgl = mk.sb([128, 4, 512], BF16, "wgl")
        for k in range(4):
            mk.dma(wgl[:, k, :], P["s5_w_glu"][oi, k * 128:(k + 1) * 128, :], q="pool")
        sg = [mk.sb([128, 512], F32) for i in range(4)]
        obuf = [mk.sb([128, 512], BF16) for i in range(4)]
        zc = [mk.sb([128, 4, 512], BF16, "zc%d" % i) for i in range(2)]
        for ch in range(NTOK // 512):
            sl = slice(ch * 512, (ch + 1) * 512)
            zch = zc[ch % 2]
            for k in range(4):
                mk.dma(zch[:, k, :], V(zTd.ap[k * 128:(k + 1) * 128, sl], zTd.bufs[k]))
            for cbo in range(4):
                pg = self.bank[cbo]
                for k in range(4):
                    mk.mm(pg, wgl[:, k, cbo * 128:(cbo + 1) * 128], zch[:, k, :], start=(k == 0), stop=(k == 3))
                mk.act(sg[cbo], pg, AF.Sigmoid)
            for cbo in range(4):
                ob_ = obuf[(ch * 4 + cbo) % 4]
                mk.tt(ob_, zch[:, cbo, :], sg[cbo], ALU.mult, e=("pool" if cbo % 2 else "dve"))
                mk.dma(V(oT.ap[(4 + cbo) * 128:(5 + cbo) * 128, sl], oT.bufs[4 + cbo]), ob_)


Builder.s5 = _s5


def _odd_layer(self, xres_in, xres_out, layer, P):
    mk = self.mk
    oi = layer // 2
    u_tm = self.scratch("u_tm", [NTOK, 1536], F32)
    uT = self.scratch("uTo", [512, NTOK], F32)
    self.proj_in(xres_in, P["norm_mix_g"][layer:layer + 1, :], P["odd_w_in"][oi], 2048,
                 [12, 13, 14, 15], uT, (0, 1536), u_tm)
    oT = self.scratch("oTd", [D, NTOK], BF16)
    if not DBG.get("no_attn"):
        self.attn(u_tm, oT, P, layer)
    if not DBG.get("no_s5"):
        self.s5(uT, oT, P, layer)
    self.proj_out(xres_in, xres_out, oT, P["odd_w_out"][oi])


Builder.odd_layer = _odd_layer


def host_odd_params(inputs):
    f = lambda a: np.ascontiguousarray(a, dtype=np.float32)
    out = {}
    out["norm_mix_g"] = f(inputs["norm_mix_g"])
    out["odd_w_in"] = f(inputs["odd_w_in"])
    out["odd_w_out"] = f(inputs["odd_w_out"])
    n_odd = inputs["odd_w_in"].shape[0]
    out["da_qk_gain"] = f(np.concatenate([np.tile(inputs["da_q_norm"], (1, 8)),
                                          np.tile(inputs["da_k_norm"], (1, 8))], axis=1))
    out["da_lam"] = f(np.stack([inputs["da_lam_q1"], inputs["da_lam_k1"],
                                inputs["da_lam_q2"], inputs["da_lam_k2"]], axis=1))
    out["da_subln"] = f(inputs["da_subln"])
    three = np.stack([inputs["s5_a_re"], inputs["s5_a_im"], inputs["s5_log_step"]], axis=1)
    t16 = three.reshape(n_odd, 3, 16, 128)
    rep = t16.reshape(n_odd, 3, 4, 4, 128)
    rep = np.transpose(rep, (0, 1, 3, 2, 4))
    rep = np.repeat(rep[:, :, :, None, :, :], 32, axis=3)
    out["s5_rep"] = f(rep.reshape(n_odd, 3, 128, 4, 128))
    out["s5_st"] = f(np.transpose(t16, (0, 1, 3, 2)))
    for nm, key in (("s5_bbd_re", "s5_b_re"), ("s5_bbd_im", "s5_b_im")):
        Bm = inputs[key]
        bd = np.zeros((n_odd, 4, 2, 16, 4, 2, 64), np.float32)
        for j in range(4):
            for q in range(4):
                for gl in range(2):
                    g = 2 * (4 * j + q) + gl
                    bd[:, q, gl, :, j, gl, :] = np.transpose(Bm[:, g], (0, 2, 1))
        out[nm] = f(bd.reshape(n_odd, 128, 4, 128))
    for nm, key in (("s5_cbd_re", "s5_c_re"), ("s5_cbd_im", "s5_c_im")):
        Cm = inputs[key]
        bd = np.zeros((n_odd, 2, 64, 16, 2, 16), np.float32)
        for sbi in range(16):
            for gl in range(2):
                bd[:, gl, :, sbi, gl, :] = np.transpose(Cm[:, 2 * sbi + gl], (0, 2, 1))
        out[nm] = f(bd.reshape(n_odd, 128, 16, 32))
    out["s5_dcol"] = f(np.transpose(inputs["s5_d"].reshape(n_odd, 4, 128), (0, 2, 1)))
    out["s5_w_glu"] = f(inputs["s5_w_glu"])
    return out


LCH = 64
NCH = T // LCH
DECAY_C = 0.6065306597126334


def _load_shift_mix(self, dst, uT, blk, s, mu_col, U, dtmp):
    mk = self.mk
    mk.dma(U[:, 1:T + 1], V(uT.ap[blk * 128:(blk + 1) * 128, s * T:(s + 1) * T], uT.bufs[blk]))
    mk.tt(dtmp, U[:, 0:T], U[:, 1:T + 1], ALU.subtract, e=DBG.get("rw_eng", "dve"))
    mk.stt(dst, dtmp, mu_col, U[:, 1:T + 1], ALU.mult, ALU.add)


Builder.load_shift_mix = _load_shift_mix


def _rwkv(self, uT, oTd, P, layer, vfirst):
    mk = self.mk
    ei = layer // 2
    has_vres = layer > 0
    with mk.scope():
        mu = mk.sb([128, 14], F32, "mu")
        mk.dma(mu, P["rw_mu_col"][ei])
        cols = mk.sb([128, 7, 4], F32, "cols")
        mk.dma(cols, P["rw_cols"][ei])
        w0c, a0c, kkc, kac, rkc, lngc, lnbc = [cols[:, i, :] for i in range(7)]
        w2a2 = mk.sb([128, 512], BF16, "w2a2")
        mk.dma(w2a2, P["rw_w2a2"][ei], q="pool")
        g2b = mk.sb([128, 512], BF16, "g2b")
        mk.dma(g2b, P["rw_g2"][ei], q="pool")
        blk = mk.sb([128, 128], BF16, "blk")
        mk.dma(blk, self.inp["c_blk"], q="pool")
        masks = mk.sb([128, 3, 128], BF16, "masks")
        mk.dma(masks, self.inp["c_masks"].re("a p c -> p a c"), q="pool")

        def mb(i):
            return V(masks.ap[:, i, :].unsqueeze(1).to_broadcast([128, 2, 128]), masks.buf)
        mLs, mUs, mUi = mb(0), mb(1), mb(2)
        rmask = mk.sb([128, T], BF16, "rmask")
        tB = mk.sb([128, T], F32, "tB")
        r32 = mk.sb([128, T], F32, "r32")
        mk.op("pool", lambda eng: eng.iota(tB.ap.rearrange("p (c l) -> p c l", l=LCH), [[0, NCH], [1, LCH]],
                                            base=0, channel_multiplier=0, allow_small_or_imprecise_dtypes=True),
              writes=[tB])
        mk.ts(rmask, tB, 1.0, None, op0=ALU.min)
        lneps = mk.sb([128, 1], F32)
        mk.memset(lneps, 64e-5)
        zb = mk.sb([128, 128], BF16, "zb")
        mk.memset(zb, 0.0)
        U = mk.sb([128, T + 1], F32, "U")
        mk.memset(U[:, 0:1], 0.0)
        dtmp = tB
        m_ = r32
        lr12 = mk.sb([128, T], BF16, "lr12")
        sdg = mk.sb([128, T], BF16, "sdg")
        tbf = mk.sb([128, T], BF16, "tbf")
        if has_vres:
            v0c = mk.sb([128, 4], F32, "v0c")
            mk.dma(v0c, P["rw_v0col"][ei - 1])
            v1b = mk.sb([128, 4, 32], BF16, "v1b")
            mk.dma(v1b, P["rw_v1"][ei - 1].re("(k p) n -> p k n", p=128), q="pool")
            v2b = mk.sb([32, 512], BF16, "v2b")
            mk.dma(v2b, P["rw_v2"][ei - 1], q="pool")
            t32b = mk.sb([32, T], BF16, "t32b")
        k32 = mk.sb([128, T], F32, "k32")
        a16 = mk.sb([128, T], BF16, "a16")
        lw = mk.sb([128, T], F32, "lw")
        cl = mk.sb([128, T], F32, "cl")
        v32 = cl
        tA = mk.sb([128, T], F32, "tA")
        gT = mk.sb([128, T], BF16, "gT")
        bon = mk.sb([128, T], BF16, "bon")
        aT_ = mk.sb([128, T], BF16, "aT_")
        bT_ = mk.sb([128, T], BF16, "bT_")
        kT_ = mk.sb([128, T], BF16, "kT_")
        vT_ = tbf
        a_bd = mk.sb([128, 2, T], BF16, "a_bd")
        b_bd = mk.sb([128, 2, T], BF16, "b_bd")
        r_bd = mk.sb([128, 2, T], BF16, "r_bd")
        for t_ in (a_bd, b_bd, r_bd):
            mk.memset(t_, 0.0)
        TMav = mk.sb([128, 16, 2, 128], BF16, "TMav")
        TMbk = mk.sb([128, 16, 2, 2, 128], BF16, "TMbk")
        mk.memset(TMbk, 0.0)
        DL = mk.sb([128, NCH], F32, "DL")
        H32 = mk.sb([128, 128], F32, "H32")
        Hb = mk.sb([128, 128], BF16, "Hb")
        ht1 = mk.sb([128, 128], F32, "ht1")
        oacc = mk.sb([128, T], BF16, "oacc")

        def grp():
            d = dict(X=[mk.sb([128, 2, 128], BF16) for _ in range(2)], XT=[mk.sb([128, 2, 128], BF16) for _ in range(2)],
                     AakT=mk.sb([128, 2, 128], BF16), ArbT=mk.sb([128, 2, 128], BF16), ArkT=mk.sb([128, 2, 128], BF16),
                     Z=mk.sb([128, 2, 2, 64], BF16), T1T=mk.sb([128, 2, 128], BF16), G1=mk.sb([128, 2, 128], F32),
                     QT=mk.sb([128, 2, 128], BF16), yn=mk.sb([128, 128], BF16), ot=mk.sb([128, 128], F32),
                     st6=mk.sb([128, 2, 6], F32), mv=mk.sb([128, 2, 2], F32), rs=mk.sb([128, 2], F32))
            mk.memset(d["T1T"], 0.0)
            mk.memset(d["G1"], 0.0)
            mk.memset(d["QT"], 0.0)
            return d
        NGS = DBG.get("rw_depth", 1) + 1
        G = [grp() for _ in range(NGS)]
        B_ = self.bank

        def reg(b, c0, c1):
            return V(B_[b].ap[:, c0:c1], B_[b].buf)
        pN, pNT = reg(0, 0, 256), reg(0, 256, 512)
        pAk, pRb = reg(1, 0, 256), reg(1, 256, 512)
        pRk, pZ2 = reg(2, 0, 256), reg(2, 256, 384)
        pZa = [reg(3, 0, 256), reg(3, 256, 512)]
        pX, pXT = reg(4, 0, 256), reg(4, 256, 512)
        pHp, pTr = reg(5, 0, 128), reg(5, 128, 256)
        pT, pG = reg(6, 0, 256), reg(6, 256, 512)
        pQ, pY = reg(3, 0, 256), reg(7, 0, 128)
        pbig = [B_[5], B_[6], B_[7]]

        def v3(t):
            return t.re("p (h c) -> p h c", h=2)

        for s in range(NSEQ):
            tsl = slice(s * T, (s + 1) * T)
            self.load_shift_mix(m_, uT, 12, s, mu[:, 12:13], U, dtmp)
            mk.act(lr12[0:64, :], m_[0:64, :], AF.Tanh)
            mk.copy(lr12[64:128, :], m_[64:128, :], e=DBG.get("rw_eng", "dve"))
            self.load_shift_mix(m_, uT, 13, s, mu[:, 13:14], U, dtmp)
            mk.act(sdg, m_, AF.Sigmoid)
            if has_vres:
                pv = [B_[i] for i in range(4)]
                for hb in range(4):
                    self.load_shift_mix(m_, uT, 8 + hb, s, mu[:, 8 + hb:9 + hb], U, dtmp)
                    mk.copy(tbf, m_, e="act")
                    for ch in range(4):
                        mk.mm(pv[ch][0:32, :], v1b[:, hb, :], tbf[:, ch * 512:(ch + 1) * 512],
                              start=(hb == 0), stop=(hb == 3))
                for ch in range(4):
                    mk.copy(t32b[:, ch * 512:(ch + 1) * 512], pv[ch][0:32, :], e="act")
            for hb in range(4):
                hsl = slice(hb * 128, (hb + 1) * 128)
                self.load_shift_mix(r32, uT, hb, s, mu[:, hb:hb + 1], U, dtmp)
                self.load_shift_mix(k32, uT, 4 + hb, s, mu[:, 4 + hb:5 + hb], U, dtmp)
                self.load_shift_mix(v32, uT, 8 + hb, s, mu[:, 8 + hb:9 + hb], U, dtmp)
                for ch in range(4):
                    csl = slice(ch * 512, (ch + 1) * 512)
                    pb = pbig[ch % 3]
                    mk.mm(pb, w2a2[0:64, hsl], lr12[0:64, csl])
                    mk.act(lw[:, csl], pb, AF.Sigmoid, bias=w0c[:, hb:hb + 1])
                    pb = pbig[(ch + 1) % 3]
                    mk.mm(pb, w2a2[64:128, hsl], lr12[64:128, csl])
                    mk.act(a16[:, csl], pb, AF.Sigmoid, bias=a0c[:, hb:hb + 1])
                    pb = pbig[(ch + 2) % 3]
                    mk.mm(pb, g2b[:, hsl], sdg[:, csl])
                    mk.copy(gT[:, csl], pb, e="act")
                    if has_vres:
                        pb = pbig[ch % 3]
                        mk.mm(pb, v2b[:, hsl], t32b[:, csl])
                        mk.act(tA[:, csl], pb, AF.Sigmoid, bias=v0c[:, hb:hb + 1])
                mk.ts(lw, lw, -DECAY_C, None, op0=ALU.mult)
                if has_vres:
                    mk.dma(tB, V(vfirst.ap[hb * 128:(hb + 1) * 128, tsl], vfirst.bufs[hb]))
                    mk.tt(tB, tB, v32, ALU.subtract, e=DBG.get("rw_eng", "dve"))
                    mk.tt(tB, tB, tA, ALU.mult, e=DBG.get("rw_eng", "dve"))
                    mk.tt(v32, v32, tB, ALU.add, e=DBG.get("rw_eng", "dve"))
                else:
                    mk.dma(V(vfirst.ap[hb * 128:(hb + 1) * 128, tsl], vfirst.bufs[hb]), v32, q=DBG.get("st_q", "pool"))
                mk.ts(tA, k32, kkc[:, hb:hb + 1], None, op0=ALU.mult)
                mk.tt(tbf, tA, tA, ALU.mult, e=DBG.get("rw_eng", "dve"))
                for ch in range(4):
                    csl = slice(ch * 512, (ch + 1) * 512)
                    pb = pbig[ch % 3]
                    mk.mm(pb, blk, tbf[:, csl])
                    mk.act(tB[:, csl], pb, AF.Sqrt)
                mk.ts(tB, tB, 1e-12, None, op0=ALU.max)
                mk.recip(tB, tB)
                mk.tt(tA, tA, tB, ALU.mult)
                mk.ts(tB, a16, -1.0, kac[:, hb:hb + 1], op0=ALU.add, op1=ALU.mult)
                mk.ts(tB, tB, 1.0, None, op0=ALU.add)
                mk.tt(k32, k32, tB, ALU.mult)
                mk.tt(tB, r32, k32, ALU.mult, e=DBG.get("rw_eng", "dve"))
                mk.ts(tbf, tB, rkc[:, hb:hb + 1], None, op0=ALU.mult)
                for ch in range(4):
                    csl = slice(ch * 512, (ch + 1) * 512)
                    pb = pbig[ch % 3]
                    mk.mm(pb, blk, tbf[:, csl])
                    mk.tt(bon[:, csl], pb, v32[:, csl], ALU.mult)
                mk.copy(vT_, v32, e="act")
                mk.scan(cl, rmask, lw, 0.0)
                mk.act(tB, cl, AF.Exp)
                mk.copy(DL, tB.re("p (c l) -> p c l", l=LCH)[:, :, LCH - 1], e=DBG.get("rw_eng", "dve"))
                mk.tt(r_bd[0:64, 0, :], r32[0:64, :], tB[0:64, :], ALU.mult)
                mk.tt(r_bd[64:128, 1, :], r32[64:128, :], tB[64:128, :], ALU.mult, e=DBG.get("rw_eng", "dve"))
                mk.tt(cl, cl, lw, ALU.subtract, e=DBG.get("rw_eng", "dve"))
                mk.act(tB, cl, AF.Exp)
                mk.tt(tB, tB, tA, ALU.mult)
                mk.ts(aT_, tB, -1.0, None, op0=ALU.mult)
                mk.copy(a_bd[0:64, 0, :], aT_[0:64, :], e=DBG.get("rw_eng", "dve"))
                mk.copy(a_bd[64:128, 1, :], aT_[64:128, :], e="act")
                mk.tt(cl, cl, lw, ALU.add, e=DBG.get("rw_eng", "dve"))
                mk.act(tB, cl, AF.Exp, scale=-1.0)
                mk.tt(kT_, k32, tB, ALU.mult)
                mk.tt(tA, tA, a16, ALU.mult, e=DBG.get("rw_eng", "dve"))
                mk.tt(bT_, tA, tB, ALU.mult)
                mk.copy(b_bd[0:64, 0, :], bT_[0:64, :], e=DBG.get("rw_eng", "dve"))
                mk.copy(b_bd[64:128, 1, :], bT_[64:128, :], e="act")
                for p in range(16):
                    ptm = B_[5 + p % 2].bitcast(BF16)
                    psl = slice(p * 128, (p + 1) * 128)
                    for qi, src in enumerate((aT_, vT_, bT_, kT_)):
                        mk.transpose(ptm[:, qi * 128:(qi + 1) * 128], src[:, psl], self.identb)
                    mk.copy(TMav[:, p, :, :], ptm[:, 0:256].re("p (q c) -> p q c", q=2), e="act")
                    for c in range(2):
                        mk.copy(TMbk[64 * c:64 * c + 64, p, c, :, :],
                                ptm[64 * c:64 * c + 64, 256:512].re("p (q c) -> p q c", q=2), e=("dve" if c else "act"))
                mk.memset(H32, 0.0)
                mk.memset(Hb, 0.0)
                def local(p):
                    g = G[p % NGS]
                    t0 = p * 128
                    gsl = slice(t0, t0 + 128)
                    mk.mm(pN, aT_[:, gsl], b_bd[:, :, gsl])
                    mk.mm(pNT, bT_[:, gsl], a_bd[:, :, gsl])
                    mk.mm(pAk, kT_[:, gsl], a_bd[:, :, gsl])
                    mk.mm(pRb, bT_[:, gsl], r_bd[:, :, gsl])
                    mk.mm(pRk, kT_[:, gsl], r_bd[:, :, gsl])
                    yield
                    mk.tt(g["X"][0], v3(pN), mLs, ALU.mult)
                    mk.tt(g["XT"][0], v3(pNT), mUs, ALU.mult)
                    mk.tt(g["AakT"], v3(pAk), mUs, ALU.mult)
                    mk.tt(g["ArbT"], v3(pRb), mUi, ALU.mult)
                    mk.tt(g["ArkT"], v3(pRk), mUi, ALU.mult)
                    yield
                    for hd in range(2):
                        mk.mm(pZ2[:, 64 * hd:64 * hd + 64], g["AakT"][:, hd, :], TMav[:, p, 1, 64 * hd:64 * hd + 64])
                    Z = g["Z"]
                    mk.copy(Z[:, 0, :, :], TMav[:, p, 0, :].re("p (h j) -> p h j", h=2), e=DBG.get("rw_eng", "dve"))
                    yield
                    mk.copy(Z[:, 1, :, :], pZ2.re("p (h i) -> p h i", h=2), e="act")
                    yield
                    for lev in range(6):
                        X, XT = g["X"][lev % 2], g["XT"][lev % 2]
                        pz = pZa[lev % 2]
                        for hd in range(2):
                            mk.mm(pz[:, hd * 128:(hd + 1) * 128], XT[:, hd, :], Z[:, :, hd, :])
                        if lev < 5:
                            Xn, XTn = g["X"][(lev + 1) % 2], g["XT"][(lev + 1) % 2]
                            for hd in range(2):
                                mk.mm(pX[:, hd * 128:(hd + 1) * 128], XT[:, hd, :], X[:, hd, :])
                                mk.mm(pXT[:, hd * 128:(hd + 1) * 128], X[:, hd, :], XT[:, hd, :])
                        yield
                        if lev < 5:
                            mk.copy(Xn.re("p h c -> p (h c)"), pX, e="act")
                            mk.copy(XTn.re("p h c -> p (h c)"), pXT, e="act")
                        mk.tt(Z, Z, pz.re("p (h w c) -> p w h c", h=2, w=2), ALU.add)
                        yield
                    Wb_ = Z[:, 0, :, :].re("p h j -> p (h j)")
                    Ub_ = Z[:, 1, :, :].re("p h j -> p (h j)")
                    mk.mm(pT, Wb_, TMbk[:, p, :, 0, :])
                    for c in range(2):
                        mk.mm(pG[:, 128 * c:128 * c + 128], TMbk[:, p, c, 0, :], Ub_, start=True, stop=False)
                        mk.mm(pG[:, 128 * c:128 * c + 128], TMbk[:, p, c, 1, :], TMav[:, p, 1, :],
                              start=False, stop=True)
                    mk.mm(pQ, Wb_, g["ArbT"].re("p h t -> p (h t)"))
                    yield
                    for hd in range(2):
                        hs = slice(64 * hd, 64 * hd + 64)
                        mk.copy(g["T1T"][hs, :, hs], pT[hs, :].re("p (c h j) -> p c h j", c=2, h=2)[:, :, hd, :], e="act")
                        mk.copy(g["G1"][hs, :, hs], pG[hs, :].re("p (c h i) -> p c h i", c=2, h=2)[:, :, hd, :], e="act")
                        for c in range(2):
                            mk.tt(g["QT"][hs, c, 64 * c:64 * c + 64], pQ[hs, hd * 128 + 64 * c:hd * 128 + 64 * c + 64],
                                  r_bd[hs, hd, t0 + 64 * c:t0 + 64 * c + 64], ALU.add)

                def rec(p):
                    g = G[p % NGS]
                    t0 = p * 128
                    gsl = slice(t0, t0 + 128)
                    Z = g["Z"]
                    mk.mm(pY, zb, zb)
                    for hd in range(2):
                        hs = slice(64 * hd, 64 * hd + 64)
                        mk.mm(pY[:, hs], g["ArbT"][:, hd, :], Z[:, 1, hd, :], start=False, stop=False,
                              skip_group_check=True)
                        mk.mm(pY[:, hs], g["ArkT"][:, hd, :], TMav[:, p, 1, hs], start=False, stop=False,
                              skip_group_check=True)
                    for c in range(2):
                        ci = 2 * p + c
                        mk.mm(pY, g["QT"][:, c, :], Hb, start=False, stop=True, skip_group_check=True)
                        mk.mm(pHp, g["T1T"][:, c, :], Hb)
                        yield
                        mk.tt(ht1, pHp, H32, ALU.add)
                        mk.tt(ht1, ht1, g["G1"][:, c, :], ALU.add)
                        mk.ts(H32, ht1, DL[:, ci:ci + 1], None, op0=ALU.mult)
                        yield
                        mk.copy(Hb, H32, e="act")
                        yield
                    for hd in range(2):
                        ysl = pY[:, 64 * hd:64 * hd + 64]
                        mk.op("dve", lambda eng, o=g["st6"][:, hd, :], i_=ysl: eng.bn_stats(out=o.ap, in_=i_.ap),
                              reads=[ysl], writes=[g["st6"]])
                        mk.op("dve", lambda eng, o=g["mv"][:, hd, :], i_=g["st6"][:, hd, :]: eng.bn_aggr(out=o.ap, in_=i_.ap),
                              reads=[g["st6"]], writes=[g["mv"]])
                    yield
                    mk.act(g["rs"], g["mv"][:, :, 1], AF.Sqrt, bias=lneps)
                    yield
                    mk.recip(g["rs"], g["rs"])
                    for hd in range(2):
                        mk.ts(g["yn"][:, 64 * hd:64 * hd + 64], pY[:, 64 * hd:64 * hd + 64], g["mv"][:, hd, 0:1],
                              g["rs"][:, hd:hd + 1], op0=ALU.subtract, op1=ALU.mult)
                    yield
                    ptr = pTr.bitcast(BF16)
                    mk.transpose(ptr[:, 0:128], g["yn"], self.identb)
                    yield
                    mk.ts(g["ot"], ptr[:, 0:128], lngc[:, hb:hb + 1], lnbc[:, hb:hb + 1], op0=ALU.mult, op1=ALU.add)
                    mk.tt(g["ot"], g["ot"], bon[:, gsl], ALU.add, e=DBG.get("rw_eng", "dve"))
                    mk.tt(oacc[:, gsl], g["ot"], gT[:, gsl], ALU.mult, e=DBG.get("rw_eng", "dve"))

                def drive(gens):
                    gens = list(gens)
                    while gens:
                        for gg in list(gens):
                            try:
                                next(gg)
                            except StopIteration:
                                gens.remove(gg)

                NG = DBG.get("ngrp", 16)
                DEPTH = DBG.get("rw_depth", 1)
                started = {}

                def get_local(p):
                    if p not in started:
                        started[p] = [local(p), False]
                    return started[p]

                def step(ent):
                    if ent[1]:
                        return
                    try:
                        next(ent[0])
                    except StopIteration:
                        ent[1] = True

                if NG:
                    ent = get_local(0)
                    while not ent[1]:
                        step(ent)
                for p in range(NG):
                    r = [rec(p), False]
                    need = get_local(p + 1) if p + 1 < NG else None
                    extra = [get_local(p + k) for k in range(2, DEPTH + 1) if p + k < NG]
                    while not r[1] or (need is not None and not need[1]):
                        step(r)
                        if need is not None:
                            step(need)
                        for e_ in extra:
                            step(e_)
                mk.dma(V(oTd.ap[hb * 128:(hb + 1) * 128, tsl], oTd.bufs[hb]), oacc, q=DBG.get("st_q", "pool"))


Builder.rwkv = _rwkv


def _pool(self, uT, oT, P, layer):
    mk = self.mk
    ei = layer // 2
    with mk.scope():
        pwb = mk.sb([128, 4, 128], BF16, "pwb")
        mk.dma(pwb, P["pool_w"][ei].re("g c d -> c g d"), q="pool")
        psc = mk.sb([128, 4], F32, "psc")
        mk.dma(psc, P["pool_scale_col"][ei])
        A = [mk.sb([128, 16 + T], F32, "pA%d" % i) for i in range(2)]
        U0 = mk.sb([128, 16 + T], F32, "pU")
        for t_ in A + [U0]:
            mk.memset(t_[:, 0:16], 0.0)
        rcw = mk.sb([128, T], F32, "rcw")
        dT = mk.sb([128, T], BF16, "dT")
        tmp = mk.sb([128, T], F32, "ptmp")
        pob = [mk.sb([128, 512], BF16) for i in range(2)]
        for gi in range(4):
            win = 2 ** (gi + 1)
            mk.op("pool", lambda eng: eng.iota(rcw.ap, [[1, T]], base=1, channel_multiplier=0,
                                                allow_small_or_imprecise_dtypes=True), writes=[rcw])
            mk.ts(rcw, rcw, float(win), None, op0=ALU.min)
            mk.recip(rcw, rcw)
            for s in range(NSEQ if DBG.get("pool_stage", 9) >= 2 else 0):
                mk.dma(U0[:, 16:], V(uT.ap[(14 + gi) * 128:(15 + gi) * 128, s * T:(s + 1) * T], uT.bufs[14 + gi]))
                src = U0
                for lev in range(gi + 1):
                    sh = 2 ** lev
                    dst = A[lev % 2]
                    mk.tt(dst[:, 16:], src[:, 16:], src[:, 16 - sh:16 - sh + T], ALU.add, e=("dve" if DBG.get("pm_eng", "dve") == "dve" else ("pool" if lev % 2 else "dve")))
                    src = dst
                mk.tt(tmp, src[:, 16:], rcw, ALU.mult)
                mk.tt(dT, tmp, U0[:, 16:], ALU.subtract, e=DBG.get("pm_eng", "dve"))
                for ch in range(4 if DBG.get("pool_stage", 9) >= 3 else 0):
                    pb = self.bank[ch % 2]
                    mk.mm(pb, pwb[:, gi, :], dT[:, ch * 512:(ch + 1) * 512])
                    ob_ = pob[ch % 2]
                    if DBG.get("pool_stage", 9) >= 4:
                        mk.ts(ob_, pb, psc[:, gi:gi + 1], None, op0=ALU.mult)
                    if DBG.get("pool_stage", 9) >= 5:
                        mk.dma(V(oT.ap[(4 + gi) * 128:(5 + gi) * 128, s * T + ch * 512:s * T + (ch + 1) * 512],
                                 oT.bufs[4 + gi]), ob_, q=DBG.get("pool_q", "pool"))


Builder.pool = _pool


def _even_layer(self, xres_in, xres_out, layer, P, vfirst):
    mk = self.mk
    ei = layer // 2
    uT = self.scratch("uTe", [2304, NTOK], F32)
    self.proj_in(xres_in, P["norm_mix_g"][layer:layer + 1, :], P["even_w_in"][ei], 2304,
                 list(range(18)), uT, None, None)
    oT = self.scratch("oTd", [D, NTOK], BF16)
    if not DBG.get("no_rwkv"):
        self.rwkv(uT, oT, P, layer, vfirst)
    if not DBG.get("no_pool"):
        self.pool(uT, oT, P, layer)
    self.proj_out(xres_in, xres_out, oT, P["even_w_out"][ei])


Builder.even_layer = _even_layer


def host_even_params(inputs):
    f = lambda a: np.ascontiguousarray(a, dtype=np.float32)
    out = {}
    out["norm_mix_g"] = f(inputs["norm_mix_g"])
    out["even_w_in"] = f(inputs["even_w_in"])
    out["even_w_out"] = f(inputs["even_w_out"])
    n_even = inputs["even_w_in"].shape[0]
    out["rw_mu_col"] = f(np.transpose(inputs["rw_mu"].reshape(n_even, 14, 128), (0, 2, 1)))
    colp = [inputs["rw_w0"], inputs["rw_a0"], inputs["rw_k_k"], inputs["rw_k_a"],
            inputs["rw_r_k"].reshape(n_even, 512), inputs["rw_ln_g"], inputs["rw_ln_b"]]
    out["rw_cols"] = f(np.stack([np.transpose(c.reshape(n_even, 4, 128), (0, 2, 1)) for c in colp], axis=2))
    out["rw_w2a2"] = f(np.concatenate([inputs["rw_w2"], inputs["rw_a2"]], axis=1))
    out["rw_g2"] = f(inputs["rw_g2"])
    nv = inputs["rw_v0"].shape[0]
    out["rw_v0col"] = f(np.transpose(inputs["rw_v0"].reshape(nv, 4, 128), (0, 2, 1)))
    out["rw_v1"] = f(inputs["rw_v1"])
    out["rw_v2"] = f(inputs["rw_v2"])
    out["pool_w"] = f(inputs["pool_w"])
    out["pool_scale_col"] = f(np.transpose(inputs["pool_scale"].reshape(n_even, 4, 128), (0, 2, 1)))
    return out


def host_consts2():
    c = host_consts()
    blk = np.kron(np.eye(2, dtype=np.float32), np.ones((64, 64), np.float32))
    lo = np.tril(np.ones((64, 64), np.float32), -1)
    e2 = np.eye(2, dtype=np.float32)
    mLs = np.kron(e2, lo)
    mUs = np.kron(e2, lo.T)
    mUi = np.kron(e2, np.triu(np.ones((64, 64), np.float32)))
    c["c_blk"] = blk
    c["c_masks"] = np.stack([mLs, mUs, mUi]).astype(np.float32)
    return c


def mk_check(mk):
    val = {}
    pos = {e: 0 for e in ENGS}
    total = sum(len(mk.ops[e]) for e in ENGS)
    done = 0
    while done < total:
        prog = False
        for e in ENGS:
            lst = mk.ops[e]
            while pos[e] < len(lst):
                waits, fn, inc = lst[pos[e]]
                if all(val.get(id(s), 0) >= v for s, v in waits):
                    val[id(inc[0])] = val.get(id(inc[0]), 0) + inc[1]
                    pos[e] += 1
                    done += 1
                    prog = True
                else:
                    break
        if not prog:
            names = {id(v): k for k, v in mk.semobj.items()}
            for e in ENGS:
                if pos[e] < len(mk.ops[e]):
                    waits, fn, inc = mk.ops[e][pos[e]]
                    print("STUCK", e, pos[e], [(names.get(id(s)), v, val.get(id(s), 0)) for s, v in waits])
            return False
    return True


_PROG = {}


def host_params(inputs):
    hp = {}
    hp.update(host_even_params(inputs))
    hp.update(host_odd_params(inputs))
    hp.update(host_moe_params(inputs))
    hp.update(host_consts2())
    return hp


def build_program(shapes, n_layers=4):
    nc = bass.Bass("TRN2", target_bir_lowering=False)
    with ExitStack() as st:
        B = Builder(nc, st)
        mk = B.mk
        x_in = DR(mk, "x_in", [NTOK, D], F32, kind="ExternalInput")
        y = DR(mk, "y", [NTOK, D], F32, kind="ExternalOutput")
        P = {}
        for k, shp in shapes.items():
            if k in B.inp:
                continue
            P[k] = B.ext_in(k, list(shp))
        vf = B.scratch("vfirst", [512, NTOK], F32)
        for layer in range(n_layers):
            xin = x_in if layer == 0 else y
            if layer % 2 == 0:
                B.even_layer(xin, y, layer, P, vf)
            else:
                B.odd_layer(xin, y, layer, P)
            B.moe(y, layer, P)
        mk.emit()
    return nc


def kernel(**inputs):
    inputs = {k: np.asarray(v) for k, v in inputs.items()}
    hp = host_params(inputs)
    key = "full"
    if key not in _PROG:
        _PROG[key] = build_program({k: v.shape for k, v in hp.items()})
    nc = _PROG[key]
    x = np.ascontiguousarray(inputs["x"], dtype=np.float32)
    nb = x.shape[0]
    n_cores = 8
    per = nb // n_cores
    in_maps = []
    for c in range(n_cores):
        m = dict(hp)
        m["x_in"] = np.ascontiguousarray(x[c * per:(c + 1) * per].reshape(NTOK, D))
        in_maps.append(m)
    res = run_bass_kernel_spmd(nc, in_maps, core_ids=list(range(n_cores)))
    out = np.stack([np.asarray(r["y"], dtype=np.float32).reshape(per, T, D) for r in res.results], axis=0)
    return out.reshape(nb, T, D)
```
